# Optimizing a Trainium2 kernel written in Bass

```python
import math
import jax, jax.numpy as jnp
from jax import lax
import numpy as np

D_MODEL = 1024
BATCH = 32
SEQ = 2048
DEPTH = 1

MIX_WIDTH = D_MODEL
LRU_WIDTH = MIX_WIDTH // 2
LRU_BLOCKS = 8
LRU_BLOCK_DIM = LRU_WIDTH // LRU_BLOCKS
CONV_WIDTH = 4
LRU_C = 8.0
MLA_HEADS = 8
QK_NOPE_DIM = 64
QK_ROPE_DIM = 32
V_HEAD_DIM = (MIX_WIDTH - LRU_WIDTH) // MLA_HEADS
Q_LORA_RANK = D_MODEL // 4
KV_LORA_RANK = D_MODEL // 8
ROPE_THETA = 10000.0
Q_BLOCK = 128
IN_SPLITS = (LRU_WIDTH, LRU_WIDTH, Q_LORA_RANK, KV_LORA_RANK, QK_ROPE_DIM)
IN_WIDTH = sum(IN_SPLITS)
PEER_HEADS = 8
PEER_N_KEYS = 128
PEER_N_EXPERTS = PEER_N_KEYS * PEER_N_KEYS
PEER_HALF_DIM = 128
PEER_QUERY_DIM = 2 * PEER_HALF_DIM
PEER_TOPK = 16
PEER_TOKEN_CHUNK = 128
EPS = 1e-6

kernel_name = "hymba_rglru_mla_peer_adaln_block"


def rmsnorm(x, g):
    xf = x.astype(jnp.float32)
    y = xf * lax.rsqrt(jnp.mean(xf * xf, axis=-1, keepdims=True) + EPS)
    return (y * g.astype(jnp.float32)).astype(x.dtype)


def modulate(h, shift, scale):
    return h * (1.0 + scale[:, None, :]) + shift[:, None, :]


def split_cols(x, sizes):
    outs, start = [], 0
    for s in sizes:
        outs.append(x[..., start:start + s])
        start += s
    return outs


def apply_rope(x, cos, sin):
    xf = x.astype(jnp.float32)
    half = xf.shape[-1] // 2
    x1, x2 = xf[..., :half], xf[..., half:]
    return jnp.concatenate([x1 * cos - x2 * sin, x2 * cos + x1 * sin], axis=-1).astype(x.dtype)


def causal_depthwise_conv(x, w, b):
    S = x.shape[1]
    xp = jnp.pad(x, ((0, 0), (CONV_WIDTH - 1, 0), (0, 0)))
    y = b
    for k in range(CONV_WIDTH):
        y = y + w[k] * xp[:, k:k + S, :]
    return y


def rg_lru(xc, wa, ba, wx, bx, lam):
    B, S, _ = xc.shape
    xb = xc.reshape(B, S, LRU_BLOCKS, LRU_BLOCK_DIM)
    r = jax.nn.sigmoid(jnp.einsum('bshi,hij->bshj', xb, wa).reshape(B, S, LRU_WIDTH) + ba)
    i = jax.nn.sigmoid(jnp.einsum('bshi,hij->bshj', xb, wx).reshape(B, S, LRU_WIDTH) + bx)
    log_a = -LRU_C * r.astype(jnp.float32) * jax.nn.softplus(-lam.astype(jnp.float32))
    a = jnp.exp(log_a)
    b = jnp.sqrt(-jnp.expm1(2.0 * log_a)) * (i * xc).astype(jnp.float32)

    def combine(left, right):
        a_l, b_l = left
        a_r, b_r = right
        return a_l * a_r, a_r * b_l + b_r

    _, h = lax.associative_scan(combine, (a, b), axis=1)
    return h.astype(xc.dtype)


def mla_attention(q_lat, kv_lat, k_rope, positions, q_norm_g, w_uq, kv_norm_g, w_ukv):
    B, S, _ = q_lat.shape
    q = (rmsnorm(q_lat, q_norm_g) @ w_uq).reshape(B, S, MLA_HEADS, QK_NOPE_DIM + QK_ROPE_DIM)
    q_nope, q_rope = q[..., :QK_NOPE_DIM], q[..., QK_NOPE_DIM:]
    kv = (rmsnorm(kv_lat, kv_norm_g) @ w_ukv).reshape(B, S, MLA_HEADS, QK_NOPE_DIM + V_HEAD_DIM)
    k_nope, v = kv[..., :QK_NOPE_DIM], kv[..., QK_NOPE_DIM:]

    inv_freq = 1.0 / (ROPE_THETA ** (jnp.arange(0, QK_ROPE_DIM, 2, dtype=jnp.float32) / QK_ROPE_DIM))
    ang = positions.astype(jnp.float32)[..., None] * inv_freq
    cos, sin = jnp.cos(ang), jnp.sin(ang)
    q_rope = apply_rope(q_rope, cos[:, :, None, :], sin[:, :, None, :])
    k_rope = apply_rope(k_rope, cos, sin)
    scale = (QK_NOPE_DIM + QK_ROPE_DIM) ** -0.5

    nb = S // Q_BLOCK
    qn_blocks = q_nope.reshape(B, nb, Q_BLOCK, MLA_HEADS, QK_NOPE_DIM).transpose(1, 0, 2, 3, 4)
    qr_blocks = q_rope.reshape(B, nb, Q_BLOCK, MLA_HEADS, QK_ROPE_DIM).transpose(1, 0, 2, 3, 4)
    starts = jnp.arange(nb, dtype=jnp.int32) * Q_BLOCK
    key_idx = jnp.arange(S, dtype=jnp.int32)

    def attend_block(args):
        qn, qr, start = args
        s = (jnp.einsum('bqhd,bkhd->bhqk', qn, k_nope)
             + jnp.einsum('bqhd,bkd->bhqk', qr, k_rope)).astype(jnp.float32) * scale
        q_idx = start + jnp.arange(Q_BLOCK, dtype=jnp.int32)
        mask = key_idx[None, :] <= q_idx[:, None]
        s = jnp.where(mask[None, None], s, -jnp.inf)
        p = jax.nn.softmax(s, axis=-1).astype(v.dtype)
        return jnp.einsum('bhqk,bkhd->bqhd', p, v)

    out = lax.map(attend_block, (qn_blocks, qr_blocks, starts))
    return out.transpose(1, 0, 2, 3, 4).reshape(B, S, MLA_HEADS * V_HEAD_DIM)


def hybrid_mixer(h, positions, w_in, conv_w, conv_b, lru_wa, lru_ba, lru_wx, lru_bx, lru_lambda,
                 q_norm_g, w_uq, kv_norm_g, w_ukv, lru_out_g, mla_out_g, w_out):
    proj = h @ w_in
    x_lru, gate_lru, q_lat, kv_lat, k_rope = split_cols(proj, IN_SPLITS)
    xc = causal_depthwise_conv(x_lru, conv_w, conv_b)
    y_lru = jax.nn.gelu(gate_lru) * rg_lru(xc, lru_wa, lru_ba, lru_wx, lru_bx, lru_lambda)
    y_mla = mla_attention(q_lat, kv_lat, k_rope, positions, q_norm_g, w_uq, kv_norm_g, w_ukv)
    y = jnp.concatenate([rmsnorm(y_lru, lru_out_g), rmsnorm(y_mla, mla_out_g)], axis=-1)
    return y @ w_out


def peer_ffn(h, wq, keys, u, v):
    B, S, D = h.shape
    T = B * S
    hf = h.reshape(T, D)
    q = (hf @ wq).reshape(T, PEER_HEADS, 2, PEER_HALF_DIM).astype(jnp.float32)
    s = jnp.einsum('thpd,hpkd->thpk', q, keys.astype(jnp.float32))
    s_top, i_top = lax.top_k(s, PEER_TOPK)
    cand_s = (s_top[:, :, 0, :, None] + s_top[:, :, 1, None, :]).reshape(T, PEER_HEADS, PEER_TOPK * PEER_TOPK)
    cand_i = (i_top[:, :, 0, :, None] * PEER_N_KEYS + i_top[:, :, 1, None, :]).reshape(T, PEER_HEADS, PEER_TOPK * PEER_TOPK)
    best_s, pos = lax.top_k(cand_s, PEER_TOPK)
    idx = jnp.take_along_axis(cand_i, pos, axis=-1)
    g = jax.nn.softmax(best_s, axis=-1).astype(h.dtype)

    nc = T // PEER_TOKEN_CHUNK
    hc = hf.reshape(nc, PEER_TOKEN_CHUNK, D)
    ic = idx.reshape(nc, PEER_TOKEN_CHUNK, PEER_HEADS, PEER_TOPK)
    gc = g.reshape(nc, PEER_TOKEN_CHUNK, PEER_HEADS, PEER_TOPK)

    def expert_chunk(args):
        xc, ii, gg = args
        act = jax.nn.gelu(jnp.einsum('chkd,cd->chk', u[ii], xc))
        return jnp.einsum('chk,chkd->cd', gg * act, v[ii])

    out = lax.map(expert_chunk, (hc, ic, gc))
    return out.reshape(B, S, D)


def setup_inputs(seed: int = 0) -> dict:
    key = jax.random.key(seed)
    ks = jax.random.split(key, 32)
    L, D = DEPTH, D_MODEL

    def nrm(k, shape, scale):
        return jax.random.normal(k, shape, jnp.float32) * scale

    def gain(k, shape):
        return 1.0 + 0.02 * jax.random.normal(k, shape, jnp.float32)

    x = nrm(ks[0], (BATCH, SEQ, D), 1.0)
    c = nrm(ks[1], (BATCH, D), 1.0)
    positions = (jax.random.randint(ks[2], (BATCH, 1), 0, 4096, dtype=jnp.int32)
                 + jnp.arange(SEQ, dtype=jnp.int32)[None, :])
    a0 = jax.random.uniform(ks[13], (L, LRU_WIDTH), jnp.float32, minval=0.9, maxval=0.999) ** (1.0 / LRU_C)
    return {
        "x": x,
        "c": c,
        "positions": positions,
        "w_ada": nrm(ks[3], (L, D, 6 * D), 0.5 * D ** -0.5),
        "b_ada": nrm(ks[4], (L, 6 * D), 0.01),
        "norm1_g": gain(ks[5], (L, D)),
        "w_in": nrm(ks[6], (L, D, IN_WIDTH), D ** -0.5),
        "conv_w": nrm(ks[7], (L, CONV_WIDTH, LRU_WIDTH), CONV_WIDTH ** -0.5),
        "conv_b": nrm(ks[8], (L, LRU_WIDTH), 0.01),
        "lru_wa": nrm(ks[9], (L, LRU_BLOCKS, LRU_BLOCK_DIM, LRU_BLOCK_DIM), LRU_BLOCK_DIM ** -0.5),
        "lru_ba": nrm(ks[10], (L, LRU_WIDTH), 0.01),
        "lru_wx": nrm(ks[11], (L, LRU_BLOCKS, LRU_BLOCK_DIM, LRU_BLOCK_DIM), LRU_BLOCK_DIM ** -0.5),
        "lru_bx": nrm(ks[12], (L, LRU_WIDTH), 0.01),
        "lru_lambda": jnp.log(a0) - jnp.log1p(-a0),
        "q_norm_g": gain(ks[14], (L, Q_LORA_RANK)),
        "w_uq": nrm(ks[15], (L, Q_LORA_RANK, MLA_HEADS * (QK_NOPE_DIM + QK_ROPE_DIM)), Q_LORA_RANK ** -0.5),
        "kv_norm_g": gain(ks[16], (L, KV_LORA_RANK)),
        "w_ukv": nrm(ks[17], (L, KV_LORA_RANK, MLA_HEADS * (QK_NOPE_DIM + V_HEAD_DIM)), KV_LORA_RANK ** -0.5),
        "lru_out_g": gain(ks[18], (L, LRU_WIDTH)),
        "mla_out_g": gain(ks[19], (L, MLA_HEADS * V_HEAD_DIM)),
        "w_out": nrm(ks[20], (L, MIX_WIDTH, D), MIX_WIDTH ** -0.5),
        "norm2_g": gain(ks[21], (L, D)),
        "peer_wq": nrm(ks[22], (L, D, PEER_HEADS * PEER_QUERY_DIM), D ** -0.5),
        "peer_keys": nrm(ks[23], (L, PEER_HEADS, 2, PEER_N_KEYS, PEER_HALF_DIM), PEER_HALF_DIM ** -0.5),
        "peer_u": nrm(ks[24], (L, PEER_N_EXPERTS, D), D ** -0.5),
        "peer_v": nrm(ks[25], (L, PEER_N_EXPERTS, D), PEER_HEADS ** -0.5),
        "final_g": gain(ks[26], (D,)),
    }


def reference(x, c, positions, w_ada, b_ada, norm1_g, w_in, conv_w, conv_b, lru_wa, lru_ba,
              lru_wx, lru_bx, lru_lambda, q_norm_g, w_uq, kv_norm_g, w_ukv, lru_out_g, mla_out_g,
              w_out, norm2_g, peer_wq, peer_keys, peer_u, peer_v, final_g):
    c_act = jax.nn.silu(c)
    for l in range(DEPTH):
        mod = c_act @ w_ada[l] + b_ada[l]
        shift1, scale1, gate1, shift2, scale2, gate2 = jnp.split(mod, 6, axis=-1)
        h = modulate(rmsnorm(x, norm1_g[l]), shift1, scale1)
        mix = hybrid_mixer(h, positions, w_in[l], conv_w[l], conv_b[l], lru_wa[l], lru_ba[l],
                           lru_wx[l], lru_bx[l], lru_lambda[l], q_norm_g[l], w_uq[l],
                           kv_norm_g[l], w_ukv[l], lru_out_g[l], mla_out_g[l], w_out[l])
        x = x + gate1[:, None, :] * mix
        h = modulate(rmsnorm(x, norm2_g[l]), shift2, scale2)
        x = x + gate2[:, None, :] * peer_ffn(h, peer_wq[l], peer_keys[l], peer_u[l], peer_v[l])
    return rmsnorm(x, final_g)
```

```python
from contextlib import ExitStack
import threading
import math
import numpy as np
import concourse.bass as bass
import concourse.mybir as mybir
from concourse.bass_utils import run_bass_kernel_spmd

F32 = mybir.dt.float32
BF16 = mybir.dt.bfloat16
I32 = mybir.dt.int32
U32 = mybir.dt.uint32
ALU = mybir.AluOpType
AF = mybir.ActivationFunctionType
AX = mybir.AxisListType

D = 1024
S = 2048
NCORES = 8
NSEQ = 4
C = 256
NCH = S // C
DC = 8
EPS = 1e-6
WIN_COLS = 1472
NEG = -1.0e30
TWO_PI = 2.0 * math.pi

V_N1G, V_N2G, V_FG = 0, 8, 16
V_CONVW, V_CONVB, V_BA, V_BX, V_LAM = 24, 40, 44, 48, 52
V_QNG, V_KVNG, V_LOG, V_MOG = 56, 58, 59, 63
V_BADA = 67
V_INVF, V_SGN = 115, 116
NV = 117


class Sched:
    ENG = ("pe", "act", "dve", "pool", "sp")

    def __init__(self, nc, es):
        self.nc = nc
        self.es = es
        self.eng = {"pe": nc.tensor, "act": nc.scalar, "dve": nc.vector, "pool": nc.gpsimd, "sp": nc.sync}
        self.sem = {e: es.enter_context(nc.semaphore("sem_" + e)) for e in self.ENG}
        self.cnt = {e: 0 for e in self.ENG}
        self.seen = {e: {} for e in self.ENG}
        self.last_w = {}
        self.readers = {}
        self.dsem = {}
        self.dcnt = {}
        self.dead = [False]
        self.co = None
        self.last_b = {e: 0 for e in self.ENG}
        self.b_dcnt = {}
        self.in_b = False
        self.b_active = False
        self.tail = {e: 0.0 for e in self.ENG}
        self.tw = {}
        self.tr = {}
        self.DUR = {"pe": 0.15, "act": 0.45, "dve": 0.3, "pool": 0.25, "sp": 0.1}
        self.LAT = 0.3
        self.MARGIN = {"pe": 24.0, "act": 18.0, "dve": 16.0, "pool": 6.0, "sp": 60.0}

    def _deps(self, reads, writes):
        need = {}
        def add(tok):
            k, v = tok
            if need.get(k, 0) < v:
                need[k] = v
        for k in list(reads) + list(writes):
            t = self.last_w.get(k)
            if t is not None:
                add(t)
        for k in writes:
            for tok in self.readers.get(k, {}).items():
                add(tok)
        return need

    def _emit_waits(self, e, need):
        eng = self.eng[e]
        seen = self.seen[e]
        for k, v in need.items():
            if k == e and e == "pe":
                continue
            if seen.get(k, 0) >= v:
                continue
            if k in self.sem:
                eng.wait_ge(self.sem[k], v)
            else:
                eng.wait_ge(self.dsem[k], v)
            seen[k] = v

    def _record(self, tok, reads, writes):
        for k in writes:
            self.last_w[k] = tok
            self.readers[k] = {}
        for k in reads:
            r = self.readers.setdefault(k, {})
            if r.get(tok[0], 0) < tok[1]:
                r[tok[0]] = tok[1]

    def _ready(self, reads, writes):
        t = 0.0
        for k in list(reads) + list(writes):
            v = self.tw.get(k)
            if v is not None and v > t:
                t = v
        for k in writes:
            v = self.tr.get(k)
            if v is not None and v > t:
                t = v
        return t

    def _model(self, e, reads, writes, dur, extra=0.0):
        start = max(self._ready(reads, writes) + self.LAT, self.tail[e])
        fin = start + dur
        self.tail[e] = fin
        for k in writes:
            self.tw[k] = fin + extra
            self.tr[k] = 0.0
        for k in reads:
            if self.tr.get(k, 0.0) < fin + extra:
                self.tr[k] = fin + extra

    def op(self, e, fn, reads=(), writes=(), cost=None):
        dur = self.DUR[e] if cost is None else cost
        if self.co is not None:
            self.co.tick(self, e, reads, writes, dur)
        self.in_b = threading.current_thread() is getattr(self, "in_b_thread", None) and self.b_active
        self._model(e, reads, writes, dur)
        self._emit_waits(e, self._deps(reads, writes))
        ins = fn()
        ins.then_inc(self.sem[e], 1)
        self.cnt[e] += 1
        if self.in_b:
            self.last_b[e] = self.cnt[e]
        self._record((e, self.cnt[e]), reads, writes)

    def dma(self, e, fn, reads=(), writes=(), ch=None, cost=None):
        dur = 0.1 if cost is None else cost
        if self.co is not None:
            self.co.tick(self, e, reads, writes, dur)
        self.in_b = threading.current_thread() is getattr(self, "in_b_thread", None) and self.b_active
        self._model(e, reads, writes, dur, extra=2.5)
        if ch not in self.dsem:
            self.dsem[ch] = self.es.enter_context(self.nc.semaphore("dsem_%d" % len(self.dsem)))
            self.dcnt[ch] = 0
        need = self._deps(reads, writes)
        if self.dcnt[ch]:
            need[ch] = max(need.get(ch, 0), self.dcnt[ch])
        self._emit_waits(e, need)
        ins = fn()
        ins.then_inc(self.dsem[ch], 16)
        self.dcnt[ch] += 16
        if self.in_b:
            self.b_dcnt[ch] = self.dcnt[ch]
        self._record((ch, self.dcnt[ch]), reads, writes)

    def barrier_b(self):
        need = {e: v for e, v in self.last_b.items() if v}
        need.update({ch: v for ch, v in self.b_dcnt.items() if v})
        for e in self.ENG:
            self._emit_waits(e, dict(need))

    def barrier(self):
        need = {e: self.cnt[e] for e in self.ENG if self.cnt[e]}
        for ch, v in self.dcnt.items():
            if v:
                need[ch] = v
        for e in self.ENG:
            self._emit_waits(e, dict(need))
        self.last_w = {}
        self.readers = {}

    def finish(self):
        need = {e: self.cnt[e] for e in self.ENG if self.cnt[e]}
        for ch, v in self.dcnt.items():
            if v:
                need[ch] = v
        self._emit_waits("sp", need)


class Co:
    def __init__(self):
        self.thread = None
        self.quota = 0
        self.b_go = threading.Semaphore(0)
        self.m_go = threading.Semaphore(0)
        self.done = True
        self.exc = None
        self.count = 0
        self.free_run = False
        self.SLACK = 0.3

    def start(self, fn):
        self.done = False
        self.count = 0
        self.exc = None

        def run():
            self.b_go.acquire()
            try:
                fn()
            except BaseException as e:
                self.exc = e
            self.done = True
            self.m_go.release()
        self.thread = threading.Thread(target=run)
        self.thread.start()

    def give(self, q):
        if self.done:
            return
        self.quota = q
        self.free_run = q >= (1 << 50)
        self.b_go.release()
        self.m_go.acquire()
        if self.exc is not None:
            raise self.exc

    def tick(self, sched=None, e=None, reads=(), writes=(), dur=0.0):
        if self.thread is not None and threading.current_thread() is self.thread:
            self.count += 1
            while not getattr(self, "free_run", False):
                self.quota -= 1
                blocked = False
                if sched is not None:
                    fin = max(sched._ready(reads, writes) + sched.LAT, sched.tail[e]) + dur
                    blocked = fin > sched.tail["pool"] + sched.MARGIN[e]
                if self.quota >= 0 and not blocked:
                    break
                self.m_go.release()
                self.b_go.acquire()

    def drain(self):
        while not self.done:
            self.give(1 << 60)
        if self.thread is not None:
            self.thread.join()
            self.thread = None
        if self.exc is not None:
            raise self.exc


def build_program(nseq=NSEQ, nch=NCH, stop=99, overlap=True):
    nc = bass.Bass("TRN2", target_bir_lowering=False)
    dr = lambda name, shape, dt, kind="ExternalInput": nc.dram_tensor(name, shape, dt, kind=kind).ap()
    xT_d = dr("xT", [nseq, DC, 128, S], F32)
    cT_d = dr("cT", [128, DC, nseq], F32)
    pos_d = dr("pos", [nseq, S], I32)
    wada_d = dr("w_ada", [DC, 128, 6 * D], F32)
    vecs_d = dr("vecs", [128, NV], F32)
    win_d = dr("w_in", [DC, 128, WIN_COLS], F32)
    wabd_d = dr("wa_bd", [4, 128, 128], F32)
    wxbd_d = dr("wx_bd", [4, 128, 128], F32)
    wuq_d = dr("w_uq", [2, 128, 1536], F32)
    wukv_d = dr("w_ukv", [128, 1024], F32)
    wout_d = dr("w_out", [DC, 128, D], F32)
    wq_d = dr("peer_wq", [DC, 128, 2048], F32)
    keysT_d = dr("keysT", [128, 16, 128], F32)
    uv_d = dr("uv", [16384, 2048], F32)
    outT_d = dr("outT", [nseq, DC, 128, S], F32, kind="ExternalOutput")
    winb_d = dr("winb", [13, 128, DC, 128], BF16, kind="Internal")
    woutb_d = dr("woutb", [8, 128, DC, 128], BF16, kind="Internal")
    wqb_d = dr("wqb", [16, 128, DC, 128], BF16, kind="Internal")
    uvb_d = dr("uvb", [16384, 2048], BF16, kind="Internal")

    with ExitStack() as es:
        sc = Sched(nc, es)
        co = Co()
        sc.co = co
        PE, ACT, DVE, POOL, SP = nc.tensor, nc.scalar, nc.vector, nc.gpsimd, nc.sync
        uid = [0]

        def sb(stack, name, shape, dt):
            uid[0] += 1
            return stack.enter_context(nc.sbuf_tensor("%s_%d" % (name, uid[0]), shape, dt))

        w_uq = sb(es, "w_uq", [128, 2, 1536], BF16)
        w_ukv = sb(es, "w_ukv", [128, 1024], BF16)
        keysT = sb(es, "keysT", [128, 16, 128], F32)
        wa_bd = sb(es, "wa_bd", [128, 4, 128], BF16)
        wx_bd = sb(es, "wx_bd", [128, 4, 128], BF16)
        vecs = sb(es, "vecs", [128, NV], F32)
        modT = sb(es, "modT", [128, 48, nseq], F32)
        A1 = sb(es, "A1", [128, DC, nseq], F32)
        A2 = sb(es, "A2", [128, DC, nseq], F32)
        nsp = sb(es, "nsp", [128, 4], F32)
        consts = sb(es, "consts", [128, 4], F32)
        ones_bf = sb(es, "ones_bf", [128, 128], BF16)
        ident_f = sb(es, "ident_f", [128, 128], F32)
        ident_b = sb(es, "ident_b", [128, 128], BF16)
        tri_b = sb(es, "tri_b", [128, 128], BF16)
        iota16 = sb(es, "iota16", [128, 16], F32)
        KT = sb(es, "KT", [96, 8, S], BF16)
        VC = sb(es, "VC", [128, S // 128, 8, 65], BF16)
        xl = sb(es, "xl", [128, 4, C + 3], F32)
        hst = sb(es, "hst", [128, 4], F32)
        xTs = [sb(es, "xT%d" % i, [128, DC, C], F32) for i in range(2)]
        hT = sb(es, "hT", [128, DC, C], BF16)
        sq = sb(es, "sq", [128, DC, C], BF16)
        rstd = sb(es, "rstd", [128, C], F32)
        tmpf = sb(es, "tmpf", [128, C], F32)
        wr = [sb(es, "wr%d" % i, [128, DC, 128], BF16) for i in range(4)]
        NBUF = 8
        G = [sb(es, "G%d" % i, [128, 2048], BF16) for i in range(NBUF)]
        junkb_t = sb(es, "junkb", [128, 2, D], BF16)
        junkb = [junkb_t[:, 0, :], junkb_t[:, 1, :]]
        diag = [sb(es, "diag%d" % i, [128, 128], BF16) for i in range(3)]
        dots = sb(es, "dots", [128, 128], F32)
        acts = sb(es, "acts", [128, 128], F32)
        zz = sb(es, "zz", [128, 128], F32)
        petm = sb(es, "petm", [128, D], F32)
        sqA = junkb_t[:, :, :].rearrange("p a (b c) -> p (a b) c", c=C)
        rstdA = sb(es, "rstdA", [128, C], F32)
        otmp = [sb(es, "otmp%d" % i, [128, C], F32) for i in range(2)]
        idxs = [[sb(es, "idx%d%d" % (p, j), [128, 128], I32) for j in range(2)] for p in range(2)]
        ggs = [[sb(es, "gg%d%d" % (p, j), [128, 8, 16], F32) for j in range(2)] for p in range(2)]
        h2s = [[sb(es, "h2%d%d" % (p, j), [128, D], BF16) for j in range(2)] for p in range(2)]

        PS = [es.enter_context(nc.psum_tensor("ps%d" % i, [128, 512], F32)) for i in (0, 1, 2)]
        PS3 = es.enter_context(nc.psum_tensor("ps3", [128, 1024], BF16))
        PS += [None] + [es.enter_context(nc.psum_tensor("ps%d" % i, [128, 512], F32)) for i in (4, 5, 6, 7)]

        def vcol(c0, n=1):
            return vecs[:, c0:c0 + n]

        with ExitStack() as pes:
            stage = sb(pes, "stage", [128, 4096], F32)
            cT = sb(pes, "cT", [128, DC, nseq], F32)
            iot_i = sb(pes, "iot_i", [128, 128], I32)
            iot_f = sb(pes, "iot_f", [128, 128], F32)
            sc.dma("sp", lambda: SP.dma_start(out=vecs[:], in_=vecs_d[:, :]), writes=["vecs"], ch="vecs")
            sc.dma("sp", lambda: SP.dma_start(out=cT[:], in_=cT_d[:, :, :]), writes=["cT"], ch="cT")
            sc.dma("sp", lambda: SP.dma_start(out=keysT[:], in_=keysT_d[:, :, :]), writes=["keysT"], ch="keysT")
            sc.op("dve", lambda: DVE.memset(consts[:, 0:1], EPS), writes=["consts"])
            sc.op("dve", lambda: DVE.memset(consts[:, 1:2], 1.0), writes=["consts"])
            sc.op("dve", lambda: DVE.memset(consts[:, 2:3], 0.0), writes=["consts"])
            sc.op("dve", lambda: DVE.memset(ones_bf[:], 1.0), writes=["ones_bf"])
            sc.op("dve", lambda: DVE.memset(VC[:], 1.0), writes=["VC"])
            sc.op("dve", lambda: DVE.memset(KT[:], 0.0), writes=["KT"])
            sc.op("pool", lambda: POOL.iota(iot_i[:], pattern=[[1, 128]], base=0, channel_multiplier=-1), writes=["iot_i"])
            sc.op("dve", lambda: DVE.tensor_copy(iot_f[:], iot_i[:]), reads=["iot_i"], writes=["iot_f"])
            sc.op("dve", lambda: DVE.tensor_scalar(ident_f[:], iot_f[:], 0.0, None, op0=ALU.is_equal), reads=["iot_f"], writes=["ident_f"])
            sc.op("dve", lambda: DVE.tensor_copy(ident_b[:], ident_f[:]), reads=["ident_f"], writes=["ident_b"])
            sc.op("dve", lambda: DVE.tensor_scalar(tri_b[:], iot_f[:], 0.0, None, op0=ALU.is_ge), reads=["iot_f"], writes=["tri_b"])
            sc.op("pool", lambda: POOL.iota(iot_i[:, 0:16], pattern=[[1, 16]], base=0, channel_multiplier=0), reads=["iot_f"], writes=["iot_i"])
            sc.op("dve", lambda: DVE.tensor_copy(iota16[:], iot_i[:, 0:16]), reads=["iot_i"], writes=["iota16"])

            stb = [sb(pes, "stb%d" % i, [128, 2048], BF16) for i in range(2)]

            def cast_op(k, dst_ap, st, key, wkey):
                eng = ("dve", "act", "pool")[k % 3]
                if eng == "dve":
                    sc.op("dve", lambda: DVE.tensor_copy(dst_ap, st), reads=[key], writes=[wkey])
                elif eng == "act":
                    sc.op("act", lambda: ACT.copy(dst_ap, st), reads=[key], writes=[wkey])
                else:
                    sc.op("pool", lambda: POOL.tensor_copy(dst_ap, st), reads=[key], writes=[wkey])

            def load_cast(dst_ap, src_ap, ncols, k, outs=None):
                st = stage[:, 0:ncols] if k % 2 == 0 else stage[:, 2048:2048 + ncols]
                key = "stage%d" % (k % 2)
                sc.dma("sp", lambda: SP.dma_start(out=st, in_=src_ap), writes=[key], ch=key)
                if outs is None:
                    cast_op(k, dst_ap, st, key, "W")
                    return
                bkey = "stb%d" % (k % 2)
                sbt = stb[k % 2]
                cast_op(k, sbt[:, 0:ncols], st, key, bkey)
                for oi, (d_ap, s_ap) in enumerate(outs(sbt)):
                    sc.dma("sp", lambda: SP.dma_start(out=d_ap, in_=s_ap), reads=[bkey], writes=["wscr"], ch="%so%d" % (bkey, oi))
            k = 0
            for dc in range(DC):
                load_cast(None, win_d[dc], WIN_COLS, k, outs=lambda t, dc=dc: [
                    (winb_d[0:11, :, dc, :].rearrange("oc p n -> p oc n"), t[:, 0:1408].rearrange("p (oc n) -> p oc n", n=128)),
                    (winb_d[11, :, dc, 0:96], t[:, 1344:1440]),
                    (winb_d[12, :, dc, 0:96], t[:, 1376:1472])]); k += 1
                load_cast(None, wout_d[dc], D, k, outs=lambda t, dc=dc: [
                    (woutb_d[:, :, dc, :].rearrange("oc p n -> p oc n"), t[:, 0:1024].rearrange("p (oc n) -> p oc n", n=128))]); k += 1
                load_cast(None, wq_d[dc], 2048, k, outs=lambda t, dc=dc: [
                    (wqb_d[:, :, dc, :].rearrange("oc p n -> p oc n"), t[:, 0:2048].rearrange("p (oc n) -> p oc n", n=128))]); k += 1
            for kc in range(2):
                load_cast(w_uq[:, kc, :], wuq_d[kc], 1536, k); k += 1
            load_cast(w_ukv[:, :], wukv_d[:, :], 1024, k); k += 1
            for ci in range(4):
                load_cast(wa_bd[:, ci, :], wabd_d[ci], 128, k); k += 1
                load_cast(wx_bd[:, ci, :], wxbd_d[ci], 128, k); k += 1

            uv_v = uv_d.rearrange("(p r) n -> p r n", p=128)
            uvb_v = uvb_d.rearrange("(p r) n -> p r n", p=128)
            for st_i in range(128):
                i2 = st_i % 2
                st = stage[:, i2 * 2048:(i2 + 1) * 2048]
                skey, bkey = "stage%d" % i2, "stb%d" % i2
                sc.dma("sp", lambda: SP.dma_start(out=st, in_=uv_v[:, st_i, :]), writes=[skey], ch=skey)
                if st_i % 2 == 0:
                    sc.op("dve", lambda: DVE.tensor_copy(stb[i2][:], st), reads=[skey], writes=[bkey])
                else:
                    sc.op("act", lambda: ACT.copy(stb[i2][:], st), reads=[skey], writes=[bkey])
                sc.dma("act", lambda: ACT.dma_start(out=uvb_v[:, st_i, :], in_=stb[i2][:]), reads=[bkey], writes=["uvscr"], ch=bkey + "u")
            sc.op("act", lambda: ACT.activation(out=nsp[:], in_=vcol(V_LAM, 4), func=AF.Exp, scale=-1.0), reads=["vecs"], writes=["nsp"])
            sc.op("act", lambda: ACT.activation(out=nsp[:], in_=nsp[:], func=AF.Ln, bias=consts[:, 1:2], scale=1.0), reads=["nsp", "consts"], writes=["nsp"])
            sc.op("dve", lambda: DVE.tensor_scalar(nsp[:], nsp[:], -16.0, None, op0=ALU.mult), reads=["nsp"], writes=["nsp"])

            sc.op("act", lambda: ACT.activation(out=cT[:], in_=cT[:], func=AF.Silu), reads=["cT"], writes=["cT"])
            ps_mod = PS[0][:, 0:48 * nseq].rearrange("p (n b) -> p n b", b=nseq)
            for n in range(48):
                key = "stage%d" % (n % 2)
                st = stage[:, (n % 2) * 2048:(n % 2) * 2048 + 1024].rearrange("p (dc n) -> p dc n", n=128)
                sc.dma("sp", lambda: SP.dma_start(out=st, in_=wada_d[:, :, n * 128:(n + 1) * 128].rearrange("dc p n -> p dc n")), writes=[key], ch=key)
                for dc in range(DC):
                    sc.op("pe", lambda: PE.matmul(ps_mod[:, n, :], st[:, dc, :], cT[:, dc, :], start=(dc == 0), stop=(dc == DC - 1)),
                          reads=[key, "cT"], writes=["ps0"])
            sc.op("dve", lambda: DVE.tensor_tensor(out=modT[:], in0=ps_mod, in1=vcol(V_BADA, 48).unsqueeze(2).to_broadcast([128, 48, nseq]), op=ALU.add),
                  reads=["ps0", "vecs"], writes=["modT"])
            for dc in range(DC):
                sc.op("dve", lambda: DVE.tensor_scalar(A1[:, dc, :], modT[:, 8 + dc, :], 1.0, vcol(V_N1G + dc), op0=ALU.add, op1=ALU.mult),
                      reads=["modT", "vecs"], writes=["A1"])
                sc.op("dve", lambda: DVE.tensor_scalar(A2[:, dc, :], modT[:, 32 + dc, :], 1.0, vcol(V_N2G + dc), op0=ALU.add, op1=ALU.mult),
                      reads=["modT", "vecs"], writes=["A2"])
            sc.barrier()

        def rms_stats(src_tile, nchunks, inv_n, srckey, sq_t=None, rstd_t=None, bank=0, sfx="", sqkeys=None):
            sq_t = sq if sq_t is None else sq_t
            rstd_t = rstd if rstd_t is None else rstd_t
            sk, rk, pk = "sq" + sfx, "rstd" + sfx, "ps%d" % bank
            sks = [sk] if sqkeys is None else list(sqkeys)
            sc.op("act", lambda: ACT.activation(out=sq_t[:, 0:nchunks, :], in_=src_tile, func=AF.Square), reads=[srckey], writes=sks, cost=0.25 + 0.21 * nchunks)
            for i in range(nchunks):
                sc.op("pe", lambda: PE.matmul(PS[bank][:, 0:C], ones_bf[:], sq_t[:, i, :], start=(i == 0), stop=(i == nchunks - 1)),
                      reads=sks, writes=[pk])
            sc.op("act", lambda: ACT.activation(out=rstd_t[:], in_=PS[bank][:, 0:C], func=AF.Sqrt, bias=consts[:, 0:1], scale=inv_n), reads=[pk], writes=[rk])
            sc.op("dve", lambda: DVE.reciprocal(rstd_t[:], rstd_t[:]), reads=[rk], writes=[rk])

        def modulated_norm(xT, xk, Acol, shift_chunk0, b):
            rms_stats(xT[:], DC, 1.0 / D, xk)
            for dc in range(DC):
                sc.op("dve", lambda: DVE.scalar_tensor_tensor(out=tmpf[:], in0=xT[:, dc, :], scalar=Acol[:, dc, b:b + 1], in1=rstd[:], op0=ALU.mult, op1=ALU.mult),
                      reads=[xk, "rstd"], writes=["tmpf"])
                sc.op("act", lambda: ACT.activation(out=hT[:, dc, :], in_=tmpf[:], func=AF.Identity, bias=modT[:, shift_chunk0 + dc, b:b + 1], scale=1.0),
                      reads=["tmpf"], writes=["hT"])

        gen_i = [0]

        def gen_ps():
            bnk = (1, 2)[gen_i[0] % 2]
            gen_i[0] += 1
            return PS[bnk][:, 0:C], "ps%d" % bnk

        ring_i = [0]

        def wload(src_ap, ncols=128):
            i = ring_i[0] % 4
            ring_i[0] += 1
            key = "wr%d" % i
            sc.dma("sp", lambda: SP.dma_start(out=wr[i][:, :, 0:ncols], in_=src_ap), reads=["wscr"], writes=[key], ch=key)
            return wr[i], key

        class WStream:
            def __init__(self, blocks, depth=3):
                self.blocks = list(blocks)
                self.pend = []
                self.depth = depth
                for _ in range(depth):
                    self._issue()

            def _issue(self):
                if self.blocks:
                    src, ncols = self.blocks.pop(0)
                    self.pend.append(wload(src, ncols))

            def next(self):
                w, key = self.pend.pop(0)
                self._issue()
                return w, key

        def emit_B(b, c, par):
            sc.in_b_thread = threading.current_thread()
            sc.b_active = True
            t0 = c * C
            xT = xTs[par]
            xk = "xT%d" % par
            if c == 0:
                sc.op("dve", lambda: DVE.memset(xl[:], 0.0), writes=["xl0", "xl1", "xl2", "xl3"])
                sc.op("dve", lambda: DVE.memset(hst[:], 0.0), writes=["hst"])
            with ExitStack() as mes:
                gT = sb(mes, "gT", [128, 4, C], F32)
                qlat = sb(mes, "qlat", [128, 2, C], F32)
                kvlat = sb(mes, "kvlat", [128, C], F32)
                qs = sb(mes, "qs", [128, 2, C], BF16)
                kvs = sb(mes, "kvs", [128, C], BF16)
                xc = sb(mes, "xc", [128, C], F32)
                xcb = sb(mes, "xcb", [128, C], BF16)
                ra = sb(mes, "ra", [128, C], F32)
                ib = sb(mes, "ib", [128, C], F32)
                hh = sb(mes, "hh", [128, C], F32)
                ylru = sb(mes, "ylru", [128, 4, C], F32)
                yT = sb(mes, "yT", [128, 8, C], BF16)
                QT = sb(mes, "QT", [96, 8, C], BF16)
                posi = sb(mes, "posi", [96, C], I32)
                ang = sb(mes, "ang", [96, C], F32)
                kf = sb(mes, "kf", [96, C], F32)
                cos2 = sb(mes, "cos2", [96, C], F32)
                sin2 = sb(mes, "sin2", [96, C], F32)
                t1 = sb(mes, "t1", [96, C], F32)
                t2 = sb(mes, "t2", [96, C], F32)
                krb = sb(mes, "krb", [96, C], BF16)
                pT = [sb(mes, "pT%d" % i, [128, C], BF16) for i in range(2)]
                ymla = sb(mes, "ymla", [128, 2, 512], F32)
                ymn = sb(mes, "ymn", [128, 2, 512], BF16)
                rinv = sb(mes, "rinv", [128, 2], F32)
                sst = sb(mes, "sst", [128, 2], F32)

                win_blocks = [(winb_d[oc, :, :, :], 128) for oc in range(11)] + [(winb_d[11, :, :, 0:96], 96), (winb_d[12, :, :, 0:96], 96)]
                wst = WStream(win_blocks)
                sc.dma("sp", lambda: SP.dma_start(out=xT[:], in_=xT_d[b, :, :, t0:t0 + C].rearrange("dc p t -> p dc t")), writes=[xk], ch=xk)
                sc.dma("sp", lambda: SP.dma_start(out=posi[64:96, :], in_=pos_d[b:b + 1, t0:t0 + C].partition_broadcast(32)), writes=["posi"], ch="posi")
                modulated_norm(xT, xk, A1, 0, b)

                R = slice(64, 96)
                sc.op("dve", lambda: DVE.tensor_copy(ang[R, :], posi[R, :]), reads=["posi"], writes=["ang"])
                sc.op("dve", lambda: DVE.tensor_scalar(ang[R, :], ang[R, :], vecs[R, V_INVF:V_INVF + 1], None, op0=ALU.mult), reads=["ang", "vecs"], writes=["ang"])
                for shift, dst, use_sgn in ((0.0, sin2, True), (math.pi / 2, cos2, False)):
                    sc.op("dve", lambda: DVE.tensor_scalar(kf[R, :], ang[R, :], shift, 1.0 / TWO_PI, op0=ALU.add, op1=ALU.mult), reads=["ang"], writes=["kf"])
                    sc.op("dve", lambda: DVE.tensor_copy(posi[R, :], kf[R, :]), reads=["kf"], writes=["posi"])
                    sc.op("dve", lambda: DVE.tensor_copy(kf[R, :], posi[R, :]), reads=["posi"], writes=["kf"])
                    sc.op("dve", lambda: DVE.scalar_tensor_tensor(out=kf[R, :], in0=kf[R, :], scalar=-TWO_PI, in1=ang[R, :], op0=ALU.mult, op1=ALU.add),
                          reads=["kf", "ang"], writes=["kf"])
                    sc.op("dve", lambda: DVE.tensor_scalar(kf[R, :], kf[R, :], shift, None, op0=ALU.add), reads=["kf"], writes=["kf"])
                    sc.op("dve", lambda: DVE.tensor_scalar(kf[R, :], kf[R, :], 3.1415925, -3.1415925, op0=ALU.min, op1=ALU.max), reads=["kf"], writes=["kf"])
                    if use_sgn:
                        sc.op("act", lambda: ACT.activation(out=dst[R, :], in_=kf[R, :], func=AF.Sin, scale=vecs[R, V_SGN:V_SGN + 1]), reads=["kf", "vecs"], writes=["rope"])
                    else:
                        sc.op("act", lambda: ACT.activation(out=dst[R, :], in_=kf[R, :], func=AF.Sin), reads=["kf"], writes=["rope"])
                ROPE = ["rope"]

                def inproj(ncols=128):
                    w, wkey = wst.next()
                    ps, key = gen_ps()
                    for dc in range(DC):
                        sc.op("pe", lambda: PE.matmul(ps[0:ncols, :], w[:, dc, 0:ncols], hT[:, dc, :], start=(dc == 0), stop=(dc == DC - 1)),
                              reads=["hT", wkey], writes=[key])
                    return ps, key
                for ci in range(4):
                    ps, key = inproj()
                    sc.op("act", lambda: ACT.copy(xl[:, ci, 3:3 + C], ps), reads=[key], writes=["xl%d" % ci])
                for ci in range(4):
                    ps, key = inproj()
                    sc.op("act", lambda: ACT.activation(out=gT[:, ci, :], in_=ps, func=AF.Gelu_apprx_tanh), reads=[key], writes=["gT"])
                for kc in range(2):
                    ps, key = inproj()
                    sc.op("dve", lambda: DVE.tensor_copy(qlat[:, kc, :], ps), reads=[key], writes=["qlat"])
                ps, key = inproj()
                sc.op("dve", lambda: DVE.tensor_copy(kvlat[:], ps), reads=[key], writes=["kvlat"])
                ps_kr, key_kr = inproj(96)
                ps_krr, key_krr = inproj(96)
                sc.op("dve", lambda: DVE.tensor_tensor(out=t1[R, :], in0=ps_kr[R, :], in1=cos2[R, :], op=ALU.mult), reads=[key_kr] + ROPE, writes=["t1"])
                sc.op("dve", lambda: DVE.tensor_tensor(out=t2[R, :], in0=ps_krr[R, :], in1=sin2[R, :], op=ALU.mult), reads=[key_krr] + ROPE, writes=["t2"])
                sc.op("dve", lambda: DVE.tensor_tensor(out=krb[R, :], in0=t1[R, :], in1=t2[R, :], op=ALU.add), reads=["t1", "t2"], writes=["krb"])
                for h in range(8):
                    if h % 2 == 0:
                        sc.op("act", lambda: ACT.copy(KT[R, h, t0:t0 + C], krb[R, :]), reads=["krb"], writes=["KT"])
                    else:
                        sc.op("dve", lambda: DVE.tensor_copy(KT[R, h, t0:t0 + C], krb[R, :]), reads=["krb"], writes=["KT"])

                for ci in range(4):
                    xk_ = "xl%d" % ci
                    cw = V_CONVW + ci * 4
                    sc.op("dve", lambda: DVE.tensor_scalar(xc[:], xl[:, ci, 0:C], vcol(cw), vcol(V_CONVB + ci), op0=ALU.mult, op1=ALU.add),
                          reads=[xk_, "vecs"], writes=["xc"])
                    for kk in range(1, 4):
                        sc.op("dve", lambda: DVE.scalar_tensor_tensor(out=xc[:], in0=xl[:, ci, kk:kk + C], scalar=vcol(cw + kk), in1=xc[:], op0=ALU.mult, op1=ALU.add),
                              reads=[xk_, "xc"], writes=["xc"])
                    sc.op("act", lambda: ACT.copy(xl[:, ci, 0:3], xl[:, ci, C:C + 3]), reads=[xk_, "xc"], writes=[xk_])
                    sc.op("act", lambda: ACT.copy(xcb[:], xc[:]), reads=["xc"], writes=["xcb"])
                    ps_r, key_r = gen_ps()
                    sc.op("pe", lambda: PE.matmul(ps_r, wa_bd[:, ci, :], xcb[:], start=True, stop=True), reads=["xcb"], writes=[key_r])
                    ps_i, key_i = gen_ps()
                    sc.op("pe", lambda: PE.matmul(ps_i, wx_bd[:, ci, :], xcb[:], start=True, stop=True), reads=["xcb"], writes=[key_i])
                    sc.op("act", lambda: ACT.activation(out=ra[:], in_=ps_r, func=AF.Sigmoid, bias=vcol(V_BA + ci), scale=1.0), reads=[key_r], writes=["ra"])
                    sc.op("act", lambda: ACT.activation(out=ib[:], in_=ps_i, func=AF.Sigmoid, bias=vcol(V_BX + ci), scale=1.0), reads=[key_i], writes=["ib"])
                    sc.op("act", lambda: ACT.activation(out=hh[:], in_=ra[:], func=AF.Exp, scale=nsp[:, ci:ci + 1]), reads=["ra"], writes=["hh"])
                    sc.op("dve", lambda: DVE.tensor_scalar(ra[:], ra[:], nsp[:, ci:ci + 1], 0.5, op0=ALU.mult, op1=ALU.mult), reads=["ra", "hh"], writes=["ra"])
                    sc.op("act", lambda: ACT.activation(out=ra[:], in_=ra[:], func=AF.Exp), reads=["ra"], writes=["ra"])
                    sc.op("act", lambda: ACT.activation(out=hh[:], in_=hh[:], func=AF.Sqrt, bias=consts[:, 1:2], scale=-1.0), reads=["hh"], writes=["hh"])
                    sc.op("dve", lambda: DVE.tensor_tensor(out=ib[:], in0=ib[:], in1=xc[:], op=ALU.mult), reads=["ib", "xc"], writes=["ib"])
                    sc.op("dve", lambda: DVE.tensor_tensor(out=ib[:], in0=ib[:], in1=hh[:], op=ALU.mult), reads=["ib", "hh"], writes=["ib"])
                    sc.op("dve", lambda: DVE.tensor_tensor_scan(out=hh[:], data0=ra[:], data1=ib[:], initial=hst[:, ci:ci + 1], op0=ALU.mult, op1=ALU.add),
                          reads=["ra", "ib", "hst"], writes=["hh"], cost=0.6)
                    sc.op("dve", lambda: DVE.tensor_copy(hst[:, ci:ci + 1], hh[:, C - 1:C]), reads=["hh"], writes=["hst"])
                    sc.op("dve", lambda: DVE.tensor_tensor(out=ylru[:, ci, :], in0=hh[:], in1=gT[:, ci, :], op=ALU.mult), reads=["hh", "gT"], writes=["ylru"])
                rms_stats(ylru[:], 4, 1.0 / 512, "ylru")
                for ci in range(4):
                    sc.op("dve", lambda: DVE.scalar_tensor_tensor(out=yT[:, ci, :], in0=ylru[:, ci, :], scalar=vcol(V_LOG + ci), in1=rstd[:], op0=ALU.mult, op1=ALU.mult),
                          reads=["ylru", "rstd"], writes=["yT"])

                rms_stats(qlat[:], 2, 1.0 / 256, "qlat")
                for kc in range(2):
                    sc.op("dve", lambda: DVE.scalar_tensor_tensor(out=qs[:, kc, :], in0=qlat[:, kc, :], scalar=vcol(V_QNG + kc), in1=rstd[:], op0=ALU.mult, op1=ALU.mult),
                          reads=["qlat", "rstd"], writes=["qs"])
                rms_stats(kvlat[:].unsqueeze(1), 1, 1.0 / 128, "kvlat")
                sc.op("dve", lambda: DVE.scalar_tensor_tensor(out=kvs[:], in0=kvlat[:], scalar=vcol(V_KVNG), in1=rstd[:], op0=ALU.mult, op1=ALU.mult),
                      reads=["kvlat", "rstd"], writes=["kvs"])
                for h in range(8):
                    ps_q, key_q = gen_ps()
                    ps_qr, key_qr = gen_ps()
                    for kc in range(2):
                        sc.op("pe", lambda: PE.matmul(ps_q[0:96, :], w_uq[:, kc, h * 192:h * 192 + 96], qs[:, kc, :], start=(kc == 0), stop=(kc == 1)), reads=["qs"], writes=[key_q])
                    for kc in range(2):
                        sc.op("pe", lambda: PE.matmul(ps_qr[0:96, :], w_uq[:, kc, h * 192 + 96:h * 192 + 192], qs[:, kc, :], start=(kc == 0), stop=(kc == 1)), reads=["qs"], writes=[key_qr])
                    sc.op("act", lambda: ACT.copy(QT[0:64, h, :], ps_q[0:64, :]), reads=[key_q], writes=["QT"])
                    sc.op("dve", lambda: DVE.tensor_tensor(out=t1[R, :], in0=ps_q[R, :], in1=cos2[R, :], op=ALU.mult), reads=[key_q] + ROPE, writes=["t1"])
                    sc.op("dve", lambda: DVE.tensor_tensor(out=t2[R, :], in0=ps_qr[R, :], in1=sin2[R, :], op=ALU.mult), reads=[key_qr] + ROPE, writes=["t2"])
                    sc.op("dve", lambda: DVE.tensor_tensor(out=QT[R, h, :], in0=t1[R, :], in1=t2[R, :], op=ALU.add), reads=["t1", "t2"], writes=["QT"])
                    ps_k, key_k = gen_ps()
                    sc.op("pe", lambda: PE.matmul(ps_k[0:64, :], w_ukv[:, h * 128:h * 128 + 64], kvs[:], start=True, stop=True), reads=["kvs"], writes=[key_k])
                    sc.op("act", lambda: ACT.copy(KT[0:64, h, t0:t0 + C], ps_k[0:64, :]), reads=[key_k], writes=["KT"])
                wv = w_ukv[:, :].rearrange("p (h x) -> p h x", x=128)[:, :, 64:128]
                for j in range(C // 128):
                    tile_i = (t0 // 128) + j
                    sc.op("pe", lambda: PE.matmul(PS[4][:, :].rearrange("p (h x) -> p h x", x=64), kvs[:, j * 128:(j + 1) * 128], wv, start=True, stop=True),
                          reads=["kvs"], writes=["ps4"])
                    sc.op("dve", lambda: DVE.tensor_copy(VC[:, tile_i, :, 0:64], PS[4][:, :].rearrange("p (h x) -> p h x", x=64)), reads=["ps4"], writes=["VC"])

                wso = WStream([(woutb_d[oc, :, :, :], 128) for oc in range(8)])

                scale = 96.0 ** -0.5
                nkt = (t0 + C) // 128
                kdiag0 = t0 // 128
                it = 0
                abk = (1, 2)
                akeys = ["ps1", "ps2"]
                for h in range(8):
                    for kt in range(nkt):
                        sbank = 4 + (it % 2)
                        skey = "ps%d" % sbank
                        pt = pT[it % 2]
                        pkey = "pT%d" % (it % 2)
                        it += 1
                        sc.op("pe", lambda: PE.matmul(PS[sbank][:, 0:C], KT[0:96, h, kt * 128:(kt + 1) * 128], QT[0:96, h, :], start=True, stop=True),
                              reads=["KT", "QT"], writes=[skey])
                        sc.op("act", lambda: ACT.activation(out=pt[:], in_=PS[sbank][:, 0:C], func=AF.Exp, scale=scale), reads=[skey], writes=[pkey])
                        jk = kt - kdiag0
                        if jk >= 0:
                            sc.op("dve", lambda: DVE.tensor_tensor(out=pt[:, jk * 128:(jk + 1) * 128], in0=pt[:, jk * 128:(jk + 1) * 128], in1=tri_b[:], op=ALU.mult),
                                  reads=[pkey], writes=[pkey])
                        for jq in range(C // 128):
                            if jk > jq:
                                continue
                            last = kdiag0 + jq
                            sc.op("pe", lambda: PE.matmul(PS[abk[jq]][:, 0:65], pt[:, jq * 128:(jq + 1) * 128], VC[:, kt, h, :], start=(kt == 0), stop=(kt == last)),
                                  reads=[pkey, "VC"], writes=[akeys[jq]])
                    for jq in range(C // 128):
                        sc.op("dve", lambda: DVE.reciprocal(rinv[:, jq:jq + 1], PS[abk[jq]][:, 64:65]), reads=[akeys[jq]], writes=["rinv"])
                        sc.op("dve", lambda: DVE.tensor_scalar(ymla[:, jq, h * 64:(h + 1) * 64], PS[abk[jq]][:, 0:64], rinv[:, jq:jq + 1], None, op0=ALU.mult),
                              reads=[akeys[jq], "rinv"], writes=["ymla"])
                for jq in range(C // 128):
                    sc.op("dve", lambda: DVE.scalar_tensor_tensor(out=ymn[:, jq, :], in0=ymla[:, jq, :], scalar=1.0, in1=ymla[:, jq, :], op0=ALU.mult, op1=ALU.mult, accum_out=sst[:, jq:jq + 1]),
                          reads=["ymla"], writes=["ymn", "sst"])
                sc.op("act", lambda: ACT.activation(out=sst[:], in_=sst[:], func=AF.Sqrt, bias=consts[:, 0:1], scale=1.0 / 512), reads=["sst"], writes=["sst"])
                sc.op("dve", lambda: DVE.reciprocal(sst[:], sst[:]), reads=["sst"], writes=["sst"])
                for jq in range(C // 128):
                    sc.op("dve", lambda: DVE.tensor_scalar(ymn[:, jq, :], ymla[:, jq, :], sst[:, jq:jq + 1], None, op0=ALU.mult), reads=["ymla", "sst"], writes=["ymn"])
                    for fc in range(4):
                        sc.op("pe", lambda: PE.transpose(PS3[:, fc * 128:(fc + 1) * 128], ymn[:, jq, fc * 128:(fc + 1) * 128], ident_b[:]), reads=["ymn"], writes=["ps3"])
                    for fc in range(4):
                        sc.op("dve", lambda: DVE.tensor_scalar(yT[:, 4 + fc, jq * 128:(jq + 1) * 128], PS3[:, fc * 128:(fc + 1) * 128], vcol(V_MOG + fc), None, op0=ALU.mult),
                              reads=["ps3"], writes=["yT"])
                for oc in range(DC):
                    w, wkey = wso.next()
                    ps, key = gen_ps()
                    for cc in range(8):
                        sc.op("pe", lambda: PE.matmul(ps, w[:, cc, :], yT[:, cc, :], start=(cc == 0), stop=(cc == 7)), reads=["yT", wkey], writes=[key])
                    sc.op("dve", lambda: DVE.scalar_tensor_tensor(out=xT[:, oc, :], in0=ps, scalar=modT[:, 16 + oc, b:b + 1], in1=xT[:, oc, :], op0=ALU.mult, op1=ALU.add),
                          reads=[key, xk], writes=[xk])
                sc.barrier_b()

            with ExitStack() as pes2:
                qT = sb(pes2, "qT", [128, 16, 128], F32)
                scs = sb(pes2, "scs", [128, 2048], F32)
                work = sb(pes2, "work", [128, 2048], F32)
                top = sb(pes2, "top", [128, 16, 16], F32)
                tix = sb(pes2, "tix", [128, 16, 16], U32)
                tixf = sb(pes2, "tixf", [128, 16, 16], F32)
                best = sb(pes2, "best", [128, 8, 16], F32)
                posu = sb(pes2, "posu", [128, 8, 16], U32)
                pa_i = sb(pes2, "pa_i", [128, 8, 16], I32)
                pa_f = sb(pes2, "pa_f", [128, 8, 16], F32)
                pb_f = sb(pes2, "pb_f", [128, 8, 16], F32)
                gsum = sb(pes2, "gsum", [128, 8], F32)
                isel = sb(pes2, "isel", [128, 8, 16], F32)
                jsel = sb(pes2, "jsel", [128, 8, 16], F32)

                modulated_norm(xT, xk, A2, 24, b)
                for j in range(C // 128):
                    ts = slice(j * 128, (j + 1) * 128)
                    h2tm = h2s[par][j]
                    gg = ggs[par][j]
                    idxi = idxs[par][j]
                    hkey, gkey_, ikey = "h2%d%d" % (par, j), "gg%d%d" % (par, j), "idx%d%d" % (par, j)
                    wsq = WStream([(wqb_d[hp, :, :, :], 128) for hp in range(16)])
                    for dc in range(DC):
                        sc.op("pe", lambda: PE.transpose(PS3[:, dc * 128:(dc + 1) * 128], hT[:, dc, ts], ident_b[:]), reads=["hT"], writes=["ps3"])
                    sc.op("act", lambda: ACT.copy(h2tm[:], PS3[:, :]), reads=["ps3"], writes=[hkey], cost=1.0)
                    for hp in range(16):
                        w, wkey = wsq.next()
                        pq = PS[0][:, 0:128]
                        for dc in range(DC):
                            sc.op("pe", lambda: PE.matmul(pq, w[:, dc, :], hT[:, dc, ts], start=(dc == 0), stop=(dc == DC - 1)), reads=["hT", wkey], writes=["ps0"])
                        if hp % 2 == 0:
                            sc.op("act", lambda: ACT.copy(qT[:, hp, :], pq), reads=["ps0"], writes=["qT%d" % hp])
                        else:
                            sc.op("dve", lambda: DVE.tensor_copy(qT[:, hp, :], pq), reads=["ps0"], writes=["qT%d" % hp])
                    sbanks = (1, 2, 4, 5)
                    for hp in range(16):
                        bnk = sbanks[hp // 4]
                        sc.op("pe", lambda: PE.matmul(PS[bnk][:, (hp % 4) * 128:(hp % 4 + 1) * 128], qT[:, hp, :], keysT[:, hp, :], start=True, stop=True),
                              reads=["qT%d" % hp], writes=["ps%d" % bnk])
                    for q in range(4):
                        bnk = sbanks[q]
                        if q % 2 == 0:
                            sc.op("act", lambda: ACT.copy(scs[:, q * 512:(q + 1) * 512], PS[bnk][:, :]), reads=["ps%d" % bnk], writes=["scs%d" % q])
                        else:
                            sc.op("dve", lambda: DVE.tensor_copy(scs[:, q * 512:(q + 1) * 512], PS[bnk][:, :]), reads=["ps%d" % bnk], writes=["scs%d" % q])
                    TOPK = ["top%d" % hp for hp in range(16)]
                    TIXK = ["tix%d" % hp for hp in range(16)]
                    WRKK = ["work%d" % hp for hp in range(16)]
                    sks = ["scs%d" % (hp // 4) for hp in range(16)]
                    svs = [scs[:, hp * 128:(hp + 1) * 128] for hp in range(16)]
                    wvs = [work[:, hp * 128:(hp + 1) * 128] for hp in range(16)]
                    for hp in range(16):
                        sc.op("dve", lambda: DVE.max(out=top[:, hp, 0:8], in_=svs[hp]), reads=[sks[hp]], writes=[TOPK[hp]], cost=0.2)
                    for hp in range(16):
                        sc.op("dve", lambda: DVE.max_index(out=tix[:, hp, 0:8], in_max=top[:, hp, 0:8], in_values=svs[hp]), reads=[sks[hp], TOPK[hp]], writes=[TIXK[hp]], cost=0.25)
                    for hp in range(16):
                        sc.op("dve", lambda: DVE.match_replace(out=wvs[hp], in_to_replace=top[:, hp, 0:8], in_values=svs[hp], imm_value=NEG), reads=[sks[hp], TOPK[hp]], writes=[WRKK[hp]], cost=0.25)
                    for hp in range(16):
                        sc.op("dve", lambda: DVE.max(out=top[:, hp, 8:16], in_=wvs[hp]), reads=[WRKK[hp]], writes=[TOPK[hp]], cost=0.2)
                    for hp in range(16):
                        sc.op("dve", lambda: DVE.max_index(out=tix[:, hp, 8:16], in_max=top[:, hp, 8:16], in_values=wvs[hp]), reads=[WRKK[hp], TOPK[hp]], writes=[TIXK[hp]], cost=0.25)
                    sc.op("dve", lambda: DVE.tensor_copy(tixf[:], tix[:]), reads=TIXK, writes=["tixf"])
                    top4 = top[:].rearrange("p (h two) k -> p h two k", two=2)
                    tix4 = tixf[:].rearrange("p (h two) k -> p h two k", two=2)
                    cand = work[:].rearrange("p (h a b) -> p h a b", a=16, b=16)
                    cand3 = work[:].rearrange("p (h ab) -> p h ab", ab=256)
                    cand2 = scs[:].rearrange("p (h ab) -> p h ab", ab=256)
                    ALLS = ["scs0", "scs1", "scs2", "scs3"]
                    WH = [[WRKK[2 * h], WRKK[2 * h + 1]] for h in range(8)]
                    SH = ["scs%d" % (h // 2) for h in range(8)]
                    BK = ["best%d" % h for h in range(8)]
                    PK = ["posu%d" % h for h in range(8)]
                    sc.op("dve", lambda: DVE.tensor_tensor(out=cand, in0=top4[:, :, 0, :].unsqueeze(3).to_broadcast([128, 8, 16, 16]),
                                                           in1=top4[:, :, 1, :].unsqueeze(2).to_broadcast([128, 8, 16, 16]), op=ALU.add),
                          reads=TOPK, writes=WRKK, cost=2.2)
                    for h in range(8):
                        sc.op("dve", lambda: DVE.max(out=best[:, h, 0:8], in_=cand3[:, h, :]), reads=WH[h], writes=[BK[h]], cost=0.35)
                    for h in range(8):
                        sc.op("dve", lambda: DVE.max_index(out=posu[:, h, 0:8], in_max=best[:, h, 0:8], in_values=cand3[:, h, :]), reads=WH[h] + [BK[h]], writes=[PK[h]], cost=0.4)
                    for h in range(8):
                        sc.op("dve", lambda: DVE.match_replace(out=cand2[:, h, :], in_to_replace=best[:, h, 0:8], in_values=cand3[:, h, :], imm_value=NEG), reads=WH[h] + [BK[h]], writes=[SH[h]], cost=0.4)
                    for h in range(8):
                        sc.op("dve", lambda: DVE.max(out=best[:, h, 8:16], in_=cand2[:, h, :]), reads=[SH[h]], writes=[BK[h]], cost=0.35)
                    for h in range(8):
                        sc.op("dve", lambda: DVE.max_index(out=posu[:, h, 8:16], in_max=best[:, h, 8:16], in_values=cand2[:, h, :]), reads=[SH[h], BK[h]], writes=[PK[h]], cost=0.4)
                    sc.op("dve", lambda: DVE.tensor_copy(pb_f[:], posu[:]), reads=PK, writes=["pb_f"])
                    sc.op("dve", lambda: DVE.tensor_scalar(pa_f[:], pb_f[:], 1.0 / 16, -0.46875, op0=ALU.mult, op1=ALU.add), reads=["pb_f"], writes=["pa_f"])
                    sc.op("dve", lambda: DVE.tensor_copy(pa_i[:], pa_f[:]), reads=["pa_f"], writes=["pa_i"])
                    sc.op("dve", lambda: DVE.tensor_copy(pa_f[:], pa_i[:]), reads=["pa_i"], writes=["pa_f"])
                    sc.op("dve", lambda: DVE.scalar_tensor_tensor(out=pb_f[:], in0=pa_f[:], scalar=-16.0, in1=pb_f[:], op0=ALU.mult, op1=ALU.add), reads=["pa_f", "pb_f"], writes=["pb_f"])
                    sc.op("dve", lambda: DVE.tensor_tensor(out=gg[:], in0=best[:], in1=best[:, :, 0:1].to_broadcast([128, 8, 16]), op=ALU.subtract), reads=BK, writes=[gkey_])
                    sc.op("act", lambda: ACT.activation(out=gg[:], in_=gg[:], func=AF.Exp), reads=[gkey_], writes=[gkey_])
                    sc.op("dve", lambda: DVE.tensor_reduce(out=gsum[:], in_=gg[:], axis=AX.X, op=ALU.add), reads=[gkey_], writes=["gsum"])
                    sc.op("dve", lambda: DVE.reciprocal(gsum[:], gsum[:]), reads=["gsum"], writes=["gsum"])
                    sc.op("dve", lambda: DVE.tensor_tensor(out=gg[:], in0=gg[:], in1=gsum[:].unsqueeze(2).to_broadcast([128, 8, 16]), op=ALU.mult), reads=[gkey_, "gsum"], writes=[gkey_])
                    eq = work[:].rearrange("p (h k a) -> p h k a", k=16, a=16)
                    io4 = iota16[:].unsqueeze(1).unsqueeze(1).to_broadcast([128, 8, 16, 16])
                    for (pf, two, dst) in ((pa_f, 0, isel), (pb_f, 1, jsel)):
                        sc.op("dve", lambda: DVE.tensor_tensor(out=eq, in0=io4, in1=pf[:].unsqueeze(3).to_broadcast([128, 8, 16, 16]), op=ALU.is_equal),
                              reads=["pa_f", "pb_f"] + BK + PK, writes=WRKK, cost=2.2)
                        sc.op("dve", lambda: DVE.tensor_tensor(out=eq, in0=eq, in1=tix4[:, :, two, :].unsqueeze(2).to_broadcast([128, 8, 16, 16]), op=ALU.mult),
                              reads=WRKK + ["tixf"], writes=WRKK, cost=2.2)
                        sc.op("dve", lambda: DVE.tensor_reduce(out=dst[:], in_=eq, axis=AX.X, op=ALU.add), reads=WRKK, writes=["sel%d" % two], cost=2.2)
                    sc.op("dve", lambda: DVE.scalar_tensor_tensor(out=isel[:], in0=isel[:], scalar=128.0, in1=jsel[:], op0=ALU.mult, op1=ALU.add),
                          reads=["sel0", "sel1"], writes=["sel0"])
                    sc.op("dve", lambda: DVE.tensor_copy(idxi[:], isel[:].rearrange("p h k -> p (h k)")), reads=["sel0"], writes=[ikey])
                sc.barrier_b()
            sc.b_active = False

        def emit_A(b, c, par, give):
            t0 = c * C
            xT = xTs[par]
            xk = "xT%d" % par
            for j in range(C // 128):
                ts = slice(j * 128, (j + 1) * 128)
                h2tm = h2s[par][j]
                ggf = ggs[par][j][:].rearrange("p h k -> p (h k)")
                idxi = idxs[par][j]
                hkey, gkey_, ikey = "h2%d%d" % (par, j), "gg%d%d" % (par, j), "idx%d%d" % (par, j)
                for step in range(130):
                    hk = step
                    if hk < 128:
                        Gb = G[hk % NBUF]
                        gkey = "G%d" % (hk % NBUF)
                        sc.dma("pool", lambda: POOL.indirect_dma_start(out=Gb[:], out_offset=None, in_=uvb_d,
                                                                       in_offset=bass.IndirectOffsetOnAxis(ap=idxi[:, hk:hk + 1], axis=0)),
                               reads=[ikey, "uvscr"], writes=[gkey], ch=gkey, cost=1.9)
                        sc.op("dve", lambda: DVE.scalar_tensor_tensor(out=junkb[hk % 2], in0=Gb[:, 0:D], scalar=1.0, in1=h2tm[:], op0=ALU.mult, op1=ALU.mult, accum_out=dots[:, hk:hk + 1]),
                              reads=[gkey, hkey], writes=["dots%d" % hk, "junkb%d" % (hk % 2)], cost=1.3)
                        sc.op("act", lambda: ACT.activation(out=acts[:, hk:hk + 1], in_=dots[:, hk:hk + 1], func=AF.Gelu_apprx_tanh), reads=["dots%d" % hk], writes=["acts%d" % hk])
                    k1 = step - 1
                    if 0 <= k1 < 128:
                        sc.op("act", lambda: ACT.activation(out=zz[:, k1:k1 + 1], in_=acts[:, k1:k1 + 1], func=AF.Identity, scale=ggf[:, k1:k1 + 1]),
                              reads=["acts%d" % k1, gkey_], writes=["zz%d" % k1])
                    k2 = step - 2
                    if 0 <= k2 < 128:
                        Gp = G[k2 % NBUF]
                        gpkey = "G%d" % (k2 % NBUF)
                        dg = diag[k2 % 3]
                        dkey = "diag%d" % (k2 % 3)
                        sc.op("act", lambda: ACT.activation(out=dg[:], in_=ident_b[:], func=AF.Identity, scale=zz[:, k2:k2 + 1]),
                              reads=["zz%d" % k2], writes=[dkey])
                        for half in range(2):
                            bnk = 6 + half
                            sc.op("pe", lambda: PE.matmul(PS[bnk][:, :], dg[:], Gp[:, D + half * 512:D + (half + 1) * 512], start=(k2 == 0), stop=(k2 == 127)),
                                  reads=[dkey, gpkey], writes=["ps%d" % bnk])
                    give()
                sc.op("act", lambda: ACT.copy(petm[:, 0:512], PS[6][:, :]), reads=["ps6"], writes=["petm"])
                sc.op("dve", lambda: DVE.tensor_copy(petm[:, 512:1024], PS[7][:, :]), reads=["ps7"], writes=["petm"])
                for dc in range(DC):
                    bnk = 6 + dc // 4
                    sc.op("pe", lambda: PE.transpose(PS[bnk][:, (dc % 4) * 128:(dc % 4 + 1) * 128], petm[:, dc * 128:(dc + 1) * 128], ident_f[:]), reads=["petm"], writes=["ps%d" % bnk])
                for dc in range(DC):
                    bnk = 6 + dc // 4
                    sc.op("dve", lambda: DVE.scalar_tensor_tensor(out=xT[:, dc, ts], in0=PS[bnk][:, (dc % 4) * 128:(dc % 4 + 1) * 128], scalar=modT[:, 40 + dc, b:b + 1], in1=xT[:, dc, ts], op0=ALU.mult, op1=ALU.add),
                          reads=["ps%d" % bnk, xk], writes=[xk])
                give()
            rms_stats(xT[:], DC, 1.0 / D, xk, sq_t=sqA, rstd_t=rstdA, bank=6, sfx="A", sqkeys=["junkb0", "junkb1"])
            for dc in range(DC):
                ot = otmp[dc % 2]
                okey = "otmp%d" % (dc % 2)
                sc.op("dve", lambda: DVE.scalar_tensor_tensor(out=ot[:], in0=xT[:, dc, :], scalar=vcol(V_FG + dc), in1=rstdA[:], op0=ALU.mult, op1=ALU.mult),
                      reads=[xk, "rstdA"], writes=[okey])
                sc.dma("sp", lambda: SP.dma_start(out=outT_d[b, dc, :, t0:t0 + C], in_=ot[:]), reads=[okey], writes=["outd"], ch=okey)
            give()

        chunks = [(b, c) for b in range(nseq) for c in range(nch)]
        emit_B(chunks[0][0], chunks[0][1], 0)
        last_b_count = 2200
        for i, (b, c) in enumerate(chunks):
            par = i % 2
            if overlap and i + 1 < len(chunks):
                nb, ncn = chunks[i + 1]
                co.start(lambda nb=nb, ncn=ncn, par=par: emit_B(nb, ncn, 1 - par))
                q = 200
                emit_A(b, c, par, lambda q=q: co.give(q))
                last_b_count = max(co.count, 1) if co.done else last_b_count
                co.drain()
                last_b_count = max(co.count, 1)
            else:
                emit_A(b, c, par, lambda: None)
                if i + 1 < len(chunks):
                    nb, ncn = chunks[i + 1]
                    emit_B(nb, ncn, 1 - par)
        sc.finish()
    return nc


def _pack_inputs(inp, core, nseq=NSEQ):
    f32 = np.float32
    b0 = core * nseq
    x = inp["x"][b0:b0 + nseq]
    xT = np.ascontiguousarray(np.transpose(x, (0, 2, 1))).reshape(nseq, DC, 128, S)
    c = inp["c"][b0:b0 + nseq]
    cT = np.ascontiguousarray(c.T.reshape(DC, 128, nseq).transpose(1, 0, 2))
    pos = np.ascontiguousarray(inp["positions"][b0:b0 + nseq]).astype(np.int32)
    return {"xT": xT.astype(f32), "cT": cT.astype(f32), "pos": pos}


def _pack_weights(inp):
    f32 = np.float32
    col = lambda v, n: np.ascontiguousarray(np.asarray(v, f32).reshape(n, 128).T)
    vecs = np.zeros((128, NV), f32)
    vecs[:, V_N1G:V_N1G + 8] = col(inp["norm1_g"][0], 8)
    vecs[:, V_N2G:V_N2G + 8] = col(inp["norm2_g"][0], 8)
    vecs[:, V_FG:V_FG + 8] = col(inp["final_g"], 8)
    cw = np.asarray(inp["conv_w"][0], f32)
    for ci in range(4):
        for k in range(4):
            vecs[:, V_CONVW + ci * 4 + k] = cw[k, ci * 128:(ci + 1) * 128]
    vecs[:, V_CONVB:V_CONVB + 4] = col(inp["conv_b"][0], 4)
    vecs[:, V_BA:V_BA + 4] = col(inp["lru_ba"][0], 4)
    vecs[:, V_BX:V_BX + 4] = col(inp["lru_bx"][0], 4)
    vecs[:, V_LAM:V_LAM + 4] = col(inp["lru_lambda"][0], 4)
    vecs[:, V_QNG:V_QNG + 2] = col(inp["q_norm_g"][0], 2)
    vecs[:, V_KVNG:V_KVNG + 1] = col(inp["kv_norm_g"][0], 1)
    vecs[:, V_LOG:V_LOG + 4] = col(inp["lru_out_g"][0], 4)
    vecs[:, V_MOG:V_MOG + 4] = col(inp["mla_out_g"][0], 4)
    vecs[:, V_BADA:V_BADA + 48] = col(inp["b_ada"][0], 48)
    inv_freq = (1.0 / (10000.0 ** (np.arange(0, 32, 2, dtype=np.float32) / np.float32(32)))).astype(f32)
    for p in range(64, 96):
        vecs[p, V_INVF] = inv_freq[(p - 64) % 16]
        vecs[p, V_SGN] = -1.0 if p < 80 else 1.0
    w_in = np.asarray(inp["w_in"][0], f32)
    kr = w_in[:, 1408:1440]
    w_in_ext = np.concatenate([w_in, kr[:, 16:32], kr[:, 0:16]], axis=1)
    wa = np.asarray(inp["lru_wa"][0], f32)
    wx = np.asarray(inp["lru_wx"][0], f32)
    wa_bd = np.zeros((4, 128, 128), f32)
    wx_bd = np.zeros((4, 128, 128), f32)
    for ci in range(4):
        for s in range(2):
            wa_bd[ci, s * 64:(s + 1) * 64, s * 64:(s + 1) * 64] = wa[2 * ci + s]
            wx_bd[ci, s * 64:(s + 1) * 64, s * 64:(s + 1) * 64] = wx[2 * ci + s]
    w_uq = np.asarray(inp["w_uq"][0], f32)
    parts = []
    for h in range(8):
        blk = w_uq[:, h * 96:(h + 1) * 96]
        parts += [blk, blk[:, 0:64], blk[:, 80:96], blk[:, 64:80]]
    w_uq_ext = np.concatenate(parts, axis=1)
    keys = np.asarray(inp["peer_keys"][0], f32)
    keysT = np.ascontiguousarray(keys.reshape(16, 128, 128).transpose(2, 0, 1))
    uv = np.concatenate([np.asarray(inp["peer_u"][0], f32), np.asarray(inp["peer_v"][0], f32)], axis=1)
    return {
        "w_ada": np.ascontiguousarray(np.asarray(inp["w_ada"][0], f32).reshape(DC, 128, 6 * D)),
        "vecs": vecs,
        "w_in": np.ascontiguousarray(w_in_ext.reshape(DC, 128, WIN_COLS)),
        "wa_bd": wa_bd, "wx_bd": wx_bd,
        "w_uq": np.ascontiguousarray(w_uq_ext.reshape(2, 128, 1536)),
        "w_ukv": np.ascontiguousarray(np.asarray(inp["w_ukv"][0], f32)),
        "w_out": np.ascontiguousarray(np.asarray(inp["w_out"][0], f32).reshape(DC, 128, D)),
        "peer_wq": np.ascontiguousarray(np.asarray(inp["peer_wq"][0], f32).reshape(DC, 128, 2048)),
        "keysT": keysT,
        "uv": np.ascontiguousarray(uv),
    }


def kernel(**inputs):
    inp = {k: np.asarray(v) for k, v in inputs.items()}
    nc = build_program(NSEQ, NCH)
    wts = _pack_weights(inp)
    in_maps = []
    for core in range(NCORES):
        m = dict(wts)
        m.update(_pack_inputs(inp, core))
        in_maps.append(m)
    res = run_bass_kernel_spmd(nc, in_maps, core_ids=list(range(NCORES)))
    outs = []
    for core in range(NCORES):
        oT = np.asarray(res.results[core]["outT"]).reshape(NSEQ, D, S)
        outs.append(np.transpose(oT, (0, 2, 1)))
    return np.ascontiguousarray(np.concatenate(outs, axis=0)).astype(np.float32)
```

```python
from contextlib import ExitStack
import threading
import math
import numpy as np
import concourse.bass as bass
import concourse.mybir as mybir
from concourse.bass_utils import run_bass_kernel_spmd

F32 = mybir.dt.float32
BF16 = mybir.dt.bfloat16
I32 = mybir.dt.int32
U32 = mybir.dt.uint32
ALU = mybir.AluOpType
AF = mybir.ActivationFunctionType
AX = mybir.AxisListType

D = 1024
S = 2048
NCORES = 8
NSEQ = 4
C = 256
NCH = S // C
DC = 8
EPS = 1e-6
WIN_COLS = 1472
NEG = -1.0e30
TWO_PI = 2.0 * math.pi

V_N1G, V_N2G, V_FG = 0, 8, 16
V_CONVW, V_CONVB, V_BA, V_BX, V_LAM = 24, 40, 44, 48, 52
V_QNG, V_KVNG, V_LOG, V_MOG = 56, 58, 59, 63
V_BADA = 67
V_INVF, V_SGN = 115, 116
NV = 117


class Sched:
    ENG = ("pe", "act", "dve", "pool", "sp")

    def __init__(self, nc, es):
        self.nc = nc
        self.es = es
        self.eng = {"pe": nc.tensor, "act": nc.scalar, "dve": nc.vector, "pool": nc.gpsimd, "sp": nc.sync}
        self.sem = {e: es.enter_context(nc.semaphore("sem_" + e)) for e in self.ENG}
        self.cnt = {e: 0 for e in self.ENG}
        self.seen = {e: {} for e in self.ENG}
        self.last_w = {}
        self.readers = {}
        self.dsem = {}
        self.dcnt = {}
        self.dead = [False]
        self.co = None
        self.last_b = {e: 0 for e in self.ENG}
        self.b_dcnt = {}
        self.in_b = False
        self.b_active = False
        self.tail = {e: 0.0 for e in self.ENG}
        self.tw = {}
        self.tr = {}
        self.DUR = {"pe": 0.15, "act": 0.45, "dve": 0.3, "pool": 0.25, "sp": 0.1}
        self.LAT = 0.3
        self.MARGIN = {"pe": 24.0, "act": 18.0, "dve": 16.0, "pool": 6.0, "sp": 60.0}

    def _deps(self, reads, writes):
        need = {}
        def add(tok):
            k, v = tok
            if need.get(k, 0) < v:
                need[k] = v
        for k in list(reads) + list(writes):
            t = self.last_w.get(k)
            if t is not None:
                add(t)
        for k in writes:
            for tok in self.readers.get(k, {}).items():
                add(tok)
        return need

    def _emit_waits(self, e, need):
        eng = self.eng[e]
        seen = self.seen[e]
        for k, v in need.items():
            if k == e and e == "pe":
                continue
            if seen.get(k, 0) >= v:
                continue
            if k in self.sem:
                eng.wait_ge(self.sem[k], v)
            else:
                eng.wait_ge(self.dsem[k], v)
            seen[k] = v

    def _record(self, tok, reads, writes):
        for k in writes:
            self.last_w[k] = tok
            self.readers[k] = {}
        for k in reads:
            r = self.readers.setdefault(k, {})
            if r.get(tok[0], 0) < tok[1]:
                r[tok[0]] = tok[1]

    def _ready(self, reads, writes):
        t = 0.0
        for k in list(reads) + list(writes):
            v = self.tw.get(k)
            if v is not None and v > t:
                t = v
        for k in writes:
            v = self.tr.get(k)
            if v is not None and v > t:
                t = v
        return t

    def _model(self, e, reads, writes, dur, extra=0.0):
        start = max(self._ready(reads, writes) + self.LAT, self.tail[e])
        fin = start + dur
        self.tail[e] = fin
        for k in writes:
            self.tw[k] = fin + extra
            self.tr[k] = 0.0
        for k in reads:
            if self.tr.get(k, 0.0) < fin + extra:
                self.tr[k] = fin + extra

    def op(self, e, fn, reads=(), writes=(), cost=None):
        dur = self.DUR[e] if cost is None else cost
        if self.co is not None:
            self.co.tick(self, e, reads, writes, dur)
        self.in_b = threading.current_thread() is getattr(self, "in_b_thread", None) and self.b_active
        self._model(e, reads, writes, dur)
        self._emit_waits(e, self._deps(reads, writes))
        ins = fn()
        ins.then_inc(self.sem[e], 1)
        self.cnt[e] += 1
        if self.in_b:
            self.last_b[e] = self.cnt[e]
        self._record((e, self.cnt[e]), reads, writes)

    def dma(self, e, fn, reads=(), writes=(), ch=None, cost=None):
        dur = 0.1 if cost is None else cost
        if self.co is not None:
            self.co.tick(self, e, reads, writes, dur)
        self.in_b = threading.current_thread() is getattr(self, "in_b_thread", None) and self.b_active
        self._model(e, reads, writes, dur, extra=2.5)
        if ch not in self.dsem:
            self.dsem[ch] = self.es.enter_context(self.nc.semaphore("dsem_%d" % len(self.dsem)))
            self.dcnt[ch] = 0
        need = self._deps(reads, writes)
        if self.dcnt[ch]:
            need[ch] = max(need.get(ch, 0), self.dcnt[ch])
        self._emit_waits(e, need)
        ins = fn()
        ins.then_inc(self.dsem[ch], 16)
        self.dcnt[ch] += 16
        if self.in_b:
            self.b_dcnt[ch] = self.dcnt[ch]
        self._record((ch, self.dcnt[ch]), reads, writes)

    def barrier_b(self):
        need = {e: v for e, v in self.last_b.items() if v}
        need.update({ch: v for ch, v in self.b_dcnt.items() if v})
        for e in self.ENG:
            self._emit_waits(e, dict(need))

    def barrier(self):
        need = {e: self.cnt[e] for e in self.ENG if self.cnt[e]}
        for ch, v in self.dcnt.items():
            if v:
                need[ch] = v
        for e in self.ENG:
            self._emit_waits(e, dict(need))
        self.last_w = {}
        self.readers = {}

    def finish(self):
        need = {e: self.cnt[e] for e in self.ENG if self.cnt[e]}
        for ch, v in self.dcnt.items():
            if v:
                need[ch] = v
        self._emit_waits("sp", need)


class Co:
    def __init__(self):
        self.thread = None
        self.quota = 0
        self.b_go = threading.Semaphore(0)
        self.m_go = threading.Semaphore(0)
        self.done = True
        self.exc = None
        self.count = 0
        self.free_run = False
        self.SLACK = 0.3

    def start(self, fn):
        self.done = False
        self.count = 0
        self.exc = None

        def run():
            self.b_go.acquire()
            try:
                fn()
            except BaseException as e:
                self.exc = e
            self.done = True
            self.m_go.release()
        self.thread = threading.Thread(target=run)
        self.thread.start()

    def give(self, q):
        if self.done:
            return
        self.quota = q
        self.free_run = q >= (1 << 50)
        self.b_go.release()
        self.m_go.acquire()
        if self.exc is not None:
            raise self.exc

    def tick(self, sched=None, e=None, reads=(), writes=(), dur=0.0):
        if self.thread is not None and threading.current_thread() is self.thread:
            self.count += 1
            while not getattr(self, "free_run", False):
                self.quota -= 1
                blocked = False
                if sched is not None:
                    fin = max(sched._ready(reads, writes) + sched.LAT, sched.tail[e]) + dur
                    blocked = fin > sched.tail["pool"] + sched.MARGIN[e]
                if self.quota >= 0 and not blocked:
                    break
                self.m_go.release()
                self.b_go.acquire()

    def drain(self):
        while not self.done:
            self.give(1 << 60)
        if self.thread is not None:
            self.thread.join()
            self.thread = None
        if self.exc is not None:
            raise self.exc


def build_program(nseq=NSEQ, nch=NCH, stop=99, overlap=True):
    nc = bass.Bass("TRN2", target_bir_lowering=False)
    dr = lambda name, shape, dt, kind="ExternalInput": nc.dram_tensor(name, shape, dt, kind=kind).ap()
    xT_d = dr("xT", [nseq, DC, 128, S], F32)
    cT_d = dr("cT", [128, DC, nseq], F32)
    pos_d = dr("pos", [nseq, S], I32)
    wada_d = dr("w_ada", [DC, 128, 6 * D], F32)
    vecs_d = dr("vecs", [128, NV], F32)
    win_d = dr("w_in", [DC, 128, WIN_COLS], F32)
    wabd_d = dr("wa_bd", [4, 128, 128], F32)
    wxbd_d = dr("wx_bd", [4, 128, 128], F32)
    wuq_d = dr("w_uq", [2, 128, 1536], F32)
    wukv_d = dr("w_ukv", [128, 1024], F32)
    wout_d = dr("w_out", [DC, 128, D], F32)
    wq_d = dr("peer_wq", [DC, 128, 2048], F32)
    keysT_d = dr("keysT", [128, 16, 128], F32)
    uv_d = dr("uv", [16384, 2048], F32)
    outT_d = dr("outT", [nseq, DC, 128, S], F32, kind="ExternalOutput")
    winb_d = dr("winb", [13, 128, DC, 128], BF16, kind="Internal")
    woutb_d = dr("woutb", [8, 128, DC, 128], BF16, kind="Internal")
    wqb_d = dr("wqb", [16, 128, DC, 128], BF16, kind="Internal")
    uvb_d = dr("uvb", [16384, 2048], BF16, kind="Internal")

    with ExitStack() as es:
        sc = Sched(nc, es)
        co = Co()
        sc.co = co
        PE, ACT, DVE, POOL, SP = nc.tensor, nc.scalar, nc.vector, nc.gpsimd, nc.sync
        uid = [0]

        def sb(stack, name, shape, dt):
            uid[0] += 1
            return stack.enter_context(nc.sbuf_tensor("%s_%d" % (name, uid[0]), shape, dt))

        w_uq = sb(es, "w_uq", [128, 2, 1536], BF16)
        w_ukv = sb(es, "w_ukv", [128, 1024], BF16)
        keysT = sb(es, "keysT", [128, 16, 128], F32)
        wa_bd = sb(es, "wa_bd", [128, 4, 128], BF16)
        wx_bd = sb(es, "wx_bd", [128, 4, 128], BF16)
        vecs = sb(es, "vecs", [128, NV], F32)
        modT = sb(es, "modT", [128, 48, nseq], F32)
        A1 = sb(es, "A1", [128, DC, nseq], F32)
        A2 = sb(es, "A2", [128, DC, nseq], F32)
        nsp = sb(es, "nsp", [128, 4], F32)
        consts = sb(es, "consts", [128, 4], F32)
        ones_bf = sb(es, "ones_bf", [128, 128], BF16)
        ident_f = sb(es, "ident_f", [128, 128], F32)
        ident_b = sb(es, "ident_b", [128, 128], BF16)
        tri_b = sb(es, "tri_b", [128, 128], BF16)
        iota16 = sb(es, "iota16", [128, 16], F32)
        KT = sb(es, "KT", [96, 8, S], BF16)
        VC = sb(es, "VC", [128, S // 128, 8, 65], BF16)
        xl = sb(es, "xl", [128, 4, C + 3], F32)
        hst = sb(es, "hst", [128, 4], F32)
        xTs = [sb(es, "xT%d" % i, [128, DC, C], F32) for i in range(2)]
        hT = sb(es, "hT", [128, DC, C], BF16)
        sq = sb(es, "sq", [128, DC, C], BF16)
        rstd = sb(es, "rstd", [128, C], F32)
        tmpf = sb(es, "tmpf", [128, C], F32)
        wr = [sb(es, "wr%d" % i, [128, DC, 128], BF16) for i in range(4)]
        NBUF = 8
        G = [sb(es, "G%d" % i, [128, 2048], BF16) for i in range(NBUF)]
        junkb_t = sb(es, "junkb", [128, 2, D], BF16)
        junkb = [junkb_t[:, 0, :], junkb_t[:, 1, :]]
        diag = [sb(es, "diag%d" % i, [128, 128], BF16) for i in range(3)]
        dots = sb(es, "dots", [128, 128], F32)
        acts = sb(es, "acts", [128, 128], F32)
        zz = sb(es, "zz", [128, 128], F32)
        petm = sb(es, "petm", [128, D], F32)
        sqA = junkb_t[:, :, :].rearrange("p a (b c) -> p (a b) c", c=C)
        rstdA = sb(es, "rstdA", [128, C], F32)
        otmp = [sb(es, "otmp%d" % i, [128, C], F32) for i in range(2)]
        idxs = [[sb(es, "idx%d%d" % (p, j), [128, 128], I32) for j in range(2)] for p in range(2)]
        ggs = [[sb(es, "gg%d%d" % (p, j), [128, 8, 16], F32) for j in range(2)] for p in range(2)]
        h2s = [[sb(es, "h2%d%d" % (p, j), [128, D], BF16) for j in range(2)] for p in range(2)]

        PS = [es.enter_context(nc.psum_tensor("ps%d" % i, [128, 512], F32)) for i in (0, 1, 2)]
        PS3 = es.enter_context(nc.psum_tensor("ps3", [128, 1024], BF16))
        PS += [None] + [es.enter_context(nc.psum_tensor("ps%d" % i, [128, 512], F32)) for i in (4, 5, 6, 7)]

        def vcol(c0, n=1):
            return vecs[:, c0:c0 + n]

        with ExitStack() as pes:
            stage = sb(pes, "stage", [128, 4096], F32)
            cT = sb(pes, "cT", [128, DC, nseq], F32)
            iot_i = sb(pes, "iot_i", [128, 128], I32)
            iot_f = sb(pes, "iot_f", [128, 128], F32)
            sc.dma("sp", lambda: SP.dma_start(out=vecs[:], in_=vecs_d[:, :]), writes=["vecs"], ch="vecs")
            sc.dma("sp", lambda: SP.dma_start(out=cT[:], in_=cT_d[:, :, :]), writes=["cT"], ch="cT")
            sc.dma("sp", lambda: SP.dma_start(out=keysT[:], in_=keysT_d[:, :, :]), writes=["keysT"], ch="keysT")
            sc.op("dve", lambda: DVE.memset(consts[:, 0:1], EPS), writes=["consts"])
            sc.op("dve", lambda: DVE.memset(consts[:, 1:2], 1.0), writes=["consts"])
            sc.op("dve", lambda: DVE.memset(consts[:, 2:3], 0.0), writes=["consts"])
            sc.op("dve", lambda: DVE.memset(ones_bf[:], 1.0), writes=["ones_bf"])
            sc.op("dve", lambda: DVE.memset(VC[:], 1.0), writes=["VC"])
            sc.op("dve", lambda: DVE.memset(KT[:], 0.0), writes=["KT"])
            sc.op("pool", lambda: POOL.iota(iot_i[:], pattern=[[1, 128]], base=0, channel_multiplier=-1), writes=["iot_i"])
            sc.op("dve", lambda: DVE.tensor_copy(iot_f[:], iot_i[:]), reads=["iot_i"], writes=["iot_f"])
            sc.op("dve", lambda: DVE.tensor_scalar(ident_f[:], iot_f[:], 0.0, None, op0=ALU.is_equal), reads=["iot_f"], writes=["ident_f"])
            sc.op("dve", lambda: DVE.tensor_copy(ident_b[:], ident_f[:]), reads=["ident_f"], writes=["ident_b"])
            sc.op("dve", lambda: DVE.tensor_scalar(tri_b[:], iot_f[:], 0.0, None, op0=ALU.is_ge), reads=["iot_f"], writes=["tri_b"])
            sc.op("pool", lambda: POOL.iota(iot_i[:, 0:16], pattern=[[1, 16]], base=0, channel_multiplier=0), reads=["iot_f"], writes=["iot_i"])
            sc.op("dve", lambda: DVE.tensor_copy(iota16[:], iot_i[:, 0:16]), reads=["iot_i"], writes=["iota16"])

            stb = [sb(pes, "stb%d" % i, [128, 2048], BF16) for i in range(2)]

            def cast_op(k, dst_ap, st, key, wkey):
                eng = ("dve", "act", "pool")[k % 3]
                if eng == "dve":
                    sc.op("dve", lambda: DVE.tensor_copy(dst_ap, st), reads=[key], writes=[wkey])
                elif eng == "act":
                    sc.op("act", lambda: ACT.copy(dst_ap, st), reads=[key], writes=[wkey])
                else:
                    sc.op("pool", lambda: POOL.tensor_copy(dst_ap, st), reads=[key], writes=[wkey])

            def load_cast(dst_ap, src_ap, ncols, k, outs=None):
                st = stage[:, 0:ncols] if k % 2 == 0 else stage[:, 2048:2048 + ncols]
                key = "stage%d" % (k % 2)
                sc.dma("sp", lambda: SP.dma_start(out=st, in_=src_ap), writes=[key], ch=key)
                if outs is None:
                    cast_op(k, dst_ap, st, key, "W")
                    return
                bkey = "stb%d" % (k % 2)
                sbt = stb[k % 2]
                cast_op(k, sbt[:, 0:ncols], st, key, bkey)
                for oi, (d_ap, s_ap) in enumerate(outs(sbt)):
                    sc.dma("sp", lambda: SP.dma_start(out=d_ap, in_=s_ap), reads=[bkey], writes=["wscr"], ch="%so%d" % (bkey, oi))
            k = 0
            for dc in range(DC):
                load_cast(None, win_d[dc], WIN_COLS, k, outs=lambda t, dc=dc: [
                    (winb_d[0:11, :, dc, :].rearrange("oc p n -> p oc n"), t[:, 0:1408].rearrange("p (oc n) -> p oc n", n=128)),
                    (winb_d[11, :, dc, 0:96], t[:, 1344:1440]),
                    (winb_d[12, :, dc, 0:96], t[:, 1376:1472])]); k += 1
                load_cast(None, wout_d[dc], D, k, outs=lambda t, dc=dc: [
                    (woutb_d[:, :, dc, :].rearrange("oc p n -> p oc n"), t[:, 0:1024].rearrange("p (oc n) -> p oc n", n=128))]); k += 1
                load_cast(None, wq_d[dc], 2048, k, outs=lambda t, dc=dc: [
                    (wqb_d[:, :, dc, :].rearrange("oc p n -> p oc n"), t[:, 0:2048].rearrange("p (oc n) -> p oc n", n=128))]); k += 1
            for kc in range(2):
                load_cast(w_uq[:, kc, :], wuq_d[kc], 1536, k); k += 1
            load_cast(w_ukv[:, :], wukv_d[:, :], 1024, k); k += 1
            for ci in range(4):
                load_cast(wa_bd[:, ci, :], wabd_d[ci], 128, k); k += 1
                load_cast(wx_bd[:, ci, :], wxbd_d[ci], 128, k); k += 1

            uv_v = uv_d.rearrange("(p r) n -> p r n", p=128)
            uvb_v = uvb_d.rearrange("(p r) n -> p r n", p=128)
            for st_i in range(128):
                i2 = st_i % 2
                st = stage[:, i2 * 2048:(i2 + 1) * 2048]
                skey, bkey = "stage%d" % i2, "stb%d" % i2
                sc.dma("sp", lambda: SP.dma_start(out=st, in_=uv_v[:, st_i, :]), writes=[skey], ch=skey)
                if st_i % 2 == 0:
                    sc.op("dve", lambda: DVE.tensor_copy(stb[i2][:], st), reads=[skey], writes=[bkey])
                else:
                    sc.op("act", lambda: ACT.copy(stb[i2][:], st), reads=[skey], writes=[bkey])
                sc.dma("act", lambda: ACT.dma_start(out=uvb_v[:, st_i, :], in_=stb[i2][:]), reads=[bkey], writes=["uvscr"], ch=bkey + "u")
            sc.op("act", lambda: ACT.activation(out=nsp[:], in_=vcol(V_LAM, 4), func=AF.Exp, scale=-1.0), reads=["vecs"], writes=["nsp"])
            sc.op("act", lambda: ACT.activation(out=nsp[:], in_=nsp[:], func=AF.Ln, bias=consts[:, 1:2], scale=1.0), reads=["nsp", "consts"], writes=["nsp"])
            sc.op("dve", lambda: DVE.tensor_scalar(nsp[:], nsp[:], -16.0, None, op0=ALU.mult), reads=["nsp"], writes=["nsp"])

            sc.op("act", lambda: ACT.activation(out=cT[:], in_=cT[:], func=AF.Silu), reads=["cT"], writes=["cT"])
            ps_mod = PS[0][:, 0:48 * nseq].rearrange("p (n b) -> p n b", b=nseq)
            for n in range(48):
                key = "stage%d" % (n % 2)
                st = stage[:, (n % 2) * 2048:(n % 2) * 2048 + 1024].rearrange("p (dc n) -> p dc n", n=128)
                sc.dma("sp", lambda: SP.dma_start(out=st, in_=wada_d[:, :, n * 128:(n + 1) * 128].rearrange("dc p n -> p dc n")), writes=[key], ch=key)
                for dc in range(DC):
                    sc.op("pe", lambda: PE.matmul(ps_mod[:, n, :], st[:, dc, :], cT[:, dc, :], start=(dc == 0), stop=(dc == DC - 1)),
                          reads=[key, "cT"], writes=["ps0"])
            sc.op("dve", lambda: DVE.tensor_tensor(out=modT[:], in0=ps_mod, in1=vcol(V_BADA, 48).unsqueeze(2).to_broadcast([128, 48, nseq]), op=ALU.add),
                  reads=["ps0", "vecs"], writes=["modT"])
            for dc in range(DC):
                sc.op("dve", lambda: DVE.tensor_scalar(A1[:, dc, :], modT[:, 8 + dc, :], 1.0, vcol(V_N1G + dc), op0=ALU.add, op1=ALU.mult),
                      reads=["modT", "vecs"], writes=["A1"])
                sc.op("dve", lambda: DVE.tensor_scalar(A2[:, dc, :], modT[:, 32 + dc, :], 1.0, vcol(V_N2G + dc), op0=ALU.add, op1=ALU.mult),
                      reads=["modT", "vecs"], writes=["A2"])
            sc.barrier()

        def rms_stats(src_tile, nchunks, inv_n, srckey, sq_t=None, rstd_t=None, bank=0, sfx="", sqkeys=None):
            sq_t = sq if sq_t is None else sq_t
            rstd_t = rstd if rstd_t is None else rstd_t
            sk, rk, pk = "sq" + sfx, "rstd" + sfx, "ps%d" % bank
            sks = [sk] if sqkeys is None else list(sqkeys)
            sc.op("act", lambda: ACT.activation(out=sq_t[:, 0:nchunks, :], in_=src_tile, func=AF.Square), reads=[srckey], writes=sks, cost=0.25 + 0.21 * nchunks)
            for i in range(nchunks):
                sc.op("pe", lambda: PE.matmul(PS[bank][:, 0:C], ones_bf[:], sq_t[:, i, :], start=(i == 0), stop=(i == nchunks - 1)),
                      reads=sks, writes=[pk])
            sc.op("act", lambda: ACT.activation(out=rstd_t[:], in_=PS[bank][:, 0:C], func=AF.Sqrt, bias=consts[:, 0:1], scale=inv_n), reads=[pk], writes=[rk])
            sc.op("dve", lambda: DVE.reciprocal(rstd_t[:], rstd_t[:]), reads=[rk], writes=[rk])

        def modulated_norm(xT, xk, Acol, shift_chunk0, b):
            rms_stats(xT[:], DC, 1.0 / D, xk)
            for dc in range(DC):
                sc.op("dve", lambda: DVE.scalar_tensor_tensor(out=tmpf[:], in0=xT[:, dc, :], scalar=Acol[:, dc, b:b + 1], in1=rstd[:], op0=ALU.mult, op1=ALU.mult),
                      reads=[xk, "rstd"], writes=["tmpf"])
                sc.op("act", lambda: ACT.activation(out=hT[:, dc, :], in_=tmpf[:], func=AF.Identity, bias=modT[:, shift_chunk0 + dc, b:b + 1], scale=1.0),
                      reads=["tmpf"], writes=["hT"])

        gen_i = [0]

        def gen_ps():
            bnk = (1, 2)[gen_i[0] % 2]
            gen_i[0] += 1
            return PS[bnk][:, 0:C], "ps%d" % bnk

        ring_i = [0]

        def wload(src_ap, ncols=128):
            i = ring_i[0] % 4
            ring_i[0] += 1
            key = "wr%d" % i
            sc.dma("sp", lambda: SP.dma_start(out=wr[i][:, :, 0:ncols], in_=src_ap), reads=["wscr"], writes=[key], ch=key)
            return wr[i], key

        class WStream:
            def __init__(self, blocks, depth=3):
                self.blocks = list(blocks)
                self.pend = []
                self.depth = depth
                for _ in range(depth):
                    self._issue()

            def _issue(self):
                if self.blocks:
                    src, ncols = self.blocks.pop(0)
                    self.pend.append(wload(src, ncols))

            def next(self):
                w, key = self.pend.pop(0)
                self._issue()
                return w, key

        def emit_B(b, c, par):
            sc.in_b_thread = threading.current_thread()
            sc.b_active = True
            t0 = c * C
            xT = xTs[par]
            xk = "xT%d" % par
            if c == 0:
                sc.op("dve", lambda: DVE.memset(xl[:], 0.0), writes=["xl0", "xl1", "xl2", "xl3"])
                sc.op("dve", lambda: DVE.memset(hst[:], 0.0), writes=["hst"])
            with ExitStack() as mes:
                gT = sb(mes, "gT", [128, 4, C], F32)
                qlat = sb(mes, "qlat", [128, 2, C], F32)
                kvlat = sb(mes, "kvlat", [128, C], F32)
                qs = sb(mes, "qs", [128, 2, C], BF16)
                kvs = sb(mes, "kvs", [128, C], BF16)
                xc = sb(mes, "xc", [128, C], F32)
                xcb = sb(mes, "xcb", [128, C], BF16)
                ra = sb(mes, "ra", [128, C], F32)
                ib = sb(mes, "ib", [128, C], F32)
                hh = sb(mes, "hh", [128, C], F32)
                ylru = sb(mes, "ylru", [128, 4, C], F32)
                yT = sb(mes, "yT", [128, 8, C], BF16)
                QT = sb(mes, "QT", [96, 8, C], BF16)
                posi = sb(mes, "posi", [96, C], I32)
                ang = sb(mes, "ang", [96, C], F32)
                kf = sb(mes, "kf", [96, C], F32)
                cos2 = sb(mes, "cos2", [96, C], F32)
                sin2 = sb(mes, "sin2", [96, C], F32)
                t1 = sb(mes, "t1", [96, C], F32)
                t2 = sb(mes, "t2", [96, C], F32)
                krb = sb(mes, "krb", [96, C], BF16)
                pT = [sb(mes, "pT%d" % i, [128, C], BF16) for i in range(4)]
                ymla = sb(mes, "ymla", [128, 2, 512], F32)
                ymn = sb(mes, "ymn", [128, 2, 512], BF16)
                rinv = sb(mes, "rinv", [128, 2], F32)
                sst = sb(mes, "sst", [128, 2], F32)

                win_blocks = [(winb_d[oc, :, :, :], 128) for oc in range(11)] + [(winb_d[11, :, :, 0:96], 96), (winb_d[12, :, :, 0:96], 96)]
                wst = WStream(win_blocks)
                sc.dma("sp", lambda: SP.dma_start(out=xT[:], in_=xT_d[b, :, :, t0:t0 + C].rearrange("dc p t -> p dc t")), writes=[xk], ch=xk)
                sc.dma("sp", lambda: SP.dma_start(out=posi[64:96, :], in_=pos_d[b:b + 1, t0:t0 + C].partition_broadcast(32)), writes=["posi"], ch="posi")
                modulated_norm(xT, xk, A1, 0, b)

                R = slice(64, 96)
                sc.op("dve", lambda: DVE.tensor_copy(ang[R, :], posi[R, :]), reads=["posi"], writes=["ang"])
                sc.op("dve", lambda: DVE.tensor_scalar(ang[R, :], ang[R, :], vecs[R, V_INVF:V_INVF + 1], None, op0=ALU.mult), reads=["ang", "vecs"], writes=["ang"])
                for shift, dst, use_sgn in ((0.0, sin2, True), (math.pi / 2, cos2, False)):
                    sc.op("dve", lambda: DVE.tensor_scalar(kf[R, :], ang[R, :], shift, 1.0 / TWO_PI, op0=ALU.add, op1=ALU.mult), reads=["ang"], writes=["kf"])
                    sc.op("dve", lambda: DVE.tensor_copy(posi[R, :], kf[R, :]), reads=["kf"], writes=["posi"])
                    sc.op("dve", lambda: DVE.tensor_copy(kf[R, :], posi[R, :]), reads=["posi"], writes=["kf"])
                    sc.op("dve", lambda: DVE.scalar_tensor_tensor(out=kf[R, :], in0=kf[R, :], scalar=-TWO_PI, in1=ang[R, :], op0=ALU.mult, op1=ALU.add),
                          reads=["kf", "ang"], writes=["kf"])
                    sc.op("dve", lambda: DVE.tensor_scalar(kf[R, :], kf[R, :], shift, None, op0=ALU.add), reads=["kf"], writes=["kf"])
                    sc.op("dve", lambda: DVE.tensor_scalar(kf[R, :], kf[R, :], 3.1415925, -3.1415925, op0=ALU.min, op1=ALU.max), reads=["kf"], writes=["kf"])
                    if use_sgn:
                        sc.op("act", lambda: ACT.activation(out=dst[R, :], in_=kf[R, :], func=AF.Sin, scale=vecs[R, V_SGN:V_SGN + 1]), reads=["kf", "vecs"], writes=["rope"])
                    else:
                        sc.op("act", lambda: ACT.activation(out=dst[R, :], in_=kf[R, :], func=AF.Sin), reads=["kf"], writes=["rope"])
                ROPE = ["rope"]

                def inproj(ncols=128):
                    w, wkey = wst.next()
                    ps, key = gen_ps()
                    for dc in range(DC):
                        sc.op("pe", lambda: PE.matmul(ps[0:ncols, :], w[:, dc, 0:ncols], hT[:, dc, :], start=(dc == 0), stop=(dc == DC - 1)),
                              reads=["hT", wkey], writes=[key])
                    return ps, key
                for ci in range(4):
                    ps, key = inproj()
                    sc.op("act", lambda: ACT.copy(xl[:, ci, 3:3 + C], ps), reads=[key], writes=["xl%d" % ci])
                for ci in range(4):
                    ps, key = inproj()
                    sc.op("act", lambda: ACT.activation(out=gT[:, ci, :], in_=ps, func=AF.Gelu_apprx_tanh), reads=[key], writes=["gT"])
                for kc in range(2):
                    ps, key = inproj()
                    sc.op("dve", lambda: DVE.tensor_copy(qlat[:, kc, :], ps), reads=[key], writes=["qlat"])
                ps, key = inproj()
                sc.op("dve", lambda: DVE.tensor_copy(kvlat[:], ps), reads=[key], writes=["kvlat"])
                ps_kr, key_kr = inproj(96)
                ps_krr, key_krr = inproj(96)
                sc.op("dve", lambda: DVE.tensor_tensor(out=t1[R, :], in0=ps_kr[R, :], in1=cos2[R, :], op=ALU.mult), reads=[key_kr] + ROPE, writes=["t1"])
                sc.op("dve", lambda: DVE.tensor_tensor(out=t2[R, :], in0=ps_krr[R, :], in1=sin2[R, :], op=ALU.mult), reads=[key_krr] + ROPE, writes=["t2"])
                sc.op("dve", lambda: DVE.tensor_tensor(out=krb[R, :], in0=t1[R, :], in1=t2[R, :], op=ALU.add), reads=["t1", "t2"], writes=["krb"])
                for h in range(8):
                    if h % 2 == 0:
                        sc.op("act", lambda: ACT.copy(KT[R, h, t0:t0 + C], krb[R, :]), reads=["krb"], writes=["KT"])
                    else:
                        sc.op("dve", lambda: DVE.tensor_copy(KT[R, h, t0:t0 + C], krb[R, :]), reads=["krb"], writes=["KT"])

                for ci in range(4):
                    xk_ = "xl%d" % ci
                    cw = V_CONVW + ci * 4
                    sc.op("dve", lambda: DVE.tensor_scalar(xc[:], xl[:, ci, 0:C], vcol(cw), vcol(V_CONVB + ci), op0=ALU.mult, op1=ALU.add),
                          reads=[xk_, "vecs"], writes=["xc"])
                    for kk in range(1, 4):
                        sc.op("dve", lambda: DVE.scalar_tensor_tensor(out=xc[:], in0=xl[:, ci, kk:kk + C], scalar=vcol(cw + kk), in1=xc[:], op0=ALU.mult, op1=ALU.add),
                              reads=[xk_, "xc"], writes=["xc"])
                    sc.op("act", lambda: ACT.copy(xl[:, ci, 0:3], xl[:, ci, C:C + 3]), reads=[xk_, "xc"], writes=[xk_])
                    sc.op("act", lambda: ACT.copy(xcb[:], xc[:]), reads=["xc"], writes=["xcb"])
                    ps_r, key_r = gen_ps()
                    sc.op("pe", lambda: PE.matmul(ps_r, wa_bd[:, ci, :], xcb[:], start=True, stop=True), reads=["xcb"], writes=[key_r])
                    ps_i, key_i = gen_ps()
                    sc.op("pe", lambda: PE.matmul(ps_i, wx_bd[:, ci, :], xcb[:], start=True, stop=True), reads=["xcb"], writes=[key_i])
                    sc.op("act", lambda: ACT.activation(out=ra[:], in_=ps_r, func=AF.Sigmoid, bias=vcol(V_BA + ci), scale=1.0), reads=[key_r], writes=["ra"])
                    sc.op("act", lambda: ACT.activation(out=ib[:], in_=ps_i, func=AF.Sigmoid, bias=vcol(V_BX + ci), scale=1.0), reads=[key_i], writes=["ib"])
                    sc.op("act", lambda: ACT.activation(out=hh[:], in_=ra[:], func=AF.Exp, scale=nsp[:, ci:ci + 1]), reads=["ra"], writes=["hh"])
                    sc.op("dve", lambda: DVE.tensor_scalar(ra[:], ra[:], nsp[:, ci:ci + 1], 0.5, op0=ALU.mult, op1=ALU.mult), reads=["ra", "hh"], writes=["ra"])
                    sc.op("act", lambda: ACT.activation(out=ra[:], in_=ra[:], func=AF.Exp), reads=["ra"], writes=["ra"])
                    sc.op("act", lambda: ACT.activation(out=hh[:], in_=hh[:], func=AF.Sqrt, bias=consts[:, 1:2], scale=-1.0), reads=["hh"], writes=["hh"])
                    sc.op("dve", lambda: DVE.tensor_tensor(out=ib[:], in0=ib[:], in1=xc[:], op=ALU.mult), reads=["ib", "xc"], writes=["ib"])
                    sc.op("dve", lambda: DVE.tensor_tensor(out=ib[:], in0=ib[:], in1=hh[:], op=ALU.mult), reads=["ib", "hh"], writes=["ib"])
                    sc.op("dve", lambda: DVE.tensor_tensor_scan(out=hh[:], data0=ra[:], data1=ib[:], initial=hst[:, ci:ci + 1], op0=ALU.mult, op1=ALU.add),
                          reads=["ra", "ib", "hst"], writes=["hh"], cost=0.6)
                    sc.op("dve", lambda: DVE.tensor_copy(hst[:, ci:ci + 1], hh[:, C - 1:C]), reads=["hh"], writes=["hst"])
                    sc.op("dve", lambda: DVE.tensor_tensor(out=ylru[:, ci, :], in0=hh[:], in1=gT[:, ci, :], op=ALU.mult), reads=["hh", "gT"], writes=["ylru"])
                rms_stats(ylru[:], 4, 1.0 / 512, "ylru")
                for ci in range(4):
                    sc.op("dve", lambda: DVE.scalar_tensor_tensor(out=yT[:, ci, :], in0=ylru[:, ci, :], scalar=vcol(V_LOG + ci), in1=rstd[:], op0=ALU.mult, op1=ALU.mult),
                          reads=["ylru", "rstd"], writes=["yT"])

                rms_stats(qlat[:], 2, 1.0 / 256, "qlat")
                for kc in range(2):
                    sc.op("dve", lambda: DVE.scalar_tensor_tensor(out=qs[:, kc, :], in0=qlat[:, kc, :], scalar=vcol(V_QNG + kc), in1=rstd[:], op0=ALU.mult, op1=ALU.mult),
                          reads=["qlat", "rstd"], writes=["qs"])
                rms_stats(kvlat[:].unsqueeze(1), 1, 1.0 / 128, "kvlat")
                sc.op("dve", lambda: DVE.scalar_tensor_tensor(out=kvs[:], in0=kvlat[:], scalar=vcol(V_KVNG), in1=rstd[:], op0=ALU.mult, op1=ALU.mult),
                      reads=["kvlat", "rstd"], writes=["kvs"])
                for h in range(8):
                    ps_q, key_q = gen_ps()
                    ps_qr, key_qr = gen_ps()
                    for kc in range(2):
                        sc.op("pe", lambda: PE.matmul(ps_q[0:96, :], w_uq[:, kc, h * 192:h * 192 + 96], qs[:, kc, :], start=(kc == 0), stop=(kc == 1)), reads=["qs"], writes=[key_q])
                    for kc in range(2):
                        sc.op("pe", lambda: PE.matmul(ps_qr[0:96, :], w_uq[:, kc, h * 192 + 96:h * 192 + 192], qs[:, kc, :], start=(kc == 0), stop=(kc == 1)), reads=["qs"], writes=[key_qr])
                    sc.op("act", lambda: ACT.copy(QT[0:64, h, :], ps_q[0:64, :]), reads=[key_q], writes=["QT"])
                    sc.op("dve", lambda: DVE.tensor_tensor(out=t1[R, :], in0=ps_q[R, :], in1=cos2[R, :], op=ALU.mult), reads=[key_q] + ROPE, writes=["t1"])
                    sc.op("dve", lambda: DVE.tensor_tensor(out=t2[R, :], in0=ps_qr[R, :], in1=sin2[R, :], op=ALU.mult), reads=[key_qr] + ROPE, writes=["t2"])
                    sc.op("dve", lambda: DVE.tensor_tensor(out=QT[R, h, :], in0=t1[R, :], in1=t2[R, :], op=ALU.add), reads=["t1", "t2"], writes=["QT"])
                    ps_k, key_k = gen_ps()
                    sc.op("pe", lambda: PE.matmul(ps_k[0:64, :], w_ukv[:, h * 128:h * 128 + 64], kvs[:], start=True, stop=True), reads=["kvs"], writes=[key_k])
                    sc.op("act", lambda: ACT.copy(KT[0:64, h, t0:t0 + C], ps_k[0:64, :]), reads=[key_k], writes=["KT"])
                wv = w_ukv[:, :].rearrange("p (h x) -> p h x", x=128)[:, :, 64:128]
                for j in range(C // 128):
                    tile_i = (t0 // 128) + j
                    sc.op("pe", lambda: PE.matmul(PS[4][:, :].rearrange("p (h x) -> p h x", x=64), kvs[:, j * 128:(j + 1) * 128], wv, start=True, stop=True),
                          reads=["kvs"], writes=["ps4"])
                    sc.op("dve", lambda: DVE.tensor_copy(VC[:, tile_i, :, 0:64], PS[4][:, :].rearrange("p (h x) -> p h x", x=64)), reads=["ps4"], writes=["VC"])

                wso = WStream([(woutb_d[oc, :, :, :], 128) for oc in range(8)])

                scale = 96.0 ** -0.5
                nkt = (t0 + C) // 128
                kdiag0 = t0 // 128
                it = 0
                abk = (1, 2)
                akeys = ["ps1", "ps2"]
                for h in range(8):
                    for kt in range(nkt):
                        sbank = (4, 5, 0)[it % 3]
                        skey = "ps%d" % sbank
                        pt = pT[it % 4]
                        pkey = "pT%d" % (it % 4)
                        it += 1
                        sc.op("pe", lambda: PE.matmul(PS[sbank][:, 0:C], KT[0:96, h, kt * 128:(kt + 1) * 128], QT[0:96, h, :], start=True, stop=True),
                              reads=["KT", "QT"], writes=[skey])
                        sc.op("act", lambda: ACT.activation(out=pt[:], in_=PS[sbank][:, 0:C], func=AF.Exp, scale=scale), reads=[skey], writes=[pkey])
                        jk = kt - kdiag0
                        if jk >= 0:
                            sc.op("dve", lambda: DVE.tensor_tensor(out=pt[:, jk * 128:(jk + 1) * 128], in0=pt[:, jk * 128:(jk + 1) * 128], in1=tri_b[:], op=ALU.mult),
                                  reads=[pkey], writes=[pkey])
                        for jq in range(C // 128):
                            if jk > jq:
                                continue
                            last = kdiag0 + jq
                            sc.op("pe", lambda: PE.matmul(PS[abk[jq]][:, 0:65], pt[:, jq * 128:(jq + 1) * 128], VC[:, kt, h, :], start=(kt == 0), stop=(kt == last)),
                                  reads=[pkey, "VC"], writes=[akeys[jq]])
                    for jq in range(C // 128):
                        sc.op("dve", lambda: DVE.reciprocal(rinv[:, jq:jq + 1], PS[abk[jq]][:, 64:65]), reads=[akeys[jq]], writes=["rinv"])
                        sc.op("dve", lambda: DVE.tensor_scalar(ymla[:, jq, h * 64:(h + 1) * 64], PS[abk[jq]][:, 0:64], rinv[:, jq:jq + 1], None, op0=ALU.mult),
                              reads=[akeys[jq], "rinv"], writes=["ymla"])
                for jq in range(C // 128):
                    sc.op("dve", lambda: DVE.scalar_tensor_tensor(out=ymn[:, jq, :], in0=ymla[:, jq, :], scalar=1.0, in1=ymla[:, jq, :], op0=ALU.mult, op1=ALU.mult, accum_out=sst[:, jq:jq + 1]),
                          reads=["ymla"], writes=["ymn", "sst"])
                sc.op("act", lambda: ACT.activation(out=sst[:], in_=sst[:], func=AF.Sqrt, bias=consts[:, 0:1], scale=1.0 / 512), reads=["sst"], writes=["sst"])
                sc.op("dve", lambda: DVE.reciprocal(sst[:], sst[:]), reads=["sst"], writes=["sst"])
                for jq in range(C // 128):
                    sc.op("dve", lambda: DVE.tensor_scalar(ymn[:, jq, :], ymla[:, jq, :], sst[:, jq:jq + 1], None, op0=ALU.mult), reads=["ymla", "sst"], writes=["ymn"])
                    for fc in range(4):
                        sc.op("pe", lambda: PE.transpose(PS3[:, fc * 128:(fc + 1) * 128], ymn[:, jq, fc * 128:(fc + 1) * 128], ident_b[:]), reads=["ymn"], writes=["ps3"])
                    for fc in range(4):
                        sc.op("dve", lambda: DVE.tensor_scalar(yT[:, 4 + fc, jq * 128:(jq + 1) * 128], PS3[:, fc * 128:(fc + 1) * 128], vcol(V_MOG + fc), None, op0=ALU.mult),
                              reads=["ps3"], writes=["yT"])
                for oc in range(DC):
                    w, wkey = wso.next()
                    ps, key = gen_ps()
                    for cc in range(8):
                        sc.op("pe", lambda: PE.matmul(ps, w[:, cc, :], yT[:, cc, :], start=(cc == 0), stop=(cc == 7)), reads=["yT", wkey], writes=[key])
                    sc.op("dve", lambda: DVE.scalar_tensor_tensor(out=xT[:, oc, :], in0=ps, scalar=modT[:, 16 + oc, b:b + 1], in1=xT[:, oc, :], op0=ALU.mult, op1=ALU.add),
                          reads=[key, xk], writes=[xk])
                sc.barrier_b()

            with ExitStack() as pes2:
                qT = sb(pes2, "qT", [128, 16, C], F32)
                scs = sb(pes2, "scs", [128, 2048], F32)
                work = sb(pes2, "work", [128, 2048], F32)
                top = sb(pes2, "top", [128, 16, 16], F32)
                tix = sb(pes2, "tix", [128, 16, 16], U32)
                tixf = sb(pes2, "tixf", [128, 16, 16], F32)
                best = sb(pes2, "best", [128, 8, 16], F32)
                posu = sb(pes2, "posu", [128, 8, 16], U32)
                pa_i = sb(pes2, "pa_i", [128, 8, 16], I32)
                pa_f = sb(pes2, "pa_f", [128, 8, 16], F32)
                pb_f = sb(pes2, "pb_f", [128, 8, 16], F32)
                gsum = sb(pes2, "gsum", [128, 8], F32)
                isel = sb(pes2, "isel", [128, 8, 16], F32)
                jsel = sb(pes2, "jsel", [128, 8, 16], F32)

                modulated_norm(xT, xk, A2, 24, b)
                wsq = WStream([(wqb_d[hp, :, :, :], 128) for hp in range(16)])
                for hp in range(16):
                    w, wkey = wsq.next()
                    qb = (0, 1, 2, 4, 5)[hp % 5]
                    qk = "ps%d" % qb
                    pq = PS[qb][:, 0:C]
                    for dc in range(DC):
                        sc.op("pe", lambda: PE.matmul(pq, w[:, dc, :], hT[:, dc, :], start=(dc == 0), stop=(dc == DC - 1)), reads=["hT", wkey], writes=[qk])
                    if hp % 2 == 0:
                        sc.op("act", lambda: ACT.copy(qT[:, hp, :], pq), reads=[qk], writes=["qT%d" % hp])
                    else:
                        sc.op("dve", lambda: DVE.tensor_copy(qT[:, hp, :], pq), reads=[qk], writes=["qT%d" % hp])
                for j in range(C // 128):
                    ts = slice(j * 128, (j + 1) * 128)
                    h2tm = h2s[par][j]
                    gg = ggs[par][j]
                    idxi = idxs[par][j]
                    hkey, gkey_, ikey = "h2%d%d" % (par, j), "gg%d%d" % (par, j), "idx%d%d" % (par, j)
                    for dc in range(DC):
                        sc.op("pe", lambda: PE.transpose(PS3[:, dc * 128:(dc + 1) * 128], hT[:, dc, ts], ident_b[:]), reads=["hT"], writes=["ps3"])
                    sc.op("act", lambda: ACT.copy(h2tm[:], PS3[:, :]), reads=["ps3"], writes=[hkey], cost=1.0)
                    sbanks = (1, 2, 4, 5)
                    for hp in range(16):
                        bnk = sbanks[hp // 4]
                        sc.op("pe", lambda: PE.matmul(PS[bnk][:, (hp % 4) * 128:(hp % 4 + 1) * 128], qT[:, hp, ts], keysT[:, hp, :], start=True, stop=True),
                              reads=["qT%d" % hp], writes=["ps%d" % bnk])
                    for q in range(4):
                        bnk = sbanks[q]
                        if q % 2 == 0:
                            sc.op("act", lambda: ACT.copy(scs[:, q * 512:(q + 1) * 512], PS[bnk][:, :]), reads=["ps%d" % bnk], writes=["scs%d" % q])
                        else:
                            sc.op("dve", lambda: DVE.tensor_copy(scs[:, q * 512:(q + 1) * 512], PS[bnk][:, :]), reads=["ps%d" % bnk], writes=["scs%d" % q])
                    TOPK = ["top%d" % hp for hp in range(16)]
                    TIXK = ["tix%d" % hp for hp in range(16)]
                    WRKK = ["work%d" % hp for hp in range(16)]
                    sks = ["scs%d" % (hp // 4) for hp in range(16)]
                    svs = [scs[:, hp * 128:(hp + 1) * 128] for hp in range(16)]
                    wvs = [work[:, hp * 128:(hp + 1) * 128] for hp in range(16)]
                    for hp in range(16):
                        sc.op("dve", lambda: DVE.max(out=top[:, hp, 0:8], in_=svs[hp]), reads=[sks[hp]], writes=[TOPK[hp]], cost=0.2)
                    for hp in range(16):
                        sc.op("dve", lambda: DVE.max_index(out=tix[:, hp, 0:8], in_max=top[:, hp, 0:8], in_values=svs[hp]), reads=[sks[hp], TOPK[hp]], writes=[TIXK[hp]], cost=0.25)
                    for hp in range(16):
                        sc.op("dve", lambda: DVE.match_replace(out=wvs[hp], in_to_replace=top[:, hp, 0:8], in_values=svs[hp], imm_value=NEG), reads=[sks[hp], TOPK[hp]], writes=[WRKK[hp]], cost=0.25)
                    for hp in range(16):
                        sc.op("dve", lambda: DVE.max(out=top[:, hp, 8:16], in_=wvs[hp]), reads=[WRKK[hp]], writes=[TOPK[hp]], cost=0.2)
                    for hp in range(16):
                        sc.op("dve", lambda: DVE.max_index(out=tix[:, hp, 8:16], in_max=top[:, hp, 8:16], in_values=wvs[hp]), reads=[WRKK[hp], TOPK[hp]], writes=[TIXK[hp]], cost=0.25)
                    sc.op("dve", lambda: DVE.tensor_copy(tixf[:], tix[:]), reads=TIXK, writes=["tixf"])
                    top4 = top[:].rearrange("p (h two) k -> p h two k", two=2)
                    tix4 = tixf[:].rearrange("p (h two) k -> p h two k", two=2)
                    cand = work[:].rearrange("p (h a b) -> p h a b", a=16, b=16)
                    cand3 = work[:].rearrange("p (h ab) -> p h ab", ab=256)
                    cand2 = scs[:].rearrange("p (h ab) -> p h ab", ab=256)
                    ALLS = ["scs0", "scs1", "scs2", "scs3"]
                    WH = [[WRKK[2 * h], WRKK[2 * h + 1]] for h in range(8)]
                    SH = ["scs%d" % (h // 2) for h in range(8)]
                    BK = ["best%d" % h for h in range(8)]
                    PK = ["posu%d" % h for h in range(8)]
                    sc.op("dve", lambda: DVE.tensor_tensor(out=cand, in0=top4[:, :, 0, :].unsqueeze(3).to_broadcast([128, 8, 16, 16]),
                                                           in1=top4[:, :, 1, :].unsqueeze(2).to_broadcast([128, 8, 16, 16]), op=ALU.add),
                          reads=TOPK, writes=WRKK, cost=2.2)
                    for h in range(8):
                        sc.op("dve", lambda: DVE.max(out=best[:, h, 0:8], in_=cand3[:, h, :]), reads=WH[h], writes=[BK[h]], cost=0.35)
                    for h in range(8):
                        sc.op("dve", lambda: DVE.max_index(out=posu[:, h, 0:8], in_max=best[:, h, 0:8], in_values=cand3[:, h, :]), reads=WH[h] + [BK[h]], writes=[PK[h]], cost=0.4)
                    for h in range(8):
                        sc.op("dve", lambda: DVE.match_replace(out=cand2[:, h, :], in_to_replace=best[:, h, 0:8], in_values=cand3[:, h, :], imm_value=NEG), reads=WH[h] + [BK[h]], writes=[SH[h]], cost=0.4)
                    for h in range(8):
                        sc.op("dve", lambda: DVE.max(out=best[:, h, 8:16], in_=cand2[:, h, :]), reads=[SH[h]], writes=[BK[h]], cost=0.35)
                    for h in range(8):
                        sc.op("dve", lambda: DVE.max_index(out=posu[:, h, 8:16], in_max=best[:, h, 8:16], in_values=cand2[:, h, :]), reads=[SH[h], BK[h]], writes=[PK[h]], cost=0.4)
                    sc.op("dve", lambda: DVE.tensor_copy(pb_f[:], posu[:]), reads=PK, writes=["pb_f"])
                    sc.op("dve", lambda: DVE.tensor_scalar(pa_f[:], pb_f[:], 1.0 / 16, -0.46875, op0=ALU.mult, op1=ALU.add), reads=["pb_f"], writes=["pa_f"])
                    sc.op("dve", lambda: DVE.tensor_copy(pa_i[:], pa_f[:]), reads=["pa_f"], writes=["pa_i"])
                    sc.op("dve", lambda: DVE.tensor_copy(pa_f[:], pa_i[:]), reads=["pa_i"], writes=["pa_f"])
                    sc.op("dve", lambda: DVE.scalar_tensor_tensor(out=pb_f[:], in0=pa_f[:], scalar=-16.0, in1=pb_f[:], op0=ALU.mult, op1=ALU.add), reads=["pa_f", "pb_f"], writes=["pb_f"])
                    sc.op("dve", lambda: DVE.tensor_tensor(out=gg[:], in0=best[:], in1=best[:, :, 0:1].to_broadcast([128, 8, 16]), op=ALU.subtract), reads=BK, writes=[gkey_])
                    sc.op("act", lambda: ACT.activation(out=gg[:], in_=gg[:], func=AF.Exp), reads=[gkey_], writes=[gkey_])
                    sc.op("dve", lambda: DVE.tensor_reduce(out=gsum[:], in_=gg[:], axis=AX.X, op=ALU.add), reads=[gkey_], writes=["gsum"])
                    sc.op("dve", lambda: DVE.reciprocal(gsum[:], gsum[:]), reads=["gsum"], writes=["gsum"])
                    sc.op("dve", lambda: DVE.tensor_tensor(out=gg[:], in0=gg[:], in1=gsum[:].unsqueeze(2).to_broadcast([128, 8, 16]), op=ALU.mult), reads=[gkey_, "gsum"], writes=[gkey_])
                    eq = work[:].rearrange("p (h k a) -> p h k a", k=16, a=16)
                    io4 = iota16[:].unsqueeze(1).unsqueeze(1).to_broadcast([128, 8, 16, 16])
                    for (pf, two, dst) in ((pa_f, 0, isel), (pb_f, 1, jsel)):
                        sc.op("dve", lambda: DVE.tensor_tensor(out=eq, in0=io4, in1=pf[:].unsqueeze(3).to_broadcast([128, 8, 16, 16]), op=ALU.is_equal),
                              reads=["pa_f", "pb_f"] + BK + PK, writes=WRKK, cost=2.2)
                        sc.op("dve", lambda: DVE.tensor_tensor(out=eq, in0=eq, in1=tix4[:, :, two, :].unsqueeze(2).to_broadcast([128, 8, 16, 16]), op=ALU.mult),
                              reads=WRKK + ["tixf"], writes=WRKK, cost=2.2)
                        sc.op("dve", lambda: DVE.tensor_reduce(out=dst[:], in_=eq, axis=AX.X, op=ALU.add), reads=WRKK, writes=["sel%d" % two], cost=2.2)
                    sc.op("dve", lambda: DVE.scalar_tensor_tensor(out=isel[:], in0=isel[:], scalar=128.0, in1=jsel[:], op0=ALU.mult, op1=ALU.add),
                          reads=["sel0", "sel1"], writes=["sel0"])
                    sc.op("dve", lambda: DVE.tensor_copy(idxi[:], isel[:].rearrange("p h k -> p (h k)")), reads=["sel0"], writes=[ikey])
                sc.barrier_b()
            sc.b_active = False

        def emit_A(b, c, par, give):
            t0 = c * C
            xT = xTs[par]
            xk = "xT%d" % par
            for j in range(C // 128):
                ts = slice(j * 128, (j + 1) * 128)
                h2tm = h2s[par][j]
                ggf = ggs[par][j][:].rearrange("p h k -> p (h k)")
                idxi = idxs[par][j]
                hkey, gkey_, ikey = "h2%d%d" % (par, j), "gg%d%d" % (par, j), "idx%d%d" % (par, j)
                for step in range(130):
                    hk = step
                    if hk < 128:
                        Gb = G[hk % NBUF]
                        gkey = "G%d" % (hk % NBUF)
                        sc.dma("pool", lambda: POOL.indirect_dma_start(out=Gb[:], out_offset=None, in_=uvb_d,
                                                                       in_offset=bass.IndirectOffsetOnAxis(ap=idxi[:, hk:hk + 1], axis=0)),
                               reads=[ikey, "uvscr"], writes=[gkey], ch=gkey, cost=1.9)
                        sc.op("dve", lambda: DVE.scalar_tensor_tensor(out=junkb[hk % 2], in0=Gb[:, 0:D], scalar=1.0, in1=h2tm[:], op0=ALU.mult, op1=ALU.mult, accum_out=dots[:, hk:hk + 1]),
                              reads=[gkey, hkey], writes=["dots%d" % hk, "junkb%d" % (hk % 2)], cost=1.3)
                        sc.op("act", lambda: ACT.activation(out=acts[:, hk:hk + 1], in_=dots[:, hk:hk + 1], func=AF.Gelu_apprx_tanh), reads=["dots%d" % hk], writes=["acts%d" % hk])
                    k1 = step - 1
                    if 0 <= k1 < 128:
                        sc.op("act", lambda: ACT.activation(out=zz[:, k1:k1 + 1], in_=acts[:, k1:k1 + 1], func=AF.Identity, scale=ggf[:, k1:k1 + 1]),
                              reads=["acts%d" % k1, gkey_], writes=["zz%d" % k1])
                    k2 = step - 2
                    if 0 <= k2 < 128:
                        Gp = G[k2 % NBUF]
                        gpkey = "G%d" % (k2 % NBUF)
                        dg = diag[k2 % 3]
                        dkey = "diag%d" % (k2 % 3)
                        sc.op("act", lambda: ACT.activation(out=dg[:], in_=ident_b[:], func=AF.Identity, scale=zz[:, k2:k2 + 1]),
                              reads=["zz%d" % k2], writes=[dkey])
                        for half in range(2):
                            bnk = 6 + half
                            sc.op("pe", lambda: PE.matmul(PS[bnk][:, :], dg[:], Gp[:, D + half * 512:D + (half + 1) * 512], start=(k2 == 0), stop=(k2 == 127)),
                                  reads=[dkey, gpkey], writes=["ps%d" % bnk])
                    give()
                sc.op("act", lambda: ACT.copy(petm[:, 0:512], PS[6][:, :]), reads=["ps6"], writes=["petm"])
                sc.op("dve", lambda: DVE.tensor_copy(petm[:, 512:1024], PS[7][:, :]), reads=["ps7"], writes=["petm"])
                for dc in range(DC):
                    bnk = 6 + dc // 4
                    sc.op("pe", lambda: PE.transpose(PS[bnk][:, (dc % 4) * 128:(dc % 4 + 1) * 128], petm[:, dc * 128:(dc + 1) * 128], ident_f[:]), reads=["petm"], writes=["ps%d" % bnk])
                for dc in range(DC):
                    bnk = 6 + dc // 4
                    sc.op("dve", lambda: DVE.scalar_tensor_tensor(out=xT[:, dc, ts], in0=PS[bnk][:, (dc % 4) * 128:(dc % 4 + 1) * 128], scalar=modT[:, 40 + dc, b:b + 1], in1=xT[:, dc, ts], op0=ALU.mult, op1=ALU.add),
                          reads=["ps%d" % bnk, xk], writes=[xk])
                give()
            rms_stats(xT[:], DC, 1.0 / D, xk, sq_t=sqA, rstd_t=rstdA, bank=6, sfx="A", sqkeys=["junkb0", "junkb1"])
            for dc in range(DC):
                ot = otmp[dc % 2]
                okey = "otmp%d" % (dc % 2)
                sc.op("dve", lambda: DVE.scalar_tensor_tensor(out=ot[:], in0=xT[:, dc, :], scalar=vcol(V_FG + dc), in1=rstdA[:], op0=ALU.mult, op1=ALU.mult),
                      reads=[xk, "rstdA"], writes=[okey])
                sc.dma("sp", lambda: SP.dma_start(out=outT_d[b, dc, :, t0:t0 + C], in_=ot[:]), reads=[okey], writes=["outd"], ch=okey)
            give()

        chunks = [(b, c) for b in range(nseq) for c in range(nch)]
        emit_B(chunks[0][0], chunks[0][1], 0)
        last_b_count = 2200
        for i, (b, c) in enumerate(chunks):
            par = i % 2
            if overlap and i + 1 < len(chunks):
                nb, ncn = chunks[i + 1]
                co.start(lambda nb=nb, ncn=ncn, par=par: emit_B(nb, ncn, 1 - par))
                q = 200
                emit_A(b, c, par, lambda q=q: co.give(q))
                last_b_count = max(co.count, 1) if co.done else last_b_count
                co.drain()
                last_b_count = max(co.count, 1)
            else:
                emit_A(b, c, par, lambda: None)
                if i + 1 < len(chunks):
                    nb, ncn = chunks[i + 1]
                    emit_B(nb, ncn, 1 - par)
        sc.finish()
    return nc


def _pack_inputs(inp, core, nseq=NSEQ):
    f32 = np.float32
    b0 = core * nseq
    x = inp["x"][b0:b0 + nseq]
    xT = np.ascontiguousarray(np.transpose(x, (0, 2, 1))).reshape(nseq, DC, 128, S)
    c = inp["c"][b0:b0 + nseq]
    cT = np.ascontiguousarray(c.T.reshape(DC, 128, nseq).transpose(1, 0, 2))
    pos = np.ascontiguousarray(inp["positions"][b0:b0 + nseq]).astype(np.int32)
    return {"xT": xT.astype(f32), "cT": cT.astype(f32), "pos": pos}


def _pack_weights(inp):
    f32 = np.float32
    col = lambda v, n: np.ascontiguousarray(np.asarray(v, f32).reshape(n, 128).T)
    vecs = np.zeros((128, NV), f32)
    vecs[:, V_N1G:V_N1G + 8] = col(inp["norm1_g"][0], 8)
    vecs[:, V_N2G:V_N2G + 8] = col(inp["norm2_g"][0], 8)
    vecs[:, V_FG:V_FG + 8] = col(inp["final_g"], 8)
    cw = np.asarray(inp["conv_w"][0], f32)
    for ci in range(4):
        for k in range(4):
            vecs[:, V_CONVW + ci * 4 + k] = cw[k, ci * 128:(ci + 1) * 128]
    vecs[:, V_CONVB:V_CONVB + 4] = col(inp["conv_b"][0], 4)
    vecs[:, V_BA:V_BA + 4] = col(inp["lru_ba"][0], 4)
    vecs[:, V_BX:V_BX + 4] = col(inp["lru_bx"][0], 4)
    vecs[:, V_LAM:V_LAM + 4] = col(inp["lru_lambda"][0], 4)
    vecs[:, V_QNG:V_QNG + 2] = col(inp["q_norm_g"][0], 2)
    vecs[:, V_KVNG:V_KVNG + 1] = col(inp["kv_norm_g"][0], 1)
    vecs[:, V_LOG:V_LOG + 4] = col(inp["lru_out_g"][0], 4)
    vecs[:, V_MOG:V_MOG + 4] = col(inp["mla_out_g"][0], 4)
    vecs[:, V_BADA:V_BADA + 48] = col(inp["b_ada"][0], 48)
    inv_freq = (1.0 / (10000.0 ** (np.arange(0, 32, 2, dtype=np.float32) / np.float32(32)))).astype(f32)
    for p in range(64, 96):
        vecs[p, V_INVF] = inv_freq[(p - 64) % 16]
        vecs[p, V_SGN] = -1.0 if p < 80 else 1.0
    w_in = np.asarray(inp["w_in"][0], f32)
    kr = w_in[:, 1408:1440]
    w_in_ext = np.concatenate([w_in, kr[:, 16:32], kr[:, 0:16]], axis=1)
    wa = np.asarray(inp["lru_wa"][0], f32)
    wx = np.asarray(inp["lru_wx"][0], f32)
    wa_bd = np.zeros((4, 128, 128), f32)
    wx_bd = np.zeros((4, 128, 128), f32)
    for ci in range(4):
        for s in range(2):
            wa_bd[ci, s * 64:(s + 1) * 64, s * 64:(s + 1) * 64] = wa[2 * ci + s]
            wx_bd[ci, s * 64:(s + 1) * 64, s * 64:(s + 1) * 64] = wx[2 * ci + s]
    w_uq = np.asarray(inp["w_uq"][0], f32)
    parts = []
    for h in range(8):
        blk = w_uq[:, h * 96:(h + 1) * 96]
        parts += [blk, blk[:, 0:64], blk[:, 80:96], blk[:, 64:80]]
    w_uq_ext = np.concatenate(parts, axis=1)
    keys = np.asarray(inp["peer_keys"][0], f32)
    keysT = np.ascontiguousarray(keys.reshape(16, 128, 128).transpose(2, 0, 1))
    uv = np.concatenate([np.asarray(inp["peer_u"][0], f32), np.asarray(inp["peer_v"][0], f32)], axis=1)
    return {
        "w_ada": np.ascontiguousarray(np.asarray(inp["w_ada"][0], f32).reshape(DC, 128, 6 * D)),
        "vecs": vecs,
        "w_in": np.ascontiguousarray(w_in_ext.reshape(DC, 128, WIN_COLS)),
        "wa_bd": wa_bd, "wx_bd": wx_bd,
        "w_uq": np.ascontiguousarray(w_uq_ext.reshape(2, 128, 1536)),
        "w_ukv": np.ascontiguousarray(np.asarray(inp["w_ukv"][0], f32)),
        "w_out": np.ascontiguousarray(np.asarray(inp["w_out"][0], f32).reshape(DC, 128, D)),
        "peer_wq": np.ascontiguousarray(np.asarray(inp["peer_wq"][0], f32).reshape(DC, 128, 2048)),
        "keysT": keysT,
        "uv": np.ascontiguousarray(uv),
    }


def kernel(**inputs):
    inp = {k: np.asarray(v) for k, v in inputs.items()}
    nc = build_program(NSEQ, NCH)
    wts = _pack_weights(inp)
    in_maps = []
    for core in range(NCORES):
        m = dict(wts)
        m.update(_pack_inputs(inp, core))
        in_maps.append(m)
    res = run_bass_kernel_spmd(nc, in_maps, core_ids=list(range(NCORES)))
    outs = []
    for core in range(NCORES):
        oT = np.asarray(res.results[core]["outT"]).reshape(NSEQ, D, S)
        outs.append(np.transpose(oT, (0, 2, 1)))
    return np.ascontiguousarray(np.concatenate(outs, axis=0)).astype(np.float32)
```

```python
from contextlib import ExitStack
import threading
import math
import numpy as np
import concourse.bass as bass
import concourse.mybir as mybir
from concourse.bass_utils import run_bass_kernel_spmd

F32 = mybir.dt.float32
BF16 = mybir.dt.bfloat16
I32 = mybir.dt.int32
U32 = mybir.dt.uint32
ALU = mybir.AluOpType
AF = mybir.ActivationFunctionType
AX = mybir.AxisListType

D = 1024
S = 2048
NCORES = 8
NSEQ = 4
C = 256
NCH = S // C
DC = 8
EPS = 1e-6
WIN_COLS = 1472
NEG = -1.0e30
TWO_PI = 2.0 * math.pi

V_N1G, V_N2G, V_FG = 0, 8, 16
V_CONVW, V_CONVB, V_BA, V_BX, V_LAM = 24, 40, 44, 48, 52
V_QNG, V_KVNG, V_LOG, V_MOG = 56, 58, 59, 63
V_BADA = 67
V_INVF, V_SGN = 115, 116
NV = 117


class Sched:
    ENG = ("pe", "act", "dve", "pool", "sp")

    def __init__(self, nc, es):
        self.nc = nc
        self.es = es
        self.eng = {"pe": nc.tensor, "act": nc.scalar, "dve": nc.vector, "pool": nc.gpsimd, "sp": nc.sync}
        self.sem = {e: es.enter_context(nc.semaphore("sem_" + e)) for e in self.ENG}
        self.cnt = {e: 0 for e in self.ENG}
        self.seen = {e: {} for e in self.ENG}
        self.last_w = {}
        self.readers = {}
        self.dsem = {}
        self.dcnt = {}
        self.dead = [False]
        self.co = None
        self.last_b = {e: 0 for e in self.ENG}
        self.b_dcnt = {}
        self.in_b = False
        self.b_active = False
        self.tail = {e: 0.0 for e in self.ENG}
        self.tw = {}
        self.tr = {}
        self.DUR = {"pe": 0.15, "act": 0.45, "dve": 0.3, "pool": 0.25, "sp": 0.1}
        self.LAT = 0.3
        self.MARGIN = {"pe": 24.0, "act": 18.0, "dve": 16.0, "pool": 6.0, "sp": 60.0}

    def _deps(self, reads, writes):
        need = {}
        def add(tok):
            k, v = tok
            if need.get(k, 0) < v:
                need[k] = v
        for k in list(reads) + list(writes):
            t = self.last_w.get(k)
            if t is not None:
                add(t)
        for k in writes:
            for tok in self.readers.get(k, {}).items():
                add(tok)
        return need

    def _emit_waits(self, e, need):
        eng = self.eng[e]
        seen = self.seen[e]
        for k, v in need.items():
            if k == e and e == "pe":
                continue
            if seen.get(k, 0) >= v:
                continue
            if k in self.sem:
                eng.wait_ge(self.sem[k], v)
            else:
                eng.wait_ge(self.dsem[k], v)
            seen[k] = v

    def _record(self, tok, reads, writes):
        for k in writes:
            self.last_w[k] = tok
            self.readers[k] = {}
        for k in reads:
            r = self.readers.setdefault(k, {})
            if r.get(tok[0], 0) < tok[1]:
                r[tok[0]] = tok[1]

    def _ready(self, reads, writes):
        t = 0.0
        for k in list(reads) + list(writes):
            v = self.tw.get(k)
            if v is not None and v > t:
                t = v
        for k in writes:
            v = self.tr.get(k)
            if v is not None and v > t:
                t = v
        return t

    def _model(self, e, reads, writes, dur, extra=0.0):
        start = max(self._ready(reads, writes) + self.LAT, self.tail[e])
        fin = start + dur
        self.tail[e] = fin
        for k in writes:
            self.tw[k] = fin + extra
            self.tr[k] = 0.0
        for k in reads:
            if self.tr.get(k, 0.0) < fin + extra:
                self.tr[k] = fin + extra

    def op(self, e, fn, reads=(), writes=(), cost=None):
        dur = self.DUR[e] if cost is None else cost
        if self.co is not None:
            self.co.tick(self, e, reads, writes, dur)
        self.in_b = threading.current_thread() is getattr(self, "in_b_thread", None) and self.b_active
        self._model(e, reads, writes, dur)
        self._emit_waits(e, self._deps(reads, writes))
        ins = fn()
        ins.then_inc(self.sem[e], 1)
        self.cnt[e] += 1
        if self.in_b:
            self.last_b[e] = self.cnt[e]
        self._record((e, self.cnt[e]), reads, writes)

    def dma(self, e, fn, reads=(), writes=(), ch=None, cost=None):
        dur = 0.1 if cost is None else cost
        if self.co is not None:
            self.co.tick(self, e, reads, writes, dur)
        self.in_b = threading.current_thread() is getattr(self, "in_b_thread", None) and self.b_active
        self._model(e, reads, writes, dur, extra=2.5)
        if ch not in self.dsem:
            self.dsem[ch] = self.es.enter_context(self.nc.semaphore("dsem_%d" % len(self.dsem)))
            self.dcnt[ch] = 0
        need = self._deps(reads, writes)
        if self.dcnt[ch]:
            need[ch] = max(need.get(ch, 0), self.dcnt[ch])
        self._emit_waits(e, need)
        ins = fn()
        ins.then_inc(self.dsem[ch], 16)
        self.dcnt[ch] += 16
        if self.in_b:
            self.b_dcnt[ch] = self.dcnt[ch]
        self._record((ch, self.dcnt[ch]), reads, writes)

    def barrier_b(self):
        need = {e: v for e, v in self.last_b.items() if v}
        need.update({ch: v for ch, v in self.b_dcnt.items() if v})
        for e in self.ENG:
            self._emit_waits(e, dict(need))

    def barrier(self):
        need = {e: self.cnt[e] for e in self.ENG if self.cnt[e]}
        for ch, v in self.dcnt.items():
            if v:
                need[ch] = v
        for e in self.ENG:
            self._emit_waits(e, dict(need))
        self.last_w = {}
        self.readers = {}

    def finish(self):
        need = {e: self.cnt[e] for e in self.ENG if self.cnt[e]}
        for ch, v in self.dcnt.items():
            if v:
                need[ch] = v
        self._emit_waits("sp", need)


class Co:
    def __init__(self):
        self.thread = None
        self.quota = 0
        self.b_go = threading.Semaphore(0)
        self.m_go = threading.Semaphore(0)
        self.done = True
        self.exc = None
        self.count = 0
        self.free_run = False
        self.SLACK = 0.3

    def start(self, fn):
        self.done = False
        self.count = 0
        self.exc = None

        def run():
            self.b_go.acquire()
            try:
                fn()
            except BaseException as e:
                self.exc = e
            self.done = True
            self.m_go.release()
        self.thread = threading.Thread(target=run)
        self.thread.start()

    def give(self, q):
        if self.done:
            return
        self.quota = q
        self.free_run = q >= (1 << 50)
        self.b_go.release()
        self.m_go.acquire()
        if self.exc is not None:
            raise self.exc

    def tick(self, sched=None, e=None, reads=(), writes=(), dur=0.0):
        if self.thread is not None and threading.current_thread() is self.thread:
            self.count += 1
            while not getattr(self, "free_run", False):
                self.quota -= 1
                blocked = False
                if sched is not None and not getattr(self, "no_pace", False):
                    fin = max(sched._ready(reads, writes) + sched.LAT, sched.tail[e]) + dur
                    blocked = fin > sched.tail["pool"] + sched.MARGIN[e]
                if self.quota >= 0 and not blocked:
                    break
                self.m_go.release()
                self.b_go.acquire()

    def drain(self):
        while not self.done:
            self.give(1 << 60)
        if self.thread is not None:
            self.thread.join()
            self.thread = None
        if self.exc is not None:
            raise self.exc


def build_program(nseq=NSEQ, nch=NCH, stop=99, overlap=True):
    nc = bass.Bass("TRN2", target_bir_lowering=False)
    dr = lambda name, shape, dt, kind="ExternalInput": nc.dram_tensor(name, shape, dt, kind=kind).ap()
    xT_d = dr("xT", [nseq, DC, 128, S], F32)
    cT_d = dr("cT", [128, DC, nseq], F32)
    pos_d = dr("pos", [nseq, S], I32)
    wada_d = dr("w_ada", [DC, 128, 6 * D], F32)
    vecs_d = dr("vecs", [128, NV], F32)
    win_d = dr("w_in", [DC, 128, WIN_COLS], F32)
    wabd_d = dr("wa_bd", [4, 128, 128], F32)
    wxbd_d = dr("wx_bd", [4, 128, 128], F32)
    wuq_d = dr("w_uq", [2, 128, 1536], F32)
    wukv_d = dr("w_ukv", [128, 1024], F32)
    wout_d = dr("w_out", [DC, 128, D], F32)
    wq_d = dr("peer_wq", [DC, 128, 2048], F32)
    keysT_d = dr("keysT", [128, 16, 128], F32)
    uv_d = dr("uv", [16384, 2048], F32)
    outT_d = dr("outT", [nseq, DC, 128, S], F32, kind="ExternalOutput")
    winb_d = dr("winb", [13, 128, DC, 128], BF16, kind="Internal")
    woutb_d = dr("woutb", [8, 128, DC, 128], BF16, kind="Internal")
    wqb_d = dr("wqb", [16, 128, DC, 128], BF16, kind="Internal")
    uvb_d = dr("uvb", [16384, 2048], BF16, kind="Internal")

    with ExitStack() as es:
        sc = Sched(nc, es)
        co = Co()
        sc.co = co
        PE, ACT, DVE, POOL, SP = nc.tensor, nc.scalar, nc.vector, nc.gpsimd, nc.sync
        uid = [0]

        def sb(stack, name, shape, dt):
            uid[0] += 1
            return stack.enter_context(nc.sbuf_tensor("%s_%d" % (name, uid[0]), shape, dt))

        w_uq = sb(es, "w_uq", [128, 2, 1536], BF16)
        w_ukv = sb(es, "w_ukv", [128, 1024], BF16)
        keysT = sb(es, "keysT", [128, 16, 128], F32)
        wa_bd = sb(es, "wa_bd", [128, 4, 128], BF16)
        wx_bd = sb(es, "wx_bd", [128, 4, 128], BF16)
        vecs = sb(es, "vecs", [128, NV], F32)
        modT = sb(es, "modT", [128, 48, nseq], F32)
        A1 = sb(es, "A1", [128, DC, nseq], F32)
        A2 = sb(es, "A2", [128, DC, nseq], F32)
        nsp = sb(es, "nsp", [128, 4], F32)
        consts = sb(es, "consts", [128, 4], F32)
        ones_bf = sb(es, "ones_bf", [128, 128], BF16)
        ident_f = sb(es, "ident_f", [128, 128], F32)
        ident_b = sb(es, "ident_b", [128, 128], BF16)
        tri_b = sb(es, "tri_b", [128, 128], BF16)
        iota16 = sb(es, "iota16", [128, 16], F32)
        KT = sb(es, "KT", [96, 8, S], BF16)
        VC = sb(es, "VC", [128, S // 128, 8, 65], BF16)
        xl = sb(es, "xl", [128, 4, C + 3], F32)
        hst = sb(es, "hst", [128, 4], F32)
        xTs = [sb(es, "xT%d" % i, [128, DC, C], F32) for i in range(2)]
        hT = sb(es, "hT", [128, DC, C], BF16)
        sq = sb(es, "sq", [128, DC, C], BF16)
        rstd = sb(es, "rstd", [128, C], F32)
        tmpf = sb(es, "tmpf", [128, C], F32)
        wr = [sb(es, "wr%d" % i, [128, DC, 128], BF16) for i in range(4)]
        idxs = [[sb(es, "idx%d%d" % (p, j), [128, 128], I32) for j in range(2)] for p in range(2)]
        ggs = [[sb(es, "gg%d%d" % (p, j), [128, 8, 16], F32) for j in range(2)] for p in range(2)]
        h2s = [[sb(es, "h2%d%d" % (p, j), [128, D], BF16) for j in range(2)] for p in range(2)]

        PS = [es.enter_context(nc.psum_tensor("ps%d" % i, [128, 512], F32)) for i in (0, 1, 2)]
        PS3 = es.enter_context(nc.psum_tensor("ps3", [128, 1024], BF16))
        PS += [None] + [es.enter_context(nc.psum_tensor("ps%d" % i, [128, 512], F32)) for i in (4, 5, 6, 7)]

        def vcol(c0, n=1):
            return vecs[:, c0:c0 + n]

        pes = ExitStack()
        if True:
            stage = sb(pes, "stage", [128, 4096], F32)
            cT = sb(pes, "cT", [128, DC, nseq], F32)
            iot_i = sb(pes, "iot_i", [128, 128], I32)
            iot_f = sb(pes, "iot_f", [128, 128], F32)
            sc.dma("sp", lambda: SP.dma_start(out=vecs[:], in_=vecs_d[:, :]), writes=["vecs"], ch="vecs")
            sc.dma("sp", lambda: SP.dma_start(out=cT[:], in_=cT_d[:, :, :]), writes=["cT"], ch="cT")
            sc.dma("sp", lambda: SP.dma_start(out=keysT[:], in_=keysT_d[:, :, :]), writes=["keysT"], ch="keysT")
            sc.op("dve", lambda: DVE.memset(consts[:, 0:1], EPS), writes=["consts"])
            sc.op("dve", lambda: DVE.memset(consts[:, 1:2], 1.0), writes=["consts"])
            sc.op("dve", lambda: DVE.memset(consts[:, 2:3], 0.0), writes=["consts"])
            sc.op("dve", lambda: DVE.memset(ones_bf[:], 1.0), writes=["ones_bf"])
            sc.op("dve", lambda: DVE.memset(VC[:], 1.0), writes=["VC"])
            sc.op("dve", lambda: DVE.memset(KT[:], 0.0), writes=["KT"])
            sc.op("pool", lambda: POOL.iota(iot_i[:], pattern=[[1, 128]], base=0, channel_multiplier=-1), writes=["iot_i"])
            sc.op("dve", lambda: DVE.tensor_copy(iot_f[:], iot_i[:]), reads=["iot_i"], writes=["iot_f"])
            sc.op("dve", lambda: DVE.tensor_scalar(ident_f[:], iot_f[:], 0.0, None, op0=ALU.is_equal), reads=["iot_f"], writes=["ident_f"])
            sc.op("dve", lambda: DVE.tensor_copy(ident_b[:], ident_f[:]), reads=["ident_f"], writes=["ident_b"])
            sc.op("dve", lambda: DVE.tensor_scalar(tri_b[:], iot_f[:], 0.0, None, op0=ALU.is_ge), reads=["iot_f"], writes=["tri_b"])
            sc.op("pool", lambda: POOL.iota(iot_i[:, 0:16], pattern=[[1, 16]], base=0, channel_multiplier=0), reads=["iot_f"], writes=["iot_i"])
            sc.op("dve", lambda: DVE.tensor_copy(iota16[:], iot_i[:, 0:16]), reads=["iot_i"], writes=["iota16"])

            stb = [sb(pes, "stb%d" % i, [128, 2048], BF16) for i in range(2)]

            def cast_op(k, dst_ap, st, key, wkey):
                eng = ("dve", "act", "pool")[k % 3]
                if eng == "dve":
                    sc.op("dve", lambda: DVE.tensor_copy(dst_ap, st), reads=[key], writes=[wkey])
                elif eng == "act":
                    sc.op("act", lambda: ACT.copy(dst_ap, st), reads=[key], writes=[wkey])
                else:
                    sc.op("pool", lambda: POOL.tensor_copy(dst_ap, st), reads=[key], writes=[wkey])

            def load_cast(dst_ap, src_ap, ncols, k, outs=None):
                st = stage[:, 0:ncols] if k % 2 == 0 else stage[:, 2048:2048 + ncols]
                key = "stage%d" % (k % 2)
                sc.dma("sp", lambda: SP.dma_start(out=st, in_=src_ap), writes=[key], ch=key)
                if outs is None:
                    cast_op(k, dst_ap, st, key, "W")
                    return
                bkey = "stb%d" % (k % 2)
                sbt = stb[k % 2]
                cast_op(k, sbt[:, 0:ncols], st, key, bkey)
                for oi, (d_ap, s_ap) in enumerate(outs(sbt)):
                    sc.dma("sp", lambda: SP.dma_start(out=d_ap, in_=s_ap), reads=[bkey], writes=["wscr"], ch="%so%d" % (bkey, oi))
            k = 0
            for dc in range(DC):
                load_cast(None, win_d[dc], WIN_COLS, k, outs=lambda t, dc=dc: [
                    (winb_d[0:11, :, dc, :].rearrange("oc p n -> p oc n"), t[:, 0:1408].rearrange("p (oc n) -> p oc n", n=128)),
                    (winb_d[11, :, dc, 0:96], t[:, 1344:1440]),
                    (winb_d[12, :, dc, 0:96], t[:, 1376:1472])]); k += 1
                load_cast(None, wout_d[dc], D, k, outs=lambda t, dc=dc: [
                    (woutb_d[:, :, dc, :].rearrange("oc p n -> p oc n"), t[:, 0:1024].rearrange("p (oc n) -> p oc n", n=128))]); k += 1
                load_cast(None, wq_d[dc], 2048, k, outs=lambda t, dc=dc: [
                    (wqb_d[:, :, dc, :].rearrange("oc p n -> p oc n"), t[:, 0:2048].rearrange("p (oc n) -> p oc n", n=128))]); k += 1
            for kc in range(2):
                load_cast(w_uq[:, kc, :], wuq_d[kc], 1536, k); k += 1
            load_cast(w_ukv[:, :], wukv_d[:, :], 1024, k); k += 1
            for ci in range(4):
                load_cast(wa_bd[:, ci, :], wabd_d[ci], 128, k); k += 1
                load_cast(wx_bd[:, ci, :], wxbd_d[ci], 128, k); k += 1

            sc.op("act", lambda: ACT.activation(out=nsp[:], in_=vcol(V_LAM, 4), func=AF.Exp, scale=-1.0), reads=["vecs"], writes=["nsp"])
            sc.op("act", lambda: ACT.activation(out=nsp[:], in_=nsp[:], func=AF.Ln, bias=consts[:, 1:2], scale=1.0), reads=["nsp", "consts"], writes=["nsp"])
            sc.op("dve", lambda: DVE.tensor_scalar(nsp[:], nsp[:], -16.0, None, op0=ALU.mult), reads=["nsp"], writes=["nsp"])

            sc.op("act", lambda: ACT.activation(out=cT[:], in_=cT[:], func=AF.Silu), reads=["cT"], writes=["cT"])
            ps_mod = PS[0][:, 0:48 * nseq].rearrange("p (n b) -> p n b", b=nseq)
            for n in range(48):
                key = "stage%d" % (n % 2)
                st = stage[:, (n % 2) * 2048:(n % 2) * 2048 + 1024].rearrange("p (dc n) -> p dc n", n=128)
                sc.dma("sp", lambda: SP.dma_start(out=st, in_=wada_d[:, :, n * 128:(n + 1) * 128].rearrange("dc p n -> p dc n")), writes=[key], ch=key)
                for dc in range(DC):
                    sc.op("pe", lambda: PE.matmul(ps_mod[:, n, :], st[:, dc, :], cT[:, dc, :], start=(dc == 0), stop=(dc == DC - 1)),
                          reads=[key, "cT"], writes=["ps0"])
            sc.op("dve", lambda: DVE.tensor_tensor(out=modT[:], in0=ps_mod, in1=vcol(V_BADA, 48).unsqueeze(2).to_broadcast([128, 48, nseq]), op=ALU.add),
                  reads=["ps0", "vecs"], writes=["modT"])
            for dc in range(DC):
                sc.op("dve", lambda: DVE.tensor_scalar(A1[:, dc, :], modT[:, 8 + dc, :], 1.0, vcol(V_N1G + dc), op0=ALU.add, op1=ALU.mult),
                      reads=["modT", "vecs"], writes=["A1"])
                sc.op("dve", lambda: DVE.tensor_scalar(A2[:, dc, :], modT[:, 32 + dc, :], 1.0, vcol(V_N2G + dc), op0=ALU.add, op1=ALU.mult),
                      reads=["modT", "vecs"], writes=["A2"])
            sc.barrier()

        def rms_stats(src_tile, nchunks, inv_n, srckey, sq_t=None, rstd_t=None, bank=0, sfx="", sqkeys=None):
            sq_t = sq if sq_t is None else sq_t
            rstd_t = rstd if rstd_t is None else rstd_t
            sk, rk, pk = "sq" + sfx, "rstd" + sfx, "ps%d" % bank
            sks = [sk] if sqkeys is None else list(sqkeys)
            srckeys = list(srckey) if isinstance(srckey, (list, tuple)) else [srckey]
            sc.op("act", lambda: ACT.activation(out=sq_t[:, 0:nchunks, :], in_=src_tile, func=AF.Square), reads=srckeys, writes=sks, cost=0.25 + 0.21 * nchunks)
            for i in range(nchunks):
                sc.op("pe", lambda: PE.matmul(PS[bank][:, 0:C], ones_bf[:], sq_t[:, i, :], start=(i == 0), stop=(i == nchunks - 1)),
                      reads=sks, writes=[pk])
            sc.op("act", lambda: ACT.activation(out=rstd_t[:], in_=PS[bank][:, 0:C], func=AF.Sqrt, bias=consts[:, 0:1], scale=inv_n), reads=[pk], writes=[rk])
            sc.op("dve", lambda: DVE.reciprocal(rstd_t[:], rstd_t[:]), reads=[rk], writes=[rk])

        def modulated_norm(xT, xk, Acol, shift_chunk0, b):
            rms_stats(xT[:], DC, 1.0 / D, xk)
            for dc in range(DC):
                sc.op("dve", lambda: DVE.scalar_tensor_tensor(out=tmpf[:], in0=xT[:, dc, :], scalar=Acol[:, dc, b:b + 1], in1=rstd[:], op0=ALU.mult, op1=ALU.mult),
                      reads=[xk, "rstd"], writes=["tmpf"])
                sc.op("act", lambda: ACT.activation(out=hT[:, dc, :], in_=tmpf[:], func=AF.Identity, bias=modT[:, shift_chunk0 + dc, b:b + 1], scale=1.0),
                      reads=["tmpf"], writes=["hT"])

        gen_i = [0]

        def gen_ps():
            bnk = (1, 2)[gen_i[0] % 2]
            gen_i[0] += 1
            return PS[bnk][:, 0:C], "ps%d" % bnk

        ring_i = [0]

        def wload(src_ap, ncols=128):
            i = ring_i[0] % 4
            ring_i[0] += 1
            key = "wr%d" % i
            sc.dma("sp", lambda: SP.dma_start(out=wr[i][:, :, 0:ncols], in_=src_ap), reads=["wscr"], writes=[key], ch=key)
            return wr[i], key

        class WStream:
            def __init__(self, blocks, depth=3):
                self.blocks = list(blocks)
                self.pend = []
                self.depth = depth
                for _ in range(depth):
                    self._issue()

            def _issue(self):
                if self.blocks:
                    src, ncols = self.blocks.pop(0)
                    self.pend.append(wload(src, ncols))

            def next(self):
                w, key = self.pend.pop(0)
                self._issue()
                return w, key

        def emit_B(b, c, par):
            sc.in_b_thread = threading.current_thread()
            sc.b_active = True
            t0 = c * C
            xT = xTs[par]
            xk = "xT%d" % par
            if c == 0:
                sc.op("dve", lambda: DVE.memset(xl[:], 0.0), writes=["xl0", "xl1", "xl2", "xl3"])
                sc.op("dve", lambda: DVE.memset(hst[:], 0.0), writes=["hst0", "hst1", "hst2", "hst3"])
            with ExitStack() as mes:
                gT = sb(mes, "gT", [128, 4, C], F32)
                qlat = sb(mes, "qlat", [128, 2, C], F32)
                kvlat = sb(mes, "kvlat", [128, C], F32)
                qs = sb(mes, "qs", [128, 2, C], BF16)
                kvs = sb(mes, "kvs", [128, C], BF16)
                xc = sb(mes, "xc", [128, C], F32)
                xcb = sb(mes, "xcb", [128, C], BF16)
                ra = sb(mes, "ra", [128, C], F32)
                ib = sb(mes, "ib", [128, C], F32)
                hh = sb(mes, "hh", [128, C], F32)
                xc2 = sb(mes, "xc2", [128, C], F32)
                xcb2 = sb(mes, "xcb2", [128, C], BF16)
                ra2 = sb(mes, "ra2", [128, C], F32)
                ib2 = sb(mes, "ib2", [128, C], F32)
                hh2 = sb(mes, "hh2", [128, C], F32)
                ylru = sb(mes, "ylru", [128, 4, C], F32)
                yT = sb(mes, "yT", [128, 8, C], BF16)
                QT = sb(mes, "QT", [96, 8, C], BF16)
                posi = sb(mes, "posi", [96, C], I32)
                ang = sb(mes, "ang", [96, C], F32)
                kf = sb(mes, "kf", [96, C], F32)
                cos2 = sb(mes, "cos2", [96, C], F32)
                sin2 = sb(mes, "sin2", [96, C], F32)
                t1 = tmpf
                t2 = sb(mes, "t2", [96, C], F32)
                krb = sb(mes, "krb", [96, C], BF16)
                pT = [sb(mes, "pT%d" % i, [128, C], BF16) for i in range(3)]
                ymla = sb(mes, "ymla", [128, 2, 512], F32)
                ymn = sb(mes, "ymn", [128, 2, 512], BF16)
                rinv = sb(mes, "rinv", [128, 2], F32)
                sst = sb(mes, "sst", [128, 2], F32)

                win_blocks = [(winb_d[oc, :, :, :], 128) for oc in range(11)] + [(winb_d[11, :, :, 0:96], 96), (winb_d[12, :, :, 0:96], 96)]
                wst = WStream(win_blocks)
                sc.dma("sp", lambda: SP.dma_start(out=xT[:], in_=xT_d[b, :, :, t0:t0 + C].rearrange("dc p t -> p dc t")), writes=[xk], ch=xk)
                sc.dma("sp", lambda: SP.dma_start(out=posi[64:96, :], in_=pos_d[b:b + 1, t0:t0 + C].partition_broadcast(32)), writes=["posi"], ch="posi")
                modulated_norm(xT, xk, A1, 0, b)

                R = slice(64, 96)
                sc.op("dve", lambda: DVE.tensor_copy(ang[R, :], posi[R, :]), reads=["posi"], writes=["ang"])
                sc.op("dve", lambda: DVE.tensor_scalar(ang[R, :], ang[R, :], vecs[R, V_INVF:V_INVF + 1], None, op0=ALU.mult), reads=["ang", "vecs"], writes=["ang"])
                for shift, dst, use_sgn in ((0.0, sin2, True), (math.pi / 2, cos2, False)):
                    sc.op("dve", lambda: DVE.tensor_scalar(kf[R, :], ang[R, :], shift, 1.0 / TWO_PI, op0=ALU.add, op1=ALU.mult), reads=["ang"], writes=["kf"])
                    sc.op("dve", lambda: DVE.tensor_copy(posi[R, :], kf[R, :]), reads=["kf"], writes=["posi"])
                    sc.op("dve", lambda: DVE.tensor_copy(kf[R, :], posi[R, :]), reads=["posi"], writes=["kf"])
                    sc.op("dve", lambda: DVE.scalar_tensor_tensor(out=kf[R, :], in0=kf[R, :], scalar=-TWO_PI, in1=ang[R, :], op0=ALU.mult, op1=ALU.add),
                          reads=["kf", "ang"], writes=["kf"])
                    sc.op("dve", lambda: DVE.tensor_scalar(kf[R, :], kf[R, :], shift, None, op0=ALU.add), reads=["kf"], writes=["kf"])
                    sc.op("dve", lambda: DVE.tensor_scalar(kf[R, :], kf[R, :], 3.1415925, -3.1415925, op0=ALU.min, op1=ALU.max), reads=["kf"], writes=["kf"])
                    if use_sgn:
                        sc.op("act", lambda: ACT.activation(out=dst[R, :], in_=kf[R, :], func=AF.Sin, scale=vecs[R, V_SGN:V_SGN + 1]), reads=["kf", "vecs"], writes=["rope"])
                    else:
                        sc.op("act", lambda: ACT.activation(out=dst[R, :], in_=kf[R, :], func=AF.Sin), reads=["kf"], writes=["rope"])
                ROPE = ["rope"]

                def inproj(ncols=128):
                    w, wkey = wst.next()
                    ps, key = gen_ps()
                    for dc in range(DC):
                        sc.op("pe", lambda: PE.matmul(ps[0:ncols, :], w[:, dc, 0:ncols], hT[:, dc, :], start=(dc == 0), stop=(dc == DC - 1)),
                              reads=["hT", wkey], writes=[key])
                    return ps, key
                for ci in range(4):
                    ps, key = inproj()
                    sc.op("act", lambda: ACT.copy(xl[:, ci, 3:3 + C], ps), reads=[key], writes=["xl%d" % ci])
                for ci in range(4):
                    ps, key = inproj()
                    sc.op("act", lambda: ACT.activation(out=gT[:, ci, :], in_=ps, func=AF.Gelu_apprx_tanh), reads=[key], writes=["gT"])
                for kc in range(2):
                    ps, key = inproj()
                    sc.op("dve", lambda: DVE.tensor_copy(qlat[:, kc, :], ps), reads=[key], writes=["qlat"])
                ps, key = inproj()
                sc.op("dve", lambda: DVE.tensor_copy(kvlat[:], ps), reads=[key], writes=["kvlat"])
                ps_kr, key_kr = inproj(96)
                ps_krr, key_krr = inproj(96)
                sc.op("dve", lambda: DVE.tensor_tensor(out=t1[R, :], in0=ps_kr[R, :], in1=cos2[R, :], op=ALU.mult), reads=[key_kr] + ROPE, writes=["tmpf"])
                sc.op("dve", lambda: DVE.tensor_tensor(out=t2[R, :], in0=ps_krr[R, :], in1=sin2[R, :], op=ALU.mult), reads=[key_krr] + ROPE, writes=["t2"])
                sc.op("dve", lambda: DVE.tensor_tensor(out=krb[R, :], in0=t1[R, :], in1=t2[R, :], op=ALU.add), reads=["tmpf", "t2"], writes=["krb"])
                for h in range(8):
                    if h % 2 == 0:
                        sc.op("act", lambda: ACT.copy(KT[R, h, t0:t0 + C], krb[R, :]), reads=["krb"], writes=["KT"])
                    else:
                        sc.op("dve", lambda: DVE.tensor_copy(KT[R, h, t0:t0 + C], krb[R, :]), reads=["krb"], writes=["KT"])

                def lru_stages(ci, B_, sfx, banks):
                    xc_, xcb_, ra_, ib_, hh_ = B_
                    kxc, kxcb, kra, kib, khh = ["%s%s" % (n, sfx) for n in ("xc", "xcb", "ra", "ib", "hh")]
                    xk_ = "xl%d" % ci
                    cw = V_CONVW + ci * 4
                    ps_r, key_r = PS[banks[0]][:, 0:C], "ps%d" % banks[0]
                    ps_i, key_i = PS[banks[1]][:, 0:C], "ps%d" % banks[1]
                    st = []
                    st.append(lambda: sc.op("dve", lambda: DVE.tensor_scalar(xc_[:], xl[:, ci, 0:C], vcol(cw), vcol(V_CONVB + ci), op0=ALU.mult, op1=ALU.add),
                                            reads=[xk_, "vecs"], writes=[kxc]))
                    for kk in range(1, 4):
                        st.append(lambda kk=kk: sc.op("dve", lambda: DVE.scalar_tensor_tensor(out=xc_[:], in0=xl[:, ci, kk:kk + C], scalar=vcol(cw + kk), in1=xc_[:], op0=ALU.mult, op1=ALU.add),
                                                      reads=[xk_, kxc], writes=[kxc]))
                    st.append(lambda: sc.op("act", lambda: ACT.copy(xl[:, ci, 0:3], xl[:, ci, C:C + 3]), reads=[xk_, kxc], writes=[xk_]))
                    st.append(lambda: sc.op("act", lambda: ACT.copy(xcb_[:], xc_[:]), reads=[kxc], writes=[kxcb]))
                    st.append(lambda: sc.op("pe", lambda: PE.matmul(ps_r, wa_bd[:, ci, :], xcb_[:], start=True, stop=True), reads=[kxcb], writes=[key_r]))
                    st.append(lambda: sc.op("pe", lambda: PE.matmul(ps_i, wx_bd[:, ci, :], xcb_[:], start=True, stop=True), reads=[kxcb], writes=[key_i]))
                    st.append(lambda: sc.op("act", lambda: ACT.activation(out=ra_[:], in_=ps_r, func=AF.Sigmoid, bias=vcol(V_BA + ci), scale=1.0), reads=[key_r], writes=[kra]))
                    st.append(lambda: sc.op("act", lambda: ACT.activation(out=ib_[:], in_=ps_i, func=AF.Sigmoid, bias=vcol(V_BX + ci), scale=1.0), reads=[key_i], writes=[kib]))
                    st.append(lambda: sc.op("act", lambda: ACT.activation(out=hh_[:], in_=ra_[:], func=AF.Exp, scale=nsp[:, ci:ci + 1]), reads=[kra], writes=[khh]))
                    st.append(lambda: sc.op("dve", lambda: DVE.tensor_scalar(ra_[:], ra_[:], nsp[:, ci:ci + 1], 0.5, op0=ALU.mult, op1=ALU.mult), reads=[kra, khh], writes=[kra]))
                    st.append(lambda: sc.op("act", lambda: ACT.activation(out=ra_[:], in_=ra_[:], func=AF.Exp), reads=[kra], writes=[kra]))
                    st.append(lambda: sc.op("act", lambda: ACT.activation(out=hh_[:], in_=hh_[:], func=AF.Sqrt, bias=consts[:, 1:2], scale=-1.0), reads=[khh], writes=[khh]))
                    st.append(lambda: sc.op("dve", lambda: DVE.tensor_tensor(out=ib_[:], in0=ib_[:], in1=xc_[:], op=ALU.mult), reads=[kib, kxc], writes=[kib]))
                    st.append(lambda: sc.op("dve", lambda: DVE.tensor_tensor(out=ib_[:], in0=ib_[:], in1=hh_[:], op=ALU.mult), reads=[kib, khh], writes=[kib]))
                    st.append(lambda: sc.op("dve", lambda: DVE.tensor_tensor_scan(out=hh_[:], data0=ra_[:], data1=ib_[:], initial=hst[:, ci:ci + 1], op0=ALU.mult, op1=ALU.add),
                                            reads=[kra, kib, "hst%d" % ci], writes=[khh], cost=0.6))
                    st.append(lambda: sc.op("dve", lambda: DVE.tensor_copy(hst[:, ci:ci + 1], hh_[:, C - 1:C]), reads=[khh], writes=["hst%d" % ci]))
                    st.append(lambda: sc.op("dve", lambda: DVE.tensor_tensor(out=ylru[:, ci, :], in0=hh_[:], in1=gT[:, ci, :], op=ALU.mult), reads=[khh, "gT"], writes=["ylru%d" % ci]))
                    return st
                setA = (xc, xcb, ra, ib, hh)
                setB = (xc2, xcb2, ra2, ib2, hh2)
                for pair in ((0, 1), (2, 3)):
                    sa = lru_stages(pair[0], setA, "", (1, 2))
                    sb_ = lru_stages(pair[1], setB, "b", (4, 5))
                    for fa, fb in zip(sa, sb_):
                        fa()
                        fb()
                rms_stats(ylru[:], 4, 1.0 / 512, ["ylru0", "ylru1", "ylru2", "ylru3"])
                for ci in range(4):
                    sc.op("dve", lambda: DVE.scalar_tensor_tensor(out=yT[:, ci, :], in0=ylru[:, ci, :], scalar=vcol(V_LOG + ci), in1=rstd[:], op0=ALU.mult, op1=ALU.mult),
                          reads=["ylru%d" % ci, "rstd"], writes=["yT"])

                rms_stats(qlat[:], 2, 1.0 / 256, "qlat")
                for kc in range(2):
                    sc.op("dve", lambda: DVE.scalar_tensor_tensor(out=qs[:, kc, :], in0=qlat[:, kc, :], scalar=vcol(V_QNG + kc), in1=rstd[:], op0=ALU.mult, op1=ALU.mult),
                          reads=["qlat", "rstd"], writes=["qs"])
                rms_stats(kvlat[:].unsqueeze(1), 1, 1.0 / 128, "kvlat")
                sc.op("dve", lambda: DVE.scalar_tensor_tensor(out=kvs[:], in0=kvlat[:], scalar=vcol(V_KVNG), in1=rstd[:], op0=ALU.mult, op1=ALU.mult),
                      reads=["kvlat", "rstd"], writes=["kvs"])
                for h in range(8):
                    ps_q, key_q = gen_ps()
                    ps_qr, key_qr = gen_ps()
                    for kc in range(2):
                        sc.op("pe", lambda: PE.matmul(ps_q[0:96, :], w_uq[:, kc, h * 192:h * 192 + 96], qs[:, kc, :], start=(kc == 0), stop=(kc == 1)), reads=["qs"], writes=[key_q])
                    for kc in range(2):
                        sc.op("pe", lambda: PE.matmul(ps_qr[0:96, :], w_uq[:, kc, h * 192 + 96:h * 192 + 192], qs[:, kc, :], start=(kc == 0), stop=(kc == 1)), reads=["qs"], writes=[key_qr])
                    sc.op("act", lambda: ACT.copy(QT[0:64, h, :], ps_q[0:64, :]), reads=[key_q], writes=["QT"])
                    sc.op("dve", lambda: DVE.tensor_tensor(out=t1[R, :], in0=ps_q[R, :], in1=cos2[R, :], op=ALU.mult), reads=[key_q] + ROPE, writes=["tmpf"])
                    sc.op("dve", lambda: DVE.tensor_tensor(out=t2[R, :], in0=ps_qr[R, :], in1=sin2[R, :], op=ALU.mult), reads=[key_qr] + ROPE, writes=["t2"])
                    sc.op("dve", lambda: DVE.tensor_tensor(out=QT[R, h, :], in0=t1[R, :], in1=t2[R, :], op=ALU.add), reads=["tmpf", "t2"], writes=["QT"])
                    ps_k, key_k = gen_ps()
                    sc.op("pe", lambda: PE.matmul(ps_k[0:64, :], w_ukv[:, h * 128:h * 128 + 64], kvs[:], start=True, stop=True), reads=["kvs"], writes=[key_k])
                    sc.op("act", lambda: ACT.copy(KT[0:64, h, t0:t0 + C], ps_k[0:64, :]), reads=[key_k], writes=["KT"])
                wv = w_ukv[:, :].rearrange("p (h x) -> p h x", x=128)[:, :, 64:128]
                for j in range(C // 128):
                    tile_i = (t0 // 128) + j
                    sc.op("pe", lambda: PE.matmul(PS[4][:, :].rearrange("p (h x) -> p h x", x=64), kvs[:, j * 128:(j + 1) * 128], wv, start=True, stop=True),
                          reads=["kvs"], writes=["ps4"])
                    sc.op("dve", lambda: DVE.tensor_copy(VC[:, tile_i, :, 0:64], PS[4][:, :].rearrange("p (h x) -> p h x", x=64)), reads=["ps4"], writes=["VC"])

                wso = WStream([(woutb_d[oc, :, :, :], 128) for oc in range(8)])

                scale = 96.0 ** -0.5
                nkt = (t0 + C) // 128
                kdiag0 = t0 // 128
                it = 0
                abk = (1, 2)
                akeys = ["ps1", "ps2"]
                for h in range(8):
                    for kt in range(nkt):
                        sbank = (4, 5, 0)[it % 3]
                        skey = "ps%d" % sbank
                        pt = pT[it % 3]
                        pkey = "pT%d" % (it % 3)
                        it += 1
                        sc.op("pe", lambda: PE.matmul(PS[sbank][:, 0:C], KT[0:96, h, kt * 128:(kt + 1) * 128], QT[0:96, h, :], start=True, stop=True),
                              reads=["KT", "QT"], writes=[skey])
                        sc.op("act", lambda: ACT.activation(out=pt[:], in_=PS[sbank][:, 0:C], func=AF.Exp, scale=scale), reads=[skey], writes=[pkey])
                        jk = kt - kdiag0
                        if jk >= 0:
                            sc.op("dve", lambda: DVE.tensor_tensor(out=pt[:, jk * 128:(jk + 1) * 128], in0=pt[:, jk * 128:(jk + 1) * 128], in1=tri_b[:], op=ALU.mult),
                                  reads=[pkey], writes=[pkey])
                        for jq in range(C // 128):
                            if jk > jq:
                                continue
                            last = kdiag0 + jq
                            sc.op("pe", lambda: PE.matmul(PS[abk[jq]][:, 0:65], pt[:, jq * 128:(jq + 1) * 128], VC[:, kt, h, :], start=(kt == 0), stop=(kt == last)),
                                  reads=[pkey, "VC"], writes=[akeys[jq]])
                    for jq in range(C // 128):
                        sc.op("dve", lambda: DVE.reciprocal(rinv[:, jq:jq + 1], PS[abk[jq]][:, 64:65]), reads=[akeys[jq]], writes=["rinv"])
                        sc.op("dve", lambda: DVE.tensor_scalar(ymla[:, jq, h * 64:(h + 1) * 64], PS[abk[jq]][:, 0:64], rinv[:, jq:jq + 1], None, op0=ALU.mult),
                              reads=[akeys[jq], "rinv"], writes=["ymla"])
                for jq in range(C // 128):
                    sc.op("dve", lambda: DVE.scalar_tensor_tensor(out=ymn[:, jq, :], in0=ymla[:, jq, :], scalar=1.0, in1=ymla[:, jq, :], op0=ALU.mult, op1=ALU.mult, accum_out=sst[:, jq:jq + 1]),
                          reads=["ymla"], writes=["ymn", "sst"])
                sc.op("act", lambda: ACT.activation(out=sst[:], in_=sst[:], func=AF.Sqrt, bias=consts[:, 0:1], scale=1.0 / 512), reads=["sst"], writes=["sst"])
                sc.op("dve", lambda: DVE.reciprocal(sst[:], sst[:]), reads=["sst"], writes=["sst"])
                for jq in range(C // 128):
                    sc.op("dve", lambda: DVE.tensor_scalar(ymn[:, jq, :], ymla[:, jq, :], sst[:, jq:jq + 1], None, op0=ALU.mult), reads=["ymla", "sst"], writes=["ymn"])
                    for fc in range(4):
                        sc.op("pe", lambda: PE.transpose(PS3[:, fc * 128:(fc + 1) * 128], ymn[:, jq, fc * 128:(fc + 1) * 128], ident_b[:]), reads=["ymn"], writes=["ps3"])
                    for fc in range(4):
                        sc.op("dve", lambda: DVE.tensor_scalar(yT[:, 4 + fc, jq * 128:(jq + 1) * 128], PS3[:, fc * 128:(fc + 1) * 128], vcol(V_MOG + fc), None, op0=ALU.mult),
                              reads=["ps3"], writes=["yT"])
                for oc in range(DC):
                    w, wkey = wso.next()
                    ps, key = gen_ps()
                    for cc in range(8):
                        sc.op("pe", lambda: PE.matmul(ps, w[:, cc, :], yT[:, cc, :], start=(cc == 0), stop=(cc == 7)), reads=["yT", wkey], writes=[key])
                    sc.op("dve", lambda: DVE.scalar_tensor_tensor(out=xT[:, oc, :], in0=ps, scalar=modT[:, 16 + oc, b:b + 1], in1=xT[:, oc, :], op0=ALU.mult, op1=ALU.add),
                          reads=[key, xk], writes=[xk])
                sc.barrier_b()

            with ExitStack() as pes2:
                qT = sb(pes2, "qT", [128, 16, C], F32)
                scs = sb(pes2, "scs", [128, 2048], F32)
                work = sb(pes2, "work", [128, 2048], F32)
                top = sb(pes2, "top", [128, 16, 16], F32)
                tix = sb(pes2, "tix", [128, 16, 16], U32)
                tixf = sb(pes2, "tixf", [128, 16, 16], F32)
                best = sb(pes2, "best", [128, 8, 16], F32)
                posu = sb(pes2, "posu", [128, 8, 16], U32)
                pa_i = sb(pes2, "pa_i", [128, 8, 16], I32)
                pa_f = sb(pes2, "pa_f", [128, 8, 16], F32)
                pb_f = sb(pes2, "pb_f", [128, 8, 16], F32)
                gsum = sb(pes2, "gsum", [128, 8], F32)
                isel = sb(pes2, "isel", [128, 8, 16], F32)
                jsel = sb(pes2, "jsel", [128, 8, 16], F32)

                modulated_norm(xT, xk, A2, 24, b)
                wsq = WStream([(wqb_d[hp, :, :, :], 128) for hp in range(16)])
                for hp in range(16):
                    w, wkey = wsq.next()
                    qb = (0, 1, 2, 4, 5)[hp % 5]
                    qk = "ps%d" % qb
                    pq = PS[qb][:, 0:C]
                    for dc in range(DC):
                        sc.op("pe", lambda: PE.matmul(pq, w[:, dc, :], hT[:, dc, :], start=(dc == 0), stop=(dc == DC - 1)), reads=["hT", wkey], writes=[qk])
                    if hp % 2 == 0:
                        sc.op("act", lambda: ACT.copy(qT[:, hp, :], pq), reads=[qk], writes=["qT%d" % hp])
                    else:
                        sc.op("dve", lambda: DVE.tensor_copy(qT[:, hp, :], pq), reads=[qk], writes=["qT%d" % hp])
                for j in range(C // 128):
                    ts = slice(j * 128, (j + 1) * 128)
                    h2tm = h2s[par][j]
                    gg = ggs[par][j]
                    idxi = idxs[par][j]
                    hkey, gkey_, ikey = "h2%d%d" % (par, j), "gg%d%d" % (par, j), "idx%d%d" % (par, j)
                    for dc in range(DC):
                        sc.op("pe", lambda: PE.transpose(PS3[:, dc * 128:(dc + 1) * 128], hT[:, dc, ts], ident_b[:]), reads=["hT"], writes=["ps3"])
                    sc.op("act", lambda: ACT.copy(h2tm[:], PS3[:, :]), reads=["ps3"], writes=[hkey], cost=1.0)
                    sbanks = (1, 2, 4, 5)
                    for hp in range(16):
                        bnk = sbanks[hp // 4]
                        sc.op("pe", lambda: PE.matmul(PS[bnk][:, (hp % 4) * 128:(hp % 4 + 1) * 128], qT[:, hp, ts], keysT[:, hp, :], start=True, stop=True),
                              reads=["qT%d" % hp], writes=["ps%d" % bnk])
                    for q in range(4):
                        bnk = sbanks[q]
                        if q % 2 == 0:
                            sc.op("act", lambda: ACT.copy(scs[:, q * 512:(q + 1) * 512], PS[bnk][:, :]), reads=["ps%d" % bnk], writes=["scs%d" % q])
                        else:
                            sc.op("dve", lambda: DVE.tensor_copy(scs[:, q * 512:(q + 1) * 512], PS[bnk][:, :]), reads=["ps%d" % bnk], writes=["scs%d" % q])
                    TOPK = ["top%d" % hp for hp in range(16)]
                    TIXK = ["tix%d" % hp for hp in range(16)]
                    WRKK = ["work%d" % hp for hp in range(16)]
                    sks = ["scs%d" % (hp // 4) for hp in range(16)]
                    svs = [scs[:, hp * 128:(hp + 1) * 128] for hp in range(16)]
                    wvs = [work[:, hp * 128:(hp + 1) * 128] for hp in range(16)]
                    for hp in range(16):
                        sc.op("dve", lambda: DVE.max(out=top[:, hp, 0:8], in_=svs[hp]), reads=[sks[hp]], writes=[TOPK[hp]], cost=0.2)
                    for hp in range(16):
                        sc.op("dve", lambda: DVE.max_index(out=tix[:, hp, 0:8], in_max=top[:, hp, 0:8], in_values=svs[hp]), reads=[sks[hp], TOPK[hp]], writes=[TIXK[hp]], cost=0.25)
                    for hp in range(16):
                        sc.op("dve", lambda: DVE.match_replace(out=wvs[hp], in_to_replace=top[:, hp, 0:8], in_values=svs[hp], imm_value=NEG), reads=[sks[hp], TOPK[hp]], writes=[WRKK[hp]], cost=0.25)
                    for hp in range(16):
                        sc.op("dve", lambda: DVE.max(out=top[:, hp, 8:16], in_=wvs[hp]), reads=[WRKK[hp]], writes=[TOPK[hp]], cost=0.2)
                    for hp in range(16):
                        sc.op("dve", lambda: DVE.max_index(out=tix[:, hp, 8:16], in_max=top[:, hp, 8:16], in_values=wvs[hp]), reads=[WRKK[hp], TOPK[hp]], writes=[TIXK[hp]], cost=0.25)
                    sc.op("dve", lambda: DVE.tensor_copy(tixf[:], tix[:]), reads=TIXK, writes=["tixf"])
                    top4 = top[:].rearrange("p (h two) k -> p h two k", two=2)
                    tix4 = tixf[:].rearrange("p (h two) k -> p h two k", two=2)
                    cand = work[:].rearrange("p (h a b) -> p h a b", a=16, b=16)
                    cand3 = work[:].rearrange("p (h ab) -> p h ab", ab=256)
                    cand2 = scs[:].rearrange("p (h ab) -> p h ab", ab=256)
                    ALLS = ["scs0", "scs1", "scs2", "scs3"]
                    WH = [[WRKK[2 * h], WRKK[2 * h + 1]] for h in range(8)]
                    SH = ["scs%d" % (h // 2) for h in range(8)]
                    BK = ["best%d" % h for h in range(8)]
                    PK = ["posu%d" % h for h in range(8)]
                    sc.op("dve", lambda: DVE.tensor_tensor(out=cand, in0=top4[:, :, 0, :].unsqueeze(3).to_broadcast([128, 8, 16, 16]),
                                                           in1=top4[:, :, 1, :].unsqueeze(2).to_broadcast([128, 8, 16, 16]), op=ALU.add),
                          reads=TOPK, writes=WRKK, cost=2.2)
                    for h in range(8):
                        sc.op("dve", lambda: DVE.max(out=best[:, h, 0:8], in_=cand3[:, h, :]), reads=WH[h], writes=[BK[h]], cost=0.35)
                    for h in range(8):
                        sc.op("dve", lambda: DVE.max_index(out=posu[:, h, 0:8], in_max=best[:, h, 0:8], in_values=cand3[:, h, :]), reads=WH[h] + [BK[h]], writes=[PK[h]], cost=0.4)
                    for h in range(8):
                        sc.op("dve", lambda: DVE.match_replace(out=cand2[:, h, :], in_to_replace=best[:, h, 0:8], in_values=cand3[:, h, :], imm_value=NEG), reads=WH[h] + [BK[h]], writes=[SH[h]], cost=0.4)
                    for h in range(8):
                        sc.op("dve", lambda: DVE.max(out=best[:, h, 8:16], in_=cand2[:, h, :]), reads=[SH[h]], writes=[BK[h]], cost=0.35)
                    for h in range(8):
                        sc.op("dve", lambda: DVE.max_index(out=posu[:, h, 8:16], in_max=best[:, h, 8:16], in_values=cand2[:, h, :]), reads=[SH[h], BK[h]], writes=[PK[h]], cost=0.4)
                    sc.op("dve", lambda: DVE.tensor_copy(pb_f[:], posu[:]), reads=PK, writes=["pb_f"])
                    sc.op("dve", lambda: DVE.tensor_scalar(pa_f[:], pb_f[:], 1.0 / 16, -0.46875, op0=ALU.mult, op1=ALU.add), reads=["pb_f"], writes=["pa_f"])
                    sc.op("dve", lambda: DVE.tensor_copy(pa_i[:], pa_f[:]), reads=["pa_f"], writes=["pa_i"])
                    sc.op("dve", lambda: DVE.tensor_copy(pa_f[:], pa_i[:]), reads=["pa_i"], writes=["pa_f"])
                    sc.op("dve", lambda: DVE.scalar_tensor_tensor(out=pb_f[:], in0=pa_f[:], scalar=-16.0, in1=pb_f[:], op0=ALU.mult, op1=ALU.add), reads=["pa_f", "pb_f"], writes=["pb_f"])
                    sc.op("dve", lambda: DVE.tensor_tensor(out=gg[:], in0=best[:], in1=best[:, :, 0:1].to_broadcast([128, 8, 16]), op=ALU.subtract), reads=BK, writes=[gkey_])
                    sc.op("act", lambda: ACT.activation(out=gg[:], in_=gg[:], func=AF.Exp), reads=[gkey_], writes=[gkey_])
                    sc.op("dve", lambda: DVE.tensor_reduce(out=gsum[:], in_=gg[:], axis=AX.X, op=ALU.add), reads=[gkey_], writes=["gsum"])
                    sc.op("dve", lambda: DVE.reciprocal(gsum[:], gsum[:]), reads=["gsum"], writes=["gsum"])
                    sc.op("dve", lambda: DVE.tensor_tensor(out=gg[:], in0=gg[:], in1=gsum[:].unsqueeze(2).to_broadcast([128, 8, 16]), op=ALU.mult), reads=[gkey_, "gsum"], writes=[gkey_])
                    eq = work[:].rearrange("p (h k a) -> p h k a", k=16, a=16)
                    io4 = iota16[:].unsqueeze(1).unsqueeze(1).to_broadcast([128, 8, 16, 16])
                    for (pf, two, dst) in ((pa_f, 0, isel), (pb_f, 1, jsel)):
                        sc.op("dve", lambda: DVE.tensor_tensor(out=eq, in0=io4, in1=pf[:].unsqueeze(3).to_broadcast([128, 8, 16, 16]), op=ALU.is_equal),
                              reads=["pa_f", "pb_f"] + BK + PK, writes=WRKK, cost=2.2)
                        sc.op("dve", lambda: DVE.tensor_tensor(out=eq, in0=eq, in1=tix4[:, :, two, :].unsqueeze(2).to_broadcast([128, 8, 16, 16]), op=ALU.mult),
                              reads=WRKK + ["tixf"], writes=WRKK, cost=2.2)
                        sc.op("dve", lambda: DVE.tensor_reduce(out=dst[:], in_=eq, axis=AX.X, op=ALU.add), reads=WRKK, writes=["sel%d" % two], cost=2.2)
                    sc.op("dve", lambda: DVE.scalar_tensor_tensor(out=isel[:], in0=isel[:], scalar=128.0, in1=jsel[:], op0=ALU.mult, op1=ALU.add),
                          reads=["sel0", "sel1"], writes=["sel0"])
                    sc.op("dve", lambda: DVE.tensor_copy(idxi[:], isel[:].rearrange("p h k -> p (h k)")), reads=["sel0"], writes=[ikey])
                sc.barrier_b()
            sc.b_active = False

        def emit_A(b, c, par, give):
            t0 = c * C
            xT = xTs[par]
            xk = "xT%d" % par
            for j in range(C // 128):
                ts = slice(j * 128, (j + 1) * 128)
                h2tm = h2s[par][j]
                ggf = ggs[par][j][:].rearrange("p h k -> p (h k)")
                idxi = idxs[par][j]
                hkey, gkey_, ikey = "h2%d%d" % (par, j), "gg%d%d" % (par, j), "idx%d%d" % (par, j)
                for step in range(130):
                    hk = step
                    if hk < 128:
                        Gb = G[hk % NBUF]
                        gkey = "G%d" % (hk % NBUF)
                        sc.dma("pool", lambda: POOL.indirect_dma_start(out=Gb[:], out_offset=None, in_=uvb_d,
                                                                       in_offset=bass.IndirectOffsetOnAxis(ap=idxi[:, hk:hk + 1], axis=0)),
                               reads=[ikey, "uvscr"], writes=[gkey], ch=gkey, cost=1.9)
                        sc.op("dve", lambda: DVE.scalar_tensor_tensor(out=junkb[hk % 2], in0=Gb[:, 0:D], scalar=1.0, in1=h2tm[:], op0=ALU.mult, op1=ALU.mult, accum_out=dots[:, hk:hk + 1]),
                              reads=[gkey, hkey], writes=["dots%d" % hk, "junkb%d" % (hk % 2)], cost=1.3)
                        sc.op("act", lambda: ACT.activation(out=acts[:, hk:hk + 1], in_=dots[:, hk:hk + 1], func=AF.Gelu_apprx_tanh), reads=["dots%d" % hk], writes=["dots%d" % hk])
                    k1 = step - 1
                    if 0 <= k1 < 128:
                        sc.op("act", lambda: ACT.activation(out=zz[:, k1:k1 + 1], in_=acts[:, k1:k1 + 1], func=AF.Identity, scale=ggf[:, k1:k1 + 1]),
                              reads=["dots%d" % k1, gkey_], writes=["dots%d" % k1])
                    k2 = step - 2
                    if 0 <= k2 < 128:
                        Gp = G[k2 % NBUF]
                        gpkey = "G%d" % (k2 % NBUF)
                        dg = diag[k2 % 3]
                        dkey = "diag%d" % (k2 % 3)
                        sc.op("act", lambda: ACT.activation(out=dg[:], in_=ident_b[:], func=AF.Identity, scale=zz[:, k2:k2 + 1]),
                              reads=["dots%d" % k2], writes=[dkey])
                        for half in range(2):
                            bnk = 6 + half
                            sc.op("pe", lambda: PE.matmul(PS[bnk][:, :], dg[:], Gp[:, D + half * 512:D + (half + 1) * 512], start=(k2 == 0), stop=(k2 == 127)),
                                  reads=[dkey, gpkey], writes=["ps%d" % bnk])
                    give()
                sc.op("act", lambda: ACT.copy(petm[:, 0:512], PS[6][:, :]), reads=["ps6"], writes=["petm"])
                sc.op("dve", lambda: DVE.tensor_copy(petm[:, 512:1024], PS[7][:, :]), reads=["ps7"], writes=["petm"])
                for dc in range(DC):
                    bnk = 6 + dc // 4
                    sc.op("pe", lambda: PE.transpose(PS[bnk][:, (dc % 4) * 128:(dc % 4 + 1) * 128], petm[:, dc * 128:(dc + 1) * 128], ident_f[:]), reads=["petm"], writes=["ps%d" % bnk])
                for dc in range(DC):
                    bnk = 6 + dc // 4
                    sc.op("dve", lambda: DVE.scalar_tensor_tensor(out=xT[:, dc, ts], in0=PS[bnk][:, (dc % 4) * 128:(dc % 4 + 1) * 128], scalar=modT[:, 40 + dc, b:b + 1], in1=xT[:, dc, ts], op0=ALU.mult, op1=ALU.add),
                          reads=["ps%d" % bnk, xk], writes=[xk])
                give()
            rms_stats(xT[:], DC, 1.0 / D, xk, sq_t=sqA, rstd_t=rstdA, bank=6, sfx="A", sqkeys=["junkb0", "junkb1"])
            for dc in range(DC):
                ot = otmp[dc % 2]
                okey = "otmp%d" % (dc % 2)
                sc.op("dve", lambda: DVE.scalar_tensor_tensor(out=ot[:], in0=xT[:, dc, :], scalar=vcol(V_FG + dc), in1=rstdA[:], op0=ALU.mult, op1=ALU.mult),
                      reads=[xk, "rstdA"], writes=[okey])
                sc.dma("sp", lambda: SP.dma_start(out=outT_d[b, dc, :, t0:t0 + C], in_=ot[:]), reads=[okey], writes=["outd"], ch=okey)
            give()

        chunks = [(b, c) for b in range(nseq) for c in range(nch)]
        if overlap:
            co.no_pace = True
            co.start(lambda: emit_B(chunks[0][0], chunks[0][1], 0))
        uv_v = uv_d.rearrange("(p r) n -> p r n", p=128)
        uvb_v = uvb_d.rearrange("(p r) n -> p r n", p=128)
        for st_i in range(128):
            i2 = st_i % 2
            st = stage[:, i2 * 2048:(i2 + 1) * 2048]
            skey, bkey = "stage%d" % i2, "stb%d" % i2
            sc.dma("sp", lambda: SP.dma_start(out=st, in_=uv_v[:, st_i, :]), writes=[skey], ch=skey)
            if st_i % 2 == 0:
                sc.op("dve", lambda: DVE.tensor_copy(stb[i2][:], st), reads=[skey], writes=[bkey])
            else:
                sc.op("act", lambda: ACT.copy(stb[i2][:], st), reads=[skey], writes=[bkey])
            sc.dma("act", lambda: ACT.dma_start(out=uvb_v[:, st_i, :], in_=stb[i2][:]), reads=[bkey], writes=["uvscr"], ch=bkey + "u")
            if overlap:
                co.give(24)
        if overlap:
            co.drain()
            co.no_pace = False
        else:
            emit_B(chunks[0][0], chunks[0][1], 0)
        sc.barrier()
        pes.close()
        NBUF = 8
        G = [sb(es, "G%d" % i, [128, 2048], BF16) for i in range(NBUF)]
        junkb_t = sb(es, "junkb", [128, 2, D], BF16)
        junkb = [junkb_t[:, 0, :], junkb_t[:, 1, :]]
        diag = [sb(es, "diag%d" % i, [128, 128], BF16) for i in range(3)]
        dots = sb(es, "dots", [128, 128], F32)
        acts = dots
        zz = dots
        petm = sb(es, "petm", [128, D], F32)
        sqA = junkb_t[:, :, :].rearrange("p a (b c) -> p (a b) c", c=C)
        rstdA = sb(es, "rstdA", [128, C], F32)
        otmp = [sb(es, "otmp%d" % i, [128, C], F32) for i in range(2)]

        last_b_count = 2200
        for i, (b, c) in enumerate(chunks):
            par = i % 2
            if overlap and i + 1 < len(chunks):
                nb, ncn = chunks[i + 1]
                co.start(lambda nb=nb, ncn=ncn, par=par: emit_B(nb, ncn, 1 - par))
                q = 200
                emit_A(b, c, par, lambda q=q: co.give(q))
                last_b_count = max(co.count, 1) if co.done else last_b_count
                co.drain()
                last_b_count = max(co.count, 1)
            else:
                emit_A(b, c, par, lambda: None)
                if i + 1 < len(chunks):
                    nb, ncn = chunks[i + 1]
                    emit_B(nb, ncn, 1 - par)
        sc.finish()
    return nc


def _pack_inputs(inp, core, nseq=NSEQ):
    f32 = np.float32
    b0 = core * nseq
    x = inp["x"][b0:b0 + nseq]
    xT = np.ascontiguousarray(np.transpose(x, (0, 2, 1))).reshape(nseq, DC, 128, S)
    c = inp["c"][b0:b0 + nseq]
    cT = np.ascontiguousarray(c.T.reshape(DC, 128, nseq).transpose(1, 0, 2))
    pos = np.ascontiguousarray(inp["positions"][b0:b0 + nseq]).astype(np.int32)
    return {"xT": xT.astype(f32), "cT": cT.astype(f32), "pos": pos}


def _pack_weights(inp):
    f32 = np.float32
    col = lambda v, n: np.ascontiguousarray(np.asarray(v, f32).reshape(n, 128).T)
    vecs = np.zeros((128, NV), f32)
    vecs[:, V_N1G:V_N1G + 8] = col(inp["norm1_g"][0], 8)
    vecs[:, V_N2G:V_N2G + 8] = col(inp["norm2_g"][0], 8)
    vecs[:, V_FG:V_FG + 8] = col(inp["final_g"], 8)
    cw = np.asarray(inp["conv_w"][0], f32)
    for ci in range(4):
        for k in range(4):
            vecs[:, V_CONVW + ci * 4 + k] = cw[k, ci * 128:(ci + 1) * 128]
    vecs[:, V_CONVB:V_CONVB + 4] = col(inp["conv_b"][0], 4)
    vecs[:, V_BA:V_BA + 4] = col(inp["lru_ba"][0], 4)
    vecs[:, V_BX:V_BX + 4] = col(inp["lru_bx"][0], 4)
    vecs[:, V_LAM:V_LAM + 4] = col(inp["lru_lambda"][0], 4)
    vecs[:, V_QNG:V_QNG + 2] = col(inp["q_norm_g"][0], 2)
    vecs[:, V_KVNG:V_KVNG + 1] = col(inp["kv_norm_g"][0], 1)
    vecs[:, V_LOG:V_LOG + 4] = col(inp["lru_out_g"][0], 4)
    vecs[:, V_MOG:V_MOG + 4] = col(inp["mla_out_g"][0], 4)
    vecs[:, V_BADA:V_BADA + 48] = col(inp["b_ada"][0], 48)
    inv_freq = (1.0 / (10000.0 ** (np.arange(0, 32, 2, dtype=np.float32) / np.float32(32)))).astype(f32)
    for p in range(64, 96):
        vecs[p, V_INVF] = inv_freq[(p - 64) % 16]
        vecs[p, V_SGN] = -1.0 if p < 80 else 1.0
    w_in = np.asarray(inp["w_in"][0], f32)
    kr = w_in[:, 1408:1440]
    w_in_ext = np.concatenate([w_in, kr[:, 16:32], kr[:, 0:16]], axis=1)
    wa = np.asarray(inp["lru_wa"][0], f32)
    wx = np.asarray(inp["lru_wx"][0], f32)
    wa_bd = np.zeros((4, 128, 128), f32)
    wx_bd = np.zeros((4, 128, 128), f32)
    for ci in range(4):
        for s in range(2):
            wa_bd[ci, s * 64:(s + 1) * 64, s * 64:(s + 1) * 64] = wa[2 * ci + s]
            wx_bd[ci, s * 64:(s + 1) * 64, s * 64:(s + 1) * 64] = wx[2 * ci + s]
    w_uq = np.asarray(inp["w_uq"][0], f32)
    parts = []
    for h in range(8):
        blk = w_uq[:, h * 96:(h + 1) * 96]
        parts += [blk, blk[:, 0:64], blk[:, 80:96], blk[:, 64:80]]
    w_uq_ext = np.concatenate(parts, axis=1)
    keys = np.asarray(inp["peer_keys"][0], f32)
    keysT = np.ascontiguousarray(keys.reshape(16, 128, 128).transpose(2, 0, 1))
    uv = np.concatenate([np.asarray(inp["peer_u"][0], f32), np.asarray(inp["peer_v"][0], f32)], axis=1)
    return {
        "w_ada": np.ascontiguousarray(np.asarray(inp["w_ada"][0], f32).reshape(DC, 128, 6 * D)),
        "vecs": vecs,
        "w_in": np.ascontiguousarray(w_in_ext.reshape(DC, 128, WIN_COLS)),
        "wa_bd": wa_bd, "wx_bd": wx_bd,
        "w_uq": np.ascontiguousarray(w_uq_ext.reshape(2, 128, 1536)),
        "w_ukv": np.ascontiguousarray(np.asarray(inp["w_ukv"][0], f32)),
        "w_out": np.ascontiguousarray(np.asarray(inp["w_out"][0], f32).reshape(DC, 128, D)),
        "peer_wq": np.ascontiguousarray(np.asarray(inp["peer_wq"][0], f32).reshape(DC, 128, 2048)),
        "keysT": keysT,
        "uv": np.ascontiguousarray(uv),
    }


def kernel(**inputs):
    inp = {k: np.asarray(v) for k, v in inputs.items()}
    nc = build_program(NSEQ, NCH)
    wts = _pack_weights(inp)
    in_maps = []
    for core in range(NCORES):
        m = dict(wts)
        m.update(_pack_inputs(inp, core))
        in_maps.append(m)
    res = run_bass_kernel_spmd(nc, in_maps, core_ids=list(range(NCORES)))
    outs = []
    for core in range(NCORES):
        oT = np.asarray(res.results[core]["outT"]).reshape(NSEQ, D, S)
        outs.append(np.transpose(oT, (0, 2, 1)))
    return np.ascontiguousarray(np.concatenate(outs, axis=0)).astype(np.float32)
```

```python
from contextlib import ExitStack
import threading
import math
import numpy as np
import concourse.bass as bass
import concourse.mybir as mybir
from concourse.bass_utils import run_bass_kernel_spmd

F32 = mybir.dt.float32
BF16 = mybir.dt.bfloat16
I32 = mybir.dt.int32
U32 = mybir.dt.uint32
ALU = mybir.AluOpType
AF = mybir.ActivationFunctionType
AX = mybir.AxisListType

D = 1024
S = 2048
NCORES = 8
NSEQ = 4
C = 256
NCH = S // C
DC = 8
EPS = 1e-6
WIN_COLS = 1472
NEG = -1.0e30
TWO_PI = 2.0 * math.pi

V_N1G, V_N2G, V_FG = 0, 8, 16
V_CONVW, V_CONVB, V_BA, V_BX, V_LAM = 24, 40, 44, 48, 52
V_QNG, V_KVNG, V_LOG, V_MOG = 56, 58, 59, 63
V_BADA = 67
V_INVF, V_SGN = 115, 116
NV = 117


class Sched:
    ENG = ("pe", "act", "dve", "pool", "sp")

    def __init__(self, nc, es):
        self.nc = nc
        self.es = es
        self.eng = {"pe": nc.tensor, "act": nc.scalar, "dve": nc.vector, "pool": nc.gpsimd, "sp": nc.sync}
        self.sem = {e: es.enter_context(nc.semaphore("sem_" + e)) for e in self.ENG}
        self.cnt = {e: 0 for e in self.ENG}
        self.seen = {e: {} for e in self.ENG}
        self.last_w = {}
        self.readers = {}
        self.dsem = {}
        self.dcnt = {}
        self.dead = [False]
        self.co = None
        self.last_b = {e: 0 for e in self.ENG}
        self.b_dcnt = {}
        self.in_b = False
        self.b_active = False
        self.tail = {e: 0.0 for e in self.ENG}
        self.tw = {}
        self.tr = {}
        self.DUR = {"pe": 0.15, "act": 0.45, "dve": 0.3, "pool": 0.25, "sp": 0.1}
        self.LAT = 0.3
        self.MARGIN = {"pe": 24.0, "act": 18.0, "dve": 16.0, "pool": 6.0, "sp": 60.0}

    def _deps(self, reads, writes):
        need = {}
        def add(tok):
            k, v = tok
            if need.get(k, 0) < v:
                need[k] = v
        for k in list(reads) + list(writes):
            t = self.last_w.get(k)
            if t is not None:
                add(t)
        for k in writes:
            for tok in self.readers.get(k, {}).items():
                add(tok)
        return need

    def _emit_waits(self, e, need):
        eng = self.eng[e]
        seen = self.seen[e]
        for k, v in need.items():
            if k == e and e == "pe":
                continue
            if seen.get(k, 0) >= v:
                continue
            if k in self.sem:
                eng.wait_ge(self.sem[k], v)
            else:
                eng.wait_ge(self.dsem[k], v)
            seen[k] = v

    def _record(self, tok, reads, writes):
        for k in writes:
            self.last_w[k] = tok
            self.readers[k] = {}
        for k in reads:
            r = self.readers.setdefault(k, {})
            if r.get(tok[0], 0) < tok[1]:
                r[tok[0]] = tok[1]

    def _ready(self, reads, writes):
        t = 0.0
        for k in list(reads) + list(writes):
            v = self.tw.get(k)
            if v is not None and v > t:
                t = v
        for k in writes:
            v = self.tr.get(k)
            if v is not None and v > t:
                t = v
        return t

    def _model(self, e, reads, writes, dur, extra=0.0):
        start = max(self._ready(reads, writes) + self.LAT, self.tail[e])
        fin = start + dur
        self.tail[e] = fin
        for k in writes:
            self.tw[k] = fin + extra
            self.tr[k] = 0.0
        for k in reads:
            if self.tr.get(k, 0.0) < fin + extra:
                self.tr[k] = fin + extra

    def op(self, e, fn, reads=(), writes=(), cost=None):
        dur = self.DUR[e] if cost is None else cost
        if self.co is not None:
            self.co.tick(self, e, reads, writes, dur)
        self.in_b = threading.current_thread() is getattr(self, "in_b_thread", None) and self.b_active
        if self.in_b and e == "pool" and getattr(self, "pool_pending", None):
            self._emit_waits("pool", self.pool_pending)
            self.pool_pending = None
        self._model(e, reads, writes, dur)
        self._emit_waits(e, self._deps(reads, writes))
        ins = fn()
        ins.then_inc(self.sem[e], 1)
        self.cnt[e] += 1
        if self.in_b:
            self.last_b[e] = self.cnt[e]
        self._record((e, self.cnt[e]), reads, writes)

    def dma(self, e, fn, reads=(), writes=(), ch=None, cost=None):
        dur = 0.1 if cost is None else cost
        if self.co is not None:
            self.co.tick(self, e, reads, writes, dur)
        self.in_b = threading.current_thread() is getattr(self, "in_b_thread", None) and self.b_active
        if self.in_b and e == "pool" and getattr(self, "pool_pending", None):
            self._emit_waits("pool", self.pool_pending)
            self.pool_pending = None
        self._model(e, reads, writes, dur, extra=2.5)
        if ch not in self.dsem:
            self.dsem[ch] = self.es.enter_context(self.nc.semaphore("dsem_%d" % len(self.dsem)))
            self.dcnt[ch] = 0
        need = self._deps(reads, writes)
        if self.dcnt[ch]:
            need[ch] = max(need.get(ch, 0), self.dcnt[ch])
        self._emit_waits(e, need)
        ins = fn()
        ins.then_inc(self.dsem[ch], 16)
        self.dcnt[ch] += 16
        if self.in_b:
            self.b_dcnt[ch] = self.dcnt[ch]
        self._record((ch, self.dcnt[ch]), reads, writes)

    def barrier_b(self):
        need = {e: v for e, v in self.last_b.items() if v}
        need.update({ch: v for ch, v in self.b_dcnt.items() if v})
        for e in self.ENG:
            if e == "pool":
                self.pool_pending = dict(need)
                continue
            self._emit_waits(e, dict(need))

    def barrier(self):
        need = {e: self.cnt[e] for e in self.ENG if self.cnt[e]}
        for ch, v in self.dcnt.items():
            if v:
                need[ch] = v
        for e in self.ENG:
            self._emit_waits(e, dict(need))
        self.last_w = {}
        self.readers = {}

    def finish(self):
        need = {e: self.cnt[e] for e in self.ENG if self.cnt[e]}
        for ch, v in self.dcnt.items():
            if v:
                need[ch] = v
        self._emit_waits("sp", need)


class Co:
    def __init__(self):
        self.thread = None
        self.quota = 0
        self.b_go = threading.Semaphore(0)
        self.m_go = threading.Semaphore(0)
        self.done = True
        self.exc = None
        self.count = 0
        self.free_run = False
        self.SLACK = 0.3

    def start(self, fn):
        self.done = False
        self.count = 0
        self.exc = None

        def run():
            self.b_go.acquire()
            try:
                fn()
            except BaseException as e:
                self.exc = e
            self.done = True
            self.m_go.release()
        self.thread = threading.Thread(target=run)
        self.thread.start()

    def give(self, q):
        if self.done:
            return
        self.quota = q
        self.free_run = q >= (1 << 50)
        self.b_go.release()
        self.m_go.acquire()
        if self.exc is not None:
            raise self.exc

    def tick(self, sched=None, e=None, reads=(), writes=(), dur=0.0):
        if self.thread is not None and threading.current_thread() is self.thread:
            self.count += 1
            while not getattr(self, "free_run", False):
                self.quota -= 1
                blocked = False
                if sched is not None and not getattr(self, "no_pace", False):
                    fin = max(sched._ready(reads, writes) + sched.LAT, sched.tail[e]) + dur
                    blocked = fin > sched.tail["pool"] + sched.MARGIN[e]
                if self.quota >= 0 and not blocked:
                    break
                self.m_go.release()
                self.b_go.acquire()

    def drain(self):
        while not self.done:
            self.give(1 << 60)
        if self.thread is not None:
            self.thread.join()
            self.thread = None
        if self.exc is not None:
            raise self.exc


def build_program(nseq=NSEQ, nch=NCH, stop=99, overlap=True):
    nc = bass.Bass("TRN2", target_bir_lowering=False)
    dr = lambda name, shape, dt, kind="ExternalInput": nc.dram_tensor(name, shape, dt, kind=kind).ap()
    xT_d = dr("xT", [nseq, DC, 128, S], F32)
    cT_d = dr("cT", [128, DC, nseq], F32)
    pos_d = dr("pos", [nseq, S], I32)
    wada_d = dr("w_ada", [DC, 128, 6 * D], F32)
    vecs_d = dr("vecs", [128, NV], F32)
    win_d = dr("w_in", [DC, 128, WIN_COLS], F32)
    wabd_d = dr("wa_bd", [4, 128, 128], F32)
    wxbd_d = dr("wx_bd", [4, 128, 128], F32)
    wuq_d = dr("w_uq", [2, 128, 1536], F32)
    wukv_d = dr("w_ukv", [128, 1024], F32)
    wout_d = dr("w_out", [DC, 128, D], F32)
    wq_d = dr("peer_wq", [DC, 128, 2048], F32)
    keysT_d = dr("keysT", [128, 16, 128], F32)
    uv_d = dr("uv", [16384, 2048], F32)
    outT_d = dr("outT", [nseq, DC, 128, S], F32, kind="ExternalOutput")
    winb_d = dr("winb", [13, 128, DC, 128], BF16, kind="Internal")
    woutb_d = dr("woutb", [8, 128, DC, 128], BF16, kind="Internal")
    wqb_d = dr("wqb", [16, 128, DC, 128], BF16, kind="Internal")
    uvb_d = dr("uvb", [16384, 2048], BF16, kind="Internal")

    with ExitStack() as es:
        sc = Sched(nc, es)
        co = Co()
        sc.co = co
        PE, ACT, DVE, POOL, SP = nc.tensor, nc.scalar, nc.vector, nc.gpsimd, nc.sync
        uid = [0]

        def sb(stack, name, shape, dt):
            uid[0] += 1
            return stack.enter_context(nc.sbuf_tensor("%s_%d" % (name, uid[0]), shape, dt))

        w_uq = sb(es, "w_uq", [128, 2, 1536], BF16)
        w_ukv = sb(es, "w_ukv", [128, 1024], BF16)
        keysT = sb(es, "keysT", [128, 16, 128], F32)
        wa_bd = sb(es, "wa_bd", [128, 4, 128], BF16)
        wx_bd = sb(es, "wx_bd", [128, 4, 128], BF16)
        vecs = sb(es, "vecs", [128, NV], F32)
        modT = sb(es, "modT", [128, 48, nseq], F32)
        A1 = sb(es, "A1", [128, DC, nseq], F32)
        A2 = sb(es, "A2", [128, DC, nseq], F32)
        nsp = sb(es, "nsp", [128, 4], F32)
        consts = sb(es, "consts", [128, 4], F32)
        ones_bf = sb(es, "ones_bf", [128, 128], BF16)
        ident_f = sb(es, "ident_f", [128, 128], F32)
        ident_b = sb(es, "ident_b", [128, 128], BF16)
        tri_b = sb(es, "tri_b", [128, 128], BF16)
        iota16 = sb(es, "iota16", [128, 16], F32)
        KT = sb(es, "KT", [96, 8, S], BF16)
        VC = sb(es, "VC", [128, S // 128, 8, 65], BF16)
        xl = sb(es, "xl", [128, 4, C + 3], F32)
        hst = sb(es, "hst", [128, 4], F32)
        xTs = [sb(es, "xT%d" % i, [128, DC, C], F32) for i in range(2)]
        hT = sb(es, "hT", [128, DC, C], BF16)
        sq = sb(es, "sq", [128, DC, C], BF16)
        rstd = sb(es, "rstd", [128, C], F32)
        tmpf = sb(es, "tmpf", [128, C], F32)
        wr = [sb(es, "wr%d" % i, [128, DC, 128], BF16) for i in range(4)]
        idxs = [[sb(es, "idx%d%d" % (p, j), [128, 128], I32) for j in range(2)] for p in range(2)]
        ggs = [[sb(es, "gg%d%d" % (p, j), [128, 8, 16], F32) for j in range(2)] for p in range(2)]
        h2s = [[sb(es, "h2%d%d" % (p, j), [128, D], BF16) for j in range(2)] for p in range(2)]

        PS = [es.enter_context(nc.psum_tensor("ps%d" % i, [128, 512], F32)) for i in (0, 1, 2)]
        PS3 = es.enter_context(nc.psum_tensor("ps3", [128, 1024], BF16))
        PS += [None] + [es.enter_context(nc.psum_tensor("ps%d" % i, [128, 512], F32)) for i in (4, 5, 6, 7)]

        def vcol(c0, n=1):
            return vecs[:, c0:c0 + n]

        pes = ExitStack()
        if True:
            stage = sb(pes, "stage", [128, 4096], F32)
            cT = sb(pes, "cT", [128, DC, nseq], F32)
            iot_i = sb(pes, "iot_i", [128, 128], I32)
            iot_f = sb(pes, "iot_f", [128, 128], F32)
            sc.dma("sp", lambda: SP.dma_start(out=vecs[:], in_=vecs_d[:, :]), writes=["vecs"], ch="vecs")
            sc.dma("sp", lambda: SP.dma_start(out=cT[:], in_=cT_d[:, :, :]), writes=["cT"], ch="cT")
            sc.dma("sp", lambda: SP.dma_start(out=keysT[:], in_=keysT_d[:, :, :]), writes=["keysT"], ch="keysT")
            sc.op("dve", lambda: DVE.memset(consts[:, 0:1], EPS), writes=["consts"])
            sc.op("dve", lambda: DVE.memset(consts[:, 1:2], 1.0), writes=["consts"])
            sc.op("dve", lambda: DVE.memset(consts[:, 2:3], 0.0), writes=["consts"])
            sc.op("dve", lambda: DVE.memset(ones_bf[:], 1.0), writes=["ones_bf"])
            sc.op("dve", lambda: DVE.memset(VC[:], 1.0), writes=["VC"])
            sc.op("dve", lambda: DVE.memset(KT[:], 0.0), writes=["KT"])
            sc.op("pool", lambda: POOL.iota(iot_i[:], pattern=[[1, 128]], base=0, channel_multiplier=-1), writes=["iot_i"])
            sc.op("dve", lambda: DVE.tensor_copy(iot_f[:], iot_i[:]), reads=["iot_i"], writes=["iot_f"])
            sc.op("dve", lambda: DVE.tensor_scalar(ident_f[:], iot_f[:], 0.0, None, op0=ALU.is_equal), reads=["iot_f"], writes=["ident_f"])
            sc.op("dve", lambda: DVE.tensor_copy(ident_b[:], ident_f[:]), reads=["ident_f"], writes=["ident_b"])
            sc.op("dve", lambda: DVE.tensor_scalar(tri_b[:], iot_f[:], 0.0, None, op0=ALU.is_ge), reads=["iot_f"], writes=["tri_b"])
            sc.op("pool", lambda: POOL.iota(iot_i[:, 0:16], pattern=[[1, 16]], base=0, channel_multiplier=0), reads=["iot_f"], writes=["iot_i"])
            sc.op("dve", lambda: DVE.tensor_copy(iota16[:], iot_i[:, 0:16]), reads=["iot_i"], writes=["iota16"])

            stb = [sb(pes, "stb%d" % i, [128, 2048], BF16) for i in range(2)]

            def cast_op(k, dst_ap, st, key, wkey):
                eng = ("dve", "act", "pool")[k % 3]
                if eng == "dve":
                    sc.op("dve", lambda: DVE.tensor_copy(dst_ap, st), reads=[key], writes=[wkey])
                elif eng == "act":
                    sc.op("act", lambda: ACT.copy(dst_ap, st), reads=[key], writes=[wkey])
                else:
                    sc.op("pool", lambda: POOL.tensor_copy(dst_ap, st), reads=[key], writes=[wkey])

            def load_cast(dst_ap, src_ap, ncols, k, outs=None):
                st = stage[:, 0:ncols] if k % 2 == 0 else stage[:, 2048:2048 + ncols]
                key = "stage%d" % (k % 2)
                sc.dma("sp", lambda: SP.dma_start(out=st, in_=src_ap), writes=[key], ch=key)
                if outs is None:
                    cast_op(k, dst_ap, st, key, "W")
                    return
                bkey = "stb%d" % (k % 2)
                sbt = stb[k % 2]
                cast_op(k, sbt[:, 0:ncols], st, key, bkey)
                for oi, (d_ap, s_ap) in enumerate(outs(sbt)):
                    sc.dma("sp", lambda: SP.dma_start(out=d_ap, in_=s_ap), reads=[bkey], writes=["wscr"], ch="%so%d" % (bkey, oi))
            k = 0
            for dc in range(DC):
                load_cast(None, win_d[dc], WIN_COLS, k, outs=lambda t, dc=dc: [
                    (winb_d[0:11, :, dc, :].rearrange("oc p n -> p oc n"), t[:, 0:1408].rearrange("p (oc n) -> p oc n", n=128)),
                    (winb_d[11, :, dc, 0:96], t[:, 1344:1440]),
                    (winb_d[12, :, dc, 0:96], t[:, 1376:1472])]); k += 1
                load_cast(None, wout_d[dc], D, k, outs=lambda t, dc=dc: [
                    (woutb_d[:, :, dc, :].rearrange("oc p n -> p oc n"), t[:, 0:1024].rearrange("p (oc n) -> p oc n", n=128))]); k += 1
                load_cast(None, wq_d[dc], 2048, k, outs=lambda t, dc=dc: [
                    (wqb_d[:, :, dc, :].rearrange("oc p n -> p oc n"), t[:, 0:2048].rearrange("p (oc n) -> p oc n", n=128))]); k += 1
            for kc in range(2):
                load_cast(w_uq[:, kc, :], wuq_d[kc], 1536, k); k += 1
            load_cast(w_ukv[:, :], wukv_d[:, :], 1024, k); k += 1
            for ci in range(4):
                load_cast(wa_bd[:, ci, :], wabd_d[ci], 128, k); k += 1
                load_cast(wx_bd[:, ci, :], wxbd_d[ci], 128, k); k += 1

            sc.op("act", lambda: ACT.activation(out=nsp[:], in_=vcol(V_LAM, 4), func=AF.Exp, scale=-1.0), reads=["vecs"], writes=["nsp"])
            sc.op("act", lambda: ACT.activation(out=nsp[:], in_=nsp[:], func=AF.Ln, bias=consts[:, 1:2], scale=1.0), reads=["nsp", "consts"], writes=["nsp"])
            sc.op("dve", lambda: DVE.tensor_scalar(nsp[:], nsp[:], -16.0, None, op0=ALU.mult), reads=["nsp"], writes=["nsp"])

            sc.op("act", lambda: ACT.activation(out=cT[:], in_=cT[:], func=AF.Silu), reads=["cT"], writes=["cT"])
            ps_mod = PS[0][:, 0:48 * nseq].rearrange("p (n b) -> p n b", b=nseq)
            for n in range(48):
                key = "stage%d" % (n % 2)
                st = stage[:, (n % 2) * 2048:(n % 2) * 2048 + 1024].rearrange("p (dc n) -> p dc n", n=128)
                sc.dma("sp", lambda: SP.dma_start(out=st, in_=wada_d[:, :, n * 128:(n + 1) * 128].rearrange("dc p n -> p dc n")), writes=[key], ch=key)
                for dc in range(DC):
                    sc.op("pe", lambda: PE.matmul(ps_mod[:, n, :], st[:, dc, :], cT[:, dc, :], start=(dc == 0), stop=(dc == DC - 1)),
                          reads=[key, "cT"], writes=["ps0"])
            sc.op("dve", lambda: DVE.tensor_tensor(out=modT[:], in0=ps_mod, in1=vcol(V_BADA, 48).unsqueeze(2).to_broadcast([128, 48, nseq]), op=ALU.add),
                  reads=["ps0", "vecs"], writes=["modT"])
            for dc in range(DC):
                sc.op("dve", lambda: DVE.tensor_scalar(A1[:, dc, :], modT[:, 8 + dc, :], 1.0, vcol(V_N1G + dc), op0=ALU.add, op1=ALU.mult),
                      reads=["modT", "vecs"], writes=["A1"])
                sc.op("dve", lambda: DVE.tensor_scalar(A2[:, dc, :], modT[:, 32 + dc, :], 1.0, vcol(V_N2G + dc), op0=ALU.add, op1=ALU.mult),
                      reads=["modT", "vecs"], writes=["A2"])
            sc.barrier()

        def rms_stats(src_tile, nchunks, inv_n, srckey, sq_t=None, rstd_t=None, bank=0, sfx="", sqkeys=None):
            sq_t = sq if sq_t is None else sq_t
            rstd_t = rstd if rstd_t is None else rstd_t
            sk, rk, pk = "sq" + sfx, "rstd" + sfx, "ps%d" % bank
            sks = [sk] if sqkeys is None else list(sqkeys)
            srckeys = list(srckey) if isinstance(srckey, (list, tuple)) else [srckey]
            sc.op("act", lambda: ACT.activation(out=sq_t[:, 0:nchunks, :], in_=src_tile, func=AF.Square), reads=srckeys, writes=sks, cost=0.25 + 0.21 * nchunks)
            for i in range(nchunks):
                sc.op("pe", lambda: PE.matmul(PS[bank][:, 0:C], ones_bf[:], sq_t[:, i, :], start=(i == 0), stop=(i == nchunks - 1)),
                      reads=sks, writes=[pk])
            sc.op("act", lambda: ACT.activation(out=rstd_t[:], in_=PS[bank][:, 0:C], func=AF.Sqrt, bias=consts[:, 0:1], scale=inv_n), reads=[pk], writes=[rk])
            sc.op("dve", lambda: DVE.reciprocal(rstd_t[:], rstd_t[:]), reads=[rk], writes=[rk])

        def modulated_norm(xT, xk, Acol, shift_chunk0, b):
            rms_stats(xT[:], DC, 1.0 / D, xk)
            for dc in range(DC):
                sc.op("dve", lambda: DVE.scalar_tensor_tensor(out=tmpf[:], in0=xT[:, dc, :], scalar=Acol[:, dc, b:b + 1], in1=rstd[:], op0=ALU.mult, op1=ALU.mult),
                      reads=[xk, "rstd"], writes=["tmpf"])
                sc.op("act", lambda: ACT.activation(out=hT[:, dc, :], in_=tmpf[:], func=AF.Identity, bias=modT[:, shift_chunk0 + dc, b:b + 1], scale=1.0),
                      reads=["tmpf"], writes=["hT"])

        gen_i = [0]

        def gen_ps():
            bnk = (1, 2)[gen_i[0] % 2]
            gen_i[0] += 1
            return PS[bnk][:, 0:C], "ps%d" % bnk

        ring_i = [0]

        def wload(src_ap, ncols=128):
            i = ring_i[0] % 4
            ring_i[0] += 1
            key = "wr%d" % i
            sc.dma("sp", lambda: SP.dma_start(out=wr[i][:, :, 0:ncols], in_=src_ap), reads=["wscr"], writes=[key], ch=key)
            return wr[i], key

        class WStream:
            def __init__(self, blocks, depth=3):
                self.blocks = list(blocks)
                self.pend = []
                self.depth = depth
                for _ in range(depth):
                    self._issue()

            def _issue(self):
                if self.blocks:
                    src, ncols = self.blocks.pop(0)
                    self.pend.append(wload(src, ncols))

            def next(self):
                w, key = self.pend.pop(0)
                self._issue()
                return w, key

        def emit_B(b, c, par):
            sc.in_b_thread = threading.current_thread()
            sc.b_active = True
            t0 = c * C
            xT = xTs[par]
            xk = "xT%d" % par
            if c == 0:
                sc.op("dve", lambda: DVE.memset(xl[:], 0.0), writes=["xl0", "xl1", "xl2", "xl3"])
                sc.op("dve", lambda: DVE.memset(hst[:], 0.0), writes=["hst0", "hst1", "hst2", "hst3"])
            with ExitStack() as mes:
                gT = sb(mes, "gT", [128, 4, C], F32)
                qlat = sb(mes, "qlat", [128, 2, C], F32)
                kvlat = sb(mes, "kvlat", [128, C], F32)
                qs = sb(mes, "qs", [128, 2, C], BF16)
                kvs = sb(mes, "kvs", [128, C], BF16)
                xc = sb(mes, "xc", [128, C], F32)
                xcb = sb(mes, "xcb", [128, C], BF16)
                ra = sb(mes, "ra", [128, C], F32)
                ib = sb(mes, "ib", [128, C], F32)
                hh = sb(mes, "hh", [128, C], F32)
                xc2 = sb(mes, "xc2", [128, C], F32)
                xcb2 = sb(mes, "xcb2", [128, C], BF16)
                ra2 = sb(mes, "ra2", [128, C], F32)
                ib2 = sb(mes, "ib2", [128, C], F32)
                hh2 = sb(mes, "hh2", [128, C], F32)
                ylru = sb(mes, "ylru", [128, 4, C], F32)
                yT = sb(mes, "yT", [128, 8, C], BF16)
                QT = sb(mes, "QT", [96, 8, C], BF16)
                posi = sb(mes, "posi", [96, C], I32)
                ang = sb(mes, "ang", [96, C], F32)
                kf = sb(mes, "kf", [96, C], F32)
                cos2 = sb(mes, "cos2", [96, C], F32)
                sin2 = sb(mes, "sin2", [96, C], F32)
                t1 = tmpf
                t2 = sb(mes, "t2", [96, C], F32)
                krb = sb(mes, "krb", [96, C], BF16)
                pT = [sb(mes, "pT%d" % i, [128, C], BF16) for i in range(3)]
                ymla = sb(mes, "ymla", [128, 2, 512], F32)
                ymn = sb(mes, "ymn", [128, 2, 512], BF16)
                rinv = sb(mes, "rinv", [128, 2], F32)
                sst = sb(mes, "sst", [128, 2], F32)

                win_blocks = [(winb_d[oc, :, :, :], 128) for oc in range(11)] + [(winb_d[11, :, :, 0:96], 96), (winb_d[12, :, :, 0:96], 96)]
                wst = WStream(win_blocks)
                sc.dma("sp", lambda: SP.dma_start(out=xT[:], in_=xT_d[b, :, :, t0:t0 + C].rearrange("dc p t -> p dc t")), writes=[xk], ch=xk)
                sc.dma("sp", lambda: SP.dma_start(out=posi[64:96, :], in_=pos_d[b:b + 1, t0:t0 + C].partition_broadcast(32)), writes=["posi"], ch="posi")
                modulated_norm(xT, xk, A1, 0, b)

                R = slice(64, 96)
                sc.op("dve", lambda: DVE.tensor_copy(ang[R, :], posi[R, :]), reads=["posi"], writes=["ang"])
                sc.op("dve", lambda: DVE.tensor_scalar(ang[R, :], ang[R, :], vecs[R, V_INVF:V_INVF + 1], None, op0=ALU.mult), reads=["ang", "vecs"], writes=["ang"])
                for shift, dst, use_sgn in ((0.0, sin2, True), (math.pi / 2, cos2, False)):
                    sc.op("dve", lambda: DVE.tensor_scalar(kf[R, :], ang[R, :], shift, 1.0 / TWO_PI, op0=ALU.add, op1=ALU.mult), reads=["ang"], writes=["kf"])
                    sc.op("dve", lambda: DVE.tensor_copy(posi[R, :], kf[R, :]), reads=["kf"], writes=["posi"])
                    sc.op("dve", lambda: DVE.tensor_copy(kf[R, :], posi[R, :]), reads=["posi"], writes=["kf"])
                    sc.op("dve", lambda: DVE.scalar_tensor_tensor(out=kf[R, :], in0=kf[R, :], scalar=-TWO_PI, in1=ang[R, :], op0=ALU.mult, op1=ALU.add),
                          reads=["kf", "ang"], writes=["kf"])
                    sc.op("dve", lambda: DVE.tensor_scalar(kf[R, :], kf[R, :], shift, None, op0=ALU.add), reads=["kf"], writes=["kf"])
                    sc.op("dve", lambda: DVE.tensor_scalar(kf[R, :], kf[R, :], 3.1415925, -3.1415925, op0=ALU.min, op1=ALU.max), reads=["kf"], writes=["kf"])
                    if use_sgn:
                        sc.op("act", lambda: ACT.activation(out=dst[R, :], in_=kf[R, :], func=AF.Sin, scale=vecs[R, V_SGN:V_SGN + 1]), reads=["kf", "vecs"], writes=["rope"])
                    else:
                        sc.op("act", lambda: ACT.activation(out=dst[R, :], in_=kf[R, :], func=AF.Sin), reads=["kf"], writes=["rope"])
                ROPE = ["rope"]

                def inproj(ncols=128):
                    w, wkey = wst.next()
                    ps, key = gen_ps()
                    for dc in range(DC):
                        sc.op("pe", lambda: PE.matmul(ps[0:ncols, :], w[:, dc, 0:ncols], hT[:, dc, :], start=(dc == 0), stop=(dc == DC - 1)),
                              reads=["hT", wkey], writes=[key])
                    return ps, key
                for ci in range(4):
                    ps, key = inproj()
                    sc.op("act", lambda: ACT.copy(xl[:, ci, 3:3 + C], ps), reads=[key], writes=["xl%d" % ci])
                for ci in range(4):
                    ps, key = inproj()
                    sc.op("act", lambda: ACT.activation(out=gT[:, ci, :], in_=ps, func=AF.Gelu_apprx_tanh), reads=[key], writes=["gT"])
                for kc in range(2):
                    ps, key = inproj()
                    sc.op("dve", lambda: DVE.tensor_copy(qlat[:, kc, :], ps), reads=[key], writes=["qlat"])
                ps, key = inproj()
                sc.op("dve", lambda: DVE.tensor_copy(kvlat[:], ps), reads=[key], writes=["kvlat"])
                ps_kr, key_kr = inproj(96)
                ps_krr, key_krr = inproj(96)
                sc.op("dve", lambda: DVE.tensor_tensor(out=t1[R, :], in0=ps_kr[R, :], in1=cos2[R, :], op=ALU.mult), reads=[key_kr] + ROPE, writes=["tmpf"])
                sc.op("dve", lambda: DVE.tensor_tensor(out=t2[R, :], in0=ps_krr[R, :], in1=sin2[R, :], op=ALU.mult), reads=[key_krr] + ROPE, writes=["t2"])
                sc.op("dve", lambda: DVE.tensor_tensor(out=krb[R, :], in0=t1[R, :], in1=t2[R, :], op=ALU.add), reads=["tmpf", "t2"], writes=["krb"])
                for h in range(8):
                    if h % 2 == 0:
                        sc.op("act", lambda: ACT.copy(KT[R, h, t0:t0 + C], krb[R, :]), reads=["krb"], writes=["KT"])
                    else:
                        sc.op("dve", lambda: DVE.tensor_copy(KT[R, h, t0:t0 + C], krb[R, :]), reads=["krb"], writes=["KT"])

                def lru_stages(ci, B_, sfx, banks):
                    xc_, xcb_, ra_, ib_, hh_ = B_
                    kxc, kxcb, kra, kib, khh = ["%s%s" % (n, sfx) for n in ("xc", "xcb", "ra", "ib", "hh")]
                    xk_ = "xl%d" % ci
                    cw = V_CONVW + ci * 4
                    ps_r, key_r = PS[banks[0]][:, 0:C], "ps%d" % banks[0]
                    ps_i, key_i = PS[banks[1]][:, 0:C], "ps%d" % banks[1]
                    st = []
                    st.append(lambda: sc.op("dve", lambda: DVE.tensor_scalar(xc_[:], xl[:, ci, 0:C], vcol(cw), vcol(V_CONVB + ci), op0=ALU.mult, op1=ALU.add),
                                            reads=[xk_, "vecs"], writes=[kxc]))
                    for kk in range(1, 4):
                        st.append(lambda kk=kk: sc.op("dve", lambda: DVE.scalar_tensor_tensor(out=xc_[:], in0=xl[:, ci, kk:kk + C], scalar=vcol(cw + kk), in1=xc_[:], op0=ALU.mult, op1=ALU.add),
                                                      reads=[xk_, kxc], writes=[kxc]))
                    st.append(lambda: sc.op("act", lambda: ACT.copy(xl[:, ci, 0:3], xl[:, ci, C:C + 3]), reads=[xk_, kxc], writes=[xk_]))
                    st.append(lambda: sc.op("act", lambda: ACT.copy(xcb_[:], xc_[:]), reads=[kxc], writes=[kxcb]))
                    st.append(lambda: sc.op("pe", lambda: PE.matmul(ps_r, wa_bd[:, ci, :], xcb_[:], start=True, stop=True), reads=[kxcb], writes=[key_r]))
                    st.append(lambda: sc.op("pe", lambda: PE.matmul(ps_i, wx_bd[:, ci, :], xcb_[:], start=True, stop=True), reads=[kxcb], writes=[key_i]))
                    st.append(lambda: sc.op("act", lambda: ACT.activation(out=ra_[:], in_=ps_r, func=AF.Sigmoid, bias=vcol(V_BA + ci), scale=1.0), reads=[key_r], writes=[kra]))
                    st.append(lambda: sc.op("act", lambda: ACT.activation(out=ib_[:], in_=ps_i, func=AF.Sigmoid, bias=vcol(V_BX + ci), scale=1.0), reads=[key_i], writes=[kib]))
                    st.append(lambda: sc.op("act", lambda: ACT.activation(out=hh_[:], in_=ra_[:], func=AF.Exp, scale=nsp[:, ci:ci + 1]), reads=[kra], writes=[khh]))
                    st.append(lambda: sc.op("dve", lambda: DVE.tensor_scalar(ra_[:], ra_[:], nsp[:, ci:ci + 1], 0.5, op0=ALU.mult, op1=ALU.mult), reads=[kra, khh], writes=[kra]))
                    st.append(lambda: sc.op("act", lambda: ACT.activation(out=ra_[:], in_=ra_[:], func=AF.Exp), reads=[kra], writes=[kra]))
                    st.append(lambda: sc.op("act", lambda: ACT.activation(out=hh_[:], in_=hh_[:], func=AF.Sqrt, bias=consts[:, 1:2], scale=-1.0), reads=[khh], writes=[khh]))
                    st.append(lambda: sc.op("dve", lambda: DVE.tensor_tensor(out=ib_[:], in0=ib_[:], in1=xc_[:], op=ALU.mult), reads=[kib, kxc], writes=[kib]))
                    st.append(lambda: sc.op("dve", lambda: DVE.tensor_tensor(out=ib_[:], in0=ib_[:], in1=hh_[:], op=ALU.mult), reads=[kib, khh], writes=[kib]))
                    st.append(lambda: sc.op("dve", lambda: DVE.tensor_tensor_scan(out=hh_[:], data0=ra_[:], data1=ib_[:], initial=hst[:, ci:ci + 1], op0=ALU.mult, op1=ALU.add),
                                            reads=[kra, kib, "hst%d" % ci], writes=[khh], cost=0.6))
                    st.append(lambda: sc.op("dve", lambda: DVE.tensor_copy(hst[:, ci:ci + 1], hh_[:, C - 1:C]), reads=[khh], writes=["hst%d" % ci]))
                    st.append(lambda: sc.op("dve", lambda: DVE.tensor_tensor(out=ylru[:, ci, :], in0=hh_[:], in1=gT[:, ci, :], op=ALU.mult), reads=[khh, "gT"], writes=["ylru%d" % ci]))
                    return st
                setA = (xc, xcb, ra, ib, hh)
                setB = (xc2, xcb2, ra2, ib2, hh2)
                for pair in ((0, 1), (2, 3)):
                    sa = lru_stages(pair[0], setA, "", (1, 2))
                    sb_ = lru_stages(pair[1], setB, "b", (4, 5))
                    for fa, fb in zip(sa, sb_):
                        fa()
                        fb()
                rms_stats(ylru[:], 4, 1.0 / 512, ["ylru0", "ylru1", "ylru2", "ylru3"])
                for ci in range(4):
                    sc.op("dve", lambda: DVE.scalar_tensor_tensor(out=yT[:, ci, :], in0=ylru[:, ci, :], scalar=vcol(V_LOG + ci), in1=rstd[:], op0=ALU.mult, op1=ALU.mult),
                          reads=["ylru%d" % ci, "rstd"], writes=["yT"])

                rms_stats(qlat[:], 2, 1.0 / 256, "qlat")
                for kc in range(2):
                    sc.op("dve", lambda: DVE.scalar_tensor_tensor(out=qs[:, kc, :], in0=qlat[:, kc, :], scalar=vcol(V_QNG + kc), in1=rstd[:], op0=ALU.mult, op1=ALU.mult),
                          reads=["qlat", "rstd"], writes=["qs"])
                rms_stats(kvlat[:].unsqueeze(1), 1, 1.0 / 128, "kvlat")
                sc.op("dve", lambda: DVE.scalar_tensor_tensor(out=kvs[:], in0=kvlat[:], scalar=vcol(V_KVNG), in1=rstd[:], op0=ALU.mult, op1=ALU.mult),
                      reads=["kvlat", "rstd"], writes=["kvs"])
                for h in range(8):
                    ps_q, key_q = gen_ps()
                    ps_qr, key_qr = gen_ps()
                    for kc in range(2):
                        sc.op("pe", lambda: PE.matmul(ps_q[0:96, :], w_uq[:, kc, h * 192:h * 192 + 96], qs[:, kc, :], start=(kc == 0), stop=(kc == 1)), reads=["qs"], writes=[key_q])
                    for kc in range(2):
                        sc.op("pe", lambda: PE.matmul(ps_qr[0:96, :], w_uq[:, kc, h * 192 + 96:h * 192 + 192], qs[:, kc, :], start=(kc == 0), stop=(kc == 1)), reads=["qs"], writes=[key_qr])
                    sc.op("act", lambda: ACT.copy(QT[0:64, h, :], ps_q[0:64, :]), reads=[key_q], writes=["QT"])
                    sc.op("dve", lambda: DVE.tensor_tensor(out=t1[R, :], in0=ps_q[R, :], in1=cos2[R, :], op=ALU.mult), reads=[key_q] + ROPE, writes=["tmpf"])
                    sc.op("dve", lambda: DVE.tensor_tensor(out=t2[R, :], in0=ps_qr[R, :], in1=sin2[R, :], op=ALU.mult), reads=[key_qr] + ROPE, writes=["t2"])
                    sc.op("dve", lambda: DVE.tensor_tensor(out=QT[R, h, :], in0=t1[R, :], in1=t2[R, :], op=ALU.add), reads=["tmpf", "t2"], writes=["QT"])
                    ps_k, key_k = gen_ps()
                    sc.op("pe", lambda: PE.matmul(ps_k[0:64, :], w_ukv[:, h * 128:h * 128 + 64], kvs[:], start=True, stop=True), reads=["kvs"], writes=[key_k])
                    sc.op("act", lambda: ACT.copy(KT[0:64, h, t0:t0 + C], ps_k[0:64, :]), reads=[key_k], writes=["KT"])
                wv = w_ukv[:, :].rearrange("p (h x) -> p h x", x=128)[:, :, 64:128]
                for j in range(C // 128):
                    tile_i = (t0 // 128) + j
                    sc.op("pe", lambda: PE.matmul(PS[4][:, :].rearrange("p (h x) -> p h x", x=64), kvs[:, j * 128:(j + 1) * 128], wv, start=True, stop=True),
                          reads=["kvs"], writes=["ps4"])
                    sc.op("dve", lambda: DVE.tensor_copy(VC[:, tile_i, :, 0:64], PS[4][:, :].rearrange("p (h x) -> p h x", x=64)), reads=["ps4"], writes=["VC"])

                wso = WStream([(woutb_d[oc, :, :, :], 128) for oc in range(8)])

                scale = 96.0 ** -0.5
                nkt = (t0 + C) // 128
                kdiag0 = t0 // 128
                it = 0
                abk = (1, 2)
                akeys = ["ps1", "ps2"]
                for h in range(8):
                    for kt in range(nkt):
                        sbank = (4, 5, 0)[it % 3]
                        skey = "ps%d" % sbank
                        pt = pT[it % 3]
                        pkey = "pT%d" % (it % 3)
                        it += 1
                        sc.op("pe", lambda: PE.matmul(PS[sbank][:, 0:C], KT[0:96, h, kt * 128:(kt + 1) * 128], QT[0:96, h, :], start=True, stop=True),
                              reads=["KT", "QT"], writes=[skey])
                        sc.op("act", lambda: ACT.activation(out=pt[:], in_=PS[sbank][:, 0:C], func=AF.Exp, scale=scale), reads=[skey], writes=[pkey])
                        jk = kt - kdiag0
                        if jk >= 0:
                            sc.op("dve", lambda: DVE.tensor_tensor(out=pt[:, jk * 128:(jk + 1) * 128], in0=pt[:, jk * 128:(jk + 1) * 128], in1=tri_b[:], op=ALU.mult),
                                  reads=[pkey], writes=[pkey])
                        for jq in range(C // 128):
                            if jk > jq:
                                continue
                            last = kdiag0 + jq
                            sc.op("pe", lambda: PE.matmul(PS[abk[jq]][:, 0:65], pt[:, jq * 128:(jq + 1) * 128], VC[:, kt, h, :], start=(kt == 0), stop=(kt == last)),
                                  reads=[pkey, "VC"], writes=[akeys[jq]])
                    for jq in range(C // 128):
                        sc.op("dve", lambda: DVE.reciprocal(rinv[:, jq:jq + 1], PS[abk[jq]][:, 64:65]), reads=[akeys[jq]], writes=["rinv"])
                        sc.op("dve", lambda: DVE.tensor_scalar(ymla[:, jq, h * 64:(h + 1) * 64], PS[abk[jq]][:, 0:64], rinv[:, jq:jq + 1], None, op0=ALU.mult),
                              reads=[akeys[jq], "rinv"], writes=["ymla"])
                for jq in range(C // 128):
                    sc.op("dve", lambda: DVE.scalar_tensor_tensor(out=ymn[:, jq, :], in0=ymla[:, jq, :], scalar=1.0, in1=ymla[:, jq, :], op0=ALU.mult, op1=ALU.mult, accum_out=sst[:, jq:jq + 1]),
                          reads=["ymla"], writes=["ymn", "sst"])
                sc.op("act", lambda: ACT.activation(out=sst[:], in_=sst[:], func=AF.Sqrt, bias=consts[:, 0:1], scale=1.0 / 512), reads=["sst"], writes=["sst"])
                sc.op("dve", lambda: DVE.reciprocal(sst[:], sst[:]), reads=["sst"], writes=["sst"])
                for jq in range(C // 128):
                    sc.op("dve", lambda: DVE.tensor_scalar(ymn[:, jq, :], ymla[:, jq, :], sst[:, jq:jq + 1], None, op0=ALU.mult), reads=["ymla", "sst"], writes=["ymn"])
                    for fc in range(4):
                        sc.op("pe", lambda: PE.transpose(PS3[:, fc * 128:(fc + 1) * 128], ymn[:, jq, fc * 128:(fc + 1) * 128], ident_b[:]), reads=["ymn"], writes=["ps3"])
                    for fc in range(4):
                        sc.op("dve", lambda: DVE.tensor_scalar(yT[:, 4 + fc, jq * 128:(jq + 1) * 128], PS3[:, fc * 128:(fc + 1) * 128], vcol(V_MOG + fc), None, op0=ALU.mult),
                              reads=["ps3"], writes=["yT"])
                for oc in range(DC):
                    w, wkey = wso.next()
                    ps, key = gen_ps()
                    for cc in range(8):
                        sc.op("pe", lambda: PE.matmul(ps, w[:, cc, :], yT[:, cc, :], start=(cc == 0), stop=(cc == 7)), reads=["yT", wkey], writes=[key])
                    sc.op("dve", lambda: DVE.scalar_tensor_tensor(out=xT[:, oc, :], in0=ps, scalar=modT[:, 16 + oc, b:b + 1], in1=xT[:, oc, :], op0=ALU.mult, op1=ALU.add),
                          reads=[key, xk], writes=[xk])
                sc.barrier_b()

            with ExitStack() as pes2:
                qT = sb(pes2, "qT", [128, 16, C], F32)
                scs = sb(pes2, "scs", [128, 2048], F32)
                work = sb(pes2, "work", [128, 2048], F32)
                top = sb(pes2, "top", [128, 16, 16], F32)
                tix = sb(pes2, "tix", [128, 16, 16], U32)
                tixf = sb(pes2, "tixf", [128, 16, 16], F32)
                best = sb(pes2, "best", [128, 8, 16], F32)
                posu = sb(pes2, "posu", [128, 8, 16], U32)
                pa_i = sb(pes2, "pa_i", [128, 8, 16], I32)
                pa_f = sb(pes2, "pa_f", [128, 8, 16], F32)
                pb_f = sb(pes2, "pb_f", [128, 8, 16], F32)
                gsum = sb(pes2, "gsum", [128, 8], F32)
                isel = sb(pes2, "isel", [128, 8, 16], F32)
                jsel = sb(pes2, "jsel", [128, 8, 16], F32)

                modulated_norm(xT, xk, A2, 24, b)
                wsq = WStream([(wqb_d[hp, :, :, :], 128) for hp in range(16)])
                for hp in range(16):
                    w, wkey = wsq.next()
                    qb = (0, 1, 2, 4, 5)[hp % 5]
                    qk = "ps%d" % qb
                    pq = PS[qb][:, 0:C]
                    for dc in range(DC):
                        sc.op("pe", lambda: PE.matmul(pq, w[:, dc, :], hT[:, dc, :], start=(dc == 0), stop=(dc == DC - 1)), reads=["hT", wkey], writes=[qk])
                    if hp % 2 == 0:
                        sc.op("act", lambda: ACT.copy(qT[:, hp, :], pq), reads=[qk], writes=["qT%d" % hp])
                    else:
                        sc.op("dve", lambda: DVE.tensor_copy(qT[:, hp, :], pq), reads=[qk], writes=["qT%d" % hp])
                for j in range(C // 128):
                    ts = slice(j * 128, (j + 1) * 128)
                    h2tm = h2s[par][j]
                    gg = ggs[par][j]
                    idxi = idxs[par][j]
                    hkey, gkey_, ikey = "h2%d%d" % (par, j), "gg%d%d" % (par, j), "idx%d%d" % (par, j)
                    for dc in range(DC):
                        sc.op("pe", lambda: PE.transpose(PS3[:, dc * 128:(dc + 1) * 128], hT[:, dc, ts], ident_b[:]), reads=["hT"], writes=["ps3"])
                    sc.op("act", lambda: ACT.copy(h2tm[:], PS3[:, :]), reads=["ps3"], writes=[hkey], cost=1.0)
                    sbanks = (1, 2, 4, 5)
                    for hp in range(16):
                        bnk = sbanks[hp // 4]
                        sc.op("pe", lambda: PE.matmul(PS[bnk][:, (hp % 4) * 128:(hp % 4 + 1) * 128], qT[:, hp, ts], keysT[:, hp, :], start=True, stop=True),
                              reads=["qT%d" % hp], writes=["ps%d" % bnk])
                    for q in range(4):
                        bnk = sbanks[q]
                        if q % 2 == 0:
                            sc.op("act", lambda: ACT.copy(scs[:, q * 512:(q + 1) * 512], PS[bnk][:, :]), reads=["ps%d" % bnk], writes=["scs%d" % q])
                        else:
                            sc.op("dve", lambda: DVE.tensor_copy(scs[:, q * 512:(q + 1) * 512], PS[bnk][:, :]), reads=["ps%d" % bnk], writes=["scs%d" % q])
                    TOPK = ["top%d" % hp for hp in range(16)]
                    TIXK = ["tix%d" % hp for hp in range(16)]
                    WRKK = ["work%d" % hp for hp in range(16)]
                    sks = ["scs%d" % (hp // 4) for hp in range(16)]
                    svs = [scs[:, hp * 128:(hp + 1) * 128] for hp in range(16)]
                    wvs = [work[:, hp * 128:(hp + 1) * 128] for hp in range(16)]
                    for hp in range(16):
                        sc.op("dve", lambda: DVE.max(out=top[:, hp, 0:8], in_=svs[hp]), reads=[sks[hp]], writes=[TOPK[hp]], cost=0.2)
                    for hp in range(16):
                        sc.op("dve", lambda: DVE.max_index(out=tix[:, hp, 0:8], in_max=top[:, hp, 0:8], in_values=svs[hp]), reads=[sks[hp], TOPK[hp]], writes=[TIXK[hp]], cost=0.25)
                    for hp in range(16):
                        sc.op("dve", lambda: DVE.match_replace(out=wvs[hp], in_to_replace=top[:, hp, 0:8], in_values=svs[hp], imm_value=NEG), reads=[sks[hp], TOPK[hp]], writes=[WRKK[hp]], cost=0.25)
                    for hp in range(16):
                        sc.op("dve", lambda: DVE.max(out=top[:, hp, 8:16], in_=wvs[hp]), reads=[WRKK[hp]], writes=[TOPK[hp]], cost=0.2)
                    for hp in range(16):
                        sc.op("dve", lambda: DVE.max_index(out=tix[:, hp, 8:16], in_max=top[:, hp, 8:16], in_values=wvs[hp]), reads=[WRKK[hp], TOPK[hp]], writes=[TIXK[hp]], cost=0.25)
                    sc.op("dve", lambda: DVE.tensor_copy(tixf[:], tix[:]), reads=TIXK, writes=["tixf"])
                    top4 = top[:].rearrange("p (h two) k -> p h two k", two=2)
                    tix4 = tixf[:].rearrange("p (h two) k -> p h two k", two=2)
                    cand = work[:].rearrange("p (h a b) -> p h a b", a=16, b=16)
                    cand3 = work[:].rearrange("p (h ab) -> p h ab", ab=256)
                    cand2 = scs[:].rearrange("p (h ab) -> p h ab", ab=256)
                    ALLS = ["scs0", "scs1", "scs2", "scs3"]
                    WH = [[WRKK[2 * h], WRKK[2 * h + 1]] for h in range(8)]
                    SH = ["scs%d" % (h // 2) for h in range(8)]
                    BK = ["best%d" % h for h in range(8)]
                    PK = ["posu%d" % h for h in range(8)]
                    sc.op("dve", lambda: DVE.tensor_tensor(out=cand, in0=top4[:, :, 0, :].unsqueeze(3).to_broadcast([128, 8, 16, 16]),
                                                           in1=top4[:, :, 1, :].unsqueeze(2).to_broadcast([128, 8, 16, 16]), op=ALU.add),
                          reads=TOPK, writes=WRKK, cost=2.2)
                    for h in range(8):
                        sc.op("dve", lambda: DVE.max(out=best[:, h, 0:8], in_=cand3[:, h, :]), reads=WH[h], writes=[BK[h]], cost=0.35)
                    for h in range(8):
                        sc.op("dve", lambda: DVE.max_index(out=posu[:, h, 0:8], in_max=best[:, h, 0:8], in_values=cand3[:, h, :]), reads=WH[h] + [BK[h]], writes=[PK[h]], cost=0.4)
                    for h in range(8):
                        sc.op("dve", lambda: DVE.match_replace(out=cand2[:, h, :], in_to_replace=best[:, h, 0:8], in_values=cand3[:, h, :], imm_value=NEG), reads=WH[h] + [BK[h]], writes=[SH[h]], cost=0.4)
                    for h in range(8):
                        sc.op("dve", lambda: DVE.max(out=best[:, h, 8:16], in_=cand2[:, h, :]), reads=[SH[h]], writes=[BK[h]], cost=0.35)
                    for h in range(8):
                        sc.op("dve", lambda: DVE.max_index(out=posu[:, h, 8:16], in_max=best[:, h, 8:16], in_values=cand2[:, h, :]), reads=[SH[h], BK[h]], writes=[PK[h]], cost=0.4)
                    sc.op("dve", lambda: DVE.tensor_copy(pb_f[:], posu[:]), reads=PK, writes=["pb_f"])
                    sc.op("dve", lambda: DVE.tensor_scalar(pa_f[:], pb_f[:], 1.0 / 16, -0.46875, op0=ALU.mult, op1=ALU.add), reads=["pb_f"], writes=["pa_f"])
                    sc.op("dve", lambda: DVE.tensor_copy(pa_i[:], pa_f[:]), reads=["pa_f"], writes=["pa_i"])
                    sc.op("dve", lambda: DVE.tensor_copy(pa_f[:], pa_i[:]), reads=["pa_i"], writes=["pa_f"])
                    sc.op("dve", lambda: DVE.scalar_tensor_tensor(out=pb_f[:], in0=pa_f[:], scalar=-16.0, in1=pb_f[:], op0=ALU.mult, op1=ALU.add), reads=["pa_f", "pb_f"], writes=["pb_f"])
                    sc.op("dve", lambda: DVE.tensor_tensor(out=gg[:], in0=best[:], in1=best[:, :, 0:1].to_broadcast([128, 8, 16]), op=ALU.subtract), reads=BK, writes=[gkey_])
                    sc.op("act", lambda: ACT.activation(out=gg[:], in_=gg[:], func=AF.Exp), reads=[gkey_], writes=[gkey_])
                    sc.op("dve", lambda: DVE.tensor_reduce(out=gsum[:], in_=gg[:], axis=AX.X, op=ALU.add), reads=[gkey_], writes=["gsum"])
                    sc.op("dve", lambda: DVE.reciprocal(gsum[:], gsum[:]), reads=["gsum"], writes=["gsum"])
                    sc.op("dve", lambda: DVE.tensor_tensor(out=gg[:], in0=gg[:], in1=gsum[:].unsqueeze(2).to_broadcast([128, 8, 16]), op=ALU.mult), reads=[gkey_, "gsum"], writes=[gkey_])
                    eq = work[:].rearrange("p (h k a) -> p h k a", k=16, a=16)
                    io4 = iota16[:].unsqueeze(1).unsqueeze(1).to_broadcast([128, 8, 16, 16])
                    for (pf, two, dst) in ((pa_f, 0, isel), (pb_f, 1, jsel)):
                        sc.op("dve", lambda: DVE.tensor_tensor(out=eq, in0=io4, in1=pf[:].unsqueeze(3).to_broadcast([128, 8, 16, 16]), op=ALU.is_equal),
                              reads=["pa_f", "pb_f"] + BK + PK, writes=WRKK, cost=2.2)
                        sc.op("dve", lambda: DVE.tensor_tensor(out=eq, in0=eq, in1=tix4[:, :, two, :].unsqueeze(2).to_broadcast([128, 8, 16, 16]), op=ALU.mult),
                              reads=WRKK + ["tixf"], writes=WRKK, cost=2.2)
                        sc.op("dve", lambda: DVE.tensor_reduce(out=dst[:], in_=eq, axis=AX.X, op=ALU.add), reads=WRKK, writes=["sel%d" % two], cost=2.2)
                    sc.op("dve", lambda: DVE.scalar_tensor_tensor(out=isel[:], in0=isel[:], scalar=128.0, in1=jsel[:], op0=ALU.mult, op1=ALU.add),
                          reads=["sel0", "sel1"], writes=["sel0"])
                    sc.op("dve", lambda: DVE.tensor_copy(idxi[:], isel[:].rearrange("p h k -> p (h k)")), reads=["sel0"], writes=[ikey])
                sc.barrier_b()
            sc.b_active = False

        def emit_A(b, c, par, give):
            t0 = c * C
            xT = xTs[par]
            xk = "xT%d" % par
            for j in range(C // 128):
                ts = slice(j * 128, (j + 1) * 128)
                h2tm = h2s[par][j]
                ggf = ggs[par][j][:].rearrange("p h k -> p (h k)")
                idxi = idxs[par][j]
                hkey, gkey_, ikey = "h2%d%d" % (par, j), "gg%d%d" % (par, j), "idx%d%d" % (par, j)
                for step in range(130):
                    hk = step
                    if hk < 128:
                        Gb = G[hk % NBUF]
                        gkey = "G%d" % (hk % NBUF)
                        sc.dma("pool", lambda: POOL.indirect_dma_start(out=Gb[:], out_offset=None, in_=uvb_d,
                                                                       in_offset=bass.IndirectOffsetOnAxis(ap=idxi[:, hk:hk + 1], axis=0)),
                               reads=[ikey, "uvscr"], writes=[gkey], ch=gkey, cost=1.9)
                        sc.op("dve", lambda: DVE.scalar_tensor_tensor(out=junkb[hk % 2], in0=Gb[:, 0:D], scalar=1.0, in1=h2tm[:], op0=ALU.mult, op1=ALU.mult, accum_out=dots[:, hk:hk + 1]),
                              reads=[gkey, hkey], writes=["dots%d" % hk, "junkb%d" % (hk % 2)], cost=1.3)
                        sc.op("act", lambda: ACT.activation(out=acts[:, hk:hk + 1], in_=dots[:, hk:hk + 1], func=AF.Gelu_apprx_tanh), reads=["dots%d" % hk], writes=["dots%d" % hk])
                    k1 = step - 1
                    if 0 <= k1 < 128:
                        sc.op("act", lambda: ACT.activation(out=zz[:, k1:k1 + 1], in_=acts[:, k1:k1 + 1], func=AF.Identity, scale=ggf[:, k1:k1 + 1]),
                              reads=["dots%d" % k1, gkey_], writes=["dots%d" % k1])
                    k2 = step - 2
                    if 0 <= k2 < 128:
                        Gp = G[k2 % NBUF]
                        gpkey = "G%d" % (k2 % NBUF)
                        dg = diag[k2 % 3]
                        dkey = "diag%d" % (k2 % 3)
                        sc.op("act", lambda: ACT.activation(out=dg[:], in_=ident_b[:], func=AF.Identity, scale=zz[:, k2:k2 + 1]),
                              reads=["dots%d" % k2], writes=[dkey])
                        for half in range(2):
                            bnk = 6 + half
                            sc.op("pe", lambda: PE.matmul(PS[bnk][:, :], dg[:], Gp[:, D + half * 512:D + (half + 1) * 512], start=(k2 == 0), stop=(k2 == 127)),
                                  reads=[dkey, gpkey], writes=["ps%d" % bnk])
                    give()
                sc.op("act", lambda: ACT.copy(petm[:, 0:512], PS[6][:, :]), reads=["ps6"], writes=["petm"])
                sc.op("dve", lambda: DVE.tensor_copy(petm[:, 512:1024], PS[7][:, :]), reads=["ps7"], writes=["petm"])
                for dc in range(DC):
                    bnk = 6 + dc // 4
                    sc.op("pe", lambda: PE.transpose(PS[bnk][:, (dc % 4) * 128:(dc % 4 + 1) * 128], petm[:, dc * 128:(dc + 1) * 128], ident_f[:]), reads=["petm"], writes=["ps%d" % bnk])
                for dc in range(DC):
                    bnk = 6 + dc // 4
                    sc.op("dve", lambda: DVE.scalar_tensor_tensor(out=xT[:, dc, ts], in0=PS[bnk][:, (dc % 4) * 128:(dc % 4 + 1) * 128], scalar=modT[:, 40 + dc, b:b + 1], in1=xT[:, dc, ts], op0=ALU.mult, op1=ALU.add),
                          reads=["ps%d" % bnk, xk], writes=[xk])
                give()
            rms_stats(xT[:], DC, 1.0 / D, xk, sq_t=sqA, rstd_t=rstdA, bank=6, sfx="A", sqkeys=["junkb0", "junkb1"])
            for dc in range(DC):
                ot = otmp[dc % 2]
                okey = "otmp%d" % (dc % 2)
                sc.op("dve", lambda: DVE.scalar_tensor_tensor(out=ot[:], in0=xT[:, dc, :], scalar=vcol(V_FG + dc), in1=rstdA[:], op0=ALU.mult, op1=ALU.mult),
                      reads=[xk, "rstdA"], writes=[okey])
                sc.dma("sp", lambda: SP.dma_start(out=outT_d[b, dc, :, t0:t0 + C], in_=ot[:]), reads=[okey], writes=["outd"], ch=okey)
            give()

        chunks = [(b, c) for b in range(nseq) for c in range(nch)]
        if overlap:
            co.no_pace = True
            co.start(lambda: emit_B(chunks[0][0], chunks[0][1], 0))
        uv_v = uv_d.rearrange("(p r) n -> p r n", p=128)
        uvb_v = uvb_d.rearrange("(p r) n -> p r n", p=128)
        for st_i in range(128):
            i2 = st_i % 2
            st = stage[:, i2 * 2048:(i2 + 1) * 2048]
            skey, bkey = "stage%d" % i2, "stb%d" % i2
            sc.dma("sp", lambda: SP.dma_start(out=st, in_=uv_v[:, st_i, :]), writes=[skey], ch=skey)
            if st_i % 2 == 0:
                sc.op("dve", lambda: DVE.tensor_copy(stb[i2][:], st), reads=[skey], writes=[bkey])
            else:
                sc.op("act", lambda: ACT.copy(stb[i2][:], st), reads=[skey], writes=[bkey])
            sc.dma("act", lambda: ACT.dma_start(out=uvb_v[:, st_i, :], in_=stb[i2][:]), reads=[bkey], writes=["uvscr"], ch=bkey + "u")
            if overlap:
                co.give(24)
        if overlap:
            co.drain()
            co.no_pace = False
        else:
            emit_B(chunks[0][0], chunks[0][1], 0)
        sc.barrier()
        pes.close()
        NBUF = 8
        G = [sb(es, "G%d" % i, [128, 2048], BF16) for i in range(NBUF)]
        junkb_t = sb(es, "junkb", [128, 2, D], BF16)
        junkb = [junkb_t[:, 0, :], junkb_t[:, 1, :]]
        diag = [sb(es, "diag%d" % i, [128, 128], BF16) for i in range(3)]
        dots = sb(es, "dots", [128, 128], F32)
        acts = dots
        zz = dots
        petm = sb(es, "petm", [128, D], F32)
        sqA = junkb_t[:, :, :].rearrange("p a (b c) -> p (a b) c", c=C)
        rstdA = sb(es, "rstdA", [128, C], F32)
        otmp = [sb(es, "otmp%d" % i, [128, C], F32) for i in range(2)]

        last_b_count = 2200
        for i, (b, c) in enumerate(chunks):
            par = i % 2
            if overlap and i + 1 < len(chunks):
                nb, ncn = chunks[i + 1]
                co.start(lambda nb=nb, ncn=ncn, par=par: emit_B(nb, ncn, 1 - par))
                q = 200
                emit_A(b, c, par, lambda q=q: co.give(q))
                last_b_count = max(co.count, 1) if co.done else last_b_count
                co.drain()
                last_b_count = max(co.count, 1)
            else:
                emit_A(b, c, par, lambda: None)
                if i + 1 < len(chunks):
                    nb, ncn = chunks[i + 1]
                    emit_B(nb, ncn, 1 - par)
        sc.finish()
    return nc


def _pack_inputs(inp, core, nseq=NSEQ):
    f32 = np.float32
    b0 = core * nseq
    x = inp["x"][b0:b0 + nseq]
    xT = np.ascontiguousarray(np.transpose(x, (0, 2, 1))).reshape(nseq, DC, 128, S)
    c = inp["c"][b0:b0 + nseq]
    cT = np.ascontiguousarray(c.T.reshape(DC, 128, nseq).transpose(1, 0, 2))
    pos = np.ascontiguousarray(inp["positions"][b0:b0 + nseq]).astype(np.int32)
    return {"xT": xT.astype(f32), "cT": cT.astype(f32), "pos": pos}


def _pack_weights(inp):
    f32 = np.float32
    col = lambda v, n: np.ascontiguousarray(np.asarray(v, f32).reshape(n, 128).T)
    vecs = np.zeros((128, NV), f32)
    vecs[:, V_N1G:V_N1G + 8] = col(inp["norm1_g"][0], 8)
    vecs[:, V_N2G:V_N2G + 8] = col(inp["norm2_g"][0], 8)
    vecs[:, V_FG:V_FG + 8] = col(inp["final_g"], 8)
    cw = np.asarray(inp["conv_w"][0], f32)
    for ci in range(4):
        for k in range(4):
            vecs[:, V_CONVW + ci * 4 + k] = cw[k, ci * 128:(ci + 1) * 128]
    vecs[:, V_CONVB:V_CONVB + 4] = col(inp["conv_b"][0], 4)
    vecs[:, V_BA:V_BA + 4] = col(inp["lru_ba"][0], 4)
    vecs[:, V_BX:V_BX + 4] = col(inp["lru_bx"][0], 4)
    vecs[:, V_LAM:V_LAM + 4] = col(inp["lru_lambda"][0], 4)
    vecs[:, V_QNG:V_QNG + 2] = col(inp["q_norm_g"][0], 2)
    vecs[:, V_KVNG:V_KVNG + 1] = col(inp["kv_norm_g"][0], 1)
    vecs[:, V_LOG:V_LOG + 4] = col(inp["lru_out_g"][0], 4)
    vecs[:, V_MOG:V_MOG + 4] = col(inp["mla_out_g"][0], 4)
    vecs[:, V_BADA:V_BADA + 48] = col(inp["b_ada"][0], 48)
    inv_freq = (1.0 / (10000.0 ** (np.arange(0, 32, 2, dtype=np.float32) / np.float32(32)))).astype(f32)
    for p in range(64, 96):
        vecs[p, V_INVF] = inv_freq[(p - 64) % 16]
        vecs[p, V_SGN] = -1.0 if p < 80 else 1.0
    w_in = np.asarray(inp["w_in"][0], f32)
    kr = w_in[:, 1408:1440]
    w_in_ext = np.concatenate([w_in, kr[:, 16:32], kr[:, 0:16]], axis=1)
    wa = np.asarray(inp["lru_wa"][0], f32)
    wx = np.asarray(inp["lru_wx"][0], f32)
    wa_bd = np.zeros((4, 128, 128), f32)
    wx_bd = np.zeros((4, 128, 128), f32)
    for ci in range(4):
        for s in range(2):
            wa_bd[ci, s * 64:(s + 1) * 64, s * 64:(s + 1) * 64] = wa[2 * ci + s]
            wx_bd[ci, s * 64:(s + 1) * 64, s * 64:(s + 1) * 64] = wx[2 * ci + s]
    w_uq = np.asarray(inp["w_uq"][0], f32)
    parts = []
    for h in range(8):
        blk = w_uq[:, h * 96:(h + 1) * 96]
        parts += [blk, blk[:, 0:64], blk[:, 80:96], blk[:, 64:80]]
    w_uq_ext = np.concatenate(parts, axis=1)
    keys = np.asarray(inp["peer_keys"][0], f32)
    keysT = np.ascontiguousarray(keys.reshape(16, 128, 128).transpose(2, 0, 1))
    uv = np.concatenate([np.asarray(inp["peer_u"][0], f32), np.asarray(inp["peer_v"][0], f32)], axis=1)
    return {
        "w_ada": np.ascontiguousarray(np.asarray(inp["w_ada"][0], f32).reshape(DC, 128, 6 * D)),
        "vecs": vecs,
        "w_in": np.ascontiguousarray(w_in_ext.reshape(DC, 128, WIN_COLS)),
        "wa_bd": wa_bd, "wx_bd": wx_bd,
        "w_uq": np.ascontiguousarray(w_uq_ext.reshape(2, 128, 1536)),
        "w_ukv": np.ascontiguousarray(np.asarray(inp["w_ukv"][0], f32)),
        "w_out": np.ascontiguousarray(np.asarray(inp["w_out"][0], f32).reshape(DC, 128, D)),
        "peer_wq": np.ascontiguousarray(np.asarray(inp["peer_wq"][0], f32).reshape(DC, 128, 2048)),
        "keysT": keysT,
        "uv": np.ascontiguousarray(uv),
    }


def kernel(**inputs):
    inp = {k: np.asarray(v) for k, v in inputs.items()}
    nc = build_program(NSEQ, NCH)
    wts = _pack_weights(inp)
    in_maps = []
    for core in range(NCORES):
        m = dict(wts)
        m.update(_pack_inputs(inp, core))
        in_maps.append(m)
    res = run_bass_kernel_spmd(nc, in_maps, core_ids=list(range(NCORES)))
    outs = []
    for core in range(NCORES):
        oT = np.asarray(res.results[core]["outT"]).reshape(NSEQ, D, S)
        outs.append(np.transpose(oT, (0, 2, 1)))
    return np.ascontiguousarray(np.concatenate(outs, axis=0)).astype(np.float32)
```

```python
from contextlib import ExitStack
import threading
import math
import numpy as np
import concourse.bass as bass
import concourse.mybir as mybir
from concourse.bass_utils import run_bass_kernel_spmd

F32 = mybir.dt.float32
BF16 = mybir.dt.bfloat16
I32 = mybir.dt.int32
U32 = mybir.dt.uint32
ALU = mybir.AluOpType
AF = mybir.ActivationFunctionType
AX = mybir.AxisListType

D = 1024
S = 2048
NCORES = 8
NSEQ = 4
C = 256
NCH = S // C
DC = 8
EPS = 1e-6
WIN_COLS = 1472
NEG = -1.0e30
TWO_PI = 2.0 * math.pi

V_N1G, V_N2G, V_FG = 0, 8, 16
V_CONVW, V_CONVB, V_BA, V_BX, V_LAM = 24, 40, 44, 48, 52
V_QNG, V_KVNG, V_LOG, V_MOG = 56, 58, 59, 63
V_BADA = 67
V_INVF, V_SGN = 115, 116
NV = 117


class Sched:
    ENG = ("pe", "act", "dve", "pool", "sp")

    def __init__(self, nc, es):
        self.nc = nc
        self.es = es
        self.eng = {"pe": nc.tensor, "act": nc.scalar, "dve": nc.vector, "pool": nc.gpsimd, "sp": nc.sync}
        self.sem = {e: es.enter_context(nc.semaphore("sem_" + e)) for e in self.ENG}
        self.cnt = {e: 0 for e in self.ENG}
        self.seen = {e: {} for e in self.ENG}
        self.last_w = {}
        self.readers = {}
        self.dsem = {}
        self.dcnt = {}
        self.dead = [False]
        self.co = None
        self.last_b = {e: 0 for e in self.ENG}
        self.b_dcnt = {}
        self.in_b = False
        self.b_active = False
        self.tail = {e: 0.0 for e in self.ENG}
        self.tw = {}
        self.tr = {}
        self.DUR = {"pe": 0.15, "act": 0.45, "dve": 0.3, "pool": 0.25, "sp": 0.1}
        self.LAT = 0.3
        self.MARGIN = {"pe": 24.0, "act": 18.0, "dve": 16.0, "pool": 6.0, "sp": 60.0}

    def _deps(self, reads, writes):
        need = {}
        def add(tok):
            k, v = tok
            if need.get(k, 0) < v:
                need[k] = v
        for k in list(reads) + list(writes):
            t = self.last_w.get(k)
            if t is not None:
                add(t)
        for k in writes:
            for tok in self.readers.get(k, {}).items():
                add(tok)
        return need

    def _emit_waits(self, e, need):
        eng = self.eng[e]
        seen = self.seen[e]
        for k, v in need.items():
            if k == e and e == "pe":
                continue
            if seen.get(k, 0) >= v:
                continue
            if k in self.sem:
                eng.wait_ge(self.sem[k], v)
            else:
                eng.wait_ge(self.dsem[k], v)
            seen[k] = v

    def _record(self, tok, reads, writes):
        for k in writes:
            self.last_w[k] = tok
            self.readers[k] = {}
        for k in reads:
            r = self.readers.setdefault(k, {})
            if r.get(tok[0], 0) < tok[1]:
                r[tok[0]] = tok[1]

    def _ready(self, reads, writes):
        t = 0.0
        for k in list(reads) + list(writes):
            v = self.tw.get(k)
            if v is not None and v > t:
                t = v
        for k in writes:
            v = self.tr.get(k)
            if v is not None and v > t:
                t = v
        return t

    def _model(self, e, reads, writes, dur, extra=0.0):
        start = max(self._ready(reads, writes) + self.LAT, self.tail[e])
        fin = start + dur
        self.tail[e] = fin
        for k in writes:
            self.tw[k] = fin + extra
            self.tr[k] = 0.0
        for k in reads:
            if self.tr.get(k, 0.0) < fin + extra:
                self.tr[k] = fin + extra

    def op(self, e, fn, reads=(), writes=(), cost=None):
        dur = self.DUR[e] if cost is None else cost
        if self.co is not None:
            self.co.tick(self, e, reads, writes, dur)
        self.in_b = threading.current_thread() is getattr(self, "in_b_thread", None) and self.b_active
        if self.in_b and getattr(self, "pend", None) and self.pend.get(e):
            self._emit_waits(e, self.pend[e])
            self.pend[e] = None
        self._model(e, reads, writes, dur)
        self._emit_waits(e, self._deps(reads, writes))
        ins = fn()
        ins.then_inc(self.sem[e], 1)
        self.cnt[e] += 1
        if self.in_b:
            self.last_b[e] = self.cnt[e]
        self._record((e, self.cnt[e]), reads, writes)

    def dma(self, e, fn, reads=(), writes=(), ch=None, cost=None):
        dur = 0.1 if cost is None else cost
        if self.co is not None:
            self.co.tick(self, e, reads, writes, dur)
        self.in_b = threading.current_thread() is getattr(self, "in_b_thread", None) and self.b_active
        if self.in_b and getattr(self, "pend", None) and self.pend.get(e):
            self._emit_waits(e, self.pend[e])
            self.pend[e] = None
        self._model(e, reads, writes, dur, extra=2.5)
        if ch not in self.dsem:
            self.dsem[ch] = self.es.enter_context(self.nc.semaphore("dsem_%d" % len(self.dsem)))
            self.dcnt[ch] = 0
        need = self._deps(reads, writes)
        if self.dcnt[ch]:
            need[ch] = max(need.get(ch, 0), self.dcnt[ch])
        self._emit_waits(e, need)
        ins = fn()
        ins.then_inc(self.dsem[ch], 16)
        self.dcnt[ch] += 16
        if self.in_b:
            self.b_dcnt[ch] = self.dcnt[ch]
        self._record((ch, self.dcnt[ch]), reads, writes)

    def barrier_b(self):
        need = {e: v for e, v in self.last_b.items() if v}
        need.update({ch: v for ch, v in self.b_dcnt.items() if v})
        self.pend = {e: dict(need) for e in self.ENG}

    def barrier(self):
        need = {e: self.cnt[e] for e in self.ENG if self.cnt[e]}
        for ch, v in self.dcnt.items():
            if v:
                need[ch] = v
        for e in self.ENG:
            self._emit_waits(e, dict(need))
        self.last_w = {}
        self.readers = {}

    def finish(self):
        need = {e: self.cnt[e] for e in self.ENG if self.cnt[e]}
        for ch, v in self.dcnt.items():
            if v:
                need[ch] = v
        self._emit_waits("sp", need)


class Co:
    def __init__(self):
        self.thread = None
        self.quota = 0
        self.b_go = threading.Semaphore(0)
        self.m_go = threading.Semaphore(0)
        self.done = True
        self.exc = None
        self.count = 0
        self.free_run = False
        self.SLACK = 0.3

    def start(self, fn):
        self.done = False
        self.count = 0
        self.exc = None

        def run():
            self.b_go.acquire()
            try:
                fn()
            except BaseException as e:
                self.exc = e
            self.done = True
            self.m_go.release()
        self.thread = threading.Thread(target=run)
        self.thread.start()

    def give(self, q):
        if self.done:
            return
        self.quota = q
        self.free_run = q >= (1 << 50)
        self.b_go.release()
        self.m_go.acquire()
        if self.exc is not None:
            raise self.exc

    def tick(self, sched=None, e=None, reads=(), writes=(), dur=0.0):
        if self.thread is not None and threading.current_thread() is self.thread:
            self.count += 1
            while not getattr(self, "free_run", False):
                self.quota -= 1
                blocked = False
                if sched is not None and not getattr(self, "no_pace", False):
                    fin = max(sched._ready(reads, writes) + sched.LAT, sched.tail[e]) + dur
                    blocked = fin > sched.tail["pool"] + sched.MARGIN[e]
                if self.quota >= 0 and not blocked:
                    break
                self.m_go.release()
                self.b_go.acquire()

    def drain(self):
        while not self.done:
            self.give(1 << 60)
        if self.thread is not None:
            self.thread.join()
            self.thread = None
        if self.exc is not None:
            raise self.exc


def build_program(nseq=NSEQ, nch=NCH, stop=99, overlap=True):
    nc = bass.Bass("TRN2", target_bir_lowering=False)
    dr = lambda name, shape, dt, kind="ExternalInput": nc.dram_tensor(name, shape, dt, kind=kind).ap()
    xT_d = dr("xT", [nseq, DC, 128, S], F32)
    cT_d = dr("cT", [128, DC, nseq], F32)
    pos_d = dr("pos", [nseq, S], I32)
    wada_d = dr("w_ada", [DC, 128, 6 * D], F32)
    vecs_d = dr("vecs", [128, NV], F32)
    win_d = dr("w_in", [DC, 128, WIN_COLS], F32)
    wabd_d = dr("wa_bd", [4, 128, 128], F32)
    wxbd_d = dr("wx_bd", [4, 128, 128], F32)
    wuq_d = dr("w_uq", [2, 128, 1536], F32)
    wukv_d = dr("w_ukv", [128, 1024], F32)
    wout_d = dr("w_out", [DC, 128, D], F32)
    wq_d = dr("peer_wq", [DC, 128, 2048], F32)
    keysT_d = dr("keysT", [128, 16, 128], F32)
    uv_d = dr("uv", [16384, 2048], F32)
    outT_d = dr("outT", [nseq, DC, 128, S], F32, kind="ExternalOutput")
    winb_d = dr("winb", [13, 128, DC, 128], BF16, kind="Internal")
    woutb_d = dr("woutb", [8, 128, DC, 128], BF16, kind="Internal")
    wqb_d = dr("wqb", [16, 128, DC, 128], BF16, kind="Internal")
    uvb_d = dr("uvb", [16384, 2048], BF16, kind="Internal")

    with ExitStack() as es:
        sc = Sched(nc, es)
        co = Co()
        sc.co = co
        PE, ACT, DVE, POOL, SP = nc.tensor, nc.scalar, nc.vector, nc.gpsimd, nc.sync
        uid = [0]

        def sb(stack, name, shape, dt):
            uid[0] += 1
            return stack.enter_context(nc.sbuf_tensor("%s_%d" % (name, uid[0]), shape, dt))

        w_uq = sb(es, "w_uq", [128, 2, 1536], BF16)
        w_ukv = sb(es, "w_ukv", [128, 1024], BF16)
        keysT = sb(es, "keysT", [128, 16, 128], F32)
        wa_bd = sb(es, "wa_bd", [128, 4, 128], BF16)
        wx_bd = sb(es, "wx_bd", [128, 4, 128], BF16)
        vecs = sb(es, "vecs", [128, NV], F32)
        modT = sb(es, "modT", [128, 48, nseq], F32)
        A1 = sb(es, "A1", [128, DC, nseq], F32)
        A2 = sb(es, "A2", [128, DC, nseq], F32)
        nsp = sb(es, "nsp", [128, 4], F32)
        consts = sb(es, "consts", [128, 4], F32)
        ones_bf = sb(es, "ones_bf", [128, 128], BF16)
        ident_f = sb(es, "ident_f", [128, 128], F32)
        ident_b = sb(es, "ident_b", [128, 128], BF16)
        tri_b = sb(es, "tri_b", [128, 128], BF16)
        iota16 = sb(es, "iota16", [128, 16], F32)
        KT = sb(es, "KT", [96, 8, S], BF16)
        VC = sb(es, "VC", [128, S // 128, 8, 65], BF16)
        xl = sb(es, "xl", [128, 4, C + 3], F32)
        hst = sb(es, "hst", [128, 4], F32)
        xTs = [sb(es, "xT%d" % i, [128, DC, C], F32) for i in range(2)]
        hT = sb(es, "hT", [128, DC, C], BF16)
        sq = sb(es, "sq", [128, DC, C], BF16)
        rstd = sb(es, "rstd", [128, C], F32)
        tmpf = sb(es, "tmpf", [128, C], F32)
        wr = [sb(es, "wr%d" % i, [128, DC, 128], BF16) for i in range(4)]
        idxs = [[sb(es, "idx%d%d" % (p, j), [128, 128], I32) for j in range(2)] for p in range(2)]
        ggs = [[sb(es, "gg%d%d" % (p, j), [128, 8, 16], F32) for j in range(2)] for p in range(2)]
        h2s = [[sb(es, "h2%d%d" % (p, j), [128, D], BF16) for j in range(2)] for p in range(2)]

        PS = [es.enter_context(nc.psum_tensor("ps%d" % i, [128, 512], F32)) for i in (0, 1, 2)]
        PS3 = es.enter_context(nc.psum_tensor("ps3", [128, 1024], BF16))
        PS += [None] + [es.enter_context(nc.psum_tensor("ps%d" % i, [128, 512], F32)) for i in (4, 5, 6, 7)]

        def vcol(c0, n=1):
            return vecs[:, c0:c0 + n]

        pes = ExitStack()
        if True:
            stage = sb(pes, "stage", [128, 4096], F32)
            cT = sb(pes, "cT", [128, DC, nseq], F32)
            iot_i = sb(pes, "iot_i", [128, 128], I32)
            iot_f = sb(pes, "iot_f", [128, 128], F32)
            sc.dma("sp", lambda: SP.dma_start(out=vecs[:], in_=vecs_d[:, :]), writes=["vecs"], ch="vecs")
            sc.dma("sp", lambda: SP.dma_start(out=cT[:], in_=cT_d[:, :, :]), writes=["cT"], ch="cT")
            sc.dma("sp", lambda: SP.dma_start(out=keysT[:], in_=keysT_d[:, :, :]), writes=["keysT"], ch="keysT")
            sc.op("dve", lambda: DVE.memset(consts[:, 0:1], EPS), writes=["consts"])
            sc.op("dve", lambda: DVE.memset(consts[:, 1:2], 1.0), writes=["consts"])
            sc.op("dve", lambda: DVE.memset(consts[:, 2:3], 0.0), writes=["consts"])
            sc.op("dve", lambda: DVE.memset(ones_bf[:], 1.0), writes=["ones_bf"])
            sc.op("dve", lambda: DVE.memset(VC[:], 1.0), writes=["VC"])
            sc.op("dve", lambda: DVE.memset(KT[:], 0.0), writes=["KT"])
            sc.op("pool", lambda: POOL.iota(iot_i[:], pattern=[[1, 128]], base=0, channel_multiplier=-1), writes=["iot_i"])
            sc.op("dve", lambda: DVE.tensor_copy(iot_f[:], iot_i[:]), reads=["iot_i"], writes=["iot_f"])
            sc.op("dve", lambda: DVE.tensor_scalar(ident_f[:], iot_f[:], 0.0, None, op0=ALU.is_equal), reads=["iot_f"], writes=["ident_f"])
            sc.op("dve", lambda: DVE.tensor_copy(ident_b[:], ident_f[:]), reads=["ident_f"], writes=["ident_b"])
            sc.op("dve", lambda: DVE.tensor_scalar(tri_b[:], iot_f[:], 0.0, None, op0=ALU.is_ge), reads=["iot_f"], writes=["tri_b"])
            sc.op("pool", lambda: POOL.iota(iot_i[:, 0:16], pattern=[[1, 16]], base=0, channel_multiplier=0), reads=["iot_f"], writes=["iot_i"])
            sc.op("dve", lambda: DVE.tensor_copy(iota16[:], iot_i[:, 0:16]), reads=["iot_i"], writes=["iota16"])

            stb = [sb(pes, "stb%d" % i, [128, 2048], BF16) for i in range(2)]

            def cast_op(k, dst_ap, st, key, wkey):
                eng = ("dve", "act", "pool")[k % 3]
                if eng == "dve":
                    sc.op("dve", lambda: DVE.tensor_copy(dst_ap, st), reads=[key], writes=[wkey])
                elif eng == "act":
                    sc.op("act", lambda: ACT.copy(dst_ap, st), reads=[key], writes=[wkey])
                else:
                    sc.op("pool", lambda: POOL.tensor_copy(dst_ap, st), reads=[key], writes=[wkey])

            def load_cast(dst_ap, src_ap, ncols, k, outs=None):
                st = stage[:, 0:ncols] if k % 2 == 0 else stage[:, 2048:2048 + ncols]
                key = "stage%d" % (k % 2)
                sc.dma("sp", lambda: SP.dma_start(out=st, in_=src_ap), writes=[key], ch=key)
                if outs is None:
                    cast_op(k, dst_ap, st, key, "W")
                    return
                bkey = "stb%d" % (k % 2)
                sbt = stb[k % 2]
                cast_op(k, sbt[:, 0:ncols], st, key, bkey)
                for oi, (d_ap, s_ap) in enumerate(outs(sbt)):
                    sc.dma("sp", lambda: SP.dma_start(out=d_ap, in_=s_ap), reads=[bkey], writes=["wscr"], ch="%so%d" % (bkey, oi))
            k = 0
            for dc in range(DC):
                load_cast(None, win_d[dc], WIN_COLS, k, outs=lambda t, dc=dc: [
                    (winb_d[0:11, :, dc, :].rearrange("oc p n -> p oc n"), t[:, 0:1408].rearrange("p (oc n) -> p oc n", n=128)),
                    (winb_d[11, :, dc, 0:96], t[:, 1344:1440]),
                    (winb_d[12, :, dc, 0:96], t[:, 1376:1472])]); k += 1
                load_cast(None, wout_d[dc], D, k, outs=lambda t, dc=dc: [
                    (woutb_d[:, :, dc, :].rearrange("oc p n -> p oc n"), t[:, 0:1024].rearrange("p (oc n) -> p oc n", n=128))]); k += 1
                load_cast(None, wq_d[dc], 2048, k, outs=lambda t, dc=dc: [
                    (wqb_d[:, :, dc, :].rearrange("oc p n -> p oc n"), t[:, 0:2048].rearrange("p (oc n) -> p oc n", n=128))]); k += 1
            for kc in range(2):
                load_cast(w_uq[:, kc, :], wuq_d[kc], 1536, k); k += 1
            load_cast(w_ukv[:, :], wukv_d[:, :], 1024, k); k += 1
            for ci in range(4):
                load_cast(wa_bd[:, ci, :], wabd_d[ci], 128, k); k += 1
                load_cast(wx_bd[:, ci, :], wxbd_d[ci], 128, k); k += 1

            sc.op("act", lambda: ACT.activation(out=nsp[:], in_=vcol(V_LAM, 4), func=AF.Exp, scale=-1.0), reads=["vecs"], writes=["nsp"])
            sc.op("act", lambda: ACT.activation(out=nsp[:], in_=nsp[:], func=AF.Ln, bias=consts[:, 1:2], scale=1.0), reads=["nsp", "consts"], writes=["nsp"])
            sc.op("dve", lambda: DVE.tensor_scalar(nsp[:], nsp[:], -16.0, None, op0=ALU.mult), reads=["nsp"], writes=["nsp"])

            sc.op("act", lambda: ACT.activation(out=cT[:], in_=cT[:], func=AF.Silu), reads=["cT"], writes=["cT"])
            ps_mod = PS[0][:, 0:48 * nseq].rearrange("p (n b) -> p n b", b=nseq)
            for n in range(48):
                key = "stage%d" % (n % 2)
                st = stage[:, (n % 2) * 2048:(n % 2) * 2048 + 1024].rearrange("p (dc n) -> p dc n", n=128)
                sc.dma("sp", lambda: SP.dma_start(out=st, in_=wada_d[:, :, n * 128:(n + 1) * 128].rearrange("dc p n -> p dc n")), writes=[key], ch=key)
                for dc in range(DC):
                    sc.op("pe", lambda: PE.matmul(ps_mod[:, n, :], st[:, dc, :], cT[:, dc, :], start=(dc == 0), stop=(dc == DC - 1)),
                          reads=[key, "cT"], writes=["ps0"])
            sc.op("dve", lambda: DVE.tensor_tensor(out=modT[:], in0=ps_mod, in1=vcol(V_BADA, 48).unsqueeze(2).to_broadcast([128, 48, nseq]), op=ALU.add),
                  reads=["ps0", "vecs"], writes=["modT"])
            for dc in range(DC):
                sc.op("dve", lambda: DVE.tensor_scalar(A1[:, dc, :], modT[:, 8 + dc, :], 1.0, vcol(V_N1G + dc), op0=ALU.add, op1=ALU.mult),
                      reads=["modT", "vecs"], writes=["A1"])
                sc.op("dve", lambda: DVE.tensor_scalar(A2[:, dc, :], modT[:, 32 + dc, :], 1.0, vcol(V_N2G + dc), op0=ALU.add, op1=ALU.mult),
                      reads=["modT", "vecs"], writes=["A2"])
            sc.barrier()

        def rms_stats(src_tile, nchunks, inv_n, srckey, sq_t=None, rstd_t=None, bank=0, sfx="", sqkeys=None):
            sq_t = sq if sq_t is None else sq_t
            rstd_t = rstd if rstd_t is None else rstd_t
            sk, rk, pk = "sq" + sfx, "rstd" + sfx, "ps%d" % bank
            sks = [sk] if sqkeys is None else list(sqkeys)
            srckeys = list(srckey) if isinstance(srckey, (list, tuple)) else [srckey]
            sc.op("act", lambda: ACT.activation(out=sq_t[:, 0:nchunks, :], in_=src_tile, func=AF.Square), reads=srckeys, writes=sks, cost=0.25 + 0.21 * nchunks)
            for i in range(nchunks):
                sc.op("pe", lambda: PE.matmul(PS[bank][:, 0:C], ones_bf[:], sq_t[:, i, :], start=(i == 0), stop=(i == nchunks - 1)),
                      reads=sks, writes=[pk])
            sc.op("act", lambda: ACT.activation(out=rstd_t[:], in_=PS[bank][:, 0:C], func=AF.Sqrt, bias=consts[:, 0:1], scale=inv_n), reads=[pk], writes=[rk])
            sc.op("dve", lambda: DVE.reciprocal(rstd_t[:], rstd_t[:]), reads=[rk], writes=[rk])

        def modulated_norm(xT, xk, Acol, shift_chunk0, b):
            rms_stats(xT[:], DC, 1.0 / D, xk)
            for dc in range(DC):
                sc.op("dve", lambda: DVE.scalar_tensor_tensor(out=tmpf[:], in0=xT[:, dc, :], scalar=Acol[:, dc, b:b + 1], in1=rstd[:], op0=ALU.mult, op1=ALU.mult),
                      reads=[xk, "rstd"], writes=["tmpf"])
                sc.op("act", lambda: ACT.activation(out=hT[:, dc, :], in_=tmpf[:], func=AF.Identity, bias=modT[:, shift_chunk0 + dc, b:b + 1], scale=1.0),
                      reads=["tmpf"], writes=["hT"])

        gen_i = [0]

        def gen_ps():
            bnk = (1, 2)[gen_i[0] % 2]
            gen_i[0] += 1
            return PS[bnk][:, 0:C], "ps%d" % bnk

        ring_i = [0]

        def wload(src_ap, ncols=128):
            i = ring_i[0] % 4
            ring_i[0] += 1
            key = "wr%d" % i
            sc.dma("sp", lambda: SP.dma_start(out=wr[i][:, :, 0:ncols], in_=src_ap), reads=["wscr"], writes=[key], ch=key)
            return wr[i], key

        class WStream:
            def __init__(self, blocks, depth=3):
                self.blocks = list(blocks)
                self.pend = []
                self.depth = depth
                for _ in range(depth):
                    self._issue()

            def _issue(self):
                if self.blocks:
                    src, ncols = self.blocks.pop(0)
                    self.pend.append(wload(src, ncols))

            def next(self):
                w, key = self.pend.pop(0)
                self._issue()
                return w, key

        def emit_B(b, c, par):
            sc.in_b_thread = threading.current_thread()
            sc.b_active = True
            t0 = c * C
            xT = xTs[par]
            xk = "xT%d" % par
            if c == 0:
                sc.op("dve", lambda: DVE.memset(xl[:], 0.0), writes=["xl0", "xl1", "xl2", "xl3"])
                sc.op("dve", lambda: DVE.memset(hst[:], 0.0), writes=["hst0", "hst1", "hst2", "hst3"])
            with ExitStack() as mes:
                gT = sb(mes, "gT", [128, 4, C], F32)
                qlat = sb(mes, "qlat", [128, 2, C], F32)
                kvlat = sb(mes, "kvlat", [128, C], F32)
                qs = sb(mes, "qs", [128, 2, C], BF16)
                kvs = sb(mes, "kvs", [128, C], BF16)
                xc = sb(mes, "xc", [128, C], F32)
                xcb = sb(mes, "xcb", [128, C], BF16)
                ra = sb(mes, "ra", [128, C], F32)
                ib = sb(mes, "ib", [128, C], F32)
                hh = sb(mes, "hh", [128, C], F32)
                xc2 = sb(mes, "xc2", [128, C], F32)
                xcb2 = sb(mes, "xcb2", [128, C], BF16)
                ra2 = sb(mes, "ra2", [128, C], F32)
                ib2 = sb(mes, "ib2", [128, C], F32)
                hh2 = sb(mes, "hh2", [128, C], F32)
                ylru = sb(mes, "ylru", [128, 4, C], F32)
                yT = sb(mes, "yT", [128, 8, C], BF16)
                QT = sb(mes, "QT", [96, 8, C], BF16)
                posi = sb(mes, "posi", [96, C], I32)
                ang = sb(mes, "ang", [96, C], F32)
                kf = sb(mes, "kf", [96, C], F32)
                cos2 = sb(mes, "cos2", [96, C], F32)
                sin2 = sb(mes, "sin2", [96, C], F32)
                t1 = tmpf
                t2 = sb(mes, "t2", [96, C], F32)
                krb = sb(mes, "krb", [96, C], BF16)
                pT = [sb(mes, "pT%d" % i, [128, C], BF16) for i in range(3)]
                ymla = sb(mes, "ymla", [128, 2, 512], F32)
                ymn = sb(mes, "ymn", [128, 2, 512], BF16)
                rinv = sb(mes, "rinv", [128, 2], F32)
                sst = sb(mes, "sst", [128, 2], F32)

                win_blocks = [(winb_d[oc, :, :, :], 128) for oc in range(11)] + [(winb_d[11, :, :, 0:96], 96), (winb_d[12, :, :, 0:96], 96)]
                wst = WStream(win_blocks)
                sc.dma("sp", lambda: SP.dma_start(out=xT[:], in_=xT_d[b, :, :, t0:t0 + C].rearrange("dc p t -> p dc t")), writes=[xk], ch=xk)
                sc.dma("sp", lambda: SP.dma_start(out=posi[64:96, :], in_=pos_d[b:b + 1, t0:t0 + C].partition_broadcast(32)), writes=["posi"], ch="posi")
                modulated_norm(xT, xk, A1, 0, b)

                R = slice(64, 96)
                sc.op("dve", lambda: DVE.tensor_copy(ang[R, :], posi[R, :]), reads=["posi"], writes=["ang"])
                sc.op("dve", lambda: DVE.tensor_scalar(ang[R, :], ang[R, :], vecs[R, V_INVF:V_INVF + 1], None, op0=ALU.mult), reads=["ang", "vecs"], writes=["ang"])
                for shift, dst, use_sgn in ((0.0, sin2, True), (math.pi / 2, cos2, False)):
                    sc.op("dve", lambda: DVE.tensor_scalar(kf[R, :], ang[R, :], shift, 1.0 / TWO_PI, op0=ALU.add, op1=ALU.mult), reads=["ang"], writes=["kf"])
                    sc.op("dve", lambda: DVE.tensor_copy(posi[R, :], kf[R, :]), reads=["kf"], writes=["posi"])
                    sc.op("dve", lambda: DVE.tensor_copy(kf[R, :], posi[R, :]), reads=["posi"], writes=["kf"])
                    sc.op("dve", lambda: DVE.scalar_tensor_tensor(out=kf[R, :], in0=kf[R, :], scalar=-TWO_PI, in1=ang[R, :], op0=ALU.mult, op1=ALU.add),
                          reads=["kf", "ang"], writes=["kf"])
                    sc.op("dve", lambda: DVE.tensor_scalar(kf[R, :], kf[R, :], shift, None, op0=ALU.add), reads=["kf"], writes=["kf"])
                    sc.op("dve", lambda: DVE.tensor_scalar(kf[R, :], kf[R, :], 3.1415925, -3.1415925, op0=ALU.min, op1=ALU.max), reads=["kf"], writes=["kf"])
                    if use_sgn:
                        sc.op("act", lambda: ACT.activation(out=dst[R, :], in_=kf[R, :], func=AF.Sin, scale=vecs[R, V_SGN:V_SGN + 1]), reads=["kf", "vecs"], writes=["rope"])
                    else:
                        sc.op("act", lambda: ACT.activation(out=dst[R, :], in_=kf[R, :], func=AF.Sin), reads=["kf"], writes=["rope"])
                ROPE = ["rope"]

                def inproj(ncols=128):
                    w, wkey = wst.next()
                    ps, key = gen_ps()
                    for dc in range(DC):
                        sc.op("pe", lambda: PE.matmul(ps[0:ncols, :], w[:, dc, 0:ncols], hT[:, dc, :], start=(dc == 0), stop=(dc == DC - 1)),
                              reads=["hT", wkey], writes=[key])
                    return ps, key
                for ci in range(4):
                    ps, key = inproj()
                    sc.op("act", lambda: ACT.copy(xl[:, ci, 3:3 + C], ps), reads=[key], writes=["xl%d" % ci])
                for ci in range(4):
                    ps, key = inproj()
                    sc.op("act", lambda: ACT.activation(out=gT[:, ci, :], in_=ps, func=AF.Gelu_apprx_tanh), reads=[key], writes=["gT"])
                for kc in range(2):
                    ps, key = inproj()
                    sc.op("dve", lambda: DVE.tensor_copy(qlat[:, kc, :], ps), reads=[key], writes=["qlat"])
                ps, key = inproj()
                sc.op("dve", lambda: DVE.tensor_copy(kvlat[:], ps), reads=[key], writes=["kvlat"])
                ps_kr, key_kr = inproj(96)
                ps_krr, key_krr = inproj(96)
                sc.op("dve", lambda: DVE.tensor_tensor(out=t1[R, :], in0=ps_kr[R, :], in1=cos2[R, :], op=ALU.mult), reads=[key_kr] + ROPE, writes=["tmpf"])
                sc.op("dve", lambda: DVE.tensor_tensor(out=t2[R, :], in0=ps_krr[R, :], in1=sin2[R, :], op=ALU.mult), reads=[key_krr] + ROPE, writes=["t2"])
                sc.op("dve", lambda: DVE.tensor_tensor(out=krb[R, :], in0=t1[R, :], in1=t2[R, :], op=ALU.add), reads=["tmpf", "t2"], writes=["krb"])
                for h in range(8):
                    if h % 2 == 0:
                        sc.op("act", lambda: ACT.copy(KT[R, h, t0:t0 + C], krb[R, :]), reads=["krb"], writes=["KT"])
                    else:
                        sc.op("dve", lambda: DVE.tensor_copy(KT[R, h, t0:t0 + C], krb[R, :]), reads=["krb"], writes=["KT"])

                def lru_stages(ci, B_, sfx, banks):
                    xc_, xcb_, ra_, ib_, hh_ = B_
                    kxc, kxcb, kra, kib, khh = ["%s%s" % (n, sfx) for n in ("xc", "xcb", "ra", "ib", "hh")]
                    xk_ = "xl%d" % ci
                    cw = V_CONVW + ci * 4
                    ps_r, key_r = PS[banks[0]][:, 0:C], "ps%d" % banks[0]
                    ps_i, key_i = PS[banks[1]][:, 0:C], "ps%d" % banks[1]
                    st = []
                    st.append(lambda: sc.op("dve", lambda: DVE.tensor_scalar(xc_[:], xl[:, ci, 0:C], vcol(cw), vcol(V_CONVB + ci), op0=ALU.mult, op1=ALU.add),
                                            reads=[xk_, "vecs"], writes=[kxc]))
                    for kk in range(1, 4):
                        st.append(lambda kk=kk: sc.op("dve", lambda: DVE.scalar_tensor_tensor(out=xc_[:], in0=xl[:, ci, kk:kk + C], scalar=vcol(cw + kk), in1=xc_[:], op0=ALU.mult, op1=ALU.add),
                                                      reads=[xk_, kxc], writes=[kxc]))
                    st.append(lambda: sc.op("act", lambda: ACT.copy(xl[:, ci, 0:3], xl[:, ci, C:C + 3]), reads=[xk_, kxc], writes=[xk_]))
                    st.append(lambda: sc.op("act", lambda: ACT.copy(xcb_[:], xc_[:]), reads=[kxc], writes=[kxcb]))
                    st.append(lambda: sc.op("pe", lambda: PE.matmul(ps_r, wa_bd[:, ci, :], xcb_[:], start=True, stop=True), reads=[kxcb], writes=[key_r]))
                    st.append(lambda: sc.op("pe", lambda: PE.matmul(ps_i, wx_bd[:, ci, :], xcb_[:], start=True, stop=True), reads=[kxcb], writes=[key_i]))
                    st.append(lambda: sc.op("act", lambda: ACT.activation(out=ra_[:], in_=ps_r, func=AF.Sigmoid, bias=vcol(V_BA + ci), scale=1.0), reads=[key_r], writes=[kra]))
                    st.append(lambda: sc.op("act", lambda: ACT.activation(out=ib_[:], in_=ps_i, func=AF.Sigmoid, bias=vcol(V_BX + ci), scale=1.0), reads=[key_i], writes=[kib]))
                    st.append(lambda: sc.op("act", lambda: ACT.activation(out=hh_[:], in_=ra_[:], func=AF.Exp, scale=nsp[:, ci:ci + 1]), reads=[kra], writes=[khh]))
                    st.append(lambda: sc.op("dve", lambda: DVE.tensor_scalar(ra_[:], ra_[:], nsp[:, ci:ci + 1], 0.5, op0=ALU.mult, op1=ALU.mult), reads=[kra, khh], writes=[kra]))
                    st.append(lambda: sc.op("act", lambda: ACT.activation(out=ra_[:], in_=ra_[:], func=AF.Exp), reads=[kra], writes=[kra]))
                    st.append(lambda: sc.op("act", lambda: ACT.activation(out=hh_[:], in_=hh_[:], func=AF.Sqrt, bias=consts[:, 1:2], scale=-1.0), reads=[khh], writes=[khh]))
                    st.append(lambda: sc.op("dve", lambda: DVE.tensor_tensor(out=ib_[:], in0=ib_[:], in1=xc_[:], op=ALU.mult), reads=[kib, kxc], writes=[kib]))
                    st.append(lambda: sc.op("dve", lambda: DVE.tensor_tensor(out=ib_[:], in0=ib_[:], in1=hh_[:], op=ALU.mult), reads=[kib, khh], writes=[kib]))
                    st.append(lambda: sc.op("dve", lambda: DVE.tensor_tensor_scan(out=hh_[:], data0=ra_[:], data1=ib_[:], initial=hst[:, ci:ci + 1], op0=ALU.mult, op1=ALU.add),
                                            reads=[kra, kib, "hst%d" % ci], writes=[khh], cost=0.6))
                    st.append(lambda: sc.op("dve", lambda: DVE.tensor_copy(hst[:, ci:ci + 1], hh_[:, C - 1:C]), reads=[khh], writes=["hst%d" % ci]))
                    st.append(lambda: sc.op("dve", lambda: DVE.tensor_tensor(out=ylru[:, ci, :], in0=hh_[:], in1=gT[:, ci, :], op=ALU.mult), reads=[khh, "gT"], writes=["ylru%d" % ci]))
                    return st
                setA = (xc, xcb, ra, ib, hh)
                setB = (xc2, xcb2, ra2, ib2, hh2)
                for pair in ((0, 1), (2, 3)):
                    sa = lru_stages(pair[0], setA, "", (1, 2))
                    sb_ = lru_stages(pair[1], setB, "b", (4, 5))
                    for fa, fb in zip(sa, sb_):
                        fa()
                        fb()
                rms_stats(ylru[:], 4, 1.0 / 512, ["ylru0", "ylru1", "ylru2", "ylru3"])
                for ci in range(4):
                    sc.op("dve", lambda: DVE.scalar_tensor_tensor(out=yT[:, ci, :], in0=ylru[:, ci, :], scalar=vcol(V_LOG + ci), in1=rstd[:], op0=ALU.mult, op1=ALU.mult),
                          reads=["ylru%d" % ci, "rstd"], writes=["yT"])

                rms_stats(qlat[:], 2, 1.0 / 256, "qlat")
                for kc in range(2):
                    sc.op("dve", lambda: DVE.scalar_tensor_tensor(out=qs[:, kc, :], in0=qlat[:, kc, :], scalar=vcol(V_QNG + kc), in1=rstd[:], op0=ALU.mult, op1=ALU.mult),
                          reads=["qlat", "rstd"], writes=["qs"])
                rms_stats(kvlat[:].unsqueeze(1), 1, 1.0 / 128, "kvlat")
                sc.op("dve", lambda: DVE.scalar_tensor_tensor(out=kvs[:], in0=kvlat[:], scalar=vcol(V_KVNG), in1=rstd[:], op0=ALU.mult, op1=ALU.mult),
                      reads=["kvlat", "rstd"], writes=["kvs"])
                for h in range(8):
                    ps_q, key_q = gen_ps()
                    ps_qr, key_qr = gen_ps()
                    for kc in range(2):
                        sc.op("pe", lambda: PE.matmul(ps_q[0:96, :], w_uq[:, kc, h * 192:h * 192 + 96], qs[:, kc, :], start=(kc == 0), stop=(kc == 1)), reads=["qs"], writes=[key_q])
                    for kc in range(2):
                        sc.op("pe", lambda: PE.matmul(ps_qr[0:96, :], w_uq[:, kc, h * 192 + 96:h * 192 + 192], qs[:, kc, :], start=(kc == 0), stop=(kc == 1)), reads=["qs"], writes=[key_qr])
                    sc.op("act", lambda: ACT.copy(QT[0:64, h, :], ps_q[0:64, :]), reads=[key_q], writes=["QT"])
                    sc.op("dve", lambda: DVE.tensor_tensor(out=t1[R, :], in0=ps_q[R, :], in1=cos2[R, :], op=ALU.mult), reads=[key_q] + ROPE, writes=["tmpf"])
                    sc.op("dve", lambda: DVE.tensor_tensor(out=t2[R, :], in0=ps_qr[R, :], in1=sin2[R, :], op=ALU.mult), reads=[key_qr] + ROPE, writes=["t2"])
                    sc.op("dve", lambda: DVE.tensor_tensor(out=QT[R, h, :], in0=t1[R, :], in1=t2[R, :], op=ALU.add), reads=["tmpf", "t2"], writes=["QT"])
                    ps_k, key_k = gen_ps()
                    sc.op("pe", lambda: PE.matmul(ps_k[0:64, :], w_ukv[:, h * 128:h * 128 + 64], kvs[:], start=True, stop=True), reads=["kvs"], writes=[key_k])
                    sc.op("act", lambda: ACT.copy(KT[0:64, h, t0:t0 + C], ps_k[0:64, :]), reads=[key_k], writes=["KT"])
                wv = w_ukv[:, :].rearrange("p (h x) -> p h x", x=128)[:, :, 64:128]
                for j in range(C // 128):
                    tile_i = (t0 // 128) + j
                    sc.op("pe", lambda: PE.matmul(PS[4][:, :].rearrange("p (h x) -> p h x", x=64), kvs[:, j * 128:(j + 1) * 128], wv, start=True, stop=True),
                          reads=["kvs"], writes=["ps4"])
                    sc.op("dve", lambda: DVE.tensor_copy(VC[:, tile_i, :, 0:64], PS[4][:, :].rearrange("p (h x) -> p h x", x=64)), reads=["ps4"], writes=["VC"])

                wso = WStream([(woutb_d[oc, :, :, :], 128) for oc in range(8)])

                scale = 96.0 ** -0.5
                nkt = (t0 + C) // 128
                kdiag0 = t0 // 128
                it = 0
                abk = (1, 2)
                akeys = ["ps1", "ps2"]
                for h in range(8):
                    for kt in range(nkt):
                        sbank = (4, 5, 0)[it % 3]
                        skey = "ps%d" % sbank
                        pt = pT[it % 3]
                        pkey = "pT%d" % (it % 3)
                        it += 1
                        sc.op("pe", lambda: PE.matmul(PS[sbank][:, 0:C], KT[0:96, h, kt * 128:(kt + 1) * 128], QT[0:96, h, :], start=True, stop=True),
                              reads=["KT", "QT"], writes=[skey])
                        sc.op("act", lambda: ACT.activation(out=pt[:], in_=PS[sbank][:, 0:C], func=AF.Exp, scale=scale), reads=[skey], writes=[pkey])
                        jk = kt - kdiag0
                        if jk >= 0:
                            sc.op("dve", lambda: DVE.tensor_tensor(out=pt[:, jk * 128:(jk + 1) * 128], in0=pt[:, jk * 128:(jk + 1) * 128], in1=tri_b[:], op=ALU.mult),
                                  reads=[pkey], writes=[pkey])
                        for jq in range(C // 128):
                            if jk > jq:
                                continue
                            last = kdiag0 + jq
                            sc.op("pe", lambda: PE.matmul(PS[abk[jq]][:, 0:65], pt[:, jq * 128:(jq + 1) * 128], VC[:, kt, h, :], start=(kt == 0), stop=(kt == last)),
                                  reads=[pkey, "VC"], writes=[akeys[jq]])
                    for jq in range(C // 128):
                        sc.op("dve", lambda: DVE.reciprocal(rinv[:, jq:jq + 1], PS[abk[jq]][:, 64:65]), reads=[akeys[jq]], writes=["rinv"])
                        sc.op("dve", lambda: DVE.tensor_scalar(ymla[:, jq, h * 64:(h + 1) * 64], PS[abk[jq]][:, 0:64], rinv[:, jq:jq + 1], None, op0=ALU.mult),
                              reads=[akeys[jq], "rinv"], writes=["ymla"])
                for jq in range(C // 128):
                    sc.op("dve", lambda: DVE.scalar_tensor_tensor(out=ymn[:, jq, :], in0=ymla[:, jq, :], scalar=1.0, in1=ymla[:, jq, :], op0=ALU.mult, op1=ALU.mult, accum_out=sst[:, jq:jq + 1]),
                          reads=["ymla"], writes=["ymn", "sst"])
                sc.op("act", lambda: ACT.activation(out=sst[:], in_=sst[:], func=AF.Sqrt, bias=consts[:, 0:1], scale=1.0 / 512), reads=["sst"], writes=["sst"])
                sc.op("dve", lambda: DVE.reciprocal(sst[:], sst[:]), reads=["sst"], writes=["sst"])
                for jq in range(C // 128):
                    sc.op("dve", lambda: DVE.tensor_scalar(ymn[:, jq, :], ymla[:, jq, :], sst[:, jq:jq + 1], None, op0=ALU.mult), reads=["ymla", "sst"], writes=["ymn"])
                    for fc in range(4):
                        sc.op("pe", lambda: PE.transpose(PS3[:, fc * 128:(fc + 1) * 128], ymn[:, jq, fc * 128:(fc + 1) * 128], ident_b[:]), reads=["ymn"], writes=["ps3"])
                    for fc in range(4):
                        sc.op("dve", lambda: DVE.tensor_scalar(yT[:, 4 + fc, jq * 128:(jq + 1) * 128], PS3[:, fc * 128:(fc + 1) * 128], vcol(V_MOG + fc), None, op0=ALU.mult),
                              reads=["ps3"], writes=["yT"])
                for oc in range(DC):
                    w, wkey = wso.next()
                    ps, key = gen_ps()
                    for cc in range(8):
                        sc.op("pe", lambda: PE.matmul(ps, w[:, cc, :], yT[:, cc, :], start=(cc == 0), stop=(cc == 7)), reads=["yT", wkey], writes=[key])
                    sc.op("dve", lambda: DVE.scalar_tensor_tensor(out=xT[:, oc, :], in0=ps, scalar=modT[:, 16 + oc, b:b + 1], in1=xT[:, oc, :], op0=ALU.mult, op1=ALU.add),
                          reads=[key, xk], writes=[xk])
                sc.barrier_b()

            with ExitStack() as pes2:
                qT = sb(pes2, "qT", [128, 16, C], F32)
                scs = sb(pes2, "scs", [128, 2048], F32)
                work = sb(pes2, "work", [128, 2048], F32)
                top = sb(pes2, "top", [128, 16, 16], F32)
                tix = sb(pes2, "tix", [128, 16, 16], U32)
                tixf = sb(pes2, "tixf", [128, 16, 16], F32)
                best = sb(pes2, "best", [128, 8, 16], F32)
                posu = sb(pes2, "posu", [128, 8, 16], U32)
                pa_i = sb(pes2, "pa_i", [128, 8, 16], I32)
                pa_f = sb(pes2, "pa_f", [128, 8, 16], F32)
                pb_f = sb(pes2, "pb_f", [128, 8, 16], F32)
                gsum = sb(pes2, "gsum", [128, 8], F32)
                isel = sb(pes2, "isel", [128, 8, 16], F32)
                jsel = sb(pes2, "jsel", [128, 8, 16], F32)

                modulated_norm(xT, xk, A2, 24, b)
                wsq = WStream([(wqb_d[hp, :, :, :], 128) for hp in range(16)])
                for hp in range(16):
                    w, wkey = wsq.next()
                    qb = (0, 1, 2, 4, 5)[hp % 5]
                    qk = "ps%d" % qb
                    pq = PS[qb][:, 0:C]
                    for dc in range(DC):
                        sc.op("pe", lambda: PE.matmul(pq, w[:, dc, :], hT[:, dc, :], start=(dc == 0), stop=(dc == DC - 1)), reads=["hT", wkey], writes=[qk])
                    if hp % 2 == 0:
                        sc.op("act", lambda: ACT.copy(qT[:, hp, :], pq), reads=[qk], writes=["qT%d" % hp])
                    else:
                        sc.op("dve", lambda: DVE.tensor_copy(qT[:, hp, :], pq), reads=[qk], writes=["qT%d" % hp])
                for j in range(C // 128):
                    ts = slice(j * 128, (j + 1) * 128)
                    h2tm = h2s[par][j]
                    gg = ggs[par][j]
                    idxi = idxs[par][j]
                    hkey, gkey_, ikey = "h2%d%d" % (par, j), "gg%d%d" % (par, j), "idx%d%d" % (par, j)
                    for dc in range(DC):
                        sc.op("pe", lambda: PE.transpose(PS3[:, dc * 128:(dc + 1) * 128], hT[:, dc, ts], ident_b[:]), reads=["hT"], writes=["ps3"])
                    sc.op("act", lambda: ACT.copy(h2tm[:], PS3[:, :]), reads=["ps3"], writes=[hkey], cost=1.0)
                    sbanks = (1, 2, 4, 5)
                    for hp in range(16):
                        bnk = sbanks[hp // 4]
                        sc.op("pe", lambda: PE.matmul(PS[bnk][:, (hp % 4) * 128:(hp % 4 + 1) * 128], qT[:, hp, ts], keysT[:, hp, :], start=True, stop=True),
                              reads=["qT%d" % hp], writes=["ps%d" % bnk])
                    for q in range(4):
                        bnk = sbanks[q]
                        if q % 2 == 0:
                            sc.op("act", lambda: ACT.copy(scs[:, q * 512:(q + 1) * 512], PS[bnk][:, :]), reads=["ps%d" % bnk], writes=["scs%d" % q])
                        else:
                            sc.op("dve", lambda: DVE.tensor_copy(scs[:, q * 512:(q + 1) * 512], PS[bnk][:, :]), reads=["ps%d" % bnk], writes=["scs%d" % q])
                    TOPK = ["top%d" % hp for hp in range(16)]
                    TIXK = ["tix%d" % hp for hp in range(16)]
                    WRKK = ["work%d" % hp for hp in range(16)]
                    sks = ["scs%d" % (hp // 4) for hp in range(16)]
                    svs = [scs[:, hp * 128:(hp + 1) * 128] for hp in range(16)]
                    wvs = [work[:, hp * 128:(hp + 1) * 128] for hp in range(16)]
                    for hp in range(16):
                        sc.op("dve", lambda: DVE.max(out=top[:, hp, 0:8], in_=svs[hp]), reads=[sks[hp]], writes=[TOPK[hp]], cost=0.2)
                    for hp in range(16):
                        sc.op("dve", lambda: DVE.max_index(out=tix[:, hp, 0:8], in_max=top[:, hp, 0:8], in_values=svs[hp]), reads=[sks[hp], TOPK[hp]], writes=[TIXK[hp]], cost=0.25)
                    for hp in range(16):
                        sc.op("dve", lambda: DVE.match_replace(out=wvs[hp], in_to_replace=top[:, hp, 0:8], in_values=svs[hp], imm_value=NEG), reads=[sks[hp], TOPK[hp]], writes=[WRKK[hp]], cost=0.25)
                    for hp in range(16):
                        sc.op("dve", lambda: DVE.max(out=top[:, hp, 8:16], in_=wvs[hp]), reads=[WRKK[hp]], writes=[TOPK[hp]], cost=0.2)
                    for hp in range(16):
                        sc.op("dve", lambda: DVE.max_index(out=tix[:, hp, 8:16], in_max=top[:, hp, 8:16], in_values=wvs[hp]), reads=[WRKK[hp], TOPK[hp]], writes=[TIXK[hp]], cost=0.25)
                    sc.op("dve", lambda: DVE.tensor_copy(tixf[:], tix[:]), reads=TIXK, writes=["tixf"])
                    top4 = top[:].rearrange("p (h two) k -> p h two k", two=2)
                    tix4 = tixf[:].rearrange("p (h two) k -> p h two k", two=2)
                    cand = work[:].rearrange("p (h a b) -> p h a b", a=16, b=16)
                    cand3 = work[:].rearrange("p (h ab) -> p h ab", ab=256)
                    cand2 = scs[:].rearrange("p (h ab) -> p h ab", ab=256)
                    ALLS = ["scs0", "scs1", "scs2", "scs3"]
                    WH = [[WRKK[2 * h], WRKK[2 * h + 1]] for h in range(8)]
                    SH = ["scs%d" % (h // 2) for h in range(8)]
                    BK = ["best%d" % h for h in range(8)]
                    PK = ["posu%d" % h for h in range(8)]
                    sc.op("dve", lambda: DVE.tensor_tensor(out=cand, in0=top4[:, :, 0, :].unsqueeze(3).to_broadcast([128, 8, 16, 16]),
                                                           in1=top4[:, :, 1, :].unsqueeze(2).to_broadcast([128, 8, 16, 16]), op=ALU.add),
                          reads=TOPK, writes=WRKK, cost=2.2)
                    for h in range(8):
                        sc.op("dve", lambda: DVE.max(out=best[:, h, 0:8], in_=cand3[:, h, :]), reads=WH[h], writes=[BK[h]], cost=0.35)
                    for h in range(8):
                        sc.op("dve", lambda: DVE.max_index(out=posu[:, h, 0:8], in_max=best[:, h, 0:8], in_values=cand3[:, h, :]), reads=WH[h] + [BK[h]], writes=[PK[h]], cost=0.4)
                    for h in range(8):
                        sc.op("dve", lambda: DVE.match_replace(out=cand2[:, h, :], in_to_replace=best[:, h, 0:8], in_values=cand3[:, h, :], imm_value=NEG), reads=WH[h] + [BK[h]], writes=[SH[h]], cost=0.4)
                    for h in range(8):
                        sc.op("dve", lambda: DVE.max(out=best[:, h, 8:16], in_=cand2[:, h, :]), reads=[SH[h]], writes=[BK[h]], cost=0.35)
                    for h in range(8):
                        sc.op("dve", lambda: DVE.max_index(out=posu[:, h, 8:16], in_max=best[:, h, 8:16], in_values=cand2[:, h, :]), reads=[SH[h], BK[h]], writes=[PK[h]], cost=0.4)
                    sc.op("dve", lambda: DVE.tensor_copy(pb_f[:], posu[:]), reads=PK, writes=["pb_f"])
                    sc.op("dve", lambda: DVE.tensor_scalar(pa_f[:], pb_f[:], 1.0 / 16, -0.46875, op0=ALU.mult, op1=ALU.add), reads=["pb_f"], writes=["pa_f"])
                    sc.op("dve", lambda: DVE.tensor_copy(pa_i[:], pa_f[:]), reads=["pa_f"], writes=["pa_i"])
                    sc.op("dve", lambda: DVE.tensor_copy(pa_f[:], pa_i[:]), reads=["pa_i"], writes=["pa_f"])
                    sc.op("dve", lambda: DVE.scalar_tensor_tensor(out=pb_f[:], in0=pa_f[:], scalar=-16.0, in1=pb_f[:], op0=ALU.mult, op1=ALU.add), reads=["pa_f", "pb_f"], writes=["pb_f"])
                    sc.op("dve", lambda: DVE.tensor_tensor(out=gg[:], in0=best[:], in1=best[:, :, 0:1].to_broadcast([128, 8, 16]), op=ALU.subtract), reads=BK, writes=[gkey_])
                    sc.op("act", lambda: ACT.activation(out=gg[:], in_=gg[:], func=AF.Exp), reads=[gkey_], writes=[gkey_])
                    sc.op("dve", lambda: DVE.tensor_reduce(out=gsum[:], in_=gg[:], axis=AX.X, op=ALU.add), reads=[gkey_], writes=["gsum"])
                    sc.op("dve", lambda: DVE.reciprocal(gsum[:], gsum[:]), reads=["gsum"], writes=["gsum"])
                    sc.op("dve", lambda: DVE.tensor_tensor(out=gg[:], in0=gg[:], in1=gsum[:].unsqueeze(2).to_broadcast([128, 8, 16]), op=ALU.mult), reads=[gkey_, "gsum"], writes=[gkey_])
                    eq = work[:].rearrange("p (h k a) -> p h k a", k=16, a=16)
                    io4 = iota16[:].unsqueeze(1).unsqueeze(1).to_broadcast([128, 8, 16, 16])
                    for (pf, two, dst) in ((pa_f, 0, isel), (pb_f, 1, jsel)):
                        sc.op("dve", lambda: DVE.tensor_tensor(out=eq, in0=io4, in1=pf[:].unsqueeze(3).to_broadcast([128, 8, 16, 16]), op=ALU.is_equal),
                              reads=["pa_f", "pb_f"] + BK + PK, writes=WRKK, cost=2.2)
                        sc.op("dve", lambda: DVE.tensor_tensor(out=eq, in0=eq, in1=tix4[:, :, two, :].unsqueeze(2).to_broadcast([128, 8, 16, 16]), op=ALU.mult),
                              reads=WRKK + ["tixf"], writes=WRKK, cost=2.2)
                        sc.op("dve", lambda: DVE.tensor_reduce(out=dst[:], in_=eq, axis=AX.X, op=ALU.add), reads=WRKK, writes=["sel%d" % two], cost=2.2)
                    sc.op("dve", lambda: DVE.scalar_tensor_tensor(out=isel[:], in0=isel[:], scalar=128.0, in1=jsel[:], op0=ALU.mult, op1=ALU.add),
                          reads=["sel0", "sel1"], writes=["sel0"])
                    sc.op("dve", lambda: DVE.tensor_copy(idxi[:], isel[:].rearrange("p h k -> p (h k)")), reads=["sel0"], writes=[ikey])
                sc.barrier_b()
            sc.b_active = False

        def emit_A(b, c, par, give):
            t0 = c * C
            xT = xTs[par]
            xk = "xT%d" % par
            for j in range(C // 128):
                ts = slice(j * 128, (j + 1) * 128)
                h2tm = h2s[par][j]
                ggf = ggs[par][j][:].rearrange("p h k -> p (h k)")
                idxi = idxs[par][j]
                hkey, gkey_, ikey = "h2%d%d" % (par, j), "gg%d%d" % (par, j), "idx%d%d" % (par, j)
                for step in range(130):
                    hk = step
                    if hk < 128:
                        Gb = G[hk % NBUF]
                        gkey = "G%d" % (hk % NBUF)
                        sc.dma("pool", lambda: POOL.indirect_dma_start(out=Gb[:], out_offset=None, in_=uvb_d,
                                                                       in_offset=bass.IndirectOffsetOnAxis(ap=idxi[:, hk:hk + 1], axis=0)),
                               reads=[ikey, "uvscr"], writes=[gkey], ch=gkey, cost=1.9)
                        sc.op("dve", lambda: DVE.scalar_tensor_tensor(out=junkb[hk % 2], in0=Gb[:, 0:D], scalar=1.0, in1=h2tm[:], op0=ALU.mult, op1=ALU.mult, accum_out=dots[:, hk:hk + 1]),
                              reads=[gkey, hkey], writes=["dots%d" % hk, "junkb%d" % (hk % 2)], cost=1.3)
                        sc.op("act", lambda: ACT.activation(out=acts[:, hk:hk + 1], in_=dots[:, hk:hk + 1], func=AF.Gelu_apprx_tanh), reads=["dots%d" % hk], writes=["dots%d" % hk])
                    k1 = step - 1
                    if 0 <= k1 < 128:
                        sc.op("act", lambda: ACT.activation(out=zz[:, k1:k1 + 1], in_=acts[:, k1:k1 + 1], func=AF.Identity, scale=ggf[:, k1:k1 + 1]),
                              reads=["dots%d" % k1, gkey_], writes=["dots%d" % k1])
                    k2 = step - 2
                    if 0 <= k2 < 128:
                        Gp = G[k2 % NBUF]
                        gpkey = "G%d" % (k2 % NBUF)
                        dg = diag[k2 % 3]
                        dkey = "diag%d" % (k2 % 3)
                        sc.op("act", lambda: ACT.activation(out=dg[:], in_=ident_b[:], func=AF.Identity, scale=zz[:, k2:k2 + 1]),
                              reads=["dots%d" % k2], writes=[dkey])
                        for half in range(2):
                            bnk = 6 + half
                            sc.op("pe", lambda: PE.matmul(PS[bnk][:, :], dg[:], Gp[:, D + half * 512:D + (half + 1) * 512], start=(k2 == 0), stop=(k2 == 127)),
                                  reads=[dkey, gpkey], writes=["ps%d" % bnk])
                    give()
                sc.op("act", lambda: ACT.copy(petm[:, 0:512], PS[6][:, :]), reads=["ps6"], writes=["petm"])
                sc.op("dve", lambda: DVE.tensor_copy(petm[:, 512:1024], PS[7][:, :]), reads=["ps7"], writes=["petm"])
                for dc in range(DC):
                    bnk = 6 + dc // 4
                    sc.op("pe", lambda: PE.transpose(PS[bnk][:, (dc % 4) * 128:(dc % 4 + 1) * 128], petm[:, dc * 128:(dc + 1) * 128], ident_f[:]), reads=["petm"], writes=["ps%d" % bnk])
                for dc in range(DC):
                    bnk = 6 + dc // 4
                    sc.op("dve", lambda: DVE.scalar_tensor_tensor(out=xT[:, dc, ts], in0=PS[bnk][:, (dc % 4) * 128:(dc % 4 + 1) * 128], scalar=modT[:, 40 + dc, b:b + 1], in1=xT[:, dc, ts], op0=ALU.mult, op1=ALU.add),
                          reads=["ps%d" % bnk, xk], writes=[xk])
                give()
            rms_stats(xT[:], DC, 1.0 / D, xk, sq_t=sqA, rstd_t=rstdA, bank=6, sfx="A", sqkeys=["junkb0", "junkb1"])
            for dc in range(DC):
                ot = otmp[dc % 2]
                okey = "otmp%d" % (dc % 2)
                sc.op("dve", lambda: DVE.scalar_tensor_tensor(out=ot[:], in0=xT[:, dc, :], scalar=vcol(V_FG + dc), in1=rstdA[:], op0=ALU.mult, op1=ALU.mult),
                      reads=[xk, "rstdA"], writes=[okey])
                sc.dma("sp", lambda: SP.dma_start(out=outT_d[b, dc, :, t0:t0 + C], in_=ot[:]), reads=[okey], writes=["outd"], ch=okey)
            give()

        chunks = [(b, c) for b in range(nseq) for c in range(nch)]
        if overlap:
            co.no_pace = True
            co.start(lambda: emit_B(chunks[0][0], chunks[0][1], 0))
        uv_v = uv_d.rearrange("(p r) n -> p r n", p=128)
        uvb_v = uvb_d.rearrange("(p r) n -> p r n", p=128)
        for st_i in range(128):
            i2 = st_i % 2
            st = stage[:, i2 * 2048:(i2 + 1) * 2048]
            skey, bkey = "stage%d" % i2, "stb%d" % i2
            sc.dma("sp", lambda: SP.dma_start(out=st, in_=uv_v[:, st_i, :]), writes=[skey], ch=skey)
            if st_i % 2 == 0:
                sc.op("dve", lambda: DVE.tensor_copy(stb[i2][:], st), reads=[skey], writes=[bkey])
            else:
                sc.op("act", lambda: ACT.copy(stb[i2][:], st), reads=[skey], writes=[bkey])
            sc.dma("act", lambda: ACT.dma_start(out=uvb_v[:, st_i, :], in_=stb[i2][:]), reads=[bkey], writes=["uvscr"], ch=bkey + "u")
            if overlap:
                co.give(24)
        if overlap:
            co.drain()
            co.no_pace = False
        else:
            emit_B(chunks[0][0], chunks[0][1], 0)
        sc.barrier()
        pes.close()
        NBUF = 8
        G = [sb(es, "G%d" % i, [128, 2048], BF16) for i in range(NBUF)]
        junkb_t = sb(es, "junkb", [128, 2, D], BF16)
        junkb = [junkb_t[:, 0, :], junkb_t[:, 1, :]]
        diag = [sb(es, "diag%d" % i, [128, 128], BF16) for i in range(3)]
        dots = sb(es, "dots", [128, 128], F32)
        acts = dots
        zz = dots
        petm = sb(es, "petm", [128, D], F32)
        sqA = junkb_t[:, :, :].rearrange("p a (b c) -> p (a b) c", c=C)
        rstdA = sb(es, "rstdA", [128, C], F32)
        otmp = [sb(es, "otmp%d" % i, [128, C], F32) for i in range(2)]

        last_b_count = 2200
        for i, (b, c) in enumerate(chunks):
            par = i % 2
            if overlap and i + 1 < len(chunks):
                nb, ncn = chunks[i + 1]
                co.start(lambda nb=nb, ncn=ncn, par=par: emit_B(nb, ncn, 1 - par))
                q = 200
                emit_A(b, c, par, lambda q=q: co.give(q))
                last_b_count = max(co.count, 1) if co.done else last_b_count
                co.drain()
                last_b_count = max(co.count, 1)
            else:
                emit_A(b, c, par, lambda: None)
                if i + 1 < len(chunks):
                    nb, ncn = chunks[i + 1]
                    emit_B(nb, ncn, 1 - par)
        sc.finish()
    return nc


def _pack_inputs(inp, core, nseq=NSEQ):
    f32 = np.float32
    b0 = core * nseq
    x = inp["x"][b0:b0 + nseq]
    xT = np.ascontiguousarray(np.transpose(x, (0, 2, 1))).reshape(nseq, DC, 128, S)
    c = inp["c"][b0:b0 + nseq]
    cT = np.ascontiguousarray(c.T.reshape(DC, 128, nseq).transpose(1, 0, 2))
    pos = np.ascontiguousarray(inp["positions"][b0:b0 + nseq]).astype(np.int32)
    return {"xT": xT.astype(f32), "cT": cT.astype(f32), "pos": pos}


def _pack_weights(inp):
    f32 = np.float32
    col = lambda v, n: np.ascontiguousarray(np.asarray(v, f32).reshape(n, 128).T)
    vecs = np.zeros((128, NV), f32)
    vecs[:, V_N1G:V_N1G + 8] = col(inp["norm1_g"][0], 8)
    vecs[:, V_N2G:V_N2G + 8] = col(inp["norm2_g"][0], 8)
    vecs[:, V_FG:V_FG + 8] = col(inp["final_g"], 8)
    cw = np.asarray(inp["conv_w"][0], f32)
    for ci in range(4):
        for k in range(4):
            vecs[:, V_CONVW + ci * 4 + k] = cw[k, ci * 128:(ci + 1) * 128]
    vecs[:, V_CONVB:V_CONVB + 4] = col(inp["conv_b"][0], 4)
    vecs[:, V_BA:V_BA + 4] = col(inp["lru_ba"][0], 4)
    vecs[:, V_BX:V_BX + 4] = col(inp["lru_bx"][0], 4)
    vecs[:, V_LAM:V_LAM + 4] = col(inp["lru_lambda"][0], 4)
    vecs[:, V_QNG:V_QNG + 2] = col(inp["q_norm_g"][0], 2)
    vecs[:, V_KVNG:V_KVNG + 1] = col(inp["kv_norm_g"][0], 1)
    vecs[:, V_LOG:V_LOG + 4] = col(inp["lru_out_g"][0], 4)
    vecs[:, V_MOG:V_MOG + 4] = col(inp["mla_out_g"][0], 4)
    vecs[:, V_BADA:V_BADA + 48] = col(inp["b_ada"][0], 48)
    inv_freq = (1.0 / (10000.0 ** (np.arange(0, 32, 2, dtype=np.float32) / np.float32(32)))).astype(f32)
    for p in range(64, 96):
        vecs[p, V_INVF] = inv_freq[(p - 64) % 16]
        vecs[p, V_SGN] = -1.0 if p < 80 else 1.0
    w_in = np.asarray(inp["w_in"][0], f32)
    kr = w_in[:, 1408:1440]
    w_in_ext = np.concatenate([w_in, kr[:, 16:32], kr[:, 0:16]], axis=1)
    wa = np.asarray(inp["lru_wa"][0], f32)
    wx = np.asarray(inp["lru_wx"][0], f32)
    wa_bd = np.zeros((4, 128, 128), f32)
    wx_bd = np.zeros((4, 128, 128), f32)
    for ci in range(4):
        for s in range(2):
            wa_bd[ci, s * 64:(s + 1) * 64, s * 64:(s + 1) * 64] = wa[2 * ci + s]
            wx_bd[ci, s * 64:(s + 1) * 64, s * 64:(s + 1) * 64] = wx[2 * ci + s]
    w_uq = np.asarray(inp["w_uq"][0], f32)
    parts = []
    for h in range(8):
        blk = w_uq[:, h * 96:(h + 1) * 96]
        parts += [blk, blk[:, 0:64], blk[:, 80:96], blk[:, 64:80]]
    w_uq_ext = np.concatenate(parts, axis=1)
    keys = np.asarray(inp["peer_keys"][0], f32)
    keysT = np.ascontiguousarray(keys.reshape(16, 128, 128).transpose(2, 0, 1))
    uv = np.concatenate([np.asarray(inp["peer_u"][0], f32), np.asarray(inp["peer_v"][0], f32)], axis=1)
    return {
        "w_ada": np.ascontiguousarray(np.asarray(inp["w_ada"][0], f32).reshape(DC, 128, 6 * D)),
        "vecs": vecs,
        "w_in": np.ascontiguousarray(w_in_ext.reshape(DC, 128, WIN_COLS)),
        "wa_bd": wa_bd, "wx_bd": wx_bd,
        "w_uq": np.ascontiguousarray(w_uq_ext.reshape(2, 128, 1536)),
        "w_ukv": np.ascontiguousarray(np.asarray(inp["w_ukv"][0], f32)),
        "w_out": np.ascontiguousarray(np.asarray(inp["w_out"][0], f32).reshape(DC, 128, D)),
        "peer_wq": np.ascontiguousarray(np.asarray(inp["peer_wq"][0], f32).reshape(DC, 128, 2048)),
        "keysT": keysT,
        "uv": np.ascontiguousarray(uv),
    }


def kernel(**inputs):
    inp = {k: np.asarray(v) for k, v in inputs.items()}
    nc = build_program(NSEQ, NCH)
    wts = _pack_weights(inp)
    in_maps = []
    for core in range(NCORES):
        m = dict(wts)
        m.update(_pack_inputs(inp, core))
        in_maps.append(m)
    res = run_bass_kernel_spmd(nc, in_maps, core_ids=list(range(NCORES)))
    outs = []
    for core in range(NCORES):
        oT = np.asarray(res.results[core]["outT"]).reshape(NSEQ, D, S)
        outs.append(np.transpose(oT, (0, 2, 1)))
    return np.ascontiguousarray(np.concatenate(outs, axis=0)).astype(np.float32)
```

```python
from contextlib import ExitStack
import threading
import math
import numpy as np
import concourse.bass as bass
import concourse.mybir as mybir
from concourse.bass_utils import run_bass_kernel_spmd

F32 = mybir.dt.float32
BF16 = mybir.dt.bfloat16
I32 = mybir.dt.int32
U32 = mybir.dt.uint32
ALU = mybir.AluOpType
AF = mybir.ActivationFunctionType
AX = mybir.AxisListType

D = 1024
S = 2048
NCORES = 8
NSEQ = 4
C = 256
NCH = S // C
DC = 8
EPS = 1e-6
WIN_COLS = 1472
NEG = -1.0e30
TWO_PI = 2.0 * math.pi

V_N1G, V_N2G, V_FG = 0, 8, 16
V_CONVW, V_CONVB, V_BA, V_BX, V_LAM = 24, 40, 44, 48, 52
V_QNG, V_KVNG, V_LOG, V_MOG = 56, 58, 59, 63
V_BADA = 67
V_INVF, V_SGN = 115, 116
NV = 117


class Sched:
    ENG = ("pe", "act", "dve", "pool", "sp")

    def __init__(self, nc, es):
        self.nc = nc
        self.es = es
        self.eng = {"pe": nc.tensor, "act": nc.scalar, "dve": nc.vector, "pool": nc.gpsimd, "sp": nc.sync}
        self.sem = {e: es.enter_context(nc.semaphore("sem_" + e)) for e in self.ENG}
        self.cnt = {e: 0 for e in self.ENG}
        self.seen = {e: {} for e in self.ENG}
        self.last_w = {}
        self.readers = {}
        self.dsem = {}
        self.dcnt = {}
        self.dead = [False]
        self.co = None
        self.last_b = {e: 0 for e in self.ENG}
        self.b_dcnt = {}
        self.in_b = False
        self.b_active = False
        self.tail = {e: 0.0 for e in self.ENG}
        self.tw = {}
        self.tr = {}
        self.DUR = {"pe": 0.15, "act": 0.45, "dve": 0.3, "pool": 0.25, "sp": 0.1}
        self.LAT = 0.3
        self.MARGIN = {"pe": 24.0, "act": 18.0, "dve": 16.0, "pool": 6.0, "sp": 60.0}

    def _deps(self, reads, writes):
        need = {}
        def add(tok):
            k, v = tok
            if need.get(k, 0) < v:
                need[k] = v
        for k in list(reads) + list(writes):
            t = self.last_w.get(k)
            if t is not None:
                add(t)
        for k in writes:
            for tok in self.readers.get(k, {}).items():
                add(tok)
        return need

    def _emit_waits(self, e, need):
        eng = self.eng[e]
        seen = self.seen[e]
        for k, v in need.items():
            if k == e and e == "pe":
                continue
            if seen.get(k, 0) >= v:
                continue
            if k in self.sem:
                eng.wait_ge(self.sem[k], v)
            else:
                eng.wait_ge(self.dsem[k], v)
            seen[k] = v

    def _record(self, tok, reads, writes):
        for k in writes:
            self.last_w[k] = tok
            self.readers[k] = {}
        for k in reads:
            r = self.readers.setdefault(k, {})
            if r.get(tok[0], 0) < tok[1]:
                r[tok[0]] = tok[1]

    def _ready(self, reads, writes):
        t = 0.0
        for k in list(reads) + list(writes):
            v = self.tw.get(k)
            if v is not None and v > t:
                t = v
        for k in writes:
            v = self.tr.get(k)
            if v is not None and v > t:
                t = v
        return t

    def _model(self, e, reads, writes, dur, extra=0.0):
        start = max(self._ready(reads, writes) + self.LAT, self.tail[e])
        fin = start + dur
        self.tail[e] = fin
        for k in writes:
            self.tw[k] = fin + extra
            self.tr[k] = 0.0
        for k in reads:
            if self.tr.get(k, 0.0) < fin + extra:
                self.tr[k] = fin + extra

    def op(self, e, fn, reads=(), writes=(), cost=None):
        dur = self.DUR[e] if cost is None else cost
        if self.co is not None:
            self.co.tick(self, e, reads, writes, dur)
        self.in_b = threading.current_thread() is getattr(self, "in_b_thread", None) and self.b_active
        if self.in_b and getattr(self, "pend", None) and self.pend.get(e):
            self._emit_waits(e, self.pend[e])
            self.pend[e] = None
        self._model(e, reads, writes, dur)
        self._emit_waits(e, self._deps(reads, writes))
        ins = fn()
        ins.then_inc(self.sem[e], 1)
        self.cnt[e] += 1
        if self.in_b:
            self.last_b[e] = self.cnt[e]
        self._record((e, self.cnt[e]), reads, writes)

    def dma(self, e, fn, reads=(), writes=(), ch=None, cost=None):
        dur = 0.1 if cost is None else cost
        if self.co is not None:
            self.co.tick(self, e, reads, writes, dur)
        self.in_b = threading.current_thread() is getattr(self, "in_b_thread", None) and self.b_active
        if self.in_b and getattr(self, "pend", None) and self.pend.get(e):
            self._emit_waits(e, self.pend[e])
            self.pend[e] = None
        self._model(e, reads, writes, dur, extra=2.5)
        if ch not in self.dsem:
            self.dsem[ch] = self.es.enter_context(self.nc.semaphore("dsem_%d" % len(self.dsem)))
            self.dcnt[ch] = 0
        need = self._deps(reads, writes)
        if self.dcnt[ch]:
            need[ch] = max(need.get(ch, 0), self.dcnt[ch])
        self._emit_waits(e, need)
        ins = fn()
        ins.then_inc(self.dsem[ch], 16)
        self.dcnt[ch] += 16
        if self.in_b:
            self.b_dcnt[ch] = self.dcnt[ch]
        self._record((ch, self.dcnt[ch]), reads, writes)

    def barrier_b(self):
        need = {e: v for e, v in self.last_b.items() if v}
        need.update({ch: v for ch, v in self.b_dcnt.items() if v})
        self.pend = {e: dict(need) for e in self.ENG}

    def barrier(self):
        need = {e: self.cnt[e] for e in self.ENG if self.cnt[e]}
        for ch, v in self.dcnt.items():
            if v:
                need[ch] = v
        for e in self.ENG:
            self._emit_waits(e, dict(need))
        self.last_w = {}
        self.readers = {}

    def finish(self):
        need = {e: self.cnt[e] for e in self.ENG if self.cnt[e]}
        for ch, v in self.dcnt.items():
            if v:
                need[ch] = v
        self._emit_waits("sp", need)


class Co:
    def __init__(self):
        self.thread = None
        self.quota = 0
        self.b_go = threading.Semaphore(0)
        self.m_go = threading.Semaphore(0)
        self.done = True
        self.exc = None
        self.count = 0
        self.free_run = False
        self.SLACK = 0.3

    def start(self, fn):
        self.done = False
        self.count = 0
        self.exc = None

        def run():
            self.b_go.acquire()
            try:
                fn()
            except BaseException as e:
                self.exc = e
            self.done = True
            self.m_go.release()
        self.thread = threading.Thread(target=run)
        self.thread.start()

    def give(self, q):
        if self.done:
            return
        self.quota = q
        self.free_run = q >= (1 << 50)
        self.b_go.release()
        self.m_go.acquire()
        if self.exc is not None:
            raise self.exc

    def tick(self, sched=None, e=None, reads=(), writes=(), dur=0.0):
        if self.thread is not None and threading.current_thread() is self.thread:
            self.count += 1
            while not getattr(self, "free_run", False):
                self.quota -= 1
                blocked = False
                if sched is not None and not getattr(self, "no_pace", False):
                    fin = max(sched._ready(reads, writes) + sched.LAT, sched.tail[e]) + dur
                    blocked = fin > sched.tail["pool"] + sched.MARGIN[e]
                if self.quota >= 0 and not blocked:
                    break
                self.m_go.release()
                self.b_go.acquire()

    def drain(self):
        while not self.done:
            self.give(1 << 60)
        if self.thread is not None:
            self.thread.join()
            self.thread = None
        if self.exc is not None:
            raise self.exc


def build_program(nseq=NSEQ, nch=NCH, stop=99, overlap=True):
    nc = bass.Bass("TRN2", target_bir_lowering=False)
    dr = lambda name, shape, dt, kind="ExternalInput": nc.dram_tensor(name, shape, dt, kind=kind).ap()
    xT_d = dr("xT", [nseq, DC, 128, S], F32)
    cT_d = dr("cT", [128, DC, nseq], F32)
    pos_d = dr("pos", [nseq, S], I32)
    wada_d = dr("w_ada", [DC, 128, 6 * D], F32)
    vecs_d = dr("vecs", [128, NV], F32)
    win_d = dr("w_in", [DC, 128, WIN_COLS], F32)
    wabd_d = dr("wa_bd", [4, 128, 128], F32)
    wxbd_d = dr("wx_bd", [4, 128, 128], F32)
    wuq_d = dr("w_uq", [2, 128, 1536], F32)
    wukv_d = dr("w_ukv", [128, 1024], F32)
    wout_d = dr("w_out", [DC, 128, D], F32)
    wq_d = dr("peer_wq", [DC, 128, 2048], F32)
    keysT_d = dr("keysT", [128, 16, 128], F32)
    uv_d = dr("uv", [16384, 2048], F32)
    outT_d = dr("outT", [nseq, DC, 128, S], F32, kind="ExternalOutput")
    winb_d = dr("winb", [13, 128, DC, 128], BF16, kind="Internal")
    woutb_d = dr("woutb", [8, 128, DC, 128], BF16, kind="Internal")
    wqb_d = dr("wqb", [16, 128, DC, 128], BF16, kind="Internal")
    uvb_d = dr("uvb", [16384, 2048], BF16, kind="Internal")

    with ExitStack() as es:
        sc = Sched(nc, es)
        co = Co()
        sc.co = co
        PE, ACT, DVE, POOL, SP = nc.tensor, nc.scalar, nc.vector, nc.gpsimd, nc.sync
        uid = [0]

        def sb(stack, name, shape, dt):
            uid[0] += 1
            return stack.enter_context(nc.sbuf_tensor("%s_%d" % (name, uid[0]), shape, dt))

        w_uq = sb(es, "w_uq", [128, 2, 1536], BF16)
        w_ukv = sb(es, "w_ukv", [128, 1024], BF16)
        keysT = sb(es, "keysT", [128, 16, 128], F32)
        wa_bd = sb(es, "wa_bd", [128, 4, 128], BF16)
        wx_bd = sb(es, "wx_bd", [128, 4, 128], BF16)
        vecs = sb(es, "vecs", [128, NV], F32)
        modT = sb(es, "modT", [128, 48, nseq], F32)
        A1 = sb(es, "A1", [128, DC, nseq], F32)
        A2 = sb(es, "A2", [128, DC, nseq], F32)
        nsp = sb(es, "nsp", [128, 4], F32)
        consts = sb(es, "consts", [128, 4], F32)
        ones_bf = sb(es, "ones_bf", [128, 128], BF16)
        ident_f = sb(es, "ident_f", [128, 128], F32)
        ident_b = sb(es, "ident_b", [128, 128], BF16)
        tri_b = sb(es, "tri_b", [128, 128], BF16)
        iota16 = sb(es, "iota16", [128, 16], F32)
        KT = sb(es, "KT", [96, 8, S], BF16)
        VC = sb(es, "VC", [128, S // 128, 8, 65], BF16)
        xl = sb(es, "xl", [128, 4, C + 3], F32)
        hst = sb(es, "hst", [128, 4], F32)
        xTs = [sb(es, "xT%d" % i, [128, DC, C], F32) for i in range(2)]
        hT = sb(es, "hT", [128, DC, C], BF16)
        sq = sb(es, "sq", [128, DC, C], BF16)
        rstd = sb(es, "rstd", [128, C], F32)
        tmpf = sb(es, "tmpf", [128, C], F32)
        wr = [sb(es, "wr%d" % i, [128, DC, 128], BF16) for i in range(4)]
        idxs = [[sb(es, "idx%d%d" % (p, j), [128, 128], I32) for j in range(2)] for p in range(2)]
        ggs = [[sb(es, "gg%d%d" % (p, j), [128, 8, 16], F32) for j in range(2)] for p in range(2)]
        h2s = [[sb(es, "h2%d%d" % (p, j), [128, D], BF16) for j in range(2)] for p in range(2)]

        PS = [es.enter_context(nc.psum_tensor("ps%d" % i, [128, 512], F32)) for i in (0, 1, 2)]
        PS3 = es.enter_context(nc.psum_tensor("ps3", [128, 1024], BF16))
        PS += [None] + [es.enter_context(nc.psum_tensor("ps%d" % i, [128, 512], F32)) for i in (4, 5, 6, 7)]

        def vcol(c0, n=1):
            return vecs[:, c0:c0 + n]

        pes = ExitStack()
        if True:
            stage = sb(pes, "stage", [128, 4096], F32)
            cT = sb(pes, "cT", [128, DC, nseq], F32)
            iot_i = sb(pes, "iot_i", [128, 128], I32)
            iot_f = sb(pes, "iot_f", [128, 128], F32)
            sc.dma("sp", lambda: SP.dma_start(out=vecs[:], in_=vecs_d[:, :]), writes=["vecs"], ch="vecs")
            sc.dma("sp", lambda: SP.dma_start(out=cT[:], in_=cT_d[:, :, :]), writes=["cT"], ch="cT")
            sc.dma("sp", lambda: SP.dma_start(out=keysT[:], in_=keysT_d[:, :, :]), writes=["keysT"], ch="keysT")
            sc.op("dve", lambda: DVE.memset(consts[:, 0:1], EPS), writes=["consts"])
            sc.op("dve", lambda: DVE.memset(consts[:, 1:2], 1.0), writes=["consts"])
            sc.op("dve", lambda: DVE.memset(consts[:, 2:3], 0.0), writes=["consts"])
            sc.op("dve", lambda: DVE.memset(ones_bf[:], 1.0), writes=["ones_bf"])
            sc.op("dve", lambda: DVE.memset(VC[:], 1.0), writes=["VC"])
            sc.op("dve", lambda: DVE.memset(KT[:], 0.0), writes=["KT"])
            sc.op("pool", lambda: POOL.iota(iot_i[:], pattern=[[1, 128]], base=0, channel_multiplier=-1), writes=["iot_i"])
            sc.op("dve", lambda: DVE.tensor_copy(iot_f[:], iot_i[:]), reads=["iot_i"], writes=["iot_f"])
            sc.op("dve", lambda: DVE.tensor_scalar(ident_f[:], iot_f[:], 0.0, None, op0=ALU.is_equal), reads=["iot_f"], writes=["ident_f"])
            sc.op("dve", lambda: DVE.tensor_copy(ident_b[:], ident_f[:]), reads=["ident_f"], writes=["ident_b"])
            sc.op("dve", lambda: DVE.tensor_scalar(tri_b[:], iot_f[:], 0.0, None, op0=ALU.is_ge), reads=["iot_f"], writes=["tri_b"])
            sc.op("pool", lambda: POOL.iota(iot_i[:, 0:16], pattern=[[1, 16]], base=0, channel_multiplier=0), reads=["iot_f"], writes=["iot_i"])
            sc.op("dve", lambda: DVE.tensor_copy(iota16[:], iot_i[:, 0:16]), reads=["iot_i"], writes=["iota16"])

            stb = [sb(pes, "stb%d" % i, [128, 2048], BF16) for i in range(2)]

            def cast_op(k, dst_ap, st, key, wkey):
                eng = ("dve", "act", "pool")[k % 3]
                if eng == "dve":
                    sc.op("dve", lambda: DVE.tensor_copy(dst_ap, st), reads=[key], writes=[wkey])
                elif eng == "act":
                    sc.op("act", lambda: ACT.copy(dst_ap, st), reads=[key], writes=[wkey])
                else:
                    sc.op("pool", lambda: POOL.tensor_copy(dst_ap, st), reads=[key], writes=[wkey])

            def load_cast(dst_ap, src_ap, ncols, k, outs=None):
                st = stage[:, 0:ncols] if k % 2 == 0 else stage[:, 2048:2048 + ncols]
                key = "stage%d" % (k % 2)
                sc.dma("sp", lambda: SP.dma_start(out=st, in_=src_ap), writes=[key], ch=key)
                if outs is None:
                    cast_op(k, dst_ap, st, key, "W")
                    return
                bkey = "stb%d" % (k % 2)
                sbt = stb[k % 2]
                cast_op(k, sbt[:, 0:ncols], st, key, bkey)
                for oi, (d_ap, s_ap) in enumerate(outs(sbt)):
                    sc.dma("sp", lambda: SP.dma_start(out=d_ap, in_=s_ap), reads=[bkey], writes=["wscr"], ch="%so%d" % (bkey, oi))
            k = 0
            for dc in range(DC):
                load_cast(None, win_d[dc], WIN_COLS, k, outs=lambda t, dc=dc: [
                    (winb_d[0:11, :, dc, :].rearrange("oc p n -> p oc n"), t[:, 0:1408].rearrange("p (oc n) -> p oc n", n=128)),
                    (winb_d[11, :, dc, 0:96], t[:, 1344:1440]),
                    (winb_d[12, :, dc, 0:96], t[:, 1376:1472])]); k += 1
                load_cast(None, wout_d[dc], D, k, outs=lambda t, dc=dc: [
                    (woutb_d[:, :, dc, :].rearrange("oc p n -> p oc n"), t[:, 0:1024].rearrange("p (oc n) -> p oc n", n=128))]); k += 1
                load_cast(None, wq_d[dc], 2048, k, outs=lambda t, dc=dc: [
                    (wqb_d[:, :, dc, :].rearrange("oc p n -> p oc n"), t[:, 0:2048].rearrange("p (oc n) -> p oc n", n=128))]); k += 1
            for kc in range(2):
                load_cast(w_uq[:, kc, :], wuq_d[kc], 1536, k); k += 1
            load_cast(w_ukv[:, :], wukv_d[:, :], 1024, k); k += 1
            for ci in range(4):
                load_cast(wa_bd[:, ci, :], wabd_d[ci], 128, k); k += 1
                load_cast(wx_bd[:, ci, :], wxbd_d[ci], 128, k); k += 1

            sc.op("act", lambda: ACT.activation(out=nsp[:], in_=vcol(V_LAM, 4), func=AF.Exp, scale=-1.0), reads=["vecs"], writes=["nsp"])
            sc.op("act", lambda: ACT.activation(out=nsp[:], in_=nsp[:], func=AF.Ln, bias=consts[:, 1:2], scale=1.0), reads=["nsp", "consts"], writes=["nsp"])
            sc.op("dve", lambda: DVE.tensor_scalar(nsp[:], nsp[:], -16.0, None, op0=ALU.mult), reads=["nsp"], writes=["nsp"])

            sc.op("act", lambda: ACT.activation(out=cT[:], in_=cT[:], func=AF.Silu), reads=["cT"], writes=["cT"])
            ps_mod = PS[0][:, 0:48 * nseq].rearrange("p (n b) -> p n b", b=nseq)
            for n in range(48):
                key = "stage%d" % (n % 2)
                st = stage[:, (n % 2) * 2048:(n % 2) * 2048 + 1024].rearrange("p (dc n) -> p dc n", n=128)
                sc.dma("sp", lambda: SP.dma_start(out=st, in_=wada_d[:, :, n * 128:(n + 1) * 128].rearrange("dc p n -> p dc n")), writes=[key], ch=key)
                for dc in range(DC):
                    sc.op("pe", lambda: PE.matmul(ps_mod[:, n, :], st[:, dc, :], cT[:, dc, :], start=(dc == 0), stop=(dc == DC - 1)),
                          reads=[key, "cT"], writes=["ps0"])
            sc.op("dve", lambda: DVE.tensor_tensor(out=modT[:], in0=ps_mod, in1=vcol(V_BADA, 48).unsqueeze(2).to_broadcast([128, 48, nseq]), op=ALU.add),
                  reads=["ps0", "vecs"], writes=["modT"])
            for dc in range(DC):
                sc.op("dve", lambda: DVE.tensor_scalar(A1[:, dc, :], modT[:, 8 + dc, :], 1.0, vcol(V_N1G + dc), op0=ALU.add, op1=ALU.mult),
                      reads=["modT", "vecs"], writes=["A1"])
                sc.op("dve", lambda: DVE.tensor_scalar(A2[:, dc, :], modT[:, 32 + dc, :], 1.0, vcol(V_N2G + dc), op0=ALU.add, op1=ALU.mult),
                      reads=["modT", "vecs"], writes=["A2"])
            sc.barrier()

        def rms_stats(src_tile, nchunks, inv_n, srckey, sq_t=None, rstd_t=None, bank=0, sfx="", sqkeys=None):
            sq_t = sq if sq_t is None else sq_t
            rstd_t = rstd if rstd_t is None else rstd_t
            sk, rk, pk = "sq" + sfx, "rstd" + sfx, "ps%d" % bank
            sks = [sk] if sqkeys is None else list(sqkeys)
            srckeys = list(srckey) if isinstance(srckey, (list, tuple)) else [srckey]
            sc.op("act", lambda: ACT.activation(out=sq_t[:, 0:nchunks, :], in_=src_tile, func=AF.Square), reads=srckeys, writes=sks, cost=0.25 + 0.21 * nchunks)
            for i in range(nchunks):
                sc.op("pe", lambda: PE.matmul(PS[bank][:, 0:C], ones_bf[:], sq_t[:, i, :], start=(i == 0), stop=(i == nchunks - 1)),
                      reads=sks, writes=[pk])
            sc.op("act", lambda: ACT.activation(out=rstd_t[:], in_=PS[bank][:, 0:C], func=AF.Sqrt, bias=consts[:, 0:1], scale=inv_n), reads=[pk], writes=[rk])
            sc.op("dve", lambda: DVE.reciprocal(rstd_t[:], rstd_t[:]), reads=[rk], writes=[rk])

        def modulated_norm(xT, xk, Acol, shift_chunk0, b):
            rms_stats(xT[:], DC, 1.0 / D, xk)
            for dc in range(DC):
                sc.op("dve", lambda: DVE.scalar_tensor_tensor(out=tmpf[:], in0=xT[:, dc, :], scalar=Acol[:, dc, b:b + 1], in1=rstd[:], op0=ALU.mult, op1=ALU.mult),
                      reads=[xk, "rstd"], writes=["tmpf"])
                sc.op("act", lambda: ACT.activation(out=hT[:, dc, :], in_=tmpf[:], func=AF.Identity, bias=modT[:, shift_chunk0 + dc, b:b + 1], scale=1.0),
                      reads=["tmpf"], writes=["hT"])

        gen_i = [0]

        def gen_ps():
            bnk = (1, 2)[gen_i[0] % 2]
            gen_i[0] += 1
            return PS[bnk][:, 0:C], "ps%d" % bnk

        ring_i = [0]

        def wload(src_ap, ncols=128):
            i = ring_i[0] % 4
            ring_i[0] += 1
            key = "wr%d" % i
            sc.dma("sp", lambda: SP.dma_start(out=wr[i][:, :, 0:ncols], in_=src_ap), reads=["wscr"], writes=[key], ch=key)
            return wr[i], key

        class WStream:
            def __init__(self, blocks, depth=3):
                self.blocks = list(blocks)
                self.pend = []
                self.depth = depth
                for _ in range(depth):
                    self._issue()

            def _issue(self):
                if self.blocks:
                    src, ncols = self.blocks.pop(0)
                    self.pend.append(wload(src, ncols))

            def next(self):
                w, key = self.pend.pop(0)
                self._issue()
                return w, key

        def emit_B(b, c, par):
            sc.in_b_thread = threading.current_thread()
            sc.b_active = True
            t0 = c * C
            xT = xTs[par]
            xk = "xT%d" % par
            if c == 0:
                sc.op("dve", lambda: DVE.memset(xl[:], 0.0), writes=["xl0", "xl1", "xl2", "xl3"])
                sc.op("dve", lambda: DVE.memset(hst[:], 0.0), writes=["hst0", "hst1", "hst2", "hst3"])
            with ExitStack() as mes:
                gT = sb(mes, "gT", [128, 4, C], F32)
                qlat = sb(mes, "qlat", [128, 2, C], F32)
                kvlat = sb(mes, "kvlat", [128, C], F32)
                qs = sb(mes, "qs", [128, 2, C], BF16)
                kvs = sb(mes, "kvs", [128, C], BF16)
                xc = sb(mes, "xc", [128, C], F32)
                xcb = sb(mes, "xcb", [128, C], BF16)
                ra = sb(mes, "ra", [128, C], F32)
                ib = sb(mes, "ib", [128, C], F32)
                hh = sb(mes, "hh", [128, C], F32)
                xc2 = sb(mes, "xc2", [128, C], F32)
                xcb2 = sb(mes, "xcb2", [128, C], BF16)
                ra2 = sb(mes, "ra2", [128, C], F32)
                ib2 = sb(mes, "ib2", [128, C], F32)
                hh2 = sb(mes, "hh2", [128, C], F32)
                ylru = sb(mes, "ylru", [128, 4, C], F32)
                yT = sb(mes, "yT", [128, 8, C], BF16)
                QT = sb(mes, "QT", [96, 8, C], BF16)
                posi = sb(mes, "posi", [96, C], I32)
                ang = sb(mes, "ang", [96, C], F32)
                kf = sb(mes, "kf", [96, C], F32)
                cos2 = sb(mes, "cos2", [96, C], F32)
                sin2 = sb(mes, "sin2", [96, C], F32)
                t1 = tmpf
                t2 = sb(mes, "t2", [96, C], F32)
                krb = sb(mes, "krb", [96, C], BF16)
                pT = [sb(mes, "pT%d" % i, [128, C], BF16) for i in range(3)]
                ymla = sb(mes, "ymla", [128, 2, 512], F32)
                ymn = sb(mes, "ymn", [128, 2, 512], BF16)
                rinv = sb(mes, "rinv", [128, 2], F32)
                sst = sb(mes, "sst", [128, 2], F32)

                win_blocks = [(winb_d[oc, :, :, :], 128) for oc in range(11)] + [(winb_d[11, :, :, 0:96], 96), (winb_d[12, :, :, 0:96], 96)]
                wst = WStream(win_blocks)
                sc.dma("sp", lambda: SP.dma_start(out=xT[:], in_=xT_d[b, :, :, t0:t0 + C].rearrange("dc p t -> p dc t")), writes=[xk], ch=xk)
                sc.dma("sp", lambda: SP.dma_start(out=posi[64:96, :], in_=pos_d[b:b + 1, t0:t0 + C].partition_broadcast(32)), writes=["posi"], ch="posi")
                modulated_norm(xT, xk, A1, 0, b)

                R = slice(64, 96)
                sc.op("dve", lambda: DVE.tensor_copy(ang[R, :], posi[R, :]), reads=["posi"], writes=["ang"])
                sc.op("dve", lambda: DVE.tensor_scalar(ang[R, :], ang[R, :], vecs[R, V_INVF:V_INVF + 1], None, op0=ALU.mult), reads=["ang", "vecs"], writes=["ang"])
                for shift, dst, use_sgn in ((0.0, sin2, True), (math.pi / 2, cos2, False)):
                    sc.op("dve", lambda: DVE.tensor_scalar(kf[R, :], ang[R, :], shift, 1.0 / TWO_PI, op0=ALU.add, op1=ALU.mult), reads=["ang"], writes=["kf"])
                    sc.op("dve", lambda: DVE.tensor_copy(posi[R, :], kf[R, :]), reads=["kf"], writes=["posi"])
                    sc.op("dve", lambda: DVE.tensor_copy(kf[R, :], posi[R, :]), reads=["posi"], writes=["kf"])
                    sc.op("dve", lambda: DVE.scalar_tensor_tensor(out=kf[R, :], in0=kf[R, :], scalar=-TWO_PI, in1=ang[R, :], op0=ALU.mult, op1=ALU.add),
                          reads=["kf", "ang"], writes=["kf"])
                    sc.op("dve", lambda: DVE.tensor_scalar(kf[R, :], kf[R, :], shift, None, op0=ALU.add), reads=["kf"], writes=["kf"])
                    sc.op("dve", lambda: DVE.tensor_scalar(kf[R, :], kf[R, :], 3.1415925, -3.1415925, op0=ALU.min, op1=ALU.max), reads=["kf"], writes=["kf"])
                    if use_sgn:
                        sc.op("act", lambda: ACT.activation(out=dst[R, :], in_=kf[R, :], func=AF.Sin, scale=vecs[R, V_SGN:V_SGN + 1]), reads=["kf", "vecs"], writes=["rope"])
                    else:
                        sc.op("act", lambda: ACT.activation(out=dst[R, :], in_=kf[R, :], func=AF.Sin), reads=["kf"], writes=["rope"])
                ROPE = ["rope"]

                def inproj(ncols=128):
                    w, wkey = wst.next()
                    ps, key = gen_ps()
                    for dc in range(DC):
                        sc.op("pe", lambda: PE.matmul(ps[0:ncols, :], w[:, dc, 0:ncols], hT[:, dc, :], start=(dc == 0), stop=(dc == DC - 1)),
                              reads=["hT", wkey], writes=[key])
                    return ps, key
                for ci in range(4):
                    ps, key = inproj()
                    sc.op("act", lambda: ACT.copy(xl[:, ci, 3:3 + C], ps), reads=[key], writes=["xl%d" % ci])
                for ci in range(4):
                    ps, key = inproj()
                    sc.op("act", lambda: ACT.activation(out=gT[:, ci, :], in_=ps, func=AF.Gelu_apprx_tanh), reads=[key], writes=["gT"])
                for kc in range(2):
                    ps, key = inproj()
                    sc.op("dve", lambda: DVE.tensor_copy(qlat[:, kc, :], ps), reads=[key], writes=["qlat"])
                ps, key = inproj()
                sc.op("dve", lambda: DVE.tensor_copy(kvlat[:], ps), reads=[key], writes=["kvlat"])
                ps_kr, key_kr = inproj(96)
                ps_krr, key_krr = inproj(96)
                sc.op("dve", lambda: DVE.tensor_tensor(out=t1[R, :], in0=ps_kr[R, :], in1=cos2[R, :], op=ALU.mult), reads=[key_kr] + ROPE, writes=["tmpf"])
                sc.op("dve", lambda: DVE.tensor_tensor(out=t2[R, :], in0=ps_krr[R, :], in1=sin2[R, :], op=ALU.mult), reads=[key_krr] + ROPE, writes=["t2"])
                sc.op("dve", lambda: DVE.tensor_tensor(out=krb[R, :], in0=t1[R, :], in1=t2[R, :], op=ALU.add), reads=["tmpf", "t2"], writes=["krb"])
                for h in range(8):
                    if h % 2 == 0:
                        sc.op("act", lambda: ACT.copy(KT[R, h, t0:t0 + C], krb[R, :]), reads=["krb"], writes=["KT"])
                    else:
                        sc.op("dve", lambda: DVE.tensor_copy(KT[R, h, t0:t0 + C], krb[R, :]), reads=["krb"], writes=["KT"])

                def lru_stages(ci, B_, sfx, banks):
                    xc_, xcb_, ra_, ib_, hh_ = B_
                    kxc, kxcb, kra, kib, khh = ["%s%s" % (n, sfx) for n in ("xc", "xcb", "ra", "ib", "hh")]
                    xk_ = "xl%d" % ci
                    cw = V_CONVW + ci * 4
                    ps_r, key_r = PS[banks[0]][:, 0:C], "ps%d" % banks[0]
                    ps_i, key_i = PS[banks[1]][:, 0:C], "ps%d" % banks[1]
                    st = []
                    st.append(lambda: sc.op("dve", lambda: DVE.tensor_scalar(xc_[:], xl[:, ci, 0:C], vcol(cw), vcol(V_CONVB + ci), op0=ALU.mult, op1=ALU.add),
                                            reads=[xk_, "vecs"], writes=[kxc]))
                    for kk in range(1, 4):
                        st.append(lambda kk=kk: sc.op("dve", lambda: DVE.scalar_tensor_tensor(out=xc_[:], in0=xl[:, ci, kk:kk + C], scalar=vcol(cw + kk), in1=xc_[:], op0=ALU.mult, op1=ALU.add),
                                                      reads=[xk_, kxc], writes=[kxc]))
                    st.append(lambda: sc.op("act", lambda: ACT.copy(xl[:, ci, 0:3], xl[:, ci, C:C + 3]), reads=[xk_, kxc], writes=[xk_]))
                    st.append(lambda: sc.op("act", lambda: ACT.copy(xcb_[:], xc_[:]), reads=[kxc], writes=[kxcb]))
                    st.append(lambda: sc.op("pe", lambda: PE.matmul(ps_r, wa_bd[:, ci, :], xcb_[:], start=True, stop=True), reads=[kxcb], writes=[key_r]))
                    st.append(lambda: sc.op("pe", lambda: PE.matmul(ps_i, wx_bd[:, ci, :], xcb_[:], start=True, stop=True), reads=[kxcb], writes=[key_i]))
                    st.append(lambda: sc.op("act", lambda: ACT.activation(out=ra_[:], in_=ps_r, func=AF.Sigmoid, bias=vcol(V_BA + ci), scale=1.0), reads=[key_r], writes=[kra]))
                    st.append(lambda: sc.op("act", lambda: ACT.activation(out=ib_[:], in_=ps_i, func=AF.Sigmoid, bias=vcol(V_BX + ci), scale=1.0), reads=[key_i], writes=[kib]))
                    st.append(lambda: sc.op("act", lambda: ACT.activation(out=hh_[:], in_=ra_[:], func=AF.Exp, scale=nsp[:, ci:ci + 1]), reads=[kra], writes=[khh]))
                    st.append(lambda: sc.op("dve", lambda: DVE.tensor_scalar(ra_[:], ra_[:], nsp[:, ci:ci + 1], 0.5, op0=ALU.mult, op1=ALU.mult), reads=[kra, khh], writes=[kra]))
                    st.append(lambda: sc.op("act", lambda: ACT.activation(out=ra_[:], in_=ra_[:], func=AF.Exp), reads=[kra], writes=[kra]))
                    st.append(lambda: sc.op("act", lambda: ACT.activation(out=hh_[:], in_=hh_[:], func=AF.Sqrt, bias=consts[:, 1:2], scale=-1.0), reads=[khh], writes=[khh]))
                    st.append(lambda: sc.op("dve", lambda: DVE.tensor_tensor(out=ib_[:], in0=ib_[:], in1=xc_[:], op=ALU.mult), reads=[kib, kxc], writes=[kib]))
                    st.append(lambda: sc.op("dve", lambda: DVE.tensor_tensor(out=ib_[:], in0=ib_[:], in1=hh_[:], op=ALU.mult), reads=[kib, khh], writes=[kib]))
                    st.append(lambda: sc.op("dve", lambda: DVE.tensor_tensor_scan(out=hh_[:], data0=ra_[:], data1=ib_[:], initial=hst[:, ci:ci + 1], op0=ALU.mult, op1=ALU.add),
                                            reads=[kra, kib, "hst%d" % ci], writes=[khh], cost=0.6))
                    st.append(lambda: sc.op("dve", lambda: DVE.tensor_copy(hst[:, ci:ci + 1], hh_[:, C - 1:C]), reads=[khh], writes=["hst%d" % ci]))
                    st.append(lambda: sc.op("dve", lambda: DVE.tensor_tensor(out=ylru[:, ci, :], in0=hh_[:], in1=gT[:, ci, :], op=ALU.mult), reads=[khh, "gT"], writes=["ylru%d" % ci]))
                    return st
                setA = (xc, xcb, ra, ib, hh)
                setB = (xc2, xcb2, ra2, ib2, hh2)
                for pair in ((0, 1), (2, 3)):
                    sa = lru_stages(pair[0], setA, "", (1, 2))
                    sb_ = lru_stages(pair[1], setB, "b", (4, 5))
                    for fa, fb in zip(sa, sb_):
                        fa()
                        fb()
                rms_stats(ylru[:], 4, 1.0 / 512, ["ylru0", "ylru1", "ylru2", "ylru3"])
                for ci in range(4):
                    sc.op("dve", lambda: DVE.scalar_tensor_tensor(out=yT[:, ci, :], in0=ylru[:, ci, :], scalar=vcol(V_LOG + ci), in1=rstd[:], op0=ALU.mult, op1=ALU.mult),
                          reads=["ylru%d" % ci, "rstd"], writes=["yT"])

                rms_stats(qlat[:], 2, 1.0 / 256, "qlat")
                for kc in range(2):
                    sc.op("dve", lambda: DVE.scalar_tensor_tensor(out=qs[:, kc, :], in0=qlat[:, kc, :], scalar=vcol(V_QNG + kc), in1=rstd[:], op0=ALU.mult, op1=ALU.mult),
                          reads=["qlat", "rstd"], writes=["qs"])
                rms_stats(kvlat[:].unsqueeze(1), 1, 1.0 / 128, "kvlat")
                sc.op("dve", lambda: DVE.scalar_tensor_tensor(out=kvs[:], in0=kvlat[:], scalar=vcol(V_KVNG), in1=rstd[:], op0=ALU.mult, op1=ALU.mult),
                      reads=["kvlat", "rstd"], writes=["kvs"])
                for h in range(8):
                    ps_q, key_q = gen_ps()
                    ps_qr, key_qr = gen_ps()
                    for kc in range(2):
                        sc.op("pe", lambda: PE.matmul(ps_q[0:96, :], w_uq[:, kc, h * 192:h * 192 + 96], qs[:, kc, :], start=(kc == 0), stop=(kc == 1)), reads=["qs"], writes=[key_q])
                    for kc in range(2):
                        sc.op("pe", lambda: PE.matmul(ps_qr[0:96, :], w_uq[:, kc, h * 192 + 96:h * 192 + 192], qs[:, kc, :], start=(kc == 0), stop=(kc == 1)), reads=["qs"], writes=[key_qr])
                    sc.op("act", lambda: ACT.copy(QT[0:64, h, :], ps_q[0:64, :]), reads=[key_q], writes=["QT"])
                    sc.op("dve", lambda: DVE.tensor_tensor(out=t1[R, :], in0=ps_q[R, :], in1=cos2[R, :], op=ALU.mult), reads=[key_q] + ROPE, writes=["tmpf"])
                    sc.op("dve", lambda: DVE.tensor_tensor(out=t2[R, :], in0=ps_qr[R, :], in1=sin2[R, :], op=ALU.mult), reads=[key_qr] + ROPE, writes=["t2"])
                    sc.op("dve", lambda: DVE.tensor_tensor(out=QT[R, h, :], in0=t1[R, :], in1=t2[R, :], op=ALU.add), reads=["tmpf", "t2"], writes=["QT"])
                    ps_k, key_k = gen_ps()
                    sc.op("pe", lambda: PE.matmul(ps_k[0:64, :], w_ukv[:, h * 128:h * 128 + 64], kvs[:], start=True, stop=True), reads=["kvs"], writes=[key_k])
                    sc.op("act", lambda: ACT.copy(KT[0:64, h, t0:t0 + C], ps_k[0:64, :]), reads=[key_k], writes=["KT"])
                wv = w_ukv[:, :].rearrange("p (h x) -> p h x", x=128)[:, :, 64:128]
                for j in range(C // 128):
                    tile_i = (t0 // 128) + j
                    sc.op("pe", lambda: PE.matmul(PS[4][:, :].rearrange("p (h x) -> p h x", x=64), kvs[:, j * 128:(j + 1) * 128], wv, start=True, stop=True),
                          reads=["kvs"], writes=["ps4"])
                    sc.op("dve", lambda: DVE.tensor_copy(VC[:, tile_i, :, 0:64], PS[4][:, :].rearrange("p (h x) -> p h x", x=64)), reads=["ps4"], writes=["VC"])

                wso = WStream([(woutb_d[oc, :, :, :], 128) for oc in range(8)])

                scale = 96.0 ** -0.5
                nkt = (t0 + C) // 128
                kdiag0 = t0 // 128
                it = 0
                abk = (1, 2)
                akeys = ["ps1", "ps2"]
                for h in range(8):
                    for kt in range(nkt):
                        sbank = (4, 5, 0)[it % 3]
                        skey = "ps%d" % sbank
                        pt = pT[it % 3]
                        pkey = "pT%d" % (it % 3)
                        it += 1
                        sc.op("pe", lambda: PE.matmul(PS[sbank][:, 0:C], KT[0:96, h, kt * 128:(kt + 1) * 128], QT[0:96, h, :], start=True, stop=True),
                              reads=["KT", "QT"], writes=[skey])
                        sc.op("act", lambda: ACT.activation(out=pt[:], in_=PS[sbank][:, 0:C], func=AF.Exp, scale=scale), reads=[skey], writes=[pkey])
                        jk = kt - kdiag0
                        if jk >= 0:
                            sc.op("dve", lambda: DVE.tensor_tensor(out=pt[:, jk * 128:(jk + 1) * 128], in0=pt[:, jk * 128:(jk + 1) * 128], in1=tri_b[:], op=ALU.mult),
                                  reads=[pkey], writes=[pkey])
                        for jq in range(C // 128):
                            if jk > jq:
                                continue
                            last = kdiag0 + jq
                            sc.op("pe", lambda: PE.matmul(PS[abk[jq]][:, 0:65], pt[:, jq * 128:(jq + 1) * 128], VC[:, kt, h, :], start=(kt == 0), stop=(kt == last)),
                                  reads=[pkey, "VC"], writes=[akeys[jq]])
                    for jq in range(C // 128):
                        sc.op("dve", lambda: DVE.reciprocal(rinv[:, jq:jq + 1], PS[abk[jq]][:, 64:65]), reads=[akeys[jq]], writes=["rinv"])
                        sc.op("dve", lambda: DVE.tensor_scalar(ymla[:, jq, h * 64:(h + 1) * 64], PS[abk[jq]][:, 0:64], rinv[:, jq:jq + 1], None, op0=ALU.mult),
                              reads=[akeys[jq], "rinv"], writes=["ymla"])
                for jq in range(C // 128):
                    sc.op("dve", lambda: DVE.scalar_tensor_tensor(out=ymn[:, jq, :], in0=ymla[:, jq, :], scalar=1.0, in1=ymla[:, jq, :], op0=ALU.mult, op1=ALU.mult, accum_out=sst[:, jq:jq + 1]),
                          reads=["ymla"], writes=["ymn", "sst"])
                sc.op("act", lambda: ACT.activation(out=sst[:], in_=sst[:], func=AF.Sqrt, bias=consts[:, 0:1], scale=1.0 / 512), reads=["sst"], writes=["sst"])
                sc.op("dve", lambda: DVE.reciprocal(sst[:], sst[:]), reads=["sst"], writes=["sst"])
                for jq in range(C // 128):
                    sc.op("dve", lambda: DVE.tensor_scalar(ymn[:, jq, :], ymla[:, jq, :], sst[:, jq:jq + 1], None, op0=ALU.mult), reads=["ymla", "sst"], writes=["ymn"])
                    for fc in range(4):
                        sc.op("pe", lambda: PE.transpose(PS3[:, fc * 128:(fc + 1) * 128], ymn[:, jq, fc * 128:(fc + 1) * 128], ident_b[:]), reads=["ymn"], writes=["ps3"])
                    for fc in range(4):
                        sc.op("dve", lambda: DVE.tensor_scalar(yT[:, 4 + fc, jq * 128:(jq + 1) * 128], PS3[:, fc * 128:(fc + 1) * 128], vcol(V_MOG + fc), None, op0=ALU.mult),
                              reads=["ps3"], writes=["yT"])
                for oc in range(DC):
                    w, wkey = wso.next()
                    ps, key = gen_ps()
                    for cc in range(8):
                        sc.op("pe", lambda: PE.matmul(ps, w[:, cc, :], yT[:, cc, :], start=(cc == 0), stop=(cc == 7)), reads=["yT", wkey], writes=[key])
                    sc.op("dve", lambda: DVE.scalar_tensor_tensor(out=xT[:, oc, :], in0=ps, scalar=modT[:, 16 + oc, b:b + 1], in1=xT[:, oc, :], op0=ALU.mult, op1=ALU.add),
                          reads=[key, xk], writes=[xk])
                sc.barrier_b()

            with ExitStack() as pes2:
                qT = sb(pes2, "qT", [128, 16, C], F32)
                scs = sb(pes2, "scs", [128, 2048], F32)
                work = sb(pes2, "work", [128, 2048], F32)
                top = sb(pes2, "top", [128, 16, 16], F32)
                tix = sb(pes2, "tix", [128, 16, 16], U32)
                tixf = sb(pes2, "tixf", [128, 16, 16], F32)
                best = sb(pes2, "best", [128, 8, 16], F32)
                posu = sb(pes2, "posu", [128, 8, 16], U32)
                pa_i = sb(pes2, "pa_i", [128, 8, 16], I32)
                pa_f = sb(pes2, "pa_f", [128, 8, 16], F32)
                pb_f = sb(pes2, "pb_f", [128, 8, 16], F32)
                gsum = sb(pes2, "gsum", [128, 8], F32)
                isel = sb(pes2, "isel", [128, 8, 16], F32)
                jsel = sb(pes2, "jsel", [128, 8, 16], F32)

                modulated_norm(xT, xk, A2, 24, b)
                wsq = WStream([(wqb_d[hp, :, :, :], 128) for hp in range(16)])
                for hp in range(16):
                    w, wkey = wsq.next()
                    qb = (0, 1, 2, 4, 5)[hp % 5]
                    qk = "ps%d" % qb
                    pq = PS[qb][:, 0:C]
                    for dc in range(DC):
                        sc.op("pe", lambda: PE.matmul(pq, w[:, dc, :], hT[:, dc, :], start=(dc == 0), stop=(dc == DC - 1)), reads=["hT", wkey], writes=[qk])
                    if hp % 2 == 0:
                        sc.op("act", lambda: ACT.copy(qT[:, hp, :], pq), reads=[qk], writes=["qT%d" % hp])
                    else:
                        sc.op("dve", lambda: DVE.tensor_copy(qT[:, hp, :], pq), reads=[qk], writes=["qT%d" % hp])
                for j in range(C // 128):
                    ts = slice(j * 128, (j + 1) * 128)
                    h2tm = h2s[par][j]
                    gg = ggs[par][j]
                    idxi = idxs[par][j]
                    hkey, gkey_, ikey = "h2%d%d" % (par, j), "gg%d%d" % (par, j), "idx%d%d" % (par, j)
                    for dc in range(DC):
                        sc.op("pe", lambda: PE.transpose(PS3[:, dc * 128:(dc + 1) * 128], hT[:, dc, ts], ident_b[:]), reads=["hT"], writes=["ps3"])
                    sc.op("act", lambda: ACT.copy(h2tm[:], PS3[:, :]), reads=["ps3"], writes=[hkey], cost=1.0)
                    sbanks = (1, 2, 4, 5)
                    for hp in range(16):
                        bnk = sbanks[hp // 4]
                        sc.op("pe", lambda: PE.matmul(PS[bnk][:, (hp % 4) * 128:(hp % 4 + 1) * 128], qT[:, hp, ts], keysT[:, hp, :], start=True, stop=True),
                              reads=["qT%d" % hp], writes=["ps%d" % bnk])
                    for q in range(4):
                        bnk = sbanks[q]
                        if q % 2 == 0:
                            sc.op("act", lambda: ACT.copy(scs[:, q * 512:(q + 1) * 512], PS[bnk][:, :]), reads=["ps%d" % bnk], writes=["scs%d" % q])
                        else:
                            sc.op("dve", lambda: DVE.tensor_copy(scs[:, q * 512:(q + 1) * 512], PS[bnk][:, :]), reads=["ps%d" % bnk], writes=["scs%d" % q])
                    TOPK = ["top%d" % hp for hp in range(16)]
                    TIXK = ["tix%d" % hp for hp in range(16)]
                    WRKK = ["work%d" % hp for hp in range(16)]
                    sks = ["scs%d" % (hp // 4) for hp in range(16)]
                    svs = [scs[:, hp * 128:(hp + 1) * 128] for hp in range(16)]
                    wvs = [work[:, hp * 128:(hp + 1) * 128] for hp in range(16)]
                    for hp in range(16):
                        sc.op("dve", lambda: DVE.max(out=top[:, hp, 0:8], in_=svs[hp]), reads=[sks[hp]], writes=[TOPK[hp]], cost=0.2)
                    for hp in range(16):
                        sc.op("dve", lambda: DVE.max_index(out=tix[:, hp, 0:8], in_max=top[:, hp, 0:8], in_values=svs[hp]), reads=[sks[hp], TOPK[hp]], writes=[TIXK[hp]], cost=0.25)
                    for hp in range(16):
                        sc.op("dve", lambda: DVE.match_replace(out=wvs[hp], in_to_replace=top[:, hp, 0:8], in_values=svs[hp], imm_value=NEG), reads=[sks[hp], TOPK[hp]], writes=[WRKK[hp]], cost=0.25)
                    for hp in range(16):
                        sc.op("dve", lambda: DVE.max(out=top[:, hp, 8:16], in_=wvs[hp]), reads=[WRKK[hp]], writes=[TOPK[hp]], cost=0.2)
                    for hp in range(16):
                        sc.op("dve", lambda: DVE.max_index(out=tix[:, hp, 8:16], in_max=top[:, hp, 8:16], in_values=wvs[hp]), reads=[WRKK[hp], TOPK[hp]], writes=[TIXK[hp]], cost=0.25)
                    sc.op("dve", lambda: DVE.tensor_copy(tixf[:], tix[:]), reads=TIXK, writes=["tixf"])
                    top4 = top[:].rearrange("p (h two) k -> p h two k", two=2)
                    tix4 = tixf[:].rearrange("p (h two) k -> p h two k", two=2)
                    cand = work[:].rearrange("p (h a b) -> p h a b", a=16, b=16)
                    cand3 = work[:].rearrange("p (h ab) -> p h ab", ab=256)
                    cand2 = scs[:].rearrange("p (h ab) -> p h ab", ab=256)
                    ALLS = ["scs0", "scs1", "scs2", "scs3"]
                    WH = [[WRKK[2 * h], WRKK[2 * h + 1]] for h in range(8)]
                    SH = ["scs%d" % (h // 2) for h in range(8)]
                    BK = ["best%d" % h for h in range(8)]
                    PK = ["posu%d" % h for h in range(8)]
                    sc.op("dve", lambda: DVE.tensor_tensor(out=cand, in0=top4[:, :, 0, :].unsqueeze(3).to_broadcast([128, 8, 16, 16]),
                                                           in1=top4[:, :, 1, :].unsqueeze(2).to_broadcast([128, 8, 16, 16]), op=ALU.add),
                          reads=TOPK, writes=WRKK, cost=2.2)
                    for h in range(8):
                        sc.op("dve", lambda: DVE.max(out=best[:, h, 0:8], in_=cand3[:, h, :]), reads=WH[h], writes=[BK[h]], cost=0.35)
                    for h in range(8):
                        sc.op("dve", lambda: DVE.max_index(out=posu[:, h, 0:8], in_max=best[:, h, 0:8], in_values=cand3[:, h, :]), reads=WH[h] + [BK[h]], writes=[PK[h]], cost=0.4)
                    for h in range(8):
                        sc.op("dve", lambda: DVE.match_replace(out=cand2[:, h, :], in_to_replace=best[:, h, 0:8], in_values=cand3[:, h, :], imm_value=NEG), reads=WH[h] + [BK[h]], writes=[SH[h]], cost=0.4)
                    for h in range(8):
                        sc.op("dve", lambda: DVE.max(out=best[:, h, 8:16], in_=cand2[:, h, :]), reads=[SH[h]], writes=[BK[h]], cost=0.35)
                    for h in range(8):
                        sc.op("dve", lambda: DVE.max_index(out=posu[:, h, 8:16], in_max=best[:, h, 8:16], in_values=cand2[:, h, :]), reads=[SH[h], BK[h]], writes=[PK[h]], cost=0.4)
                    sc.op("dve", lambda: DVE.tensor_copy(pb_f[:], posu[:]), reads=PK, writes=["pb_f"])
                    sc.op("dve", lambda: DVE.tensor_scalar(pa_f[:], pb_f[:], 1.0 / 16, -0.46875, op0=ALU.mult, op1=ALU.add), reads=["pb_f"], writes=["pa_f"])
                    sc.op("dve", lambda: DVE.tensor_copy(pa_i[:], pa_f[:]), reads=["pa_f"], writes=["pa_i"])
                    sc.op("dve", lambda: DVE.tensor_copy(pa_f[:], pa_i[:]), reads=["pa_i"], writes=["pa_f"])
                    sc.op("dve", lambda: DVE.scalar_tensor_tensor(out=pb_f[:], in0=pa_f[:], scalar=-16.0, in1=pb_f[:], op0=ALU.mult, op1=ALU.add), reads=["pa_f", "pb_f"], writes=["pb_f"])
                    sc.op("dve", lambda: DVE.tensor_tensor(out=gg[:], in0=best[:], in1=best[:, :, 0:1].to_broadcast([128, 8, 16]), op=ALU.subtract), reads=BK, writes=[gkey_])
                    sc.op("act", lambda: ACT.activation(out=gg[:], in_=gg[:], func=AF.Exp), reads=[gkey_], writes=[gkey_])
                    sc.op("dve", lambda: DVE.tensor_reduce(out=gsum[:], in_=gg[:], axis=AX.X, op=ALU.add), reads=[gkey_], writes=["gsum"])
                    sc.op("dve", lambda: DVE.reciprocal(gsum[:], gsum[:]), reads=["gsum"], writes=["gsum"])
                    sc.op("dve", lambda: DVE.tensor_tensor(out=gg[:], in0=gg[:], in1=gsum[:].unsqueeze(2).to_broadcast([128, 8, 16]), op=ALU.mult), reads=[gkey_, "gsum"], writes=[gkey_])
                    eq = work[:].rearrange("p (h k a) -> p h k a", k=16, a=16)
                    io4 = iota16[:].unsqueeze(1).unsqueeze(1).to_broadcast([128, 8, 16, 16])
                    for (pf, two, dst) in ((pa_f, 0, isel), (pb_f, 1, jsel)):
                        sc.op("dve", lambda: DVE.tensor_tensor(out=eq, in0=io4, in1=pf[:].unsqueeze(3).to_broadcast([128, 8, 16, 16]), op=ALU.is_equal),
                              reads=["pa_f", "pb_f"] + BK + PK, writes=WRKK, cost=2.2)
                        sc.op("dve", lambda: DVE.tensor_tensor(out=eq, in0=eq, in1=tix4[:, :, two, :].unsqueeze(2).to_broadcast([128, 8, 16, 16]), op=ALU.mult),
                              reads=WRKK + ["tixf"], writes=WRKK, cost=2.2)
                        sc.op("dve", lambda: DVE.tensor_reduce(out=dst[:], in_=eq, axis=AX.X, op=ALU.add), reads=WRKK, writes=["sel%d" % two], cost=2.2)
                    sc.op("dve", lambda: DVE.scalar_tensor_tensor(out=isel[:], in0=isel[:], scalar=128.0, in1=jsel[:], op0=ALU.mult, op1=ALU.add),
                          reads=["sel0", "sel1"], writes=["sel0"])
                    sc.op("dve", lambda: DVE.tensor_copy(idxi[:], isel[:].rearrange("p h k -> p (h k)")), reads=["sel0"], writes=[ikey])
                sc.barrier_b()
            sc.b_active = False

        def emit_A(b, c, par, give):
            t0 = c * C
            xT = xTs[par]
            xk = "xT%d" % par
            for j in range(C // 128):
                ts = slice(j * 128, (j + 1) * 128)
                h2tm = h2s[par][j]
                ggf = ggs[par][j][:].rearrange("p h k -> p (h k)")
                idxi = idxs[par][j]
                hkey, gkey_, ikey = "h2%d%d" % (par, j), "gg%d%d" % (par, j), "idx%d%d" % (par, j)
                for step in range(130):
                    hk = step
                    if hk < 128:
                        Gb = G[hk % NBUF]
                        gkey = "G%d" % (hk % NBUF)
                        sc.dma("pool", lambda: POOL.indirect_dma_start(out=Gb[:], out_offset=None, in_=uvb_d,
                                                                       in_offset=bass.IndirectOffsetOnAxis(ap=idxi[:, hk:hk + 1], axis=0)),
                               reads=[ikey, "uvscr"], writes=[gkey], ch=gkey, cost=1.9)
                        sc.op("dve", lambda: DVE.scalar_tensor_tensor(out=junkb[hk % 2], in0=Gb[:, 0:D], scalar=1.0, in1=h2tm[:], op0=ALU.mult, op1=ALU.mult, accum_out=dots[:, hk:hk + 1]),
                              reads=[gkey, hkey], writes=["dots%d" % hk, "junkb%d" % (hk % 2)], cost=1.3)
                        sc.op("act", lambda: ACT.activation(out=acts[:, hk:hk + 1], in_=dots[:, hk:hk + 1], func=AF.Gelu_apprx_tanh), reads=["dots%d" % hk], writes=["dots%d" % hk])
                    give()
                    k1 = step - 1
                    if 0 <= k1 < 128:
                        sc.op("act", lambda: ACT.activation(out=zz[:, k1:k1 + 1], in_=acts[:, k1:k1 + 1], func=AF.Identity, scale=ggf[:, k1:k1 + 1]),
                              reads=["dots%d" % k1, gkey_], writes=["dots%d" % k1])
                    k2 = step - 2
                    if 0 <= k2 < 128:
                        Gp = G[k2 % NBUF]
                        gpkey = "G%d" % (k2 % NBUF)
                        dg = diag[k2 % 3]
                        dkey = "diag%d" % (k2 % 3)
                        sc.op("act", lambda: ACT.activation(out=dg[:], in_=ident_b[:], func=AF.Identity, scale=zz[:, k2:k2 + 1]),
                              reads=["dots%d" % k2], writes=[dkey])
                        for half in range(2):
                            bnk = 6 + half
                            sc.op("pe", lambda: PE.matmul(PS[bnk][:, :], dg[:], Gp[:, D + half * 512:D + (half + 1) * 512], start=(k2 == 0), stop=(k2 == 127)),
                                  reads=[dkey, gpkey], writes=["ps%d" % bnk])
                    give()
                sc.op("act", lambda: ACT.copy(petm[:, 0:512], PS[6][:, :]), reads=["ps6"], writes=["petm"])
                sc.op("dve", lambda: DVE.tensor_copy(petm[:, 512:1024], PS[7][:, :]), reads=["ps7"], writes=["petm"])
                for dc in range(DC):
                    bnk = 6 + dc // 4
                    sc.op("pe", lambda: PE.transpose(PS[bnk][:, (dc % 4) * 128:(dc % 4 + 1) * 128], petm[:, dc * 128:(dc + 1) * 128], ident_f[:]), reads=["petm"], writes=["ps%d" % bnk])
                for dc in range(DC):
                    bnk = 6 + dc // 4
                    sc.op("dve", lambda: DVE.scalar_tensor_tensor(out=xT[:, dc, ts], in0=PS[bnk][:, (dc % 4) * 128:(dc % 4 + 1) * 128], scalar=modT[:, 40 + dc, b:b + 1], in1=xT[:, dc, ts], op0=ALU.mult, op1=ALU.add),
                          reads=["ps%d" % bnk, xk], writes=[xk])
                give()
            rms_stats(xT[:], DC, 1.0 / D, xk, sq_t=sqA, rstd_t=rstdA, bank=6, sfx="A", sqkeys=["junkb0", "junkb1"])
            for dc in range(DC):
                ot = otmp[dc % 2]
                okey = "otmp%d" % (dc % 2)
                sc.op("dve", lambda: DVE.scalar_tensor_tensor(out=ot[:], in0=xT[:, dc, :], scalar=vcol(V_FG + dc), in1=rstdA[:], op0=ALU.mult, op1=ALU.mult),
                      reads=[xk, "rstdA"], writes=[okey])
                sc.dma("sp", lambda: SP.dma_start(out=outT_d[b, dc, :, t0:t0 + C], in_=ot[:]), reads=[okey], writes=["outd"], ch=okey)
            give()

        chunks = [(b, c) for b in range(nseq) for c in range(nch)]
        if overlap:
            co.no_pace = True
            co.start(lambda: emit_B(chunks[0][0], chunks[0][1], 0))
        uv_v = uv_d.rearrange("(p r) n -> p r n", p=128)
        uvb_v = uvb_d.rearrange("(p r) n -> p r n", p=128)
        for st_i in range(128):
            i2 = st_i % 2
            st = stage[:, i2 * 2048:(i2 + 1) * 2048]
            skey, bkey = "stage%d" % i2, "stb%d" % i2
            sc.dma("sp", lambda: SP.dma_start(out=st, in_=uv_v[:, st_i, :]), writes=[skey], ch=skey)
            if st_i % 2 == 0:
                sc.op("dve", lambda: DVE.tensor_copy(stb[i2][:], st), reads=[skey], writes=[bkey])
            else:
                sc.op("act", lambda: ACT.copy(stb[i2][:], st), reads=[skey], writes=[bkey])
            sc.dma("act", lambda: ACT.dma_start(out=uvb_v[:, st_i, :], in_=stb[i2][:]), reads=[bkey], writes=["uvscr"], ch=bkey + "u")
            if overlap:
                co.give(24)
        if overlap:
            co.drain()
            co.no_pace = False
        else:
            emit_B(chunks[0][0], chunks[0][1], 0)
        sc.barrier()
        pes.close()
        NBUF = 8
        G = [sb(es, "G%d" % i, [128, 2048], BF16) for i in range(NBUF)]
        junkb_t = sb(es, "junkb", [128, 2, D], BF16)
        junkb = [junkb_t[:, 0, :], junkb_t[:, 1, :]]
        diag = [sb(es, "diag%d" % i, [128, 128], BF16) for i in range(3)]
        dots = sb(es, "dots", [128, 128], F32)
        acts = dots
        zz = dots
        petm = sb(es, "petm", [128, D], F32)
        sqA = junkb_t[:, :, :].rearrange("p a (b c) -> p (a b) c", c=C)
        rstdA = sb(es, "rstdA", [128, C], F32)
        otmp = [sb(es, "otmp%d" % i, [128, C], F32) for i in range(2)]

        last_b_count = 2200
        for i, (b, c) in enumerate(chunks):
            par = i % 2
            if overlap and i + 1 < len(chunks):
                nb, ncn = chunks[i + 1]
                co.start(lambda nb=nb, ncn=ncn, par=par: emit_B(nb, ncn, 1 - par))
                q = 200
                emit_A(b, c, par, lambda q=q: co.give(q))
                last_b_count = max(co.count, 1) if co.done else last_b_count
                co.drain()
                last_b_count = max(co.count, 1)
            else:
                emit_A(b, c, par, lambda: None)
                if i + 1 < len(chunks):
                    nb, ncn = chunks[i + 1]
                    emit_B(nb, ncn, 1 - par)
        sc.finish()
    return nc


def _pack_inputs(inp, core, nseq=NSEQ):
    f32 = np.float32
    b0 = core * nseq
    x = inp["x"][b0:b0 + nseq]
    xT = np.ascontiguousarray(np.transpose(x, (0, 2, 1))).reshape(nseq, DC, 128, S)
    c = inp["c"][b0:b0 + nseq]
    cT = np.ascontiguousarray(c.T.reshape(DC, 128, nseq).transpose(1, 0, 2))
    pos = np.ascontiguousarray(inp["positions"][b0:b0 + nseq]).astype(np.int32)
    return {"xT": xT.astype(f32), "cT": cT.astype(f32), "pos": pos}


def _pack_weights(inp):
    f32 = np.float32
    col = lambda v, n: np.ascontiguousarray(np.asarray(v, f32).reshape(n, 128).T)
    vecs = np.zeros((128, NV), f32)
    vecs[:, V_N1G:V_N1G + 8] = col(inp["norm1_g"][0], 8)
    vecs[:, V_N2G:V_N2G + 8] = col(inp["norm2_g"][0], 8)
    vecs[:, V_FG:V_FG + 8] = col(inp["final_g"], 8)
    cw = np.asarray(inp["conv_w"][0], f32)
    for ci in range(4):
        for k in range(4):
            vecs[:, V_CONVW + ci * 4 + k] = cw[k, ci * 128:(ci + 1) * 128]
    vecs[:, V_CONVB:V_CONVB + 4] = col(inp["conv_b"][0], 4)
    vecs[:, V_BA:V_BA + 4] = col(inp["lru_ba"][0], 4)
    vecs[:, V_BX:V_BX + 4] = col(inp["lru_bx"][0], 4)
    vecs[:, V_LAM:V_LAM + 4] = col(inp["lru_lambda"][0], 4)
    vecs[:, V_QNG:V_QNG + 2] = col(inp["q_norm_g"][0], 2)
    vecs[:, V_KVNG:V_KVNG + 1] = col(inp["kv_norm_g"][0], 1)
    vecs[:, V_LOG:V_LOG + 4] = col(inp["lru_out_g"][0], 4)
    vecs[:, V_MOG:V_MOG + 4] = col(inp["mla_out_g"][0], 4)
    vecs[:, V_BADA:V_BADA + 48] = col(inp["b_ada"][0], 48)
    inv_freq = (1.0 / (10000.0 ** (np.arange(0, 32, 2, dtype=np.float32) / np.float32(32)))).astype(f32)
    for p in range(64, 96):
        vecs[p, V_INVF] = inv_freq[(p - 64) % 16]
        vecs[p, V_SGN] = -1.0 if p < 80 else 1.0
    w_in = np.asarray(inp["w_in"][0], f32)
    kr = w_in[:, 1408:1440]
    w_in_ext = np.concatenate([w_in, kr[:, 16:32], kr[:, 0:16]], axis=1)
    wa = np.asarray(inp["lru_wa"][0], f32)
    wx = np.asarray(inp["lru_wx"][0], f32)
    wa_bd = np.zeros((4, 128, 128), f32)
    wx_bd = np.zeros((4, 128, 128), f32)
    for ci in range(4):
        for s in range(2):
            wa_bd[ci, s * 64:(s + 1) * 64, s * 64:(s + 1) * 64] = wa[2 * ci + s]
            wx_bd[ci, s * 64:(s + 1) * 64, s * 64:(s + 1) * 64] = wx[2 * ci + s]
    w_uq = np.asarray(inp["w_uq"][0], f32)
    parts = []
    for h in range(8):
        blk = w_uq[:, h * 96:(h + 1) * 96]
        parts += [blk, blk[:, 0:64], blk[:, 80:96], blk[:, 64:80]]
    w_uq_ext = np.concatenate(parts, axis=1)
    keys = np.asarray(inp["peer_keys"][0], f32)
    keysT = np.ascontiguousarray(keys.reshape(16, 128, 128).transpose(2, 0, 1))
    uv = np.concatenate([np.asarray(inp["peer_u"][0], f32), np.asarray(inp["peer_v"][0], f32)], axis=1)
    return {
        "w_ada": np.ascontiguousarray(np.asarray(inp["w_ada"][0], f32).reshape(DC, 128, 6 * D)),
        "vecs": vecs,
        "w_in": np.ascontiguousarray(w_in_ext.reshape(DC, 128, WIN_COLS)),
        "wa_bd": wa_bd, "wx_bd": wx_bd,
        "w_uq": np.ascontiguousarray(w_uq_ext.reshape(2, 128, 1536)),
        "w_ukv": np.ascontiguousarray(np.asarray(inp["w_ukv"][0], f32)),
        "w_out": np.ascontiguousarray(np.asarray(inp["w_out"][0], f32).reshape(DC, 128, D)),
        "peer_wq": np.ascontiguousarray(np.asarray(inp["peer_wq"][0], f32).reshape(DC, 128, 2048)),
        "keysT": keysT,
        "uv": np.ascontiguousarray(uv),
    }


def kernel(**inputs):
    inp = {k: np.asarray(v) for k, v in inputs.items()}
    nc = build_program(NSEQ, NCH)
    wts = _pack_weights(inp)
    in_maps = []
    for core in range(NCORES):
        m = dict(wts)
        m.update(_pack_inputs(inp, core))
        in_maps.append(m)
    res = run_bass_kernel_spmd(nc, in_maps, core_ids=list(range(NCORES)))
    outs = []
    for core in range(NCORES):
        oT = np.asarray(res.results[core]["outT"]).reshape(NSEQ, D, S)
        outs.append(np.transpose(oT, (0, 2, 1)))
    return np.ascontiguousarray(np.concatenate(outs, axis=0)).astype(np.float32)
```
